# Optimizing a Trainium2 kernel written in Bass

```python
import math
import jax
import jax.numpy as jnp
from jax import lax
import numpy as np

D_MODEL = 1024
BATCH = 8
SEQ = 4096
DEPTH = 4

N_EVEN = (DEPTH + 1) // 2
N_ODD = DEPTH // 2
RMS_EPS = 1e-6

A_HEADS = 8
A_KV_HEADS = 2
A_HEAD_DIM = 64
A_WINDOW = 128
B_HEADS = 4
B_KEY_DIM = 64
B_VAL_DIM = 128
B_GATE_RANK = 16
B_GATE_TAU = 16.0
B_CHUNK = 64
C_HEADS = 8
C_HEAD_DIM = 128
C_BLOCK = 256
C_TOPK = 3
C_ROW_BLOCK = 128
REL_BUCKETS = 32
REL_MAX_DIST = 128
REL_HEADS = 8
MOE_GROUPS = 4
MOE_EXPERTS_PER_GROUP = 8
MOE_EXPERTS = MOE_GROUPS * MOE_EXPERTS_PER_GROUP
MOE_TOPK = 2
MOE_HIDDEN = 512
MOE_ROW_BLOCK = 128

EVEN_SPLITS = (A_HEADS * A_HEAD_DIM, A_KV_HEADS * A_HEAD_DIM, A_KV_HEADS * A_HEAD_DIM,
               B_HEADS * B_KEY_DIM, B_HEADS * B_KEY_DIM, B_HEADS * B_VAL_DIM,
               B_HEADS * B_VAL_DIM, B_GATE_RANK)
EVEN_IN = sum(EVEN_SPLITS)
EVEN_MIX = A_HEADS * A_HEAD_DIM + B_HEADS * B_VAL_DIM
ODD_MIX = C_HEADS * C_HEAD_DIM

kernel_name = 'hybrid_swa_gla_moba_hmoe_block'


def _rms(x, w):
    xf = x.astype(jnp.float32)
    y = xf * lax.rsqrt(jnp.mean(xf * xf, axis=-1, keepdims=True) + RMS_EPS)
    return (y * w.astype(jnp.float32)).astype(x.dtype)


def _rel_bucket(dist):
    exact = REL_BUCKETS // 2
    d = jnp.maximum(dist, 0)
    logd = jnp.log(jnp.maximum(d, 1).astype(jnp.float32) / exact) / math.log(REL_MAX_DIST / exact)
    far = jnp.minimum(exact + (logd * (REL_BUCKETS - exact)).astype(jnp.int32), REL_BUCKETS - 1)
    return jnp.where(d < exact, d, far)


def _group_rows(ids, n_groups, rows):
    n = ids.shape[0]
    n_rows = ((n + rows - 1) // rows) * rows + n_groups * rows
    counts = jnp.zeros((n_groups + 1,), jnp.int32).at[ids].add(1)[:n_groups]
    padded = ((counts + rows - 1) // rows) * rows
    start = jnp.cumsum(counts) - counts
    pend = jnp.cumsum(padded)
    pstart = pend - padded
    order = jnp.argsort(ids)
    sid = ids[order]
    gid = jnp.minimum(sid, n_groups - 1)
    dest = jnp.where(sid < n_groups, pstart[gid] + jnp.arange(n) - start[gid], n_rows)
    src = jnp.full((n_rows,), n, jnp.int32).at[dest].set(order.astype(jnp.int32), mode='drop')
    blk_group = jnp.searchsorted(pend, jnp.arange(n_rows // rows) * rows, side='right')
    return src, jnp.minimum(blk_group, n_groups - 1)


def _swa_sink_attention(q, k, v, sinks, rel_bias):
    B, S, _, hd = q.shape
    W = A_WINDOW
    nb = S // W
    G = A_HEADS // A_KV_HEADS
    f32 = jnp.float32
    qb = q.reshape(B, nb, W, A_KV_HEADS, G, hd)

    def band(t):
        tb = t.reshape(B, nb, W, A_KV_HEADS, hd)
        prev = jnp.pad(tb, ((0, 0), (1, 0), (0, 0), (0, 0), (0, 0)))[:, :-1]
        return jnp.concatenate([prev, tb], axis=2)

    kband, vband = band(k), band(v)
    lg = jnp.einsum('bnqkgd,bnskd->bkgnqs', qb, kband, preferred_element_type=f32) * hd ** -0.5
    dist = jnp.arange(W)[:, None] + W - jnp.arange(2 * W)[None, :]
    mask = ((dist >= 0) & (dist < W)
            & ((jnp.arange(nb)[:, None, None] > 0) | (jnp.arange(2 * W) >= W)[None, None, :]))
    bias = rel_bias[_rel_bucket(dist)].transpose(2, 0, 1).reshape(A_KV_HEADS, G, 1, W, 2 * W)
    lg = jnp.where(mask, lg + bias, -jnp.inf)
    sink = sinks.astype(f32).reshape(A_KV_HEADS, G, 1, 1, 1)
    m = jnp.maximum(lg.max(-1, keepdims=True), sink)
    p = jnp.exp(lg - m)
    probs = p / (p.sum(-1, keepdims=True) + jnp.exp(sink - m))
    o = jnp.einsum('bkgnqs,bnskd->bnqkgd', probs.astype(v.dtype), vband, preferred_element_type=f32)
    return o.reshape(B, S, A_HEADS * hd)


def _gla(q, k, v, log_a):
    B, S, H, dk = q.shape
    dv = v.shape[-1]
    C = B_CHUNK
    n = S // C

    def chunks(t):
        return t.reshape(B, n, C, H, t.shape[-1]).transpose(0, 3, 1, 2, 4)

    q, k, v, g = chunks(q * dk ** -0.5), chunks(k), chunks(v), chunks(log_a)
    b = jnp.cumsum(g, axis=3)
    b_last = b[:, :, :, -1:]
    q_dec = q * jnp.exp(b)
    att = jnp.einsum('bhnid,bhnjd->bhnij', q_dec, k * jnp.exp(-b))
    att = jnp.where(jnp.tril(jnp.ones((C, C), bool)), att, 0.0)
    o_intra = jnp.einsum('bhnij,bhnjv->bhniv', att, v)
    upd = jnp.einsum('bhnjd,bhnjv->bhndv', k * jnp.exp(b_last - b), v)
    decay = jnp.exp(b_last[:, :, :, 0])

    def step(state, inp):
        dec, u = inp
        return state * dec[..., None] + u, state

    _, s_in = lax.scan(step, jnp.zeros((B, H, dk, dv), q.dtype),
                       (decay.transpose(2, 0, 1, 3), upd.transpose(2, 0, 1, 3, 4)))
    o_inter = jnp.einsum('bhnid,bhndv->bhniv', q_dec, s_in.transpose(1, 2, 0, 3, 4))
    return (o_intra + o_inter).transpose(0, 2, 3, 1, 4).reshape(B, S, H, dv)


def _moba(q, k, v, rel_bias):
    B, S, H, hd = q.shape
    L = C_BLOCK
    nb = -(-S // L)
    Sp = nb * L
    k_sel = min(C_TOPK, nb)
    scale = hd ** -0.5
    f32 = jnp.float32
    q, k, v = [jnp.pad(t, ((0, 0), (0, Sp - S), (0, 0), (0, 0))).transpose(0, 2, 1, 3) for t in (q, k, v)]
    qb = q.reshape(B, H, nb, L, hd)
    kb = k.reshape(B, H, nb, L, hd)
    vb = v.reshape(B, H, nb, L, hd)
    li = jnp.arange(L)
    d_own = li[:, None] - li[None, :]
    bias_own = rel_bias[_rel_bucket(d_own)].transpose(2, 0, 1)[None, :, None]
    lg = jnp.einsum('bhnqd,bhnsd->bhnqs', qb, kb, preferred_element_type=f32) * scale + bias_own
    lg = jnp.where(d_own >= 0, lg, -jnp.inf)
    lse_own = jax.nn.logsumexp(lg, axis=-1)
    o_own = jnp.einsum('bhnqs,bhnsd->bhnqd', jnp.exp(lg - lse_own[..., None]).astype(v.dtype), vb,
                       preferred_element_type=f32)
    pos = jnp.arange(Sp)
    qblk = pos // L
    score = jnp.einsum('bhtd,bhjd->bhtj', q.astype(f32), kb.astype(f32).mean(axis=3))
    score = jnp.where(jnp.arange(nb)[None, :] < qblk[:, None], score, -jnp.inf)
    _, sel = lax.top_k(score, k_sel)
    valid = (jnp.arange(k_sel)[None, :] < jnp.minimum(qblk, k_sel)[:, None]) & (pos < S)[:, None]
    n_groups = B * H * nb
    grp = jnp.arange(B * H).reshape(B, H, 1, 1) * nb + sel
    grp = jnp.where(valid, grp, n_groups).reshape(-1)
    src, blk_grp = _group_rows(grp, n_groups, C_ROW_BLOCK)
    n_pairs = grp.shape[0]
    row_ok = src < n_pairs
    row_tok = jnp.minimum(src, n_pairs - 1) // k_sel
    n_tok = B * H * Sp
    q_rows = q.reshape(n_tok, hd)
    k_grp = kb.reshape(n_groups, L, hd)
    v_grp = vb.reshape(n_groups, L, hd)

    def attend_block(args):
        tok, g = args
        qr = q_rows[tok]
        dist = (tok % Sp)[:, None] - ((g % nb) * L + li)[None, :]
        bias = rel_bias[_rel_bucket(dist), (g // nb) % H]
        lg_r = jnp.einsum('rd,sd->rs', qr, k_grp[g], preferred_element_type=f32) * scale + bias
        lse_r = jax.nn.logsumexp(lg_r, axis=-1)
        o_r = jnp.einsum('rs,sd->rd', jnp.exp(lg_r - lse_r[:, None]).astype(v.dtype), v_grp[g],
                         preferred_element_type=f32)
        return o_r, lse_r

    o_sel, lse_sel = lax.map(attend_block, (row_tok.reshape(-1, C_ROW_BLOCK), blk_grp))
    o_sel = o_sel.reshape(-1, hd)
    lse_sel = lse_sel.reshape(-1)
    seg = jnp.where(row_ok, row_tok, n_tok)
    lse_own = lse_own.reshape(n_tok)
    m_sel = jax.ops.segment_max(jnp.where(row_ok, lse_sel, -jnp.inf), seg, num_segments=n_tok + 1)[:n_tok]
    m = jnp.maximum(lse_own, m_sel)
    w_sel = jnp.where(row_ok, jnp.exp(lse_sel - m[jnp.minimum(seg, n_tok - 1)]), 0.0)
    w_own = jnp.exp(lse_own - m)
    num = (w_own[:, None] * o_own.reshape(n_tok, hd)
           + jax.ops.segment_sum(w_sel[:, None] * o_sel, seg, num_segments=n_tok + 1)[:n_tok])
    den = w_own + jax.ops.segment_sum(w_sel, seg, num_segments=n_tok + 1)[:n_tok]
    o = (num / den[:, None]).reshape(B, H, Sp, hd)[:, :, :S]
    return o.transpose(0, 2, 1, 3).reshape(B, S, H * hd).astype(q.dtype)


def _even_mixer(h, w_in, w_out, q_norm, k_norm, sinks, gate_up, gate_bias, out_norm, rel_bias):
    B, S, _ = h.shape
    f32 = jnp.float32
    qa, ka, va, qb, kb, vb, rb, ab = jnp.split(h @ w_in, np.cumsum(EVEN_SPLITS)[:-1].tolist(), axis=-1)
    qa = _rms(qa.reshape(B, S, A_HEADS, A_HEAD_DIM), q_norm)
    ka = _rms(ka.reshape(B, S, A_KV_HEADS, A_HEAD_DIM), k_norm)
    va = va.reshape(B, S, A_KV_HEADS, A_HEAD_DIM)
    oa = _swa_sink_attention(qa, ka, va, sinks, rel_bias).astype(h.dtype)
    log_a = jax.nn.log_sigmoid((ab @ gate_up + gate_bias).astype(f32)) / B_GATE_TAU
    ob = _gla(qb.reshape(B, S, B_HEADS, B_KEY_DIM).astype(f32),
              kb.reshape(B, S, B_HEADS, B_KEY_DIM).astype(f32),
              vb.reshape(B, S, B_HEADS, B_VAL_DIM).astype(f32),
              log_a.reshape(B, S, B_HEADS, B_KEY_DIM))
    ob = _rms(ob, out_norm).astype(h.dtype) * jax.nn.silu(rb).reshape(B, S, B_HEADS, B_VAL_DIM)
    return jnp.concatenate([oa, ob.reshape(B, S, B_HEADS * B_VAL_DIM)], axis=-1) @ w_out


def _odd_mixer(h, w_in, w_out, q_norm, k_norm, rel_bias):
    B, S, _ = h.shape
    q, k, v = jnp.split(h @ w_in, 3, axis=-1)
    q = _rms(q.reshape(B, S, C_HEADS, C_HEAD_DIM), q_norm)
    k = _rms(k.reshape(B, S, C_HEADS, C_HEAD_DIM), k_norm)
    v = v.reshape(B, S, C_HEADS, C_HEAD_DIM)
    return _moba(q, k, v, rel_bias) @ w_out


def _hier_moe(h, w_group, b_group, w_expert, b_expert, w1, w3, w2):
    B, S, D = h.shape
    N = B * S
    f32 = jnp.float32
    xt = h.reshape(N, D)
    g_logits = (xt @ w_group).astype(f32) + b_group.astype(f32)
    g_sel = jnp.argmax(g_logits, axis=-1)
    p_g = jnp.take_along_axis(jax.nn.softmax(g_logits, axis=-1), g_sel[:, None], axis=-1)
    e_logits = ((xt @ w_expert).astype(f32) + b_expert.astype(f32)).reshape(N, MOE_GROUPS, MOE_EXPERTS_PER_GROUP)
    e_in = jnp.take_along_axis(e_logits, g_sel[:, None, None], axis=1)[:, 0]
    top_v, top_i = lax.top_k(e_in, MOE_TOPK)
    gate = (p_g * jax.nn.softmax(top_v, axis=-1)).reshape(-1)
    ids = (g_sel[:, None] * MOE_EXPERTS_PER_GROUP + top_i).reshape(-1)
    src, blk_e = _group_rows(ids, MOE_EXPERTS, MOE_ROW_BLOCK)
    n_pairs = ids.shape[0]
    row_ok = src < n_pairs
    pair = jnp.minimum(src, n_pairs - 1)
    row_tok = pair // MOE_TOPK

    def expert_block(args):
        tok, e = args
        xr = xt[tok]
        return (jax.nn.silu(xr @ w1[e]) * (xr @ w3[e])) @ w2[e]

    y = lax.map(expert_block, (row_tok.reshape(-1, MOE_ROW_BLOCK), blk_e)).reshape(-1, D)
    w_row = jnp.where(row_ok, gate[pair], 0.0).astype(y.dtype)
    seg = jnp.where(row_ok, row_tok, N)
    out = jax.ops.segment_sum(y * w_row[:, None], seg, num_segments=N + 1)[:N]
    return out.reshape(B, S, D)


def setup_inputs(seed: int = 0) -> dict:
    key = jax.random.key(seed)
    ks = iter(jax.random.split(key, 32))
    D = D_MODEL

    def nrm(shape, s):
        return jax.random.normal(next(ks), shape, jnp.float32) * s

    return {
        'x': nrm((BATCH, SEQ, D), 1.0),
        'c': nrm((BATCH, D), 1.0),
        'rel_bias': nrm((REL_BUCKETS, REL_HEADS), 0.5),
        'ada_w': nrm((DEPTH, D, 6 * D), 0.5 * D ** -0.5),
        'ada_b': nrm((DEPTH, 6 * D), 0.02),
        'norm1_w': 1.0 + nrm((DEPTH, D), 0.05),
        'norm2_w': 1.0 + nrm((DEPTH, D), 0.05),
        'even_w_in': nrm((N_EVEN, D, EVEN_IN), D ** -0.5),
        'even_w_out': nrm((N_EVEN, EVEN_MIX, D), EVEN_MIX ** -0.5),
        'a_q_norm': 1.0 + nrm((N_EVEN, A_HEAD_DIM), 0.05),
        'a_k_norm': 1.0 + nrm((N_EVEN, A_HEAD_DIM), 0.05),
        'a_sinks': nrm((N_EVEN, A_HEADS), 0.5),
        'b_gate_up': nrm((N_EVEN, B_GATE_RANK, B_HEADS * B_KEY_DIM), B_GATE_RANK ** -0.5),
        'b_gate_bias': nrm((N_EVEN, B_HEADS * B_KEY_DIM), 0.1),
        'b_out_norm': 1.0 + nrm((N_EVEN, B_VAL_DIM), 0.05),
        'odd_w_in': nrm((N_ODD, D, 3 * ODD_MIX), D ** -0.5),
        'odd_w_out': nrm((N_ODD, ODD_MIX, D), ODD_MIX ** -0.5),
        'c_q_norm': 1.0 + nrm((N_ODD, C_HEAD_DIM), 0.05),
        'c_k_norm': 1.0 + nrm((N_ODD, C_HEAD_DIM), 0.05),
        'moe_w_group': nrm((DEPTH, D, MOE_GROUPS), D ** -0.5),
        'moe_b_group': nrm((DEPTH, MOE_GROUPS), 0.01),
        'moe_w_expert': nrm((DEPTH, D, MOE_EXPERTS), D ** -0.5),
        'moe_b_expert': nrm((DEPTH, MOE_EXPERTS), 0.01),
        'moe_w1': nrm((DEPTH, MOE_EXPERTS, D, MOE_HIDDEN), D ** -0.5),
        'moe_w3': nrm((DEPTH, MOE_EXPERTS, D, MOE_HIDDEN), D ** -0.5),
        'moe_w2': nrm((DEPTH, MOE_EXPERTS, MOE_HIDDEN, D), MOE_HIDDEN ** -0.5),
    }


def reference(x, c, rel_bias, ada_w, ada_b, norm1_w, norm2_w, even_w_in, even_w_out, a_q_norm,
              a_k_norm, a_sinks, b_gate_up, b_gate_bias, b_out_norm, odd_w_in, odd_w_out, c_q_norm,
              c_k_norm, moe_w_group, moe_b_group, moe_w_expert, moe_b_expert, moe_w1, moe_w3, moe_w2):
    cond = jax.nn.silu(c)
    for layer in range(DEPTH):
        mod = (cond @ ada_w[layer] + ada_b[layer])[:, None, :]
        shift1, scale1, gate1, shift2, scale2, gate2 = jnp.split(mod, 6, axis=-1)
        h = _rms(x, norm1_w[layer]) * (1 + scale1) + shift1
        j = layer // 2
        if layer % 2 == 0:
            y = _even_mixer(h, even_w_in[j], even_w_out[j], a_q_norm[j], a_k_norm[j], a_sinks[j],
                            b_gate_up[j], b_gate_bias[j], b_out_norm[j], rel_bias)
        else:
            y = _odd_mixer(h, odd_w_in[j], odd_w_out[j], c_q_norm[j], c_k_norm[j], rel_bias)
        x = x + gate1 * y
        h = _rms(x, norm2_w[layer]) * (1 + scale2) + shift2
        x = x + gate2 * _hier_moe(h, moe_w_group[layer], moe_b_group[layer], moe_w_expert[layer],
                                  moe_b_expert[layer], moe_w1[layer], moe_w3[layer], moe_w2[layer])
    return x
```

```python
import math
from contextlib import ExitStack

import numpy as np
import concourse.bass as bass
import concourse.mybir as mybir
from concourse.bass_utils import run_bass_kernel_spmd

F32 = mybir.dt.float32
BF16 = mybir.dt.bfloat16
I32 = mybir.dt.int32
AF = mybir.ActivationFunctionType
ALU = mybir.AluOpType
AX = mybir.AxisListType

S = 4096
D = 1024
NT = 32
CAP = 384
NB = CAP // 128
NEG = -30000.0
BIG = 10000.0
MOH = S + 128
NBLK = 96
TABROWS = NBLK * 128
EPS = 1e-6

COMPUTE = ("pe", "act", "dve", "pool")
DMAQ_K = 6
SEM_EPOCH = 6000
DMA_EPOCH = 400


class Buf:
    __slots__ = ("name", "w", "r")

    def __init__(self, name):
        self.name = name
        self.w = []
        self.r = []


class Op:
    __slots__ = ("eng", "fn", "seq", "dma", "deps", "signal", "sig_idx", "dma_idx")

    def __init__(self, eng, fn, dma):
        self.eng = eng
        self.fn = fn
        self.dma = dma
        self.deps = []
        self.signal = False
        self.sig_idx = None
        self.dma_idx = None


def _compress(lst):
    keep = []
    cnt = {}
    for x in reversed(lst):
        key = (x.eng, x.dma)
        c = cnt.get(key, 0)
        lim = DMAQ_K if x.dma else 1
        if c < lim:
            keep.append(x)
            cnt[key] = c + 1
    keep.reverse()
    return keep


class _Rec:
    def __init__(self):
        self.calls = []

    def __getattr__(self, name):
        def f(*a, **k):
            self.calls.append((name, a, k))
            return self
        return f


def _freeze(fn):
    rec = _Rec()
    fn(rec)
    assert len(rec.calls) == 1, rec.calls
    name, a, k = rec.calls[0]
    return lambda e: getattr(e, name)(*a, **k)


class Prog:
    def __init__(self, nc):
        self.nc = nc
        self.ops = {e: [] for e in ("pe", "act", "dve", "pool", "sp")}
        self.ndma = {e: 0 for e in self.ops}
        self.known = {e: {f: -1 for f in self.ops} for e in self.ops}
        self.kd_upto = {e: {f: -1 for f in self.ops} for e in self.ops}
        self.kd_set = {e: {f: set() for f in self.ops} for e in self.ops}
        self.bufs = {}

    def buf(self, name):
        b = self.bufs.get(name)
        if b is None:
            b = Buf(name)
            self.bufs[name] = b
        return b

    def _norm(self, lst):
        out = []
        for x in lst or []:
            if isinstance(x, (list, tuple)):
                out.extend(self._norm(x))
            elif isinstance(x, str):
                out.append(self.buf(x))
            else:
                out.append(x)
        return out

    def _satisfied(self, E, d):
        if d.dma:
            return d.dma_idx <= self.kd_upto[E][d.eng] or d.dma_idx in self.kd_set[E][d.eng]
        return d.seq <= self.known[E][d.eng]

    def _learn(self, E, d):
        if d.dma:
            F = d.eng
            s = self.kd_set[E][F]
            s.add(d.dma_idx)
            u = max(self.kd_upto[E][F], d.dma_idx - DMAQ_K)
            while (u + 1) in s:
                u += 1
            self.kd_upto[E][F] = u
            if len(s) > 64:
                self.kd_set[E][F] = {x for x in s if x > u}
        elif d.seq > self.known[E][d.eng]:
            self.known[E][d.eng] = d.seq

    def op(self, eng, fn, reads=None, writes=None, dma=False, wacc=None):
        reads = self._norm(reads)
        writes = self._norm(writes)
        wacc = self._norm(wacc)
        psr = [b for b in reads if b.name[:2] in ("pT", "pM", "pS", "pO")]
        if psr:
            reads = [b for b in reads if b not in psr]
            writes = writes + [b for b in psr if b not in writes]
        if fn is not None:
            fn = _freeze(fn)
        o = Op(eng, fn, dma)
        o.seq = len(self.ops[eng])
        if dma:
            o.dma_idx = self.ndma[eng]
            self.ndma[eng] += 1
        raw = set()
        cand = []
        for b in reads:
            for d in b.w:
                raw.add(id(d))
                cand.append(d)
        for b in writes:
            cand.extend(b.w)
            cand.extend(b.r)
        for b in wacc:
            cand.extend(b.r)
        seen = set()
        uniq = []
        for d in cand:
            if id(d) not in seen:
                seen.add(id(d))
                uniq.append(d)
        uniq.sort(key=lambda d: -(d.dma_idx if d.dma else d.seq))
        for d in uniq:
            if (not d.dma) and (not dma) and d.eng == eng and id(d) not in raw:
                continue
            if self._satisfied(eng, d):
                continue
            o.deps.append(d)
            d.signal = True
            self._learn(eng, d)
        for b in writes:
            b.w = [o]
            b.r = []
        for b in wacc:
            b.w.append(o)
            if len(b.w) > 40:
                b.w = _compress(b.w)
        for b in reads:
            if fn is None:
                break
            if not dma:
                b.r = [x for x in b.r if x.dma or x.eng != eng]
            b.r.append(o)
            if len(b.r) > 40:
                b.r = _compress(b.r)
        self.ops[eng].append(o)
        return o

    def pe(self, fn, r=None, w=None):
        return self.op("pe", fn, r, w)

    def act(self, fn, r=None, w=None):
        return self.op("act", fn, r, w)

    def dve(self, fn, r=None, w=None):
        return self.op("dve", fn, r, w)

    def pool(self, fn, r=None, w=None):
        return self.op("pool", fn, r, w)

    def barrier(self, fn):
        allb = list(self.bufs.values())
        self.op("dve", fn, allb, allb + [self.buf("FENCE")])
        for e in ("pe", "act", "pool", "sp"):
            self.op(e, None, [self.buf("FENCE")], None)

    def dma(self, eng, out, in_, r=None, w=None, wacc=None):
        return self.op(eng, lambda e: e.dma_start(out=out, in_=in_), r, w, dma=True, wacc=wacc)

    def emit(self):
        nc = self.nc
        nsig = {}
        for e in COMPUTE:
            c = 0
            for o in self.ops[e]:
                if o.signal and not o.dma:
                    o.sig_idx = c
                    c += 1
            nsig[e] = c
        es = ExitStack()
        sems = {}
        for e in COMPUTE:
            n_ep = max(1, (nsig[e] + SEM_EPOCH - 1) // SEM_EPOCH)
            sems[e] = [es.enter_context(nc.semaphore(f"tl_{e}_{i}")) for i in range(n_ep)]
        dsems = {}
        per_ep = DMA_EPOCH * DMAQ_K
        for e in self.ops:
            if self.ndma[e] == 0:
                continue
            n_ep = (self.ndma[e] + per_ep - 1) // per_ep
            dsems[e] = [[es.enter_context(nc.semaphore(f"dq_{e}_{i}_{k}")) for k in range(DMAQ_K)]
                        for i in range(n_ep)]

        def dma_sem(e, idx):
            return dsems[e][idx // per_ep][idx % DMAQ_K], 16 * ((idx % per_ep) // DMAQ_K + 1)

        def wait_for(engobj, d):
            if d.dma:
                s, v = dma_sem(d.eng, d.dma_idx)
            else:
                s = sems[d.eng][d.sig_idx // SEM_EPOCH]
                v = d.sig_idx % SEM_EPOCH + 1
            engobj.wait_ge(s, v)

        prog = self

        def run_engine(ename, engobj):
            for o in prog.ops[ename]:
                for d in o.deps:
                    wait_for(engobj, d)
                if o.dma:
                    if o.dma_idx >= DMAQ_K:
                        s, v = dma_sem(ename, o.dma_idx - DMAQ_K)
                        engobj.wait_ge(s, v)
                    inst = o.fn(engobj)
                    s, v = dma_sem(ename, o.dma_idx)
                    inst.then_inc(s, 16)
                elif o.fn is not None:
                    inst = o.fn(engobj)
                    if o.signal:
                        inst.then_inc(sems[ename][o.sig_idx // SEM_EPOCH], 1)
            n = prog.ndma[ename]
            for idx in range(max(0, n - DMAQ_K), n):
                s, v = dma_sem(ename, idx)
                engobj.wait_ge(s, v)

        with nc.Block() as block:
            @block.tensor
            def _(e):
                run_engine("pe", e)

            @block.scalar
            def _(e):
                run_engine("act", e)

            @block.vector
            def _(e):
                run_engine("dve", e)

            @block.gpsimd
            def _(e):
                run_engine("pool", e)

            @block.sync
            def _(e):
                run_engine("sp", e)
        es.close()


def build_program(n_layers=4, dbg=None):
    DBG = dbg or {}
    nc = bass.Bass("TRN2", target_bir_lowering=False)
    P = Prog(nc)
    es = ExitStack()

    def din(name, shape, dt=F32):
        return nc.dram_tensor(name, list(shape), dt, kind="ExternalInput").ap()

    def dscr(name, shape, dt=F32):
        return nc.dram_tensor(name, list(shape), dt, kind="Internal").ap()

    def sb(name, shape, dt=F32):
        return es.enter_context(nc.sbuf_tensor("s_" + name, list(shape), dt))

    def ps(name, shape, dt=F32):
        return es.enter_context(nc.psum_tensor("p_" + name, list(shape), dt))

    x_in = din("x", [S, D])
    cT_in = din("cT", [128, 8])
    rel_bias = din("rel_bias", [32, 8])
    relT_in = din("relT", [8, 128, 256])
    mask0_in = din("mask0", [128, 256])
    ident_in = din("ident", [128, 128])
    tri_in = din("tri", [128, 128])
    bones_in = din("bones", [128, 128])
    cind_in = din("cind", [128, 2])
    stri_in = din("stri", [128, 128])
    tabinit_in = din("tabinit", [TABROWS, 4])
    rcst_in = din("rcst", [128, 172])
    pidx_in = din("pidx", [128, 2])
    NLW = n_layers
    ada_w = din("ada_w", [NLW, D, 6 * D])
    ada_b = din("ada_b", [4, 6 * D])
    norm1_w = din("norm1_w", [4, D])
    norm2_w = din("norm2_w", [4, D])
    even_w_in = din("even_w_in", [2, D, 2320])
    even_w_out = din("even_w_out", [2, D, D])
    a_q_norm = din("a_q_norm", [2, 64])
    a_k_norm = din("a_k_norm", [2, 64])
    a_sinks = din("a_sinks", [2, 8])
    b_gate_up = din("b_gate_up", [2, 16, 256])
    b_gate_bias = din("b_gate_bias", [2, 256])
    b_out_norm = din("b_out_norm", [2, 128])
    odd_w_in = din("odd_w_in", [2, D, 3072])
    odd_w_out = din("odd_w_out", [2, D, D])
    c_q_norm = din("c_q_norm", [2, 128])
    c_k_norm = din("c_k_norm", [2, 128])
    moe_wr = din("moe_wr", [4, D, 36])
    moe_br = din("moe_br", [4, 36])
    moe_w1 = din("moe_w1", [NLW, 32, D, 512])
    moe_w3 = din("moe_w3", [NLW, 32, D, 512])
    moe_w2 = din("moe_w2", [NLW, 32, 512, D])
    out = nc.dram_tensor("out", [S, D], F32, kind="ExternalOutput").ap()

    XR = dscr("XR", [S, D])
    H2 = dscr("H2", [S, D], BF16)
    TAB = dscr("TAB", [TABROWS, 4])
    MO = dscr("MO", [2 * MOH, D])
    ATT = dscr("ATT", [S, D], BF16)
    QT = dscr("QT", [8, 128, S], BF16)
    KT = dscr("KT", [8, 128, S], BF16)
    VA = dscr("VA", [S, 8, 129], BF16)

    R1 = sb("R1", [128, 24576], BF16)
    R2 = sb("R2", [128, 8448], F32)
    WOUT = sb("WOUT", [128, 8, 1024], BF16)
    rows = sb("rows", [128, 7, 1024])
    BS = sb("BS", [128, 8, 256])
    BM1 = sb("BM1", [128, 8, 128])
    relfar = sb("relfar", [128, 8])
    identf = sb("identf", [128, 128])
    identb = sb("identb", [128, 128], BF16)
    trif = sb("trif", [128, 128])
    bonesf = sb("bonesf", [128, 128])
    cindf = sb("cindf", [128, 2])
    strib = sb("strib", [128, 128], BF16)
    onesb = sb("onesb", [128, 128], BF16)
    bonesb = sb("bonesb", [128, 128], BF16)
    rcst = sb("rcst", [128, 172])
    pidx = sb("pidx", [128, 2])
    cTs = sb("cTs", [128, 8])
    xt0_ = sb("xt0", [128, D])
    xt = [xt0_, xt0_]
    m1 = sb("m1", [128, D])
    junk = sb("junk", [128, D])
    m2 = junk
    condbc = junk[:, :].rearrange("p (k n) -> p k n", k=8)
    tmpf = sb("tmpf", [128, D])
    hb = sb("hb", [128, D], BF16)
    hT = sb("hT", [128, 8, 128], BF16)
    proj = sb("proj", [128, 2320])
    att = sb("att", [128, D], BF16)
    h2b = hb
    sm = sb("sm", [128, 64])
    wrow = sb("wrow", [128, 640])
    WR = sb("WR", [128, 8, 36], BF16)
    brow = sb("brow", [128, 36])
    adab = m1[:, 0:512]
    EV = sb("EV", [128, 7888])

    class Carver:
        def __init__(self):
            self.off = 0

        def get(self, shape, dt=F32):
            np_ = shape[0]
            n = 1
            for d_ in shape[1:]:
                n *= d_
            sz = 2 if dt == BF16 else 4
            nb = (n * sz + 3) // 4 * 4
            c0 = self.off // 4
            self.off += nb
            assert self.off <= 7888 * 4, self.off
            v = EV[0:np_, c0:c0 + nb // 4]
            if dt != F32:
                v = v.bitcast(dt)[:, 0:n]
            if len(shape) == 3:
                v = v.rearrange("p (a b) -> p a b", a=shape[1])
            elif len(shape) == 4:
                v = v.rearrange("p (a b c) -> p a b c", a=shape[1], b=shape[2])
            return v

    cv = Carver()
    qkb = cv.get([128, 512], BF16)
    kdup = cv.get([128, 2, 128], BF16)
    qkT = [cv.get([128, 6, 128], BF16) for i in range(2)]
    vaS = [cv.get([128, 2, 65], BF16) for i in range(2)]
    sc = cv.get([128, 256])
    pexp = [cv.get([128, 256], BF16) for i in range(2)]
    abT = cv.get([32, 128], BF16)
    abp = cv.get([128, 32], BF16)
    gnb = cv.get([128, 256], BF16)
    zt = cv.get([128, 256])
    gneg = cv.get([128, 256])
    bsb = zt
    eb = cv.get([128, 256])
    enb = cv.get([128, 256])
    ekd = cv.get([128, 256])
    qd = cv.get([128, 256], BF16)
    kd = cv.get([128, 256], BF16)
    ku = cv.get([128, 256], BF16)
    vbb = cv.get([128, 512], BF16)
    sgn = cv.get([128, 512])
    dec = cv.get([64, 8])
    qdT = cv.get([64, 4, 128], BF16)
    qdm = cv.get([64, 4, 2, 128], BF16)
    kdT = cv.get([64, 4, 128], BF16)
    attT = cv.get([128, 128], BF16)
    St0 = cv.get([64, 4, 128])
    St1 = cv.get([64, 4, 128])
    St0b = cv.get([64, 4, 128], BF16)
    St1b = cv.get([64, 4, 128], BF16)
    gbias = cv.get([128, 256])
    onorm1 = cv.get([128, 128])
    gupf = cv.get([32, 256])
    gupb = cv.get([32, 256], BF16)
    esink = cv.get([128, 8])
    ev_even = cv.off
    cv = Carver()
    qnb = cv.get([128, 2048], BF16)
    vast = cv.get([128, 8, 129], BF16)
    kmsum = cv.get([128, 8, 32])
    kmean = cv.get([128, 8, 16], BF16)
    qTt = cv.get([128, 2, 256], BF16)
    scs = cv.get([128, 2, 16])
    sel = cv.get([128, 2, 2, 16])
    pT2 = [cv.get([128, 256], BF16) for i in range(4)]
    acc = cv.get([128, 2, 2, 129])
    attq = cv.get([128, 2, 256], BF16)
    sco = cv.get([128, 256])
    cv = Carver()
    xg = [cv.get([128, D], BF16) for i in range(2)]
    xgT = [cv.get([128, 8, 128], BF16) for i in range(2)]
    s1t = cv.get([128, 128])
    actT = [cv.get([128, 4, 128], BF16) for i in range(2)]
    rtab = cv.get([128, 6, 32])
    blkE = cv.get([128, 96])
    widx1f = cv.get([128, 8, 96])
    widx1i = cv.get([128, 8, 96], I32)
    widx2f = cv.get([128, 4, 96])
    widx2i = cv.get([128, 4, 96], I32)
    M12all = R2[:, 0:2048].rearrange("p (t k e) -> p t k e", t=32, k=2)
    slall = R2[:, 2048:2112].rearrange("p (t k) -> p t k", t=32)
    recall = R2[:, 2112:2368].rearrange("p (t k c) -> p t k c", t=32, k=2)
    top8 = sb("top8", [128, 8])
    lg = sb("lg", [128, 36])
    msk = sb("msk", [128, 32])
    M12 = sb("M12", [128, 2, 32])
    Mb = sb("Mb", [128, 32], BF16)
    cbase = sb("cbase", [128, 32])
    Cc = sb("Cc", [128, 32])
    prod = sb("prod", [128, 2, 32])
    desti = [sb(f"desti{i}", [128, 2], I32) for i in range(2)]
    tabt = [sb(f"tabt{i}", [128, 4]) for i in range(2)]
    idxi = [sb(f"idxi{i}", [128, 2], I32) for i in range(2)]
    yo = [m1, junk]
    print("SBUF bytes remaining/partition:", nc.sbuf_bytes_remaining)

    pT = [ps(f"pT{i}", [128, 8, 128], BF16) for i in range(2)]
    pM = [ps(f"pM{i}", [128, 512]) for i in range(2)]
    pS = [ps(f"pS{i}", [128, 512]) for i in range(2)]
    pO = [ps(f"pO{i}", [128, 512]) for i in range(2)]

    R1w = R1[:, :].rearrange("p (k n) -> p k n", k=8)

    def ew(eb_, which):
        base = eb_ * 12288
        if which == 0:
            return R1[:, base:base + 4096].rearrange("p (k n) -> p k n", k=8)
        if which == 1:
            return R1[:, base + 4096:base + 8192].rearrange("p (k n) -> p k n", k=8)
        return R1[:, base + 8192:base + 12288].rearrange("p (k n) -> p k n", k=4)

    def stg(i):
        return R2[:, i * 4096:(i + 1) * 4096]

    WINT = [f"R1_{a}_{b}" for a in range(2) for b in range(3)]

    def bc(ap):
        return ap.partition_broadcast(128).squeeze(1)

    def fence(tags, eng="dve"):
        P.op(eng, lambda e: e.memset(sm[:, 62:63], 0.0), tags, tags + ["sm62"])

    R2b = R2[:, :].bitcast(BF16)
    KTs = R2b[:, 0:8192].rearrange("p (h t) -> p h t", h=2)
    VAs = R2b[:, 8192:8192 + 8256].rearrange("p (t h c) -> p t h c", t=32, h=2)
    VAs2 = R2b[:, 8192:8192 + 8256].rearrange("p (t c) -> p t c", t=32)

    def mm(out_, lhsT, rhs, start=True, stop=True, r=None, w=None):
        P.pe(lambda e: e.matmul(out_, lhsT=lhsT, rhs=rhs, start=start, stop=stop), r=r, w=w)

    def tp(out_, in_, ident, r=None, w=None):
        P.pe(lambda e: e.transpose(out=out_, in_=in_, identity=ident), r=r, w=w)

    P.dma("sp", identf[:], ident_in, w=["identf"])
    P.dma("sp", trif[:], tri_in, w=["trif"])
    P.dma("sp", bonesf[:], bones_in, w=["bonesf"])
    P.dma("sp", cindf[:], cind_in, w=["cindf"])
    P.dma("sp", rcst[:], rcst_in, w=["rcst"])
    P.dma("sp", pidx[:], pidx_in, w=["pidx"])
    P.dma("sp", cTs[:], cT_in, w=["cTs"])
    P.dma("sp", tmpf[:, 0:128], stri_in, w=["tmpf"])
    P.dve(lambda e: e.tensor_copy(out=strib[:], in_=tmpf[:, 0:128]), r=["tmpf"], w=["strib"])
    P.dve(lambda e: e.tensor_copy(out=identb[:], in_=identf[:]), r=["identf"], w=["identb"])
    P.dve(lambda e: e.memset(onesb[:], 1.0), w=["onesb"])
    P.dve(lambda e: e.tensor_copy(out=bonesb[:], in_=bonesf[:]), r=["bonesf"], w=["bonesb"])
    P.dma("sp", BS[:], relT_in.rearrange("h k q -> k h q"), w=["BS"])
    P.dma("sp", m1[:, 0:256], mask0_in, w=["m1"])
    P.dve(lambda e: e.tensor_copy(out=BM1[:], in_=BS[:, :, 128:256]), r=["BS"], w=["BM"])
    for h in range(8):
        P.dve(lambda e, h=h: e.tensor_tensor(out=BS[:, h, :], in0=BS[:, h, :], in1=m1[:, 0:256], op=ALU.add),
              r=["BS", "m1"], w=["BS"])
    P.dma("sp", relfar[:], bc(rel_bias[31:32, :]), w=["relfar"])
    P.act(lambda e: e.activation(out=cTs[:], in_=cTs[:], func=AF.Silu), r=["cTs"], w=["cTs"])

    def rstd_from_ssq(ssq_ap, n, inv_n, tag):
        P.dve(lambda e: e.tensor_scalar(out=ssq_ap, in0=ssq_ap, scalar1=inv_n, scalar2=EPS, op0=ALU.mult, op1=ALU.add),
              r=[tag], w=[tag])
        P.act(lambda e: e.activation(out=ssq_ap, in_=ssq_ap, func=AF.Sqrt), r=[tag], w=[tag])
        P.dve(lambda e: e.reciprocal(out=ssq_ap, in_=ssq_ap), r=[tag], w=[tag])

    def load_weight_bf16(dst_view, src_ap, nk, ncols, dst_tags, engs=("act", "dve", "pool")):
        srcv = src_ap.rearrange("(k p) n -> p k n", p=128)
        cpp = 4096 // nk
        c0 = 0
        i = 0
        while c0 < ncols:
            cw = min(cpp, ncols - c0)
            part = i % 2
            sv = stg(part)[:, 0:nk * cw].rearrange("p (k n) -> p k n", k=nk)
            P.dma("sp", sv, srcv[:, :, c0:c0 + cw], w=[f"R2_{part}"])
            en = engs[i % len(engs)]
            P.op(en, lambda e, sv=sv, c0=c0, cw=cw, en=en: (e.copy if en == "act" else e.tensor_copy)(
                out=dst_view[:, :, c0:c0 + cw], in_=sv),
                 [f"R2_{part}"], None, wacc=dst_tags)
            c0 += cw
            i += 1

    def layer_start(l):
        even = (l % 2 == 0)
        j = l // 2
        P.barrier(lambda e: e.memset(sm[:, 62:63], 0.0))
        if l > 0:
            P.pool(lambda e: e.tensor_copy(out=rows[:, 6, :], in_=rows[:, 5, :]), r=["rows5"], w=["rows6"])
        P.dve(lambda e: e.memset(tmpf[:, 0:128], 1.0), w=["tmpf"])
        for k in range(8):
            P.dve(lambda e, k=k: e.tensor_scalar(out=condbc[:, k, :], in0=tmpf[:, 0:128], scalar1=cTs[:, k:k + 1],
                                                 scalar2=None, op0=ALU.mult), r=["tmpf", "cTs"], w=["junk"])
        aw = ada_w[l].rearrange("(k p) n -> p k n", p=128)
        for cch in range(12):
            part = cch % 2
            sv = stg(part)[:, :].rearrange("p (k n) -> p k n", k=8)
            P.dma("sp", sv, aw[:, :, cch * 512:(cch + 1) * 512], w=[f"R2_{part}"])
            P.dma("sp", adab[:], bc(ada_b[l:l + 1, cch * 512:(cch + 1) * 512]), w=["m1"])
            pm = pM[cch % 2]
            for k in range(8):
                mm(pm[:, :], condbc[:, k, :], sv[:, k, :], start=(k == 0), stop=(k == 7),
                   r=["junk", f"R2_{part}"], w=[f"pM{cch % 2}"])
            rr = cch // 2
            hf = cch % 2
            P.dve(lambda e, pm=pm, rr=rr, hf=hf: e.tensor_tensor(out=rows[:, rr, hf * 512:(hf + 1) * 512], in0=pm[:, :],
                                                                  in1=adab[:], op=ALU.add),
                  r=[f"pM{cch % 2}", "m1"], w=[f"rows{rr}"])
        for rr, nw in ((1, norm1_w), (4, norm2_w)):
            P.dma("sp", tmpf[:], bc(nw[l:l + 1, :]), w=["tmpf"])
            P.dve(lambda e, rr=rr: e.scalar_tensor_tensor(out=rows[:, rr, :], in0=rows[:, rr, :], scalar=1.0, in1=tmpf[:],
                                                          op0=ALU.add, op1=ALU.mult),
                  r=[f"rows{rr}", "tmpf"], w=[f"rows{rr}"])
        P.dma("sp", tmpf[:, 0:288].rearrange("p (k n) -> p k n", k=8), moe_wr[l].rearrange("(k p) n -> p k n", p=128),
              w=["tmpf"])
        P.dve(lambda e: e.tensor_copy(out=WR[:], in_=tmpf[:, 0:288].rearrange("p (k n) -> p k n", k=8)),
              r=["tmpf"], w=["WR"])
        P.dma("sp", brow[:], bc(moe_br[l:l + 1, :]), w=["brow"])
        P.dve(lambda e: e.memset(cbase[:], 0.0), w=["cbase"])
        P.dma("sp", TAB, tabinit_in, w=["TAB"])
        win = even_w_in[j] if even else odd_w_in[j]
        ncols = 2320 if even else 3072
        wo = even_w_out[j] if even else odd_w_out[j]
        load_weight_bf16(R1w[:, :, 0:ncols], win, 8, ncols, WINT)
        load_weight_bf16(WOUT[:, :, :], wo, 8, 1024, ["WOUTt"])
        if even:
            P.dma("sp", tmpf[:, 0:64], bc(a_q_norm[j:j + 1, :]), w=["tmpf"])
            P.dma("sp", tmpf[:, 64:128], bc(a_k_norm[j:j + 1, :]), w=["tmpf2"])
            for i in range(10):
                src = tmpf[:, 0:64] if i < 8 else tmpf[:, 64:128]
                P.dve(lambda e, i=i, src=src: e.tensor_copy(out=wrow[:, i * 64:(i + 1) * 64], in_=src),
                      r=["tmpf", "tmpf2"], w=["wrow"])
            P.dma("sp", gbias[:], bc(b_gate_bias[j:j + 1, :]), w=["gbias"])
            P.dve(lambda e: e.memset(gupf[:, :], 0.0), w=["gupf"])
            P.dma("sp", gupf[0:16, :], b_gate_up[j], w=["gupf"])
            P.dve(lambda e: e.tensor_copy(out=gupb[:, :], in_=gupf[:, :]), r=["gupf"], w=["gupb"])
            P.dve(lambda e: e.memset(abp[:, :], 0.0), w=["abp"])
            P.dma("sp", onorm1[:, :], bc(b_out_norm[j:j + 1, :]), w=["onorm4"])
            P.dma("sp", esink[:], bc(a_sinks[j:j + 1, :]), w=["esink"])
            P.act(lambda e: e.activation(out=esink[:], in_=esink[:], func=AF.Exp), r=["esink"], w=["esink"])
            P.dve(lambda e: e.memset(St0[:], 0.0), w=["St0"])
            P.dve(lambda e: e.memset(qdm[:], 0.0), w=["qdm"])
            for i in range(2):
                P.dve(lambda e, i=i: e.memset(vaS[i][:], 1.0), w=[f"vaS{i}"])
        else:
            P.dma("sp", wrow[:, 0:128], bc(c_q_norm[j:j + 1, :]), w=["wrow"])
            P.dma("sp", wrow[:, 128:256], bc(c_k_norm[j:j + 1, :]), w=["wrow2"])
            P.dve(lambda e: e.memset(vast[:], 1.0), w=["vast"])

    def load_x(l, t, xb):
        xs = xt[xb]
        tg = "xt0"
        rs = slice(t * 128, (t + 1) * 128)
        if l == 0:
            P.dma("sp", xs[:], x_in[rs, :], w=[tg])
        else:
            P.dma("sp", xs[:], XR[rs, :], r=[f"XR{t}"], w=[tg])
            P.dma("sp", m1[:], MO[rs, :], r=["MO"], w=["m1"])
            P.dma("sp", m2[:], MO[MOH + t * 128:MOH + (t + 1) * 128, :], r=["MO"], w=["junk"])
            P.pool(lambda e: e.tensor_tensor(out=m1[:], in0=m1[:], in1=m2[:], op=ALU.add), r=["m1", "junk"], w=["m1"])
            P.pool(lambda e: e.tensor_tensor(out=m1[:], in0=m1[:], in1=rows[:, 6, :], op=ALU.mult),
                   r=["m1", "rows6"], w=["m1"])
            P.dve(lambda e: e.tensor_tensor(out=xs[:], in0=xs[:], in1=m1[:], op=ALU.add), r=[tg, "m1"], w=[tg])

    def norm_mod(xs, xtag, grow, srow, outb, otag):
        P.act(lambda e: e.activation(out=junk[:], in_=xs[:], func=AF.Square, accum_out=sm[:, 0:1]),
              r=[xtag], w=["junk", "sm0"])
        rstd_from_ssq(sm[:, 0:1], 1, 1.0 / D, "sm0")
        P.dve(lambda e: e.scalar_tensor_tensor(out=tmpf[:], in0=xs[:], scalar=sm[:, 0:1], in1=rows[:, grow, :],
                                               op0=ALU.mult, op1=ALU.mult),
              r=[xtag, "sm0", f"rows{grow}"], w=["tmpf"])
        P.pool(lambda e: e.tensor_tensor(out=outb[:], in0=tmpf[:], in1=rows[:, srow, :], op=ALU.add),
               r=["tmpf", f"rows{srow}"], w=[otag])

    tcount = [0]

    def transpose8(src, stag, dst, dtag):
        i = tcount[0] % 2
        tcount[0] += 1
        for k in range(8):
            tp(pT[i][:, k, :], src[:, k * 128:(k + 1) * 128], identb[:], r=[stag, "identb"], w=[f"pT{i}"])
        P.act(lambda e: e.copy(out=dst[:], in_=pT[i][:]), r=[f"pT{i}"], w=[dtag])

    mcount = [0]

    def project(srcT, stag, wview, wtag, ncols, dst, dtag):
        c0 = 0
        while c0 < ncols:
            cw = min(512, ncols - c0)
            i = mcount[0] % 2
            mcount[0] += 1
            for k in range(8):
                mm(pM[i][:, 0:cw], srcT[:, k, :], wview[:, k, c0:c0 + cw], start=(k == 0), stop=(k == 7),
                   r=[stag, wtag], w=[f"pM{i}"])
            if i == 0:
                P.act(lambda e, i=i, c0=c0, cw=cw: e.copy(out=dst[:, c0:c0 + cw], in_=pM[i][:, 0:cw]),
                      r=[f"pM{i}"], w=[dtag])
            else:
                P.dve(lambda e, i=i, c0=c0, cw=cw: e.tensor_copy(out=dst[:, c0:c0 + cw], in_=pM[i][:, 0:cw]),
                      r=[f"pM{i}"], w=[dtag])
            c0 += cw

    def tail(l, t, xb, attsrc, atag):
        xs = xt[xb]
        tg = "xt0"
        rs = slice(t * 128, (t + 1) * 128)
        transpose8(attsrc, atag, hT, "hT")
        for c in range(2):
            i = mcount[0] % 2
            mcount[0] += 1
            for k in range(8):
                mm(pM[i][:, :], hT[:, k, :], WOUT[:, k, c * 512:(c + 1) * 512], start=(k == 0), stop=(k == 7),
                   r=["hT", "WOUTt"], w=[f"pM{i}"])
            P.dve(lambda e, i=i, c=c: e.tensor_tensor(out=tmpf[:, c * 512:(c + 1) * 512], in0=pM[i][:, :],
                                                      in1=rows[:, 2, c * 512:(c + 1) * 512], op=ALU.mult),
                  r=[f"pM{i}", "rows2"], w=["tmpf"])
        P.pool(lambda e: e.tensor_tensor(out=xs[:], in0=xs[:], in1=tmpf[:], op=ALU.add), r=[tg, "tmpf"], w=[tg])
        P.dma("sp", XR[rs, :], xs[:], r=[tg], w=[f"XR{t}"])
        norm_mod(xs, tg, 4, 3, h2b, "hb")
        P.dma("sp", H2[rs, :], h2b[:], r=["hb"], wacc=["H2"])
        transpose8(h2b, "hb", hT, "hT")
        i = mcount[0] % 2
        mcount[0] += 1
        for k in range(8):
            mm(pM[i][:, 0:36], hT[:, k, :], WR[:, k, :], start=(k == 0), stop=(k == 7), r=["hT", "WR"], w=[f"pM{i}"])
        P.dve(lambda e: e.tensor_tensor(out=lg[:], in0=pM[i][:, 0:36], in1=brow[:], op=ALU.add),
              r=[f"pM{i}", "brow"], w=["lg"])
        RT = ["rt"]
        V = P.dve
        V(lambda e: e.tensor_reduce(out=sm[:, 8:9], in_=lg[:, 0:4], axis=AX.X, op=ALU.max), r=["lg"], w=RT)
        V(lambda e: e.tensor_scalar(out=sm[:, 12:16], in0=lg[:, 0:4], scalar1=sm[:, 8:9], scalar2=None, op0=ALU.subtract),
          r=["lg"] + RT, w=RT)
        P.act(lambda e: e.activation(out=sm[:, 16:20], in_=sm[:, 12:16], func=AF.Exp, accum_out=sm[:, 9:10]), r=RT, w=RT)
        V(lambda e: e.reciprocal(out=sm[:, 9:10], in_=sm[:, 9:10]), r=RT, w=RT)
        V(lambda e: e.tensor_scalar(out=sm[:, 20:24], in0=sm[:, 12:16], scalar1=0.0, scalar2=None, op0=ALU.is_ge),
          r=RT, w=RT)
        V(lambda e: e.tensor_scalar(out=sm[:, 20:24], in0=sm[:, 20:24], scalar1=BIG, scalar2=-BIG, op0=ALU.mult,
                                    op1=ALU.add), r=RT, w=RT)
        V(lambda e: e.tensor_tensor(out=msk[:, :].rearrange("p (g e) -> p g e", g=4),
                                    in0=lg[:, 4:36].rearrange("p (g e) -> p g e", g=4),
                                    in1=sm[:, 20:24].unsqueeze(2).to_broadcast([128, 4, 8]), op=ALU.add),
          r=["lg"] + RT, w=["msk"])
        V(lambda e: e.max(out=top8[:], in_=msk[:]), r=["msk"], w=["top8"])
        V(lambda e: e.tensor_scalar(out=M12[:, 0, :], in0=msk[:], scalar1=top8[:, 0:1], scalar2=None, op0=ALU.is_equal),
          r=["msk", "top8"], w=["M12"])
        V(lambda e: e.tensor_scalar(out=M12[:, 1, :], in0=msk[:], scalar1=top8[:, 1:2], scalar2=None, op0=ALU.is_equal),
          r=["msk", "top8"], w=["M12"])
        V(lambda e: e.tensor_tensor(out=sm[:, 10:11], in0=top8[:, 1:2], in1=top8[:, 0:1], op=ALU.subtract),
          r=["top8"], w=RT)
        P.act(lambda e: e.activation(out=sm[:, 10:11], in_=sm[:, 10:11], func=AF.Exp), r=RT, w=RT)
        V(lambda e: e.tensor_scalar(out=sm[:, 10:11], in0=sm[:, 10:11], scalar1=1.0, scalar2=None, op0=ALU.add), r=RT, w=RT)
        V(lambda e: e.reciprocal(out=sm[:, 10:11], in_=sm[:, 10:11]), r=RT, w=RT)
        rc = recall[:, t, :, :]
        rtag = "R2_0"
        V(lambda e: e.tensor_tensor(out=rc[:, 0, 2:3], in0=sm[:, 10:11], in1=sm[:, 9:10], op=ALU.mult), r=RT, w=[rtag])
        V(lambda e: e.tensor_tensor(out=rc[:, 1, 2:3], in0=sm[:, 9:10], in1=rc[:, 0, 2:3], op=ALU.subtract),
          r=RT + [rtag], w=[rtag])
        V(lambda e: e.tensor_scalar(out=rc[:, 0, 0:2], in0=pidx[:, 0:1].to_broadcast([128, 2]), scalar1=float(t * 128),
                                    scalar2=None, op0=ALU.add), r=["pidx"], w=[rtag])
        V(lambda e: e.tensor_scalar(out=rc[:, 1, 0:1], in0=pidx[:, 0:1], scalar1=float(t * 128), scalar2=None,
                                    op0=ALU.add), r=["pidx"], w=[rtag])
        V(lambda e: e.tensor_scalar(out=rc[:, 1, 1:2], in0=pidx[:, 0:1], scalar1=float(t * 128 + MOH), scalar2=None,
                                    op0=ALU.add), r=["pidx"], w=[rtag])
        V(lambda e: e.memset(rc[:, :, 3:4], 0.0), w=[rtag])
        V(lambda e: e.tensor_tensor(out=Mb[:], in0=M12[:, 0, :], in1=M12[:, 1, :], op=ALU.add), r=["M12"], w=["Mb"])
        V(lambda e: e.tensor_copy(out=M12all[:, t, :, :], in_=M12[:]), r=["M12"], w=[rtag])
        j_ = mcount[0] % 2
        mcount[0] += 1
        mm(pM[j_][:, 0:32], strib[:], Mb[:], r=["strib", "Mb"], w=[f"pM{j_}"])
        mm(pM[j_][:, 64:96], onesb[:], Mb[:], r=["onesb", "Mb"], w=[f"pM{j_}"])
        V(lambda e: e.tensor_tensor(out=Cc[:], in0=pM[j_][:, 0:32], in1=cbase[:], op=ALU.add),
          r=[f"pM{j_}", "cbase"], w=["Cc"])
        V(lambda e: e.tensor_tensor(out=cbase[:], in0=pM[j_][:, 64:96], in1=cbase[:], op=ALU.add),
          r=[f"pM{j_}", "cbase"], w=["cbase"])
        V(lambda e: e.tensor_tensor(out=prod[:], in0=M12[:], in1=Cc[:, :].unsqueeze(1).to_broadcast([128, 2, 32]),
                                    op=ALU.mult), r=["M12", "Cc"], w=["prod"])
        V(lambda e: e.tensor_reduce(out=slall[:, t, :], in_=prod[:], axis=AX.X, op=ALU.add), r=["prod"], w=[rtag])

    def route_finalize(l):
        P.barrier(lambda e: e.memset(sm[:, 62:63], 0.0))
        V = P.dve
        FT = ["rfin"]
        for half in range(2):
            es_ = slice(half * 16, (half + 1) * 16)
            t3 = tmpf[:, :].rearrange("p (a b) -> p a b", a=16)
            V(lambda e: e.tensor_tensor(out=t3, in0=cbase[:, es_].unsqueeze(2).to_broadcast([128, 16, 64]),
                                        in1=rcst[:, 0:64].unsqueeze(1).to_broadcast([128, 16, 64]), op=ALU.is_gt),
              r=["cbase", "rcst"], w=["tmpf"])
            V(lambda e: e.tensor_reduce(out=rtab[:, 0, es_], in_=t3, axis=AX.X, op=ALU.add), r=["tmpf"], w=FT)
        V(lambda e: e.tensor_scalar(out=rtab[:, 1, :], in0=rtab[:, 0, :], scalar1=128.0, scalar2=None, op0=ALU.mult),
          r=FT, w=FT)
        V(lambda e: e.memset(rtab[:, 5, :], 0.0), w=FT)
        V(lambda e: e.tensor_tensor_scan(out=rtab[:, 2, :], data0=rtab[:, 1, :], data1=rtab[:, 5, :], initial=0.0,
                                         op0=ALU.add, op1=ALU.add), r=FT, w=FT)
        V(lambda e: e.tensor_tensor(out=rtab[:, 3, :], in0=rtab[:, 2, :], in1=rtab[:, 1, :], op=ALU.subtract),
          r=FT, w=FT)
        V(lambda e: e.memset(blkE[:, :], 0.0), w=["blkE"])
        for ex in range(32):
            V(lambda e, ex=ex: e.scalar_tensor_tensor(out=blkE[:, :], in0=rcst[:, 64:160], scalar=rtab[:, 2, ex:ex + 1],
                                                      in1=blkE[:, :], op0=ALU.is_ge, op1=ALU.add),
              r=FT + ["rcst", "blkE"], w=["blkE"])
        V(lambda e: e.tensor_scalar(out=blkE[:, :], in0=blkE[:, :], scalar1=31.0, scalar2=float(32 * l), op0=ALU.min,
                                    op1=ALU.add), r=["blkE"], w=["blkE"])
        for k in range(8):
            V(lambda e, k=k: e.tensor_scalar(out=widx1f[:, k, :], in0=blkE[:, :], scalar1=1024.0,
                                             scalar2=rcst[:, 160 + k:161 + k], op0=ALU.mult, op1=ALU.add),
              r=["blkE", "rcst"], w=["widx1f"])
        V(lambda e: e.tensor_copy(out=widx1i[:, :, :], in_=widx1f[:, :, :]), r=["widx1f"], w=["widx1i"])
        for k in range(4):
            V(lambda e, k=k: e.tensor_scalar(out=widx2f[:, k, :], in0=blkE[:, :], scalar1=512.0,
                                             scalar2=rcst[:, 168 + k:169 + k], op0=ALU.mult, op1=ALU.add),
              r=["blkE", "rcst"], w=["widx2f"])
        V(lambda e: e.tensor_copy(out=widx2i[:, :, :], in_=widx2f[:, :, :]), r=["widx2f"], w=["widx2i"])
        for t in range(NT):
            rb_ = t % 2
            di = desti[rb_]
            dtag = f"desti{rb_}"
            V(lambda e, t=t: e.tensor_tensor(out=prod[:], in0=M12all[:, t, :, :],
                                             in1=rtab[:, 3, :].unsqueeze(1).to_broadcast([128, 2, 32]), op=ALU.mult),
              r=["R2_0"] + FT, w=["prod"])
            V(lambda e: e.tensor_reduce(out=sm[:, 26:28], in_=prod[:], axis=AX.X, op=ALU.add), r=["prod"], w=["rt"])
            V(lambda e, t=t: e.tensor_tensor(out=sm[:, 26:28], in0=sm[:, 26:28], in1=slall[:, t, :], op=ALU.add),
              r=["rt", "R2_0"], w=["rt"])
            V(lambda e, di=di: e.tensor_copy(out=di[:], in_=sm[:, 26:28]), r=["rt"], w=[dtag])
            for k in range(2):
                P.op("pool", lambda e, k=k, di=di, t=t: e.indirect_dma_start(
                    out=TAB, out_offset=bass.IndirectOffsetOnAxis(ap=di[:, k:k + 1], axis=0), in_=recall[:, t, k, :],
                    in_offset=None), ["R2_0", dtag], None, dma=True, wacc=["TAB"])

    def even_layer(l):
        for t in range(DBG.get("tiles", NT)):
            xb = t % 2
            cur = t % 2
            prv = 1 - cur
            load_x(l, t, xb)
            norm_mod(xt[xb], "xt0", 1, 0, hb, "hb")
            transpose8(hb, "hb", hT, "hT")
            project(hT, "hT", R1w, WINT, 2320, proj, "proj")
            if DBG.get("cut") == 1:
                P.dma("sp", out[t * 128:(t + 1) * 128, :], proj[:, 0:1024], r=["proj"], w=[f"out{t}"])
                continue
            P.dve(lambda e: e.tensor_tensor(out=tmpf[:, 0:640], in0=proj[:, 0:640], in1=proj[:, 0:640], op=ALU.mult),
                  r=["proj"], w=["tmpf"])
            P.dve(lambda e: e.tensor_reduce(out=sm[:, 32:42], in_=tmpf[:, 0:640].rearrange("p (g d) -> p g d", g=10),
                                            axis=AX.X, op=ALU.add), r=["tmpf"], w=["sm32"])
            rstd_from_ssq(sm[:, 32:42], 10, 1.0 / 64, "sm32")
            P.dve(lambda e: e.tensor_tensor(out=tmpf[:, 0:640].rearrange("p (g d) -> p g d", g=10),
                                            in0=proj[:, 0:640].rearrange("p (g d) -> p g d", g=10),
                                            in1=sm[:, 32:42].unsqueeze(2).to_broadcast([128, 10, 64]), op=ALU.mult),
                  r=["proj", "sm32"], w=["tmpf"])
            P.pool(lambda e: e.tensor_tensor(out=qkb[:], in0=tmpf[:, 0:512], in1=wrow[:, 0:512], op=ALU.mult),
                   r=["tmpf", "wrow"], w=["qkb"])
            for hf in range(2):
                P.pool(lambda e, hf=hf: e.tensor_tensor(out=kdup[:, :, hf * 64:(hf + 1) * 64],
                                                        in0=tmpf[:, 512:640].rearrange("p (g d) -> p g d", g=2),
                                                        in1=wrow[:, 512:640].rearrange("p (g d) -> p g d", g=2),
                                                        op=ALU.mult), r=["tmpf", "wrow"], w=["kdup"])
            P.act(lambda e: e.copy(out=vaS[cur][:, :, 0:64], in_=proj[:, 640:768].rearrange("p (g d) -> p g d", g=2)),
                  r=["proj"], w=[f"vaS{cur}"])
            i = tcount[0] % 2
            tcount[0] += 1
            for pr in range(4):
                tp(pT[i][:, pr, :], qkb[:, pr * 128:(pr + 1) * 128], identb[:], r=["qkb", "identb"], w=[f"pT{i}"])
            for g in range(2):
                tp(pT[i][:, 4 + g, :], kdup[:, g, :], identb[:], r=["kdup", "identb"], w=[f"pT{i}"])
            P.act(lambda e, i=i: e.copy(out=qkT[cur][:, :, :], in_=pT[i][:, 0:6, :]), r=[f"pT{i}"], w=[f"qkT{cur}"])
            if DBG.get("cut") == 2:
                P.dve(lambda e: e.tensor_copy(out=tmpf[:, 0:768], in_=qkT[cur][:, :, :].rearrange("p a b -> p (a b)")), r=[f"qkT{cur}"], w=["tmpf"])
                P.dma("sp", out[t * 128:(t + 1) * 128, 0:768], tmpf[:, 0:768], r=["tmpf"], w=[f"out{t}"])
                continue
            for hg in range(2):
                po = pO[hg]
                for hh in range(4):
                    h = hg * 4 + hh
                    g = h // 4
                    pr = h // 2
                    b0 = (h % 2) * 64
                    psx = pS[h % 2]
                    mm(psx[:, 0:128], qkT[cur][b0:b0 + 64, 4 + g, :], qkT[cur][b0:b0 + 64, pr, :],
                       r=[f"qkT{cur}"], w=[f"pS{h % 2}"])
                    if t > 0:
                        mm(psx[:, 128:256], qkT[prv][b0:b0 + 64, 4 + g, :], qkT[cur][b0:b0 + 64, pr, :],
                           r=[f"qkT{cur}", f"qkT{prv}"], w=[f"pS{h % 2}"])
                    P.dve(lambda e, psx=psx, h=h: e.scalar_tensor_tensor(out=sc[:], in0=psx[:, 0:256], scalar=0.125,
                                                                         in1=BS[:, h, :], op0=ALU.mult, op1=ALU.add),
                          r=[f"pS{h % 2}", "BS"], w=["sc"])
                    pe_ = pexp[h % 2]
                    P.act(lambda e, pe_=pe_: e.activation(out=pe_[:], in_=sc[:], func=AF.Exp), r=["sc"], w=[f"pexp{h % 2}"])
                    mm(po[:, hh * 65:(hh + 1) * 65], pe_[:, 0:128], vaS[cur][:, g, :], start=True, stop=(t == 0),
                       r=[f"pexp{h % 2}", f"vaS{cur}"], w=[f"pO{hg}"])
                    if t > 0:
                        mm(po[:, hh * 65:(hh + 1) * 65], pe_[:, 128:256], vaS[prv][:, g, :], start=False, stop=True,
                           r=[f"pexp{h % 2}", f"vaS{prv}"], w=[f"pO{hg}"])
                pov = po[:, 0:260].rearrange("p (h c) -> p h c", h=4)
                P.dve(lambda e, pov=pov, hg=hg: e.tensor_tensor(out=sm[:, 44:48].unsqueeze(2), in0=pov[:, :, 64:65],
                                                                in1=esink[:, hg * 4:(hg + 1) * 4].unsqueeze(2), op=ALU.add),
                      r=[f"pO{hg}", "esink"], w=["sm44"])
                P.dve(lambda e: e.reciprocal(out=sm[:, 44:48], in_=sm[:, 44:48]), r=["sm44"], w=["sm44"])
                P.dve(lambda e, pov=pov, hg=hg: e.tensor_tensor(
                    out=att[:, hg * 256:(hg + 1) * 256].rearrange("p (h d) -> p h d", h=4), in0=pov[:, :, 0:64],
                    in1=sm[:, 44:48].unsqueeze(2).to_broadcast([128, 4, 64]), op=ALU.mult),
                    r=[f"pO{hg}", "sm44"], w=["att"])
            if DBG.get("cut") == 31:
                P.dve(lambda e: e.tensor_copy(out=tmpf[:, 0:256], in_=sc[:, :]), r=["sc"], w=["tmpf"])
                P.dve(lambda e: e.tensor_copy(out=tmpf[:, 256:516], in_=pO[1][:, 0:260]), r=["pO1"], w=["tmpf"])
                P.dve(lambda e: e.tensor_copy(out=tmpf[:, 520:528], in_=esink[:, :]), r=["esink"], w=["tmpf"])
                P.dve(lambda e: e.tensor_copy(out=tmpf[:, 528:532], in_=sm[:, 44:48]), r=["sm44"], w=["tmpf"])
                P.dve(lambda e: e.tensor_copy(out=tmpf[:, 532:788], in_=pexp[1][:, :]), r=["pexp1"], w=["tmpf"])
                P.dve(lambda e: e.tensor_copy(out=tmpf[:, 788:918], in_=vaS[cur][:, :, :].rearrange("p a b -> p (a b)")), r=[f"vaS{cur}"], w=["tmpf"])
                P.dma("sp", out[t * 128:(t + 1) * 128, :], tmpf[:, :], r=["tmpf"], w=[f"out{t}"])
                continue
            if DBG.get("cut") == 3:
                P.dve(lambda e: e.tensor_copy(out=tmpf[:, 0:512], in_=att[:, 0:512]), r=["att"], w=["tmpf"])
                P.dma("sp", out[t * 128:(t + 1) * 128, 0:512], tmpf[:, 0:512], r=["tmpf"], w=[f"out{t}"])
                continue
            P.dve(lambda e: e.tensor_copy(out=abp[:, 0:16], in_=proj[:, 2304:2320]), r=["proj"], w=["abp"])
            i = tcount[0] % 2
            tcount[0] += 1
            tp(pT[i][0:32, 0, :], abp[:, :], identb[:], r=["abp", "identb"], w=[f"pT{i}"])
            P.act(lambda e: e.copy(out=abT[:, :], in_=pT[i][0:32, 0, :]), r=[f"pT{i}"], w=["abT"])
            mm(pS[1][:, 0:256], abT[:, :], gupb[:, :], r=["abT", "gupb"], w=["pS1"])
            P.dve(lambda e: e.tensor_tensor(out=zt[:], in0=pS[1][:, 0:256], in1=gbias[:], op=ALU.add),
                  r=["pS1", "gbias"], w=["zt"])
            if DBG.get("cut") == 41:
                P.dve(lambda e: e.tensor_copy(out=tmpf[:, 0:256], in_=zt[:, :]), r=["zt"], w=["tmpf"])
                P.dma("sp", out[t * 128:(t + 1) * 128, 0:256], tmpf[:, 0:256], r=["tmpf"], w=[f"out{t}"])
                continue
            P.act(lambda e: e.activation(out=zt[:], in_=zt[:], func=AF.Exp, scale=-1.0), r=["zt"], w=["zt"])
            P.dve(lambda e: e.tensor_scalar(out=zt[:], in0=zt[:], scalar1=1.0, scalar2=None, op0=ALU.add), r=["zt"], w=["zt"])
            P.act(lambda e: e.activation(out=zt[:], in_=zt[:], func=AF.Ln), r=["zt"], w=["zt"])
            P.dve(lambda e: e.tensor_scalar(out=gneg[:], in0=zt[:], scalar1=-1.0 / 16.0, scalar2=None, op0=ALU.mult),
                  r=["zt"], w=["gneg"])
            if DBG.get("cut") == 42:
                P.dve(lambda e: e.tensor_copy(out=tmpf[:, 0:256], in_=gneg[:, :]), r=["gneg"], w=["tmpf"])
                P.dma("sp", out[t * 128:(t + 1) * 128, 0:256], tmpf[:, 0:256], r=["tmpf"], w=[f"out{t}"])
                continue
            mm(pS[0][:, 0:256], trif[:], gneg[:], r=["trif", "gneg"], w=["pS0"])
            mm(pS[0][:, 256:512], bonesf[:], gneg[:], r=["bonesf", "gneg"], w=["pS0"])
            P.act(lambda e: e.copy(out=bsb[:], in_=pS[0][:, 0:256]), r=["pS0"], w=["zt"])
            P.act(lambda e: e.activation(out=eb[:], in_=pS[0][:, 0:256], func=AF.Exp), r=["pS0"], w=["eb"])
            P.act(lambda e: e.activation(out=enb[:], in_=pS[0][:, 0:256], func=AF.Exp, scale=-1.0), r=["pS0"], w=["enb"])
            P.dve(lambda e: e.tensor_tensor(out=ekd[:], in0=pS[0][:, 256:512], in1=bsb[:], op=ALU.subtract),
                  r=["pS0", "zt"], w=["ekd"])
            P.act(lambda e: e.activation(out=ekd[:], in_=ekd[:], func=AF.Exp), r=["ekd"], w=["ekd"])
            if DBG.get("cut") == 43:
                P.dve(lambda e: e.tensor_copy(out=tmpf[:, 0:256], in_=ekd[:, :]), r=["ekd"], w=["tmpf"])
                P.dma("sp", out[t * 128:(t + 1) * 128, 0:256], tmpf[:, 0:256], r=["tmpf"], w=[f"out{t}"])
                continue
            P.dve(lambda e: e.scalar_tensor_tensor(out=qd[:], in0=proj[:, 768:1024], scalar=0.125, in1=eb[:],
                                                   op0=ALU.mult, op1=ALU.mult), r=["proj", "eb"], w=["qd"])
            P.pool(lambda e: e.tensor_tensor(out=kd[:], in0=proj[:, 1024:1280], in1=enb[:], op=ALU.mult),
                   r=["proj", "enb"], w=["kd"])
            P.pool(lambda e: e.tensor_tensor(out=ku[:], in0=proj[:, 1024:1280], in1=ekd[:], op=ALU.mult),
                   r=["proj", "ekd"], w=["ku"])
            P.pool(lambda e: e.tensor_copy(out=vbb[:], in_=proj[:, 1280:1792]), r=["proj"], w=["vbb"])
            P.act(lambda e: e.activation(out=sgn[:], in_=proj[:, 1792:2304], func=AF.Silu), r=["proj"], w=["sgn"])
            P.dve(lambda e: e.tensor_tensor(out=sgn[:, :].rearrange("p (h d) -> p h d", h=4),
                                             in0=sgn[:, :].rearrange("p (h d) -> p h d", h=4),
                                             in1=onorm1[:, :].unsqueeze(1).to_broadcast([128, 4, 128]), op=ALU.mult),
                   r=["sgn", "onorm4"], w=["sgn"])
            P.dve(lambda e: e.tensor_copy(out=gnb[:, :], in_=gneg[:, :]), r=["gneg"], w=["gnb"])
            for hh in range(4):
                mm(pS[1][0:64, hh * 128:(hh + 1) * 128], gnb[:, hh * 64:(hh + 1) * 64], bonesb[:, :], r=["gnb", "bonesb"], w=["pS1"])
            P.act(lambda e: e.activation(out=dec[:, :].rearrange("p (h c) -> p h c", h=4),
                                         in_=pS[1][0:64, :].rearrange("p (h c r) -> p h c r", h=4, c=2)[:, :, :, 0],
                                         func=AF.Exp), r=["pS1"], w=["dec"])
            if DBG.get("cut") == 4:
                P.dve(lambda e: e.tensor_copy(out=tmpf[:, 0:256], in_=qd[:, :]), r=["qd"], w=["tmpf"])
                P.dve(lambda e: e.tensor_copy(out=tmpf[:, 256:512], in_=ku[:, :]), r=["ku"], w=["tmpf"])
                P.dve(lambda e: e.tensor_copy(out=tmpf[0:64, 512:520], in_=dec[:, :]), r=["dec"], w=["tmpf"])
                P.dma("sp", out[t * 128:(t + 1) * 128, 0:520], tmpf[:, 0:520], r=["tmpf"], w=[f"out{t}"])
                continue
            i = tcount[0] % 2
            tcount[0] += 1
            for hh in range(4):
                tp(pT[i][0:64, hh, :], qd[:, hh * 64:(hh + 1) * 64], identb[:], r=["qd", "identb"], w=[f"pT{i}"])
                tp(pT[i][0:64, 4 + hh, :], kd[:, hh * 64:(hh + 1) * 64], identb[:], r=["kd", "identb"], w=[f"pT{i}"])
            P.act(lambda e, i=i: e.copy(out=qdT[:], in_=pT[i][0:64, 0:4, :]), r=[f"pT{i}"], w=["qdT"])
            P.act(lambda e, i=i: e.copy(out=kdT[:], in_=pT[i][0:64, 4:8, :]), r=[f"pT{i}"], w=["kdT"])
            P.dve(lambda e, i=i: e.tensor_copy(out=qdm[:, :, 0, 0:64], in_=pT[i][0:64, 0:4, 0:64]), r=[f"pT{i}"], w=["qdm"])
            P.dve(lambda e, i=i: e.tensor_copy(out=qdm[:, :, 1, 64:128], in_=pT[i][0:64, 0:4, 64:128]),
                  r=[f"pT{i}"], w=["qdm"])
            P.act(lambda e: e.copy(out=St0b[:], in_=St0[:]), r=["St0"], w=["St0b"])
            for hh in range(4):
                vs = slice(hh * 128, (hh + 1) * 128)
                ks = slice(hh * 64, (hh + 1) * 64)
                pa = pS[hh % 2]
                mm(pa[:, 0:128], kdT[:, hh, :], qdT[:, hh, :], r=["kdT", "qdT"], w=[f"pS{hh % 2}"])
                P.dve(lambda e, pa=pa: e.tensor_tensor(out=attT[:], in0=pa[:, 0:128], in1=trif[:], op=ALU.mult),
                      r=[f"pS{hh % 2}", "trif"], w=["attT"])
                mm(pO[0][0:64, vs], ku[0:64, ks], vbb[0:64, vs], r=["ku", "vbb"], w=["pO0"])
                mm(pO[1][0:64, vs], ku[64:128, ks], vbb[64:128, vs], r=["ku", "vbb"], w=["pO1"])
                P.dve(lambda e, hh=hh: e.scalar_tensor_tensor(out=St1[:, hh, :], in0=St0[:, hh, :],
                                                              scalar=dec[:, 2 * hh:2 * hh + 1], in1=pO[0][0:64, vs],
                                                              op0=ALU.mult, op1=ALU.add),
                      r=["St0", "dec", "pO0"], w=["St1"])
                P.act(lambda e, hh=hh: e.copy(out=St1b[:, hh, :], in_=St1[:, hh, :]), r=["St1"], w=["St1b"])
                mm(pa[:, 256:384], attT[:, :], vbb[:, vs], start=True, stop=False, r=["attT", "vbb"], w=[f"pS{hh % 2}"])
                mm(pa[:, 256:384], qdm[:, hh, 0, :], St0b[:, hh, :], start=False, stop=False, r=["qdm", "St0b"],
                   w=[f"pS{hh % 2}"])
                mm(pa[:, 256:384], qdm[:, hh, 1, :], St1b[:, hh, :], start=False, stop=True, r=["qdm", "St1b"],
                   w=[f"pS{hh % 2}"])
                P.dve(lambda e, hh=hh: e.scalar_tensor_tensor(out=St0[:, hh, :], in0=St1[:, hh, :],
                                                              scalar=dec[:, 2 * hh + 1:2 * hh + 2],
                                                              in1=pO[1][0:64, vs], op0=ALU.mult, op1=ALU.add),
                      r=["St1", "dec", "pO1"], w=["St0"])
                P.act(lambda e, pa=pa: e.activation(out=junk[:, 0:128], in_=pa[:, 256:384], func=AF.Square,
                                                    accum_out=sm[:, 50:51]), r=[f"pS{hh % 2}"], w=["junk", "sm50"])
                rstd_from_ssq(sm[:, 50:51], 1, 1.0 / 128, "sm50")
                P.dve(lambda e, pa=pa, hh=hh: e.scalar_tensor_tensor(out=att[:, 512 + hh * 128:512 + (hh + 1) * 128],
                                                                     in0=pa[:, 256:384], scalar=sm[:, 50:51],
                                                                     in1=sgn[:, hh * 128:(hh + 1) * 128], op0=ALU.mult,
                                                                     op1=ALU.mult),
                      r=[f"pS{hh % 2}", "sm50", "sgn"], w=["att"])
            if DBG.get("notail"):
                P.dve(lambda e: e.tensor_copy(out=tmpf[:], in_=att[:]), r=["att"], w=["tmpf"])
                P.dma("sp", out[t * 128:(t + 1) * 128, :], tmpf[:], r=["tmpf"], w=[f"out{t}"])
                continue
            tail(l, t, xb, att, "att")
            if DBG.get("dumph2"):
                P.dve(lambda e: e.tensor_copy(out=tmpf[:], in_=hb[:]), r=["hb"], w=["tmpf"])
                P.dma("sp", out[t * 128:(t + 1) * 128, :], tmpf[:], r=["tmpf"], w=[f"out{t}"])

    def odd_layer(l):
        P.dve(lambda e: e.memset(kmsum[:], 0.0), w=["kmsum"])
        for t in range(NT):
            xb = t % 2
            rs = slice(t * 128, (t + 1) * 128)
            load_x(l, t, xb)
            if l > 0:
                P.dma("sp", XR[rs, :], xt[xb][:], r=["xt0"], w=[f"XR{t}"])
            norm_mod(xt[xb], "xt0", 1, 0, hb, "hb")
            transpose8(hb, "hb", hT, "hT")
            project(hT, "hT", R1w, WINT, 2048, proj, "proj")
            for half in range(2):
                src = proj[:, half * 1024:(half + 1) * 1024]
                P.dve(lambda e, src=src: e.tensor_tensor(out=tmpf[:], in0=src, in1=src, op=ALU.mult), r=["proj"], w=["tmpf"])
                P.dve(lambda e: e.tensor_reduce(out=sm[:, 32:40], in_=tmpf[:, :].rearrange("p (g d) -> p g d", g=8),
                                                axis=AX.X, op=ALU.add), r=["tmpf"], w=["sm32"])
                rstd_from_ssq(sm[:, 32:40], 8, 1.0 / 128, "sm32")
                P.dve(lambda e, src=src: e.tensor_tensor(out=tmpf[:, :].rearrange("p (g d) -> p g d", g=8),
                                                         in0=src.rearrange("p (g d) -> p g d", g=8),
                                                         in1=sm[:, 32:40].unsqueeze(2).to_broadcast([128, 8, 128]),
                                                         op=ALU.mult), r=["proj", "sm32"], w=["tmpf"])
                P.pool(lambda e, half=half: e.tensor_tensor(
                    out=qnb[:, half * 1024:(half + 1) * 1024].rearrange("p (g d) -> p g d", g=8),
                    in0=tmpf[:, :].rearrange("p (g d) -> p g d", g=8),
                    in1=wrow[:, half * 128:(half + 1) * 128].unsqueeze(1).to_broadcast([128, 8, 128]), op=ALU.mult),
                    r=["tmpf", "wrow", "wrow2"], w=["qnb"])
            project(hT, "hT", R1w[:, :, 2048:3072], WINT, 1024, proj, "proj")
            P.act(lambda e: e.copy(out=vast[:, :, 0:128], in_=proj[:, 0:1024].rearrange("p (g d) -> p g d", g=8)),
                  r=["proj"], w=["vast"])
            P.dma("sp", VA[rs, :, :].rearrange("t h c -> t (h c)"), vast[:, :, :].rearrange("p h c -> p (h c)"), r=["vast"], wacc=["VA"])
            for half in range(2):
                dstD = QT if half == 0 else KT
                transpose8(qnb[:, half * 1024:(half + 1) * 1024], "qnb", hT, "hT")
                P.dma("sp", dstD[:, :, rs].rearrange("h d t -> d h t"), hT[:], r=["hT"], wacc=["QT" if half == 0 else "KT"])
                if half == 1:
                    P.dve(lambda e, t=t: e.tensor_reduce(out=kmsum[:, :, t], in_=hT[:], axis=AX.X, op=ALU.add),
                          r=["hT"], w=["kmsum"])
        P.dve(lambda e: e.tensor_tensor(out=tmpf[:, 0:128].rearrange("p (h b) -> p h b", h=8), in0=kmsum[:, :, :].rearrange("p h (b two) -> p h b two", two=2)[:, :, :, 0],
                                        in1=kmsum[:, :, :].rearrange("p h (b two) -> p h b two", two=2)[:, :, :, 1], op=ALU.add), r=["kmsum"], w=["tmpf"])
        P.dve(lambda e: e.tensor_scalar(out=kmean[:], in0=tmpf[:, 0:128].rearrange("p (h b) -> p h b", h=8),
                                        scalar1=1.0 / 256, scalar2=None, op0=ALU.mult), r=["tmpf"], w=["kmean"])
        pcount = [0]
        for hgp in range(4):
            h0 = hgp * 2
            P.dma("sp", KTs, KT[h0:h0 + 2, :, :].rearrange("h d t -> d h t"), r=["KT"], w=["R2_0"])
            P.dma("sp", VAs2, VA[:, h0:h0 + 2, :].rearrange("(t p) h c -> p t (h c)", p=128), r=["VA"], w=["R2_1"])
            for qb_ in range(16):
                qs = slice(qb_ * 256, (qb_ + 1) * 256)
                P.dma("sp", qTt[:], QT[h0:h0 + 2, :, qs].rearrange("h d t -> d h t"), r=["QT"], w=["qTt"])
                for hh in range(2):
                    h = h0 + hh
                    for qt in range(2):
                        mm(pO[1][:, 256 + qt * 16:256 + (qt + 1) * 16], qTt[:, hh, qt * 128:(qt + 1) * 128], kmean[:, h, :],
                           r=["qTt", "kmean"], w=["pO1"])
                    P.dve(lambda e: e.memset(scs[:], NEG), w=["scs"])
                    if qb_ > 0:
                        P.dve(lambda e, qb_=qb_: e.tensor_copy(
                            out=scs[:, :, 0:qb_], in_=pO[1][:, 256:288].rearrange("p (q b) -> p q b", q=2)[:, :, 0:qb_]),
                            r=["pO1"], w=["scs"])
                    for qt in range(2):
                        P.dve(lambda e, qt=qt: e.max(out=top8[:], in_=scs[:, qt, :]), r=["scs"], w=["top8"])
                        P.dve(lambda e, qt=qt, hh=hh: e.tensor_scalar(out=sel[:, hh, qt, :], in0=scs[:, qt, :],
                                                                      scalar1=top8[:, 2:3], scalar2=None, op0=ALU.is_ge),
                              r=["scs", "top8"], w=["sel"])
                    nkt = 2 * qb_ + 2
                    for qt in range(2):
                        qtile = 2 * qb_ + qt
                        qsl = slice(qt * 128, (qt + 1) * 128)
                        po = pO[0]
                        first = True
                        for kt in range(2 * qb_, qtile + 1):
                            dl = qtile - kt
                            psx = pS[pcount[0] % 2]
                            ptag = f"pS{pcount[0] % 2}"
                            pb = pT2[pcount[0] % 4]
                            pbt = f"pT2_{pcount[0] % 4}"
                            pcount[0] += 1
                            mm(psx[:, 0:128], KTs[:, hh, kt * 128:(kt + 1) * 128], qTt[:, hh, qsl], r=["R2_0", "qTt"], w=[ptag])
                            P.dve(lambda e, psx=psx, h=h, dl=dl: e.scalar_tensor_tensor(
                                out=sco[:, 0:128], in0=psx[:, 0:128], scalar=128 ** -0.5,
                                in1=(BS[:, h, 0:128] if dl == 0 else BM1[:, h, :]), op0=ALU.mult, op1=ALU.add), r=[ptag, "BM", "BS"], w=["sco"])
                            P.act(lambda e, pb=pb: e.activation(out=pb[:, 0:128], in_=sco[:, 0:128], func=AF.Exp),
                                  r=["sco"], w=[pbt])
                            mm(po[:, qt * 129:(qt + 1) * 129], pb[:, 0:128], VAs[:, kt, hh, :], start=first,
                               stop=(kt == qtile), r=[pbt, "R2_1"], w=["pO0"])
                            first = False
                    P.dve(lambda e, hh=hh: e.tensor_copy(out=acc[:, hh, :, :],
                                                         in_=pO[0][:, 0:258].rearrange("p (q c) -> p q c", q=2)),
                          r=["pO0"], w=["acc"])
                    for jb in range(qb_):
                        pbs = []
                        for kk in range(2):
                            kt = 2 * jb + kk
                            psx = pS[pcount[0] % 2]
                            ptag = f"pS{pcount[0] % 2}"
                            pb = pT2[pcount[0] % 4]
                            pbt = f"pT2_{pcount[0] % 4}"
                            pcount[0] += 1
                            mm(psx[:, 0:256], KTs[:, hh, kt * 128:(kt + 1) * 128], qTt[:, hh, :], r=["R2_0", "qTt"], w=[ptag])
                            if kt == 2 * qb_ - 1:
                                P.dve(lambda e, psx=psx, h=h: e.scalar_tensor_tensor(
                                    out=sco[:, 0:128], in0=psx[:, 0:128], scalar=128 ** -0.5, in1=BM1[:, h, :],
                                    op0=ALU.mult, op1=ALU.add), r=[ptag, "BM"], w=["sco"])
                                P.act(lambda e, pb=pb: e.activation(out=pb[:, 0:128], in_=sco[:, 0:128], func=AF.Exp),
                                      r=["sco"], w=[pbt])
                                P.act(lambda e, pb=pb, psx=psx, h=h: e.activation(out=pb[:, 128:256], in_=psx[:, 128:256],
                                                                                   func=AF.Exp, bias=relfar[:, h:h + 1],
                                                                                   scale=128 ** -0.5),
                                      r=[ptag, "relfar"], w=[pbt])
                            else:
                                P.act(lambda e, pb=pb, psx=psx, h=h: e.activation(out=pb[:, 0:256], in_=psx[:, 0:256],
                                                                                   func=AF.Exp, bias=relfar[:, h:h + 1],
                                                                                   scale=128 ** -0.5),
                                      r=[ptag, "relfar"], w=[pbt])
                            pbs.append((pb, pbt, kt))
                        po = pO[1]
                        for qt in range(2):
                            for kk, (pb, pbt, kt) in enumerate(pbs):
                                mm(po[:, qt * 129:(qt + 1) * 129], pb[:, qt * 128:(qt + 1) * 128], VAs[:, kt, hh, :],
                                   start=(kk == 0), stop=(kk == 1), r=[pbt, "R2_1"], w=["pO1"])
                        for qt in range(2):
                            P.dve(lambda e, qt=qt, hh=hh, jb=jb: e.scalar_tensor_tensor(
                                out=acc[:, hh, qt, :], in0=pO[1][:, qt * 129:(qt + 1) * 129], scalar=sel[:, hh, qt, jb:jb + 1],
                                in1=acc[:, hh, qt, :], op0=ALU.mult, op1=ALU.add), r=["pO1", "sel", "acc"], w=["acc"])
                    for qt in range(2):
                        P.dve(lambda e, qt=qt, hh=hh: e.reciprocal(out=sm[:, 52:53], in_=acc[:, hh, qt, 128:129]),
                              r=["acc"], w=["sm52"])
                        P.dve(lambda e, qt=qt, hh=hh: e.tensor_scalar(out=attq[:, qt, hh * 128:(hh + 1) * 128],
                                                                      in0=acc[:, hh, qt, 0:128], scalar1=sm[:, 52:53],
                                                                      scalar2=None, op0=ALU.mult),
                              r=["acc", "sm52"], w=["attq"])
                for qt in range(2):
                    t = 2 * qb_ + qt
                    P.dma("sp", ATT[t * 128:(t + 1) * 128, h0 * 128:(h0 + 2) * 128], attq[:, qt, :], r=["attq"], wacc=["ATT"])
        for t in range(NT):
            xb = t % 2
            rs = slice(t * 128, (t + 1) * 128)
            if l == 0:
                P.dma("sp", xt[xb][:], x_in[rs, :], w=["xt0"])
            else:
                P.dma("sp", xt[xb][:], XR[rs, :], r=[f"XR{t}"], w=["xt0"])
            P.dma("sp", att[:], ATT[rs, :], r=["ATT"], w=["att"])
            tail(l, t, xb, att, "att")

    def expert_phase(l):
        w1r = moe_w1.rearrange("l e k n -> (l e k) n")
        w3r = moe_w3.rearrange("l e k n -> (l e k) n")
        w2r = moe_w2.rearrange("l e k n -> (l e k) n")
        s0 = stg(0)[:, :].rearrange("p (k n) -> p k n", k=8)
        s1 = stg(1)[:, :].rearrange("p (k n) -> p k n", k=8)
        s2 = stg(0)[:, :].rearrange("p (k n) -> p k n", k=4)
        for b in range(DBG.get("nblk", NBLK)):
            eb_ = b % 2
            w1v, w3v, w2v = ew(eb_, 0), ew(eb_, 1), ew(eb_, 2)
            wt = [f"R1_{eb_}_{i}" for i in range(3)]
            tb = tabt[eb_]
            ix = idxi[eb_]
            P.dma("sp", tb[:], TAB[b * 128:(b + 1) * 128, :], r=["TAB"], w=[f"tabt{eb_}"])
            P.dve(lambda e, tb=tb, ix=ix: e.tensor_copy(out=ix[:], in_=tb[:, 0:2]), r=[f"tabt{eb_}"], w=[f"idxi{eb_}"])
            P.op("pool", lambda e, ix=ix, eb_=eb_: e.indirect_dma_start(
                out=xg[eb_][:, :], out_offset=None, in_=H2,
                in_offset=bass.IndirectOffsetOnAxis(ap=ix[:, 0:1], axis=0)),
                ["H2", f"idxi{eb_}"], [f"xg{eb_}"], dma=True)
            for k in range(8):
                P.op("pool", lambda e, k=k, b=b: e.indirect_dma_start(
                    out=s0[:, k, :], out_offset=None, in_=w1r,
                    in_offset=bass.IndirectOffsetOnAxis(ap=widx1i[:, k, b:b + 1], axis=0)),
                    ["widx1i"], ["R2_0"] if k == 0 else None, dma=True, wacc=None if k == 0 else ["R2_0"])
            P.act(lambda e, w1v=w1v: e.copy(out=w1v, in_=s0), r=["R2_0"], w=[wt[0]])
            for k in range(8):
                P.op("pool", lambda e, k=k, b=b: e.indirect_dma_start(
                    out=s1[:, k, :], out_offset=None, in_=w3r,
                    in_offset=bass.IndirectOffsetOnAxis(ap=widx1i[:, k, b:b + 1], axis=0)),
                    ["widx1i"], ["R2_1"] if k == 0 else None, dma=True, wacc=None if k == 0 else ["R2_1"])
            P.dve(lambda e, w3v=w3v: e.tensor_copy(out=w3v, in_=s1), r=["R2_1"], w=[wt[1]])
            for k in range(4):
                P.op("pool", lambda e, k=k, b=b: e.indirect_dma_start(
                    out=s2[:, k, :], out_offset=None, in_=w2r,
                    in_offset=bass.IndirectOffsetOnAxis(ap=widx2i[:, k, b:b + 1], axis=0)),
                    ["widx2i"], ["R2_0"] if k == 0 else None, dma=True, wacc=None if k == 0 else ["R2_0"])
            P.act(lambda e, w2v=w2v: e.copy(out=w2v, in_=s2), r=["R2_0"], w=[wt[2]])
            i = tcount[0] % 2
            tcount[0] += 1
            for k in range(8):
                tp(pT[i][:, k, :], xg[eb_][:, k * 128:(k + 1) * 128], identb[:], r=[f"xg{eb_}", "identb"], w=[f"pT{i}"])
            P.dve(lambda e, i=i, eb_=eb_: e.tensor_copy(out=xgT[eb_][:, :, :], in_=pT[i][:]), r=[f"pT{i}"], w=[f"xgT{eb_}"])
            for hc in range(4):
                for k in range(8):
                    mm(pM[0][:, 0:128], w1v[:, k, hc * 128:(hc + 1) * 128], xgT[eb_][:, k, :], start=(k == 0), stop=(k == 7),
                       r=[f"xgT{eb_}", wt[0]], w=["pM0"])
                for k in range(8):
                    mm(pM[1][:, 0:128], w3v[:, k, hc * 128:(hc + 1) * 128], xgT[eb_][:, k, :], start=(k == 0), stop=(k == 7),
                       r=[f"xgT{eb_}", wt[1]], w=["pM1"])
                P.act(lambda e: e.activation(out=s1t[:, :], in_=pM[0][:, 0:128], func=AF.Silu), r=["pM0"], w=["s1t"])
                P.dve(lambda e, hc=hc, eb_=eb_: e.tensor_tensor(out=actT[eb_][:, hc, :], in0=pM[1][:, 0:128], in1=s1t[:, :],
                                                                op=ALU.mult), r=["pM1", "s1t"], w=[f"actT{eb_}"])
            ytag = ("m1", "junk")[eb_]
            for half in range(2):
                for hc in range(4):
                    mm(pS[half][:, :], actT[eb_][:, hc, :], w2v[:, hc, half * 512:(half + 1) * 512],
                       start=(hc == 0), stop=(hc == 3), r=[f"actT{eb_}", wt[2]], w=[f"pS{half}"])
                if half == 0:
                    P.act(lambda e, eb_=eb_, tb=tb: e.activation(out=yo[eb_][:, 0:512], in_=pS[0][:, :], func=AF.Copy,
                                                                 scale=tb[:, 2:3]),
                          r=["pS0", f"tabt{eb_}"], w=[ytag])
                else:
                    P.dve(lambda e, eb_=eb_, tb=tb: e.tensor_scalar(out=yo[eb_][:, 512:1024], in0=pS[1][:, :],
                                                                     scalar1=tb[:, 2:3], scalar2=None, op0=ALU.mult),
                          r=["pS1", f"tabt{eb_}"], w=[ytag])
            P.op("pool", lambda e, eb_=eb_, ix=ix: e.indirect_dma_start(
                out=MO, out_offset=bass.IndirectOffsetOnAxis(ap=ix[:, 1:2], axis=0), in_=yo[eb_][:], in_offset=None),
                [ytag, f"idxi{eb_}"], None, dma=True, wacc=["MO"])

    for l in range(n_layers):
        layer_start(l)
        if DBG.get("stage") == "A":
            for r_ in range(6):
                P.dma("sp", out[r_ * 128:(r_ + 1) * 128, :], rows[:, r_, :], r=[f"rows{r_}"], w=[f"out{r_}"])
            break
        if l % 2 == 0:
            even_layer(l)
        else:
            odd_layer(l)
        if not DBG.get("noexp"):
            route_finalize(l)
            expert_phase(l)
    if DBG.get("nofinal"):
        P.emit()
        es.close()
        return nc
    P.pool(lambda e: e.tensor_copy(out=rows[:, 6, :], in_=rows[:, 5, :]), r=["rows5"], w=["rows6"])
    for t in range(NT):
        xb = t % 2
        load_x(1, t, xb)
        P.dma("sp", out[t * 128:(t + 1) * 128, :], xt[xb][:], r=["xt0"], w=[f"out{t}"])
    P.emit()
    es.close()
    return nc


def _rel_bucket_np(d):
    d = np.maximum(d, 0)
    logd = np.log(np.maximum(d, 1).astype(np.float32) / 16) / math.log(128 / 16)
    far = np.minimum(16 + (logd * 16).astype(np.int32), 31)
    return np.where(d < 16, d, far)


def _constants():
    k = np.arange(128)[:, None]
    q = np.arange(128)[None, :]
    c = {}
    c["ident"] = np.eye(128, dtype=np.float32)
    same = (k // 64) == (q // 64)
    c["tri"] = (same & (k <= q)).astype(np.float32)
    c["bones"] = same.astype(np.float32)
    c["cind"] = (np.arange(128)[:, None] // 64 == np.arange(2)[None, :]).astype(np.float32)
    c["stri"] = (k < q).astype(np.float32)
    mask0 = np.zeros((128, 256), np.float32)
    mask0[:, 0:128] = np.where(q >= k, 0.0, NEG)
    mask0[:, 128:256] = np.where(q < k, 0.0, NEG)
    c["mask0"] = mask0
    r = np.arange(TABROWS)
    tab = np.zeros((TABROWS, 4), np.float32)
    tab[:, 1] = S + (r % 128)
    c["tabinit"] = tab
    rc_ = np.zeros((128, 172), np.float32)
    rc_[:, 0:64] = (np.arange(64) * 128)[None, :]
    rc_[:, 64:160] = (np.arange(96) * 128)[None, :]
    rc_[:, 160:168] = np.arange(8)[None, :] * 128 + np.arange(128)[:, None]
    rc_[:, 168:172] = np.arange(4)[None, :] * 128 + np.arange(128)[:, None]
    c["rcst"] = rc_
    pid = np.zeros((128, 2), np.float32)
    pid[:, 0] = np.arange(128)
    pid[:, 1] = 32 * CAP + np.arange(128)
    c["pidx"] = pid
    d0 = q - k
    d1 = 128 + q - k
    c["_b0"] = _rel_bucket_np(d0)
    c["_b1"] = _rel_bucket_np(d1)
    return c


_CACHE = {}


def kernel(x, c, rel_bias, ada_w, ada_b, norm1_w, norm2_w, even_w_in, even_w_out, a_q_norm, a_k_norm, a_sinks,
           b_gate_up, b_gate_bias, b_out_norm, odd_w_in, odd_w_out, c_q_norm, c_k_norm, moe_w_group, moe_b_group,
           moe_w_expert, moe_b_expert, moe_w1, moe_w3, moe_w2, _n_layers=4, _cores=None, _dbg=None):
    f = lambda a: np.ascontiguousarray(np.asarray(a, dtype=np.float32))
    cst = _constants()
    rel_bias = f(rel_bias)
    relT = np.empty((8, 128, 256), np.float32)
    relT[:, :, 0:128] = np.transpose(rel_bias[cst["_b0"]], (2, 0, 1))
    relT[:, :, 128:256] = np.transpose(rel_bias[cst["_b1"]], (2, 0, 1))
    shared = {
        "rel_bias": rel_bias, "relT": relT, "ada_w": f(ada_w[:_n_layers]), "ada_b": f(ada_b), "norm1_w": f(norm1_w),
        "norm2_w": f(norm2_w), "even_w_in": f(even_w_in), "even_w_out": f(even_w_out), "a_q_norm": f(a_q_norm),
        "a_k_norm": f(a_k_norm), "a_sinks": f(a_sinks), "b_gate_up": f(b_gate_up), "b_gate_bias": f(b_gate_bias),
        "b_out_norm": f(b_out_norm), "odd_w_in": f(odd_w_in), "odd_w_out": f(odd_w_out), "c_q_norm": f(c_q_norm),
        "c_k_norm": f(c_k_norm),
        "moe_wr": np.ascontiguousarray(np.concatenate([f(moe_w_group), f(moe_w_expert)], axis=-1)),
        "moe_br": np.ascontiguousarray(np.concatenate([f(moe_b_group), f(moe_b_expert)], axis=-1)),
        "moe_w1": f(moe_w1[:_n_layers]), "moe_w3": f(moe_w3[:_n_layers]), "moe_w2": f(moe_w2[:_n_layers]),
    }
    for k_ in ("ident", "tri", "bones", "cind", "stri", "mask0", "tabinit", "rcst", "pidx"):
        shared[k_] = cst[k_]
    x = f(x)
    c = f(c)
    cores = list(range(8)) if _cores is None else _cores
    in_maps = []
    for b in cores:
        m = dict(shared)
        m["x"] = x[b]
        m["cT"] = np.ascontiguousarray(c[b].reshape(8, 128).T)
        in_maps.append(m)
    key = (_n_layers, str(_dbg))
    if key not in _CACHE:
        _CACHE[key] = build_program(_n_layers, _dbg)
    nc = _CACHE[key]
    res = run_bass_kernel_spmd(nc, in_maps, core_ids=list(range(len(cores))))
    outs = [r["out"] for r in res.results]
    return np.stack(outs, axis=0).astype(np.float32)
```

```python
import math
from contextlib import ExitStack

import numpy as np
import concourse.bass as bass
import concourse.mybir as mybir
from concourse.bass_utils import run_bass_kernel_spmd

F32 = mybir.dt.float32
BF16 = mybir.dt.bfloat16
I32 = mybir.dt.int32
AF = mybir.ActivationFunctionType
ALU = mybir.AluOpType
AX = mybir.AxisListType

S = 4096
D = 1024
NT = 32
CAP = 384
NB = CAP // 128
NEG = -30000.0
BIG = 10000.0
MOH = S + 128
NBLK = 96
TABROWS = NBLK * 128
EPS = 1e-6

COMPUTE = ("pe", "act", "dve", "pool")
DMAQ_K = 12
SEM_EPOCH = 6000
DMA_EPOCH = 400


class Buf:
    __slots__ = ("name", "w", "r")

    def __init__(self, name):
        self.name = name
        self.w = []
        self.r = []


class Op:
    __slots__ = ("eng", "fn", "seq", "dma", "deps", "signal", "sig_idx", "dma_idx")

    def __init__(self, eng, fn, dma):
        self.eng = eng
        self.fn = fn
        self.dma = dma
        self.deps = []
        self.signal = False
        self.sig_idx = None
        self.dma_idx = None


def _compress(lst):
    keep = []
    cnt = {}
    for x in reversed(lst):
        key = (x.eng, x.dma)
        c = cnt.get(key, 0)
        lim = DMAQ_K if x.dma else 1
        if c < lim:
            keep.append(x)
            cnt[key] = c + 1
    keep.reverse()
    return keep


class _Rec:
    def __init__(self):
        self.calls = []

    def __getattr__(self, name):
        def f(*a, **k):
            self.calls.append((name, a, k))
            return self
        return f


def _freeze(fn):
    rec = _Rec()
    fn(rec)
    assert len(rec.calls) == 1, rec.calls
    name, a, k = rec.calls[0]
    return lambda e: getattr(e, name)(*a, **k)


class Prog:
    def __init__(self, nc):
        self.nc = nc
        self.ops = {e: [] for e in ("pe", "act", "dve", "pool", "sp")}
        self.ndma = {e: 0 for e in self.ops}
        self.known = {e: {f: -1 for f in self.ops} for e in self.ops}
        self.kd_upto = {e: {f: -1 for f in self.ops} for e in self.ops}
        self.kd_set = {e: {f: set() for f in self.ops} for e in self.ops}
        self.bufs = {}

    def buf(self, name):
        b = self.bufs.get(name)
        if b is None:
            b = Buf(name)
            self.bufs[name] = b
        return b

    def _norm(self, lst):
        out = []
        for x in lst or []:
            if isinstance(x, (list, tuple)):
                out.extend(self._norm(x))
            elif isinstance(x, str):
                out.append(self.buf(x))
            else:
                out.append(x)
        return out

    def _satisfied(self, E, d):
        if d.dma:
            return d.dma_idx <= self.kd_upto[E][d.eng] or d.dma_idx in self.kd_set[E][d.eng]
        return d.seq <= self.known[E][d.eng]

    def _learn(self, E, d):
        if d.dma:
            F = d.eng
            s = self.kd_set[E][F]
            s.add(d.dma_idx)
            u = max(self.kd_upto[E][F], d.dma_idx - DMAQ_K)
            while (u + 1) in s:
                u += 1
            self.kd_upto[E][F] = u
            if len(s) > 64:
                self.kd_set[E][F] = {x for x in s if x > u}
        elif d.seq > self.known[E][d.eng]:
            self.known[E][d.eng] = d.seq

    def op(self, eng, fn, reads=None, writes=None, dma=False, wacc=None):
        reads = self._norm(reads)
        writes = self._norm(writes)
        wacc = self._norm(wacc)
        psr = [b for b in reads if b.name[:2] in ("pT", "pM", "pS", "pO")]
        if psr:
            reads = [b for b in reads if b not in psr]
            writes = writes + [b for b in psr if b not in writes]
        if fn is not None:
            fn = _freeze(fn)
        o = Op(eng, fn, dma)
        o.seq = len(self.ops[eng])
        if dma:
            o.dma_idx = self.ndma[eng]
            self.ndma[eng] += 1
        raw = set()
        cand = []
        for b in reads:
            for d in b.w:
                raw.add(id(d))
                cand.append(d)
        for b in writes:
            cand.extend(b.w)
            cand.extend(b.r)
        for b in wacc:
            cand.extend(b.r)
        seen = set()
        uniq = []
        for d in cand:
            if id(d) not in seen:
                seen.add(id(d))
                uniq.append(d)
        uniq.sort(key=lambda d: -(d.dma_idx if d.dma else d.seq))
        for d in uniq:
            if (not d.dma) and (not dma) and d.eng == eng and id(d) not in raw:
                continue
            if self._satisfied(eng, d):
                continue
            o.deps.append(d)
            d.signal = True
            self._learn(eng, d)
        for b in writes:
            b.w = [o]
            b.r = []
        for b in wacc:
            b.w.append(o)
            if len(b.w) > 40:
                b.w = _compress(b.w)
        for b in reads:
            if fn is None:
                break
            if not dma:
                b.r = [x for x in b.r if x.dma or x.eng != eng]
            b.r.append(o)
            if len(b.r) > 40:
                b.r = _compress(b.r)
        self.ops[eng].append(o)
        return o

    def pe(self, fn, r=None, w=None):
        return self.op("pe", fn, r, w)

    def act(self, fn, r=None, w=None):
        return self.op("act", fn, r, w)

    def dve(self, fn, r=None, w=None):
        return self.op("dve", fn, r, w)

    def pool(self, fn, r=None, w=None):
        return self.op("pool", fn, r, w)

    def barrier(self, fn):
        allb = list(self.bufs.values())
        self.op("dve", fn, allb, allb + [self.buf("FENCE")])
        for e in ("pe", "act", "pool", "sp"):
            self.op(e, None, [self.buf("FENCE")], None)

    def dma(self, eng, out, in_, r=None, w=None, wacc=None):
        return self.op(eng, lambda e: e.dma_start(out=out, in_=in_), r, w, dma=True, wacc=wacc)

    def emit(self):
        nc = self.nc
        nsig = {}
        for e in COMPUTE:
            c = 0
            for o in self.ops[e]:
                if o.signal and not o.dma:
                    o.sig_idx = c
                    c += 1
            nsig[e] = c
        es = ExitStack()
        sems = {}
        for e in COMPUTE:
            n_ep = max(1, (nsig[e] + SEM_EPOCH - 1) // SEM_EPOCH)
            sems[e] = [es.enter_context(nc.semaphore(f"tl_{e}_{i}")) for i in range(n_ep)]
        dsems = {}
        per_ep = DMA_EPOCH * DMAQ_K
        for e in self.ops:
            if self.ndma[e] == 0:
                continue
            n_ep = (self.ndma[e] + per_ep - 1) // per_ep
            dsems[e] = [[es.enter_context(nc.semaphore(f"dq_{e}_{i}_{k}")) for k in range(DMAQ_K)]
                        for i in range(n_ep)]

        def dma_sem(e, idx):
            return dsems[e][idx // per_ep][idx % DMAQ_K], 16 * ((idx % per_ep) // DMAQ_K + 1)

        def wait_for(engobj, d):
            if d.dma:
                s, v = dma_sem(d.eng, d.dma_idx)
            else:
                s = sems[d.eng][d.sig_idx // SEM_EPOCH]
                v = d.sig_idx % SEM_EPOCH + 1
            engobj.wait_ge(s, v)

        prog = self

        def run_engine(ename, engobj):
            for o in prog.ops[ename]:
                for d in o.deps:
                    wait_for(engobj, d)
                if o.dma:
                    if o.dma_idx >= DMAQ_K:
                        s, v = dma_sem(ename, o.dma_idx - DMAQ_K)
                        engobj.wait_ge(s, v)
                    inst = o.fn(engobj)
                    s, v = dma_sem(ename, o.dma_idx)
                    inst.then_inc(s, 16)
                elif o.fn is not None:
                    inst = o.fn(engobj)
                    if o.signal:
                        inst.then_inc(sems[ename][o.sig_idx // SEM_EPOCH], 1)
            n = prog.ndma[ename]
            for idx in range(max(0, n - DMAQ_K), n):
                s, v = dma_sem(ename, idx)
                engobj.wait_ge(s, v)

        with nc.Block() as block:
            @block.tensor
            def _(e):
                run_engine("pe", e)

            @block.scalar
            def _(e):
                run_engine("act", e)

            @block.vector
            def _(e):
                run_engine("dve", e)

            @block.gpsimd
            def _(e):
                run_engine("pool", e)

            @block.sync
            def _(e):
                run_engine("sp", e)
        es.close()


def build_program(n_layers=4, dbg=None):
    DBG = dbg or {}
    nc = bass.Bass("TRN2", target_bir_lowering=False)
    P = Prog(nc)
    es = ExitStack()

    def din(name, shape, dt=F32):
        return nc.dram_tensor(name, list(shape), dt, kind="ExternalInput").ap()

    def dscr(name, shape, dt=F32):
        return nc.dram_tensor(name, list(shape), dt, kind="Internal").ap()

    def sb(name, shape, dt=F32):
        return es.enter_context(nc.sbuf_tensor("s_" + name, list(shape), dt))

    def ps(name, shape, dt=F32):
        return es.enter_context(nc.psum_tensor("p_" + name, list(shape), dt))

    x_in = din("x", [S, D])
    cT_in = din("cT", [128, 8])
    rel_bias = din("rel_bias", [32, 8])
    relT_in = din("relT", [8, 128, 256])
    mask0_in = din("mask0", [128, 256])
    ident_in = din("ident", [128, 128])
    tri_in = din("tri", [128, 128])
    bones_in = din("bones", [128, 128])
    cind_in = din("cind", [128, 2])
    stri_in = din("stri", [128, 128])
    tabinit_in = din("tabinit", [TABROWS, 4])
    rcst_in = din("rcst", [128, 172])
    pidx_in = din("pidx", [128, 2])
    NLW = n_layers
    ada_w = din("ada_w", [NLW, D, 6 * D])
    ada_b = din("ada_b", [4, 6 * D])
    norm1_w = din("norm1_w", [4, D])
    norm2_w = din("norm2_w", [4, D])
    even_w_in = din("even_w_in", [2, D, 2320])
    even_w_out = din("even_w_out", [2, D, D])
    a_q_norm = din("a_q_norm", [2, 64])
    a_k_norm = din("a_k_norm", [2, 64])
    a_sinks = din("a_sinks", [2, 8])
    b_gate_up = din("b_gate_up", [2, 16, 256])
    b_gate_bias = din("b_gate_bias", [2, 256])
    b_out_norm = din("b_out_norm", [2, 128])
    odd_w_in = din("odd_w_in", [2, D, 3072])
    odd_w_out = din("odd_w_out", [2, D, D])
    c_q_norm = din("c_q_norm", [2, 128])
    c_k_norm = din("c_k_norm", [2, 128])
    moe_wr = din("moe_wr", [4, D, 36])
    moe_br = din("moe_br", [4, 36])
    moe_w1 = din("moe_w1", [NLW, 32, D, 512])
    moe_w3 = din("moe_w3", [NLW, 32, D, 512])
    moe_w2 = din("moe_w2", [NLW, 32, 512, D])
    out = nc.dram_tensor("out", [S, D], F32, kind="ExternalOutput").ap()

    XR = dscr("XR", [S, D])
    H2 = dscr("H2", [S, D], BF16)
    TAB = dscr("TAB", [TABROWS, 4])
    MO = dscr("MO", [2 * MOH, D])
    ATT = dscr("ATT", [S, D], BF16)
    QT = dscr("QT", [8, 128, S], BF16)
    KT = dscr("KT", [8, 128, S], BF16)
    VA = dscr("VA", [S, 8, 129], BF16)

    R1 = sb("R1", [128, 24576], BF16)
    R2 = sb("R2", [128, 8448], F32)
    WOUT = sb("WOUT", [128, 8, 1024], BF16)
    rows = sb("rows", [128, 7, 1024])
    BS = sb("BS", [128, 8, 256])
    BM1 = sb("BM1", [128, 8, 128])
    relfar = sb("relfar", [128, 8])
    identf = sb("identf", [128, 128])
    identb = sb("identb", [128, 128], BF16)
    trif = sb("trif", [128, 128])
    bonesf = sb("bonesf", [128, 128])
    cindf = sb("cindf", [128, 2])
    strib = sb("strib", [128, 128], BF16)
    onesb = sb("onesb", [128, 128], BF16)
    bonesb = sb("bonesb", [128, 128], BF16)
    rcst = sb("rcst", [128, 172])
    pidx = sb("pidx", [128, 2])
    cTs = sb("cTs", [128, 8])
    xt0_ = sb("xt0", [128, D])
    xt = [xt0_, xt0_]
    m1 = sb("m1", [128, D])
    junk = sb("junk", [128, D])
    m2 = junk
    condbc = junk[:, :].rearrange("p (k n) -> p k n", k=8)
    tmpf = sb("tmpf", [128, D])
    hb = sb("hb", [128, D], BF16)
    hT = sb("hT", [128, 8, 128], BF16)
    proj = sb("proj", [128, 2320])
    att = sb("att", [128, D], BF16)
    h2b = hb
    sm = sb("sm", [128, 64])
    wrow = sb("wrow", [128, 640])
    WR = sb("WR", [128, 8, 36], BF16)
    brow = sb("brow", [128, 36])
    adab = m1[:, 0:512]
    EV = sb("EV", [128, 7888])

    class Carver:
        def __init__(self):
            self.off = 0

        def get(self, shape, dt=F32):
            np_ = shape[0]
            n = 1
            for d_ in shape[1:]:
                n *= d_
            sz = 2 if dt == BF16 else 4
            nb = (n * sz + 3) // 4 * 4
            c0 = self.off // 4
            self.off += nb
            assert self.off <= 7888 * 4, self.off
            v = EV[0:np_, c0:c0 + nb // 4]
            if dt != F32:
                v = v.bitcast(dt)[:, 0:n]
            if len(shape) == 3:
                v = v.rearrange("p (a b) -> p a b", a=shape[1])
            elif len(shape) == 4:
                v = v.rearrange("p (a b c) -> p a b c", a=shape[1], b=shape[2])
            return v

    cv = Carver()
    qkb = cv.get([128, 512], BF16)
    kdup = cv.get([128, 2, 128], BF16)
    qkT = [cv.get([128, 6, 128], BF16) for i in range(2)]
    vaS = [cv.get([128, 2, 65], BF16) for i in range(2)]
    sc = cv.get([128, 256])
    pexp = [cv.get([128, 256], BF16) for i in range(2)]
    abT = cv.get([32, 128], BF16)
    abp = cv.get([128, 32], BF16)
    gnb = cv.get([128, 256], BF16)
    zt = cv.get([128, 256])
    gneg = cv.get([128, 256])
    bsb = zt
    eb = cv.get([128, 256])
    enb = cv.get([128, 256])
    ekd = cv.get([128, 256])
    qd = cv.get([128, 256], BF16)
    kd = cv.get([128, 256], BF16)
    ku = cv.get([128, 256], BF16)
    vbb = cv.get([128, 512], BF16)
    sgn = cv.get([128, 512])
    dec = cv.get([64, 8])
    qdT = cv.get([64, 4, 128], BF16)
    qdm = cv.get([64, 4, 2, 128], BF16)
    kdT = cv.get([64, 4, 128], BF16)
    attT = cv.get([128, 128], BF16)
    St0 = cv.get([64, 4, 128])
    St1 = cv.get([64, 4, 128])
    St0b = cv.get([64, 4, 128], BF16)
    St1b = cv.get([64, 4, 128], BF16)
    gbias = cv.get([128, 256])
    onorm1 = cv.get([128, 128])
    gupf = cv.get([32, 256])
    gupb = cv.get([32, 256], BF16)
    esink = cv.get([128, 8])
    ev_even = cv.off
    cv = Carver()
    qnb = cv.get([128, 2048], BF16)
    vast = cv.get([128, 8, 129], BF16)
    kmsum = cv.get([128, 8, 32])
    kmean = cv.get([128, 8, 16], BF16)
    qTt = cv.get([128, 2, 256], BF16)
    scs = cv.get([128, 2, 16])
    sel = cv.get([128, 2, 2, 16])
    pT2 = [cv.get([128, 256], BF16) for i in range(4)]
    acc = cv.get([128, 2, 2, 129])
    attq = cv.get([128, 2, 256], BF16)
    sco = cv.get([128, 256])
    cv = Carver()
    xg = [cv.get([128, D], BF16) for i in range(2)]
    xgT = [cv.get([128, 8, 128], BF16) for i in range(2)]
    s1t = cv.get([128, 128])
    actT = [cv.get([128, 4, 128], BF16) for i in range(2)]
    rtab = cv.get([128, 6, 32])
    blkE = cv.get([128, 96])
    widx1f = cv.get([128, 8, 96])
    widx1i = cv.get([128, 8, 96], I32)
    widx2f = cv.get([128, 4, 96])
    widx2i = cv.get([128, 4, 96], I32)
    M12all = R2[:, 0:2048].rearrange("p (t k e) -> p t k e", t=32, k=2)
    slall = R2[:, 2048:2112].rearrange("p (t k) -> p t k", t=32)
    recall = R2[:, 2112:2368].rearrange("p (t k c) -> p t k c", t=32, k=2)
    top8 = sb("top8", [128, 8])
    lg = sb("lg", [128, 36])
    msk = sb("msk", [128, 32])
    M12 = sb("M12", [128, 2, 32])
    Mb = sb("Mb", [128, 32], BF16)
    cbase = sb("cbase", [128, 32])
    Cc = sb("Cc", [128, 32])
    prod = sb("prod", [128, 2, 32])
    desti = [sb(f"desti{i}", [128, 2], I32) for i in range(2)]
    tabt = [sb(f"tabt{i}", [128, 4]) for i in range(2)]
    idxi = [sb(f"idxi{i}", [128, 2], I32) for i in range(2)]
    yo = [m1, junk]
    print("SBUF bytes remaining/partition:", nc.sbuf_bytes_remaining)

    pT = [ps(f"pT{i}", [128, 8, 128], BF16) for i in range(2)]
    pM = [ps(f"pM{i}", [128, 512]) for i in range(2)]
    pS = [ps(f"pS{i}", [128, 512]) for i in range(2)]
    pO = [ps(f"pO{i}", [128, 512]) for i in range(2)]

    R1w = R1[:, :].rearrange("p (k n) -> p k n", k=8)

    def ew(eb_, which):
        base = eb_ * 12288
        if which == 0:
            return R1[:, base:base + 4096].rearrange("p (k n) -> p k n", k=8)
        if which == 1:
            return R1[:, base + 4096:base + 8192].rearrange("p (k n) -> p k n", k=8)
        return R1[:, base + 8192:base + 12288].rearrange("p (k n) -> p k n", k=4)

    def stg(i):
        return R2[:, i * 4096:(i + 1) * 4096]

    WINT = [f"R1_{a}_{b}" for a in range(2) for b in range(3)]

    def bc(ap):
        return ap.partition_broadcast(128).squeeze(1)

    def fence(tags, eng="dve"):
        P.op(eng, lambda e: e.memset(sm[:, 62:63], 0.0), tags, tags + ["sm62"])

    R2b = R2[:, :].bitcast(BF16)
    KTs = R2b[:, 0:8192].rearrange("p (h t) -> p h t", h=2)
    VAs = R2b[:, 8192:8192 + 8256].rearrange("p (t h c) -> p t h c", t=32, h=2)
    VAs2 = R2b[:, 8192:8192 + 8256].rearrange("p (t c) -> p t c", t=32)

    def mm(out_, lhsT, rhs, start=True, stop=True, r=None, w=None):
        P.pe(lambda e: e.matmul(out_, lhsT=lhsT, rhs=rhs, start=start, stop=stop), r=r, w=w)

    def tp(out_, in_, ident, r=None, w=None):
        P.pe(lambda e: e.transpose(out=out_, in_=in_, identity=ident), r=r, w=w)

    P.dma("sp", identf[:], ident_in, w=["identf"])
    P.dma("sp", trif[:], tri_in, w=["trif"])
    P.dma("sp", bonesf[:], bones_in, w=["bonesf"])
    P.dma("sp", cindf[:], cind_in, w=["cindf"])
    P.dma("sp", rcst[:], rcst_in, w=["rcst"])
    P.dma("sp", pidx[:], pidx_in, w=["pidx"])
    P.dma("sp", cTs[:], cT_in, w=["cTs"])
    P.dma("sp", tmpf[:, 0:128], stri_in, w=["tmpf"])
    P.dve(lambda e: e.tensor_copy(out=strib[:], in_=tmpf[:, 0:128]), r=["tmpf"], w=["strib"])
    P.dve(lambda e: e.tensor_copy(out=identb[:], in_=identf[:]), r=["identf"], w=["identb"])
    P.dve(lambda e: e.memset(onesb[:], 1.0), w=["onesb"])
    P.dve(lambda e: e.tensor_copy(out=bonesb[:], in_=bonesf[:]), r=["bonesf"], w=["bonesb"])
    P.dma("sp", BS[:], relT_in.rearrange("h k q -> k h q"), w=["BS"])
    P.dma("sp", m1[:, 0:256], mask0_in, w=["m1"])
    P.dve(lambda e: e.tensor_copy(out=BM1[:], in_=BS[:, :, 128:256]), r=["BS"], w=["BM"])
    for h in range(8):
        P.dve(lambda e, h=h: e.tensor_tensor(out=BS[:, h, :], in0=BS[:, h, :], in1=m1[:, 0:256], op=ALU.add),
              r=["BS", "m1"], w=["BS"])
    P.dma("sp", relfar[:], bc(rel_bias[31:32, :]), w=["relfar"])
    P.act(lambda e: e.activation(out=cTs[:], in_=cTs[:], func=AF.Silu), r=["cTs"], w=["cTs"])

    def rstd_from_ssq(ssq_ap, n, inv_n, tag):
        P.dve(lambda e: e.tensor_scalar(out=ssq_ap, in0=ssq_ap, scalar1=inv_n, scalar2=EPS, op0=ALU.mult, op1=ALU.add),
              r=[tag], w=[tag])
        P.act(lambda e: e.activation(out=ssq_ap, in_=ssq_ap, func=AF.Sqrt), r=[tag], w=[tag])
        P.dve(lambda e: e.reciprocal(out=ssq_ap, in_=ssq_ap), r=[tag], w=[tag])

    def load_weight_bf16(dst_view, src_ap, nk, ncols, dst_tags, engs=("act", "dve", "pool")):
        srcv = src_ap.rearrange("(k p) n -> p k n", p=128)
        cpp = 4096 // nk
        c0 = 0
        i = 0
        while c0 < ncols:
            cw = min(cpp, ncols - c0)
            part = i % 2
            sv = stg(part)[:, 0:nk * cw].rearrange("p (k n) -> p k n", k=nk)
            P.dma("sp", sv, srcv[:, :, c0:c0 + cw], w=[f"R2_{part}"])
            en = engs[i % len(engs)]
            P.op(en, lambda e, sv=sv, c0=c0, cw=cw, en=en: (e.copy if en == "act" else e.tensor_copy)(
                out=dst_view[:, :, c0:c0 + cw], in_=sv),
                 [f"R2_{part}"], None, wacc=dst_tags)
            c0 += cw
            i += 1

    def layer_start(l):
        even = (l % 2 == 0)
        j = l // 2
        P.barrier(lambda e: e.memset(sm[:, 62:63], 0.0))
        if l > 0:
            P.pool(lambda e: e.tensor_copy(out=rows[:, 6, :], in_=rows[:, 5, :]), r=["rows5"], w=["rows6"])
        P.dve(lambda e: e.memset(tmpf[:, 0:128], 1.0), w=["tmpf"])
        for k in range(8):
            P.dve(lambda e, k=k: e.tensor_scalar(out=condbc[:, k, :], in0=tmpf[:, 0:128], scalar1=cTs[:, k:k + 1],
                                                 scalar2=None, op0=ALU.mult), r=["tmpf", "cTs"], w=["junk"])
        aw = ada_w[l].rearrange("(k p) n -> p k n", p=128)
        for cch in range(12):
            part = cch % 2
            sv = stg(part)[:, :].rearrange("p (k n) -> p k n", k=8)
            P.dma("sp", sv, aw[:, :, cch * 512:(cch + 1) * 512], w=[f"R2_{part}"])
            P.dma("sp", adab[:], bc(ada_b[l:l + 1, cch * 512:(cch + 1) * 512]), w=["m1"])
            pm = pM[cch % 2]
            for k in range(8):
                mm(pm[:, :], condbc[:, k, :], sv[:, k, :], start=(k == 0), stop=(k == 7),
                   r=["junk", f"R2_{part}"], w=[f"pM{cch % 2}"])
            rr = cch // 2
            hf = cch % 2
            P.dve(lambda e, pm=pm, rr=rr, hf=hf: e.tensor_tensor(out=rows[:, rr, hf * 512:(hf + 1) * 512], in0=pm[:, :],
                                                                  in1=adab[:], op=ALU.add),
                  r=[f"pM{cch % 2}", "m1"], w=[f"rows{rr}"])
        for rr, nw in ((1, norm1_w), (4, norm2_w)):
            P.dma("sp", tmpf[:], bc(nw[l:l + 1, :]), w=["tmpf"])
            P.dve(lambda e, rr=rr: e.scalar_tensor_tensor(out=rows[:, rr, :], in0=rows[:, rr, :], scalar=1.0, in1=tmpf[:],
                                                          op0=ALU.add, op1=ALU.mult),
                  r=[f"rows{rr}", "tmpf"], w=[f"rows{rr}"])
        P.dma("sp", tmpf[:, 0:288].rearrange("p (k n) -> p k n", k=8), moe_wr[l].rearrange("(k p) n -> p k n", p=128),
              w=["tmpf"])
        P.dve(lambda e: e.tensor_copy(out=WR[:], in_=tmpf[:, 0:288].rearrange("p (k n) -> p k n", k=8)),
              r=["tmpf"], w=["WR"])
        P.dma("sp", brow[:], bc(moe_br[l:l + 1, :]), w=["brow"])
        P.dve(lambda e: e.memset(cbase[:], 0.0), w=["cbase"])
        P.dma("sp", TAB, tabinit_in, w=["TAB"])
        win = even_w_in[j] if even else odd_w_in[j]
        ncols = 2320 if even else 3072
        wo = even_w_out[j] if even else odd_w_out[j]
        load_weight_bf16(R1w[:, :, 0:ncols], win, 8, ncols, WINT)
        load_weight_bf16(WOUT[:, :, :], wo, 8, 1024, ["WOUTt"])
        if even:
            P.dma("sp", tmpf[:, 0:64], bc(a_q_norm[j:j + 1, :]), w=["tmpf"])
            P.dma("sp", tmpf[:, 64:128], bc(a_k_norm[j:j + 1, :]), w=["tmpf2"])
            for i in range(10):
                src = tmpf[:, 0:64] if i < 8 else tmpf[:, 64:128]
                P.dve(lambda e, i=i, src=src: e.tensor_copy(out=wrow[:, i * 64:(i + 1) * 64], in_=src),
                      r=["tmpf", "tmpf2"], w=["wrow"])
            P.dma("sp", gbias[:], bc(b_gate_bias[j:j + 1, :]), w=["gbias"])
            P.dve(lambda e: e.memset(gupf[:, :], 0.0), w=["gupf"])
            P.dma("sp", gupf[0:16, :], b_gate_up[j], w=["gupf"])
            P.dve(lambda e: e.tensor_copy(out=gupb[:, :], in_=gupf[:, :]), r=["gupf"], w=["gupb"])
            P.dve(lambda e: e.memset(abp[:, :], 0.0), w=["abp"])
            P.dma("sp", onorm1[:, :], bc(b_out_norm[j:j + 1, :]), w=["onorm4"])
            P.dma("sp", esink[:], bc(a_sinks[j:j + 1, :]), w=["esink"])
            P.act(lambda e: e.activation(out=esink[:], in_=esink[:], func=AF.Exp), r=["esink"], w=["esink"])
            P.dve(lambda e: e.memset(St0[:], 0.0), w=["St0"])
            P.dve(lambda e: e.memset(qdm[:], 0.0), w=["qdm"])
            for i in range(2):
                P.dve(lambda e, i=i: e.memset(vaS[i][:], 1.0), w=[f"vaS{i}"])
        else:
            P.dma("sp", wrow[:, 0:128], bc(c_q_norm[j:j + 1, :]), w=["wrow"])
            P.dma("sp", wrow[:, 128:256], bc(c_k_norm[j:j + 1, :]), w=["wrow2"])
            P.dve(lambda e: e.memset(vast[:], 1.0), w=["vast"])

    def load_x(l, t, xb):
        xs = xt[xb]
        tg = "xt0"
        rs = slice(t * 128, (t + 1) * 128)
        if l == 0:
            P.dma("sp", xs[:], x_in[rs, :], w=[tg])
        else:
            P.dma("sp", xs[:], XR[rs, :], r=[f"XR{t}"], w=[tg])
            P.dma("sp", m1[:], MO[rs, :], r=["MO"], w=["m1"])
            P.dma("sp", m2[:], MO[MOH + t * 128:MOH + (t + 1) * 128, :], r=["MO"], w=["junk"])
            P.pool(lambda e: e.tensor_tensor(out=m1[:], in0=m1[:], in1=m2[:], op=ALU.add), r=["m1", "junk"], w=["m1"])
            P.pool(lambda e: e.tensor_tensor(out=m1[:], in0=m1[:], in1=rows[:, 6, :], op=ALU.mult),
                   r=["m1", "rows6"], w=["m1"])
            P.dve(lambda e: e.tensor_tensor(out=xs[:], in0=xs[:], in1=m1[:], op=ALU.add), r=[tg, "m1"], w=[tg])

    def norm_mod(xs, xtag, grow, srow, outb, otag):
        P.act(lambda e: e.activation(out=junk[:], in_=xs[:], func=AF.Square, accum_out=sm[:, 0:1]),
              r=[xtag], w=["junk", "sm0"])
        rstd_from_ssq(sm[:, 0:1], 1, 1.0 / D, "sm0")
        P.dve(lambda e: e.scalar_tensor_tensor(out=tmpf[:], in0=xs[:], scalar=sm[:, 0:1], in1=rows[:, grow, :],
                                               op0=ALU.mult, op1=ALU.mult),
              r=[xtag, "sm0", f"rows{grow}"], w=["tmpf"])
        P.pool(lambda e: e.tensor_tensor(out=outb[:], in0=tmpf[:], in1=rows[:, srow, :], op=ALU.add),
               r=["tmpf", f"rows{srow}"], w=[otag])

    tcount = [0]

    def transpose8(src, stag, dst, dtag):
        i = tcount[0] % 2
        tcount[0] += 1
        for k in range(8):
            tp(pT[i][:, k, :], src[:, k * 128:(k + 1) * 128], identb[:], r=[stag, "identb"], w=[f"pT{i}"])
        P.act(lambda e: e.copy(out=dst[:], in_=pT[i][:]), r=[f"pT{i}"], w=[dtag])

    mcount = [0]

    def project(srcT, stag, wview, wtag, ncols, dst, dtag):
        c0 = 0
        while c0 < ncols:
            cw = min(512, ncols - c0)
            i = mcount[0] % 2
            mcount[0] += 1
            for k in range(8):
                mm(pM[i][:, 0:cw], srcT[:, k, :], wview[:, k, c0:c0 + cw], start=(k == 0), stop=(k == 7),
                   r=[stag, wtag], w=[f"pM{i}"])
            if i == 0:
                P.act(lambda e, i=i, c0=c0, cw=cw: e.copy(out=dst[:, c0:c0 + cw], in_=pM[i][:, 0:cw]),
                      r=[f"pM{i}"], w=[dtag])
            else:
                P.dve(lambda e, i=i, c0=c0, cw=cw: e.tensor_copy(out=dst[:, c0:c0 + cw], in_=pM[i][:, 0:cw]),
                      r=[f"pM{i}"], w=[dtag])
            c0 += cw

    def tail(l, t, xb, attsrc, atag):
        xs = xt[xb]
        tg = "xt0"
        rs = slice(t * 128, (t + 1) * 128)
        transpose8(attsrc, atag, hT, "hT")
        for c in range(2):
            i = mcount[0] % 2
            mcount[0] += 1
            for k in range(8):
                mm(pM[i][:, :], hT[:, k, :], WOUT[:, k, c * 512:(c + 1) * 512], start=(k == 0), stop=(k == 7),
                   r=["hT", "WOUTt"], w=[f"pM{i}"])
            P.dve(lambda e, i=i, c=c: e.tensor_tensor(out=tmpf[:, c * 512:(c + 1) * 512], in0=pM[i][:, :],
                                                      in1=rows[:, 2, c * 512:(c + 1) * 512], op=ALU.mult),
                  r=[f"pM{i}", "rows2"], w=["tmpf"])
        P.pool(lambda e: e.tensor_tensor(out=xs[:], in0=xs[:], in1=tmpf[:], op=ALU.add), r=[tg, "tmpf"], w=[tg])
        P.dma("sp", XR[rs, :], xs[:], r=[tg], w=[f"XR{t}"])
        norm_mod(xs, tg, 4, 3, h2b, "hb")
        P.dma("sp", H2[rs, :], h2b[:], r=["hb"], wacc=["H2"])
        transpose8(h2b, "hb", hT, "hT")
        i = mcount[0] % 2
        mcount[0] += 1
        for k in range(8):
            mm(pM[i][:, 0:36], hT[:, k, :], WR[:, k, :], start=(k == 0), stop=(k == 7), r=["hT", "WR"], w=[f"pM{i}"])
        P.dve(lambda e: e.tensor_tensor(out=lg[:], in0=pM[i][:, 0:36], in1=brow[:], op=ALU.add),
              r=[f"pM{i}", "brow"], w=["lg"])
        RT = ["rt"]
        V = P.dve
        V(lambda e: e.tensor_reduce(out=sm[:, 8:9], in_=lg[:, 0:4], axis=AX.X, op=ALU.max), r=["lg"], w=RT)
        V(lambda e: e.tensor_scalar(out=sm[:, 12:16], in0=lg[:, 0:4], scalar1=sm[:, 8:9], scalar2=None, op0=ALU.subtract),
          r=["lg"] + RT, w=RT)
        P.act(lambda e: e.activation(out=sm[:, 16:20], in_=sm[:, 12:16], func=AF.Exp, accum_out=sm[:, 9:10]), r=RT, w=RT)
        V(lambda e: e.reciprocal(out=sm[:, 9:10], in_=sm[:, 9:10]), r=RT, w=RT)
        V(lambda e: e.tensor_scalar(out=sm[:, 20:24], in0=sm[:, 12:16], scalar1=0.0, scalar2=None, op0=ALU.is_ge),
          r=RT, w=RT)
        V(lambda e: e.tensor_scalar(out=sm[:, 20:24], in0=sm[:, 20:24], scalar1=BIG, scalar2=-BIG, op0=ALU.mult,
                                    op1=ALU.add), r=RT, w=RT)
        V(lambda e: e.tensor_tensor(out=msk[:, :].rearrange("p (g e) -> p g e", g=4),
                                    in0=lg[:, 4:36].rearrange("p (g e) -> p g e", g=4),
                                    in1=sm[:, 20:24].unsqueeze(2).to_broadcast([128, 4, 8]), op=ALU.add),
          r=["lg"] + RT, w=["msk"])
        V(lambda e: e.max(out=top8[:], in_=msk[:]), r=["msk"], w=["top8"])
        V(lambda e: e.tensor_scalar(out=M12[:, 0, :], in0=msk[:], scalar1=top8[:, 0:1], scalar2=None, op0=ALU.is_equal),
          r=["msk", "top8"], w=["M12"])
        V(lambda e: e.tensor_scalar(out=M12[:, 1, :], in0=msk[:], scalar1=top8[:, 1:2], scalar2=None, op0=ALU.is_equal),
          r=["msk", "top8"], w=["M12"])
        V(lambda e: e.tensor_tensor(out=sm[:, 10:11], in0=top8[:, 1:2], in1=top8[:, 0:1], op=ALU.subtract),
          r=["top8"], w=RT)
        P.act(lambda e: e.activation(out=sm[:, 10:11], in_=sm[:, 10:11], func=AF.Exp), r=RT, w=RT)
        V(lambda e: e.tensor_scalar(out=sm[:, 10:11], in0=sm[:, 10:11], scalar1=1.0, scalar2=None, op0=ALU.add), r=RT, w=RT)
        V(lambda e: e.reciprocal(out=sm[:, 10:11], in_=sm[:, 10:11]), r=RT, w=RT)
        rc = recall[:, t, :, :]
        rtag = "R2_0"
        V(lambda e: e.tensor_tensor(out=rc[:, 0, 2:3], in0=sm[:, 10:11], in1=sm[:, 9:10], op=ALU.mult), r=RT, w=[rtag])
        V(lambda e: e.tensor_tensor(out=rc[:, 1, 2:3], in0=sm[:, 9:10], in1=rc[:, 0, 2:3], op=ALU.subtract),
          r=RT + [rtag], w=[rtag])
        V(lambda e: e.tensor_scalar(out=rc[:, 0, 0:2], in0=pidx[:, 0:1].to_broadcast([128, 2]), scalar1=float(t * 128),
                                    scalar2=None, op0=ALU.add), r=["pidx"], w=[rtag])
        V(lambda e: e.tensor_scalar(out=rc[:, 1, 0:1], in0=pidx[:, 0:1], scalar1=float(t * 128), scalar2=None,
                                    op0=ALU.add), r=["pidx"], w=[rtag])
        V(lambda e: e.tensor_scalar(out=rc[:, 1, 1:2], in0=pidx[:, 0:1], scalar1=float(t * 128 + MOH), scalar2=None,
                                    op0=ALU.add), r=["pidx"], w=[rtag])
        V(lambda e: e.memset(rc[:, :, 3:4], 0.0), w=[rtag])
        V(lambda e: e.tensor_tensor(out=Mb[:], in0=M12[:, 0, :], in1=M12[:, 1, :], op=ALU.add), r=["M12"], w=["Mb"])
        V(lambda e: e.tensor_copy(out=M12all[:, t, :, :], in_=M12[:]), r=["M12"], w=[rtag])
        j_ = mcount[0] % 2
        mcount[0] += 1
        mm(pM[j_][:, 0:32], strib[:], Mb[:], r=["strib", "Mb"], w=[f"pM{j_}"])
        mm(pM[j_][:, 64:96], onesb[:], Mb[:], r=["onesb", "Mb"], w=[f"pM{j_}"])
        V(lambda e: e.tensor_tensor(out=Cc[:], in0=pM[j_][:, 0:32], in1=cbase[:], op=ALU.add),
          r=[f"pM{j_}", "cbase"], w=["Cc"])
        V(lambda e: e.tensor_tensor(out=cbase[:], in0=pM[j_][:, 64:96], in1=cbase[:], op=ALU.add),
          r=[f"pM{j_}", "cbase"], w=["cbase"])
        V(lambda e: e.tensor_tensor(out=prod[:], in0=M12[:], in1=Cc[:, :].unsqueeze(1).to_broadcast([128, 2, 32]),
                                    op=ALU.mult), r=["M12", "Cc"], w=["prod"])
        V(lambda e: e.tensor_reduce(out=slall[:, t, :], in_=prod[:], axis=AX.X, op=ALU.add), r=["prod"], w=[rtag])

    def route_finalize(l):
        P.barrier(lambda e: e.memset(sm[:, 62:63], 0.0))
        V = P.dve
        FT = ["rfin"]
        for half in range(2):
            es_ = slice(half * 16, (half + 1) * 16)
            t3 = tmpf[:, :].rearrange("p (a b) -> p a b", a=16)
            V(lambda e: e.tensor_tensor(out=t3, in0=cbase[:, es_].unsqueeze(2).to_broadcast([128, 16, 64]),
                                        in1=rcst[:, 0:64].unsqueeze(1).to_broadcast([128, 16, 64]), op=ALU.is_gt),
              r=["cbase", "rcst"], w=["tmpf"])
            V(lambda e: e.tensor_reduce(out=rtab[:, 0, es_], in_=t3, axis=AX.X, op=ALU.add), r=["tmpf"], w=FT)
        V(lambda e: e.tensor_scalar(out=rtab[:, 1, :], in0=rtab[:, 0, :], scalar1=128.0, scalar2=None, op0=ALU.mult),
          r=FT, w=FT)
        V(lambda e: e.memset(rtab[:, 5, :], 0.0), w=FT)
        V(lambda e: e.tensor_tensor_scan(out=rtab[:, 2, :], data0=rtab[:, 1, :], data1=rtab[:, 5, :], initial=0.0,
                                         op0=ALU.add, op1=ALU.add), r=FT, w=FT)
        V(lambda e: e.tensor_tensor(out=rtab[:, 3, :], in0=rtab[:, 2, :], in1=rtab[:, 1, :], op=ALU.subtract),
          r=FT, w=FT)
        V(lambda e: e.memset(blkE[:, :], 0.0), w=["blkE"])
        for ex in range(32):
            V(lambda e, ex=ex: e.scalar_tensor_tensor(out=blkE[:, :], in0=rcst[:, 64:160], scalar=rtab[:, 2, ex:ex + 1],
                                                      in1=blkE[:, :], op0=ALU.is_ge, op1=ALU.add),
              r=FT + ["rcst", "blkE"], w=["blkE"])
        V(lambda e: e.tensor_scalar(out=blkE[:, :], in0=blkE[:, :], scalar1=31.0, scalar2=float(32 * l), op0=ALU.min,
                                    op1=ALU.add), r=["blkE"], w=["blkE"])
        V(lambda e: e.tensor_scalar(out=widx1f[:, 0, :], in0=blkE[:, :], scalar1=128.0, scalar2=pidx[:, 0:1],
                                    op0=ALU.mult, op1=ALU.add), r=["blkE", "pidx"], w=["widx1f"])
        V(lambda e: e.tensor_copy(out=widx1i[:, 0, :], in_=widx1f[:, 0, :]), r=["widx1f"], w=["widx1i"])
        for t in range(NT):
            rb_ = t % 2
            di = desti[rb_]
            dtag = f"desti{rb_}"
            V(lambda e, t=t: e.tensor_tensor(out=prod[:], in0=M12all[:, t, :, :],
                                             in1=rtab[:, 3, :].unsqueeze(1).to_broadcast([128, 2, 32]), op=ALU.mult),
              r=["R2_0"] + FT, w=["prod"])
            V(lambda e: e.tensor_reduce(out=sm[:, 26:28], in_=prod[:], axis=AX.X, op=ALU.add), r=["prod"], w=["rt"])
            V(lambda e, t=t: e.tensor_tensor(out=sm[:, 26:28], in0=sm[:, 26:28], in1=slall[:, t, :], op=ALU.add),
              r=["rt", "R2_0"], w=["rt"])
            V(lambda e, di=di: e.tensor_copy(out=di[:], in_=sm[:, 26:28]), r=["rt"], w=[dtag])
            for k in range(2):
                P.op("pool", lambda e, k=k, di=di, t=t: e.indirect_dma_start(
                    out=TAB, out_offset=bass.IndirectOffsetOnAxis(ap=di[:, k:k + 1], axis=0), in_=recall[:, t, k, :],
                    in_offset=None), ["R2_0", dtag], None, dma=True, wacc=["TAB"])

    def even_layer(l):
        for t in range(DBG.get("tiles", NT)):
            xb = t % 2
            cur = t % 2
            prv = 1 - cur
            load_x(l, t, xb)
            norm_mod(xt[xb], "xt0", 1, 0, hb, "hb")
            transpose8(hb, "hb", hT, "hT")
            project(hT, "hT", R1w, WINT, 2320, proj, "proj")
            if DBG.get("cut") == 1:
                P.dma("sp", out[t * 128:(t + 1) * 128, :], proj[:, 0:1024], r=["proj"], w=[f"out{t}"])
                continue
            P.dve(lambda e: e.tensor_tensor(out=tmpf[:, 0:640], in0=proj[:, 0:640], in1=proj[:, 0:640], op=ALU.mult),
                  r=["proj"], w=["tmpf"])
            P.dve(lambda e: e.tensor_reduce(out=sm[:, 32:42], in_=tmpf[:, 0:640].rearrange("p (g d) -> p g d", g=10),
                                            axis=AX.X, op=ALU.add), r=["tmpf"], w=["sm32"])
            rstd_from_ssq(sm[:, 32:42], 10, 1.0 / 64, "sm32")
            P.dve(lambda e: e.tensor_tensor(out=tmpf[:, 0:640].rearrange("p (g d) -> p g d", g=10),
                                            in0=proj[:, 0:640].rearrange("p (g d) -> p g d", g=10),
                                            in1=sm[:, 32:42].unsqueeze(2).to_broadcast([128, 10, 64]), op=ALU.mult),
                  r=["proj", "sm32"], w=["tmpf"])
            P.pool(lambda e: e.tensor_tensor(out=qkb[:], in0=tmpf[:, 0:512], in1=wrow[:, 0:512], op=ALU.mult),
                   r=["tmpf", "wrow"], w=["qkb"])
            for hf in range(2):
                P.pool(lambda e, hf=hf: e.tensor_tensor(out=kdup[:, :, hf * 64:(hf + 1) * 64],
                                                        in0=tmpf[:, 512:640].rearrange("p (g d) -> p g d", g=2),
                                                        in1=wrow[:, 512:640].rearrange("p (g d) -> p g d", g=2),
                                                        op=ALU.mult), r=["tmpf", "wrow"], w=["kdup"])
            P.act(lambda e: e.copy(out=vaS[cur][:, :, 0:64], in_=proj[:, 640:768].rearrange("p (g d) -> p g d", g=2)),
                  r=["proj"], w=[f"vaS{cur}"])
            i = tcount[0] % 2
            tcount[0] += 1
            for pr in range(4):
                tp(pT[i][:, pr, :], qkb[:, pr * 128:(pr + 1) * 128], identb[:], r=["qkb", "identb"], w=[f"pT{i}"])
            for g in range(2):
                tp(pT[i][:, 4 + g, :], kdup[:, g, :], identb[:], r=["kdup", "identb"], w=[f"pT{i}"])
            P.act(lambda e, i=i: e.copy(out=qkT[cur][:, :, :], in_=pT[i][:, 0:6, :]), r=[f"pT{i}"], w=[f"qkT{cur}"])
            if DBG.get("cut") == 2:
                P.dve(lambda e: e.tensor_copy(out=tmpf[:, 0:768], in_=qkT[cur][:, :, :].rearrange("p a b -> p (a b)")), r=[f"qkT{cur}"], w=["tmpf"])
                P.dma("sp", out[t * 128:(t + 1) * 128, 0:768], tmpf[:, 0:768], r=["tmpf"], w=[f"out{t}"])
                continue
            for hg in range(2):
                po = pO[hg]
                for hh in range(4):
                    h = hg * 4 + hh
                    g = h // 4
                    pr = h // 2
                    b0 = (h % 2) * 64
                    psx = pS[h % 2]
                    mm(psx[:, 0:128], qkT[cur][b0:b0 + 64, 4 + g, :], qkT[cur][b0:b0 + 64, pr, :],
                       r=[f"qkT{cur}"], w=[f"pS{h % 2}"])
                    if t > 0:
                        mm(psx[:, 128:256], qkT[prv][b0:b0 + 64, 4 + g, :], qkT[cur][b0:b0 + 64, pr, :],
                           r=[f"qkT{cur}", f"qkT{prv}"], w=[f"pS{h % 2}"])
                    P.dve(lambda e, psx=psx, h=h: e.scalar_tensor_tensor(out=sc[:], in0=psx[:, 0:256], scalar=0.125,
                                                                         in1=BS[:, h, :], op0=ALU.mult, op1=ALU.add),
                          r=[f"pS{h % 2}", "BS"], w=["sc"])
                    pe_ = pexp[h % 2]
                    P.act(lambda e, pe_=pe_: e.activation(out=pe_[:], in_=sc[:], func=AF.Exp), r=["sc"], w=[f"pexp{h % 2}"])
                    mm(po[:, hh * 65:(hh + 1) * 65], pe_[:, 0:128], vaS[cur][:, g, :], start=True, stop=(t == 0),
                       r=[f"pexp{h % 2}", f"vaS{cur}"], w=[f"pO{hg}"])
                    if t > 0:
                        mm(po[:, hh * 65:(hh + 1) * 65], pe_[:, 128:256], vaS[prv][:, g, :], start=False, stop=True,
                           r=[f"pexp{h % 2}", f"vaS{prv}"], w=[f"pO{hg}"])
                pov = po[:, 0:260].rearrange("p (h c) -> p h c", h=4)
                P.dve(lambda e, pov=pov, hg=hg: e.tensor_tensor(out=sm[:, 44:48].unsqueeze(2), in0=pov[:, :, 64:65],
                                                                in1=esink[:, hg * 4:(hg + 1) * 4].unsqueeze(2), op=ALU.add),
                      r=[f"pO{hg}", "esink"], w=["sm44"])
                P.dve(lambda e: e.reciprocal(out=sm[:, 44:48], in_=sm[:, 44:48]), r=["sm44"], w=["sm44"])
                P.dve(lambda e, pov=pov, hg=hg: e.tensor_tensor(
                    out=att[:, hg * 256:(hg + 1) * 256].rearrange("p (h d) -> p h d", h=4), in0=pov[:, :, 0:64],
                    in1=sm[:, 44:48].unsqueeze(2).to_broadcast([128, 4, 64]), op=ALU.mult),
                    r=[f"pO{hg}", "sm44"], w=["att"])
            if DBG.get("cut") == 31:
                P.dve(lambda e: e.tensor_copy(out=tmpf[:, 0:256], in_=sc[:, :]), r=["sc"], w=["tmpf"])
                P.dve(lambda e: e.tensor_copy(out=tmpf[:, 256:516], in_=pO[1][:, 0:260]), r=["pO1"], w=["tmpf"])
                P.dve(lambda e: e.tensor_copy(out=tmpf[:, 520:528], in_=esink[:, :]), r=["esink"], w=["tmpf"])
                P.dve(lambda e: e.tensor_copy(out=tmpf[:, 528:532], in_=sm[:, 44:48]), r=["sm44"], w=["tmpf"])
                P.dve(lambda e: e.tensor_copy(out=tmpf[:, 532:788], in_=pexp[1][:, :]), r=["pexp1"], w=["tmpf"])
                P.dve(lambda e: e.tensor_copy(out=tmpf[:, 788:918], in_=vaS[cur][:, :, :].rearrange("p a b -> p (a b)")), r=[f"vaS{cur}"], w=["tmpf"])
                P.dma("sp", out[t * 128:(t + 1) * 128, :], tmpf[:, :], r=["tmpf"], w=[f"out{t}"])
                continue
            if DBG.get("cut") == 3:
                P.dve(lambda e: e.tensor_copy(out=tmpf[:, 0:512], in_=att[:, 0:512]), r=["att"], w=["tmpf"])
                P.dma("sp", out[t * 128:(t + 1) * 128, 0:512], tmpf[:, 0:512], r=["tmpf"], w=[f"out{t}"])
                continue
            P.dve(lambda e: e.tensor_copy(out=abp[:, 0:16], in_=proj[:, 2304:2320]), r=["proj"], w=["abp"])
            i = tcount[0] % 2
            tcount[0] += 1
            tp(pT[i][0:32, 0, :], abp[:, :], identb[:], r=["abp", "identb"], w=[f"pT{i}"])
            P.act(lambda e: e.copy(out=abT[:, :], in_=pT[i][0:32, 0, :]), r=[f"pT{i}"], w=["abT"])
            mm(pS[1][:, 0:256], abT[:, :], gupb[:, :], r=["abT", "gupb"], w=["pS1"])
            P.dve(lambda e: e.tensor_tensor(out=zt[:], in0=pS[1][:, 0:256], in1=gbias[:], op=ALU.add),
                  r=["pS1", "gbias"], w=["zt"])
            if DBG.get("cut") == 41:
                P.dve(lambda e: e.tensor_copy(out=tmpf[:, 0:256], in_=zt[:, :]), r=["zt"], w=["tmpf"])
                P.dma("sp", out[t * 128:(t + 1) * 128, 0:256], tmpf[:, 0:256], r=["tmpf"], w=[f"out{t}"])
                continue
            P.act(lambda e: e.activation(out=zt[:], in_=zt[:], func=AF.Exp, scale=-1.0), r=["zt"], w=["zt"])
            P.dve(lambda e: e.tensor_scalar(out=zt[:], in0=zt[:], scalar1=1.0, scalar2=None, op0=ALU.add), r=["zt"], w=["zt"])
            P.act(lambda e: e.activation(out=zt[:], in_=zt[:], func=AF.Ln), r=["zt"], w=["zt"])
            P.dve(lambda e: e.tensor_scalar(out=gneg[:], in0=zt[:], scalar1=-1.0 / 16.0, scalar2=None, op0=ALU.mult),
                  r=["zt"], w=["gneg"])
            if DBG.get("cut") == 42:
                P.dve(lambda e: e.tensor_copy(out=tmpf[:, 0:256], in_=gneg[:, :]), r=["gneg"], w=["tmpf"])
                P.dma("sp", out[t * 128:(t + 1) * 128, 0:256], tmpf[:, 0:256], r=["tmpf"], w=[f"out{t}"])
                continue
            mm(pS[0][:, 0:256], trif[:], gneg[:], r=["trif", "gneg"], w=["pS0"])
            mm(pS[0][:, 256:512], bonesf[:], gneg[:], r=["bonesf", "gneg"], w=["pS0"])
            P.act(lambda e: e.copy(out=bsb[:], in_=pS[0][:, 0:256]), r=["pS0"], w=["zt"])
            P.act(lambda e: e.activation(out=eb[:], in_=pS[0][:, 0:256], func=AF.Exp), r=["pS0"], w=["eb"])
            P.act(lambda e: e.activation(out=enb[:], in_=pS[0][:, 0:256], func=AF.Exp, scale=-1.0), r=["pS0"], w=["enb"])
            P.dve(lambda e: e.tensor_tensor(out=ekd[:], in0=pS[0][:, 256:512], in1=bsb[:], op=ALU.subtract),
                  r=["pS0", "zt"], w=["ekd"])
            P.act(lambda e: e.activation(out=ekd[:], in_=ekd[:], func=AF.Exp), r=["ekd"], w=["ekd"])
            if DBG.get("cut") == 43:
                P.dve(lambda e: e.tensor_copy(out=tmpf[:, 0:256], in_=ekd[:, :]), r=["ekd"], w=["tmpf"])
                P.dma("sp", out[t * 128:(t + 1) * 128, 0:256], tmpf[:, 0:256], r=["tmpf"], w=[f"out{t}"])
                continue
            P.dve(lambda e: e.scalar_tensor_tensor(out=qd[:], in0=proj[:, 768:1024], scalar=0.125, in1=eb[:],
                                                   op0=ALU.mult, op1=ALU.mult), r=["proj", "eb"], w=["qd"])
            P.pool(lambda e: e.tensor_tensor(out=kd[:], in0=proj[:, 1024:1280], in1=enb[:], op=ALU.mult),
                   r=["proj", "enb"], w=["kd"])
            P.pool(lambda e: e.tensor_tensor(out=ku[:], in0=proj[:, 1024:1280], in1=ekd[:], op=ALU.mult),
                   r=["proj", "ekd"], w=["ku"])
            P.pool(lambda e: e.tensor_copy(out=vbb[:], in_=proj[:, 1280:1792]), r=["proj"], w=["vbb"])
            P.act(lambda e: e.activation(out=sgn[:], in_=proj[:, 1792:2304], func=AF.Silu), r=["proj"], w=["sgn"])
            P.dve(lambda e: e.tensor_tensor(out=sgn[:, :].rearrange("p (h d) -> p h d", h=4),
                                             in0=sgn[:, :].rearrange("p (h d) -> p h d", h=4),
                                             in1=onorm1[:, :].unsqueeze(1).to_broadcast([128, 4, 128]), op=ALU.mult),
                   r=["sgn", "onorm4"], w=["sgn"])
            P.dve(lambda e: e.tensor_copy(out=gnb[:, :], in_=gneg[:, :]), r=["gneg"], w=["gnb"])
            for hh in range(4):
                mm(pS[1][0:64, hh * 128:(hh + 1) * 128], gnb[:, hh * 64:(hh + 1) * 64], bonesb[:, :], r=["gnb", "bonesb"], w=["pS1"])
            P.act(lambda e: e.activation(out=dec[:, :].rearrange("p (h c) -> p h c", h=4),
                                         in_=pS[1][0:64, :].rearrange("p (h c r) -> p h c r", h=4, c=2)[:, :, :, 0],
                                         func=AF.Exp), r=["pS1"], w=["dec"])
            if DBG.get("cut") == 4:
                P.dve(lambda e: e.tensor_copy(out=tmpf[:, 0:256], in_=qd[:, :]), r=["qd"], w=["tmpf"])
                P.dve(lambda e: e.tensor_copy(out=tmpf[:, 256:512], in_=ku[:, :]), r=["ku"], w=["tmpf"])
                P.dve(lambda e: e.tensor_copy(out=tmpf[0:64, 512:520], in_=dec[:, :]), r=["dec"], w=["tmpf"])
                P.dma("sp", out[t * 128:(t + 1) * 128, 0:520], tmpf[:, 0:520], r=["tmpf"], w=[f"out{t}"])
                continue
            i = tcount[0] % 2
            tcount[0] += 1
            for hh in range(4):
                tp(pT[i][0:64, hh, :], qd[:, hh * 64:(hh + 1) * 64], identb[:], r=["qd", "identb"], w=[f"pT{i}"])
                tp(pT[i][0:64, 4 + hh, :], kd[:, hh * 64:(hh + 1) * 64], identb[:], r=["kd", "identb"], w=[f"pT{i}"])
            P.act(lambda e, i=i: e.copy(out=qdT[:], in_=pT[i][0:64, 0:4, :]), r=[f"pT{i}"], w=["qdT"])
            P.act(lambda e, i=i: e.copy(out=kdT[:], in_=pT[i][0:64, 4:8, :]), r=[f"pT{i}"], w=["kdT"])
            P.dve(lambda e, i=i: e.tensor_copy(out=qdm[:, :, 0, 0:64], in_=pT[i][0:64, 0:4, 0:64]), r=[f"pT{i}"], w=["qdm"])
            P.dve(lambda e, i=i: e.tensor_copy(out=qdm[:, :, 1, 64:128], in_=pT[i][0:64, 0:4, 64:128]),
                  r=[f"pT{i}"], w=["qdm"])
            P.act(lambda e: e.copy(out=St0b[:], in_=St0[:]), r=["St0"], w=["St0b"])
            for hh in range(4):
                vs = slice(hh * 128, (hh + 1) * 128)
                ks = slice(hh * 64, (hh + 1) * 64)
                pa = pS[hh % 2]
                mm(pa[:, 0:128], kdT[:, hh, :], qdT[:, hh, :], r=["kdT", "qdT"], w=[f"pS{hh % 2}"])
                P.dve(lambda e, pa=pa: e.tensor_tensor(out=attT[:], in0=pa[:, 0:128], in1=trif[:], op=ALU.mult),
                      r=[f"pS{hh % 2}", "trif"], w=["attT"])
                mm(pO[0][0:64, vs], ku[0:64, ks], vbb[0:64, vs], r=["ku", "vbb"], w=["pO0"])
                mm(pO[1][0:64, vs], ku[64:128, ks], vbb[64:128, vs], r=["ku", "vbb"], w=["pO1"])
                P.dve(lambda e, hh=hh: e.scalar_tensor_tensor(out=St1[:, hh, :], in0=St0[:, hh, :],
                                                              scalar=dec[:, 2 * hh:2 * hh + 1], in1=pO[0][0:64, vs],
                                                              op0=ALU.mult, op1=ALU.add),
                      r=["St0", "dec", "pO0"], w=["St1"])
                P.act(lambda e, hh=hh: e.copy(out=St1b[:, hh, :], in_=St1[:, hh, :]), r=["St1"], w=["St1b"])
                mm(pa[:, 256:384], attT[:, :], vbb[:, vs], start=True, stop=False, r=["attT", "vbb"], w=[f"pS{hh % 2}"])
                mm(pa[:, 256:384], qdm[:, hh, 0, :], St0b[:, hh, :], start=False, stop=False, r=["qdm", "St0b"],
                   w=[f"pS{hh % 2}"])
                mm(pa[:, 256:384], qdm[:, hh, 1, :], St1b[:, hh, :], start=False, stop=True, r=["qdm", "St1b"],
                   w=[f"pS{hh % 2}"])
                P.dve(lambda e, hh=hh: e.scalar_tensor_tensor(out=St0[:, hh, :], in0=St1[:, hh, :],
                                                              scalar=dec[:, 2 * hh + 1:2 * hh + 2],
                                                              in1=pO[1][0:64, vs], op0=ALU.mult, op1=ALU.add),
                      r=["St1", "dec", "pO1"], w=["St0"])
                P.act(lambda e, pa=pa: e.activation(out=junk[:, 0:128], in_=pa[:, 256:384], func=AF.Square,
                                                    accum_out=sm[:, 50:51]), r=[f"pS{hh % 2}"], w=["junk", "sm50"])
                rstd_from_ssq(sm[:, 50:51], 1, 1.0 / 128, "sm50")
                P.dve(lambda e, pa=pa, hh=hh: e.scalar_tensor_tensor(out=att[:, 512 + hh * 128:512 + (hh + 1) * 128],
                                                                     in0=pa[:, 256:384], scalar=sm[:, 50:51],
                                                                     in1=sgn[:, hh * 128:(hh + 1) * 128], op0=ALU.mult,
                                                                     op1=ALU.mult),
                      r=[f"pS{hh % 2}", "sm50", "sgn"], w=["att"])
            if DBG.get("notail"):
                P.dve(lambda e: e.tensor_copy(out=tmpf[:], in_=att[:]), r=["att"], w=["tmpf"])
                P.dma("sp", out[t * 128:(t + 1) * 128, :], tmpf[:], r=["tmpf"], w=[f"out{t}"])
                continue
            tail(l, t, xb, att, "att")
            if DBG.get("dumph2"):
                P.dve(lambda e: e.tensor_copy(out=tmpf[:], in_=hb[:]), r=["hb"], w=["tmpf"])
                P.dma("sp", out[t * 128:(t + 1) * 128, :], tmpf[:], r=["tmpf"], w=[f"out{t}"])

    def odd_layer(l):
        P.dve(lambda e: e.memset(kmsum[:], 0.0), w=["kmsum"])
        for t in range(NT):
            xb = t % 2
            rs = slice(t * 128, (t + 1) * 128)
            load_x(l, t, xb)
            if l > 0:
                P.dma("sp", XR[rs, :], xt[xb][:], r=["xt0"], w=[f"XR{t}"])
            norm_mod(xt[xb], "xt0", 1, 0, hb, "hb")
            transpose8(hb, "hb", hT, "hT")
            project(hT, "hT", R1w, WINT, 2048, proj, "proj")
            for half in range(2):
                src = proj[:, half * 1024:(half + 1) * 1024]
                P.dve(lambda e, src=src: e.tensor_tensor(out=tmpf[:], in0=src, in1=src, op=ALU.mult), r=["proj"], w=["tmpf"])
                P.dve(lambda e: e.tensor_reduce(out=sm[:, 32:40], in_=tmpf[:, :].rearrange("p (g d) -> p g d", g=8),
                                                axis=AX.X, op=ALU.add), r=["tmpf"], w=["sm32"])
                rstd_from_ssq(sm[:, 32:40], 8, 1.0 / 128, "sm32")
                P.dve(lambda e, src=src: e.tensor_tensor(out=tmpf[:, :].rearrange("p (g d) -> p g d", g=8),
                                                         in0=src.rearrange("p (g d) -> p g d", g=8),
                                                         in1=sm[:, 32:40].unsqueeze(2).to_broadcast([128, 8, 128]),
                                                         op=ALU.mult), r=["proj", "sm32"], w=["tmpf"])
                P.pool(lambda e, half=half: e.tensor_tensor(
                    out=qnb[:, half * 1024:(half + 1) * 1024].rearrange("p (g d) -> p g d", g=8),
                    in0=tmpf[:, :].rearrange("p (g d) -> p g d", g=8),
                    in1=wrow[:, half * 128:(half + 1) * 128].unsqueeze(1).to_broadcast([128, 8, 128]), op=ALU.mult),
                    r=["tmpf", "wrow", "wrow2"], w=["qnb"])
            project(hT, "hT", R1w[:, :, 2048:3072], WINT, 1024, proj, "proj")
            P.act(lambda e: e.copy(out=vast[:, :, 0:128], in_=proj[:, 0:1024].rearrange("p (g d) -> p g d", g=8)),
                  r=["proj"], w=["vast"])
            P.dma("sp", VA[rs, :, :].rearrange("t h c -> t (h c)"), vast[:, :, :].rearrange("p h c -> p (h c)"), r=["vast"], wacc=["VA"])
            for half in range(2):
                dstD = QT if half == 0 else KT
                transpose8(qnb[:, half * 1024:(half + 1) * 1024], "qnb", hT, "hT")
                P.dma("sp", dstD[:, :, rs].rearrange("h d t -> d h t"), hT[:], r=["hT"], wacc=["QT" if half == 0 else "KT"])
                if half == 1:
                    P.dve(lambda e, t=t: e.tensor_reduce(out=kmsum[:, :, t], in_=hT[:], axis=AX.X, op=ALU.add),
                          r=["hT"], w=["kmsum"])
        P.dve(lambda e: e.tensor_tensor(out=tmpf[:, 0:128].rearrange("p (h b) -> p h b", h=8), in0=kmsum[:, :, :].rearrange("p h (b two) -> p h b two", two=2)[:, :, :, 0],
                                        in1=kmsum[:, :, :].rearrange("p h (b two) -> p h b two", two=2)[:, :, :, 1], op=ALU.add), r=["kmsum"], w=["tmpf"])
        P.dve(lambda e: e.tensor_scalar(out=kmean[:], in0=tmpf[:, 0:128].rearrange("p (h b) -> p h b", h=8),
                                        scalar1=1.0 / 256, scalar2=None, op0=ALU.mult), r=["tmpf"], w=["kmean"])
        pcount = [0]
        for hgp in range(4):
            h0 = hgp * 2
            P.dma("sp", KTs, KT[h0:h0 + 2, :, :].rearrange("h d t -> d h t"), r=["KT"], w=["R2_0"])
            P.dma("sp", VAs2, VA[:, h0:h0 + 2, :].rearrange("(t p) h c -> p t (h c)", p=128), r=["VA"], w=["R2_1"])
            for qb_ in range(16):
                qs = slice(qb_ * 256, (qb_ + 1) * 256)
                P.dma("sp", qTt[:], QT[h0:h0 + 2, :, qs].rearrange("h d t -> d h t"), r=["QT"], w=["qTt"])
                for hh in range(2):
                    h = h0 + hh
                    for qt in range(2):
                        mm(pO[1][:, 256 + qt * 16:256 + (qt + 1) * 16], qTt[:, hh, qt * 128:(qt + 1) * 128], kmean[:, h, :],
                           r=["qTt", "kmean"], w=["pO1"])
                    P.dve(lambda e: e.memset(scs[:], NEG), w=["scs"])
                    if qb_ > 0:
                        P.dve(lambda e, qb_=qb_: e.tensor_copy(
                            out=scs[:, :, 0:qb_], in_=pO[1][:, 256:288].rearrange("p (q b) -> p q b", q=2)[:, :, 0:qb_]),
                            r=["pO1"], w=["scs"])
                    for qt in range(2):
                        P.dve(lambda e, qt=qt: e.max(out=top8[:], in_=scs[:, qt, :]), r=["scs"], w=["top8"])
                        P.dve(lambda e, qt=qt, hh=hh: e.tensor_scalar(out=sel[:, hh, qt, :], in0=scs[:, qt, :],
                                                                      scalar1=top8[:, 2:3], scalar2=None, op0=ALU.is_ge),
                              r=["scs", "top8"], w=["sel"])
                    nkt = 2 * qb_ + 2
                    for qt in range(2):
                        qtile = 2 * qb_ + qt
                        qsl = slice(qt * 128, (qt + 1) * 128)
                        po = pO[0]
                        first = True
                        for kt in range(2 * qb_, qtile + 1):
                            dl = qtile - kt
                            psx = pS[pcount[0] % 2]
                            ptag = f"pS{pcount[0] % 2}"
                            pb = pT2[pcount[0] % 4]
                            pbt = f"pT2_{pcount[0] % 4}"
                            pcount[0] += 1
                            mm(psx[:, 0:128], KTs[:, hh, kt * 128:(kt + 1) * 128], qTt[:, hh, qsl], r=["R2_0", "qTt"], w=[ptag])
                            P.dve(lambda e, psx=psx, h=h, dl=dl: e.scalar_tensor_tensor(
                                out=sco[:, 0:128], in0=psx[:, 0:128], scalar=128 ** -0.5,
                                in1=(BS[:, h, 0:128] if dl == 0 else BM1[:, h, :]), op0=ALU.mult, op1=ALU.add), r=[ptag, "BM", "BS"], w=["sco"])
                            P.act(lambda e, pb=pb: e.activation(out=pb[:, 0:128], in_=sco[:, 0:128], func=AF.Exp),
                                  r=["sco"], w=[pbt])
                            mm(po[:, qt * 129:(qt + 1) * 129], pb[:, 0:128], VAs[:, kt, hh, :], start=first,
                               stop=(kt == qtile), r=[pbt, "R2_1"], w=["pO0"])
                            first = False
                    P.dve(lambda e, hh=hh: e.tensor_copy(out=acc[:, hh, :, :],
                                                         in_=pO[0][:, 0:258].rearrange("p (q c) -> p q c", q=2)),
                          r=["pO0"], w=["acc"])
                    for jb in range(qb_):
                        pbs = []
                        for kk in range(2):
                            kt = 2 * jb + kk
                            psx = pS[pcount[0] % 2]
                            ptag = f"pS{pcount[0] % 2}"
                            pb = pT2[pcount[0] % 4]
                            pbt = f"pT2_{pcount[0] % 4}"
                            pcount[0] += 1
                            mm(psx[:, 0:256], KTs[:, hh, kt * 128:(kt + 1) * 128], qTt[:, hh, :], r=["R2_0", "qTt"], w=[ptag])
                            if kt == 2 * qb_ - 1:
                                P.dve(lambda e, psx=psx, h=h: e.scalar_tensor_tensor(
                                    out=sco[:, 0:128], in0=psx[:, 0:128], scalar=128 ** -0.5, in1=BM1[:, h, :],
                                    op0=ALU.mult, op1=ALU.add), r=[ptag, "BM"], w=["sco"])
                                P.act(lambda e, pb=pb: e.activation(out=pb[:, 0:128], in_=sco[:, 0:128], func=AF.Exp),
                                      r=["sco"], w=[pbt])
                                P.act(lambda e, pb=pb, psx=psx, h=h: e.activation(out=pb[:, 128:256], in_=psx[:, 128:256],
                                                                                   func=AF.Exp, bias=relfar[:, h:h + 1],
                                                                                   scale=128 ** -0.5),
                                      r=[ptag, "relfar"], w=[pbt])
                            else:
                                P.act(lambda e, pb=pb, psx=psx, h=h: e.activation(out=pb[:, 0:256], in_=psx[:, 0:256],
                                                                                   func=AF.Exp, bias=relfar[:, h:h + 1],
                                                                                   scale=128 ** -0.5),
                                      r=[ptag, "relfar"], w=[pbt])
                            pbs.append((pb, pbt, kt))
                        po = pO[1]
                        for qt in range(2):
                            for kk, (pb, pbt, kt) in enumerate(pbs):
                                mm(po[:, qt * 129:(qt + 1) * 129], pb[:, qt * 128:(qt + 1) * 128], VAs[:, kt, hh, :],
                                   start=(kk == 0), stop=(kk == 1), r=[pbt, "R2_1"], w=["pO1"])
                        for qt in range(2):
                            P.dve(lambda e, qt=qt, hh=hh, jb=jb: e.scalar_tensor_tensor(
                                out=acc[:, hh, qt, :], in0=pO[1][:, qt * 129:(qt + 1) * 129], scalar=sel[:, hh, qt, jb:jb + 1],
                                in1=acc[:, hh, qt, :], op0=ALU.mult, op1=ALU.add), r=["pO1", "sel", "acc"], w=["acc"])
                    for qt in range(2):
                        P.dve(lambda e, qt=qt, hh=hh: e.reciprocal(out=sm[:, 52:53], in_=acc[:, hh, qt, 128:129]),
                              r=["acc"], w=["sm52"])
                        P.dve(lambda e, qt=qt, hh=hh: e.tensor_scalar(out=attq[:, qt, hh * 128:(hh + 1) * 128],
                                                                      in0=acc[:, hh, qt, 0:128], scalar1=sm[:, 52:53],
                                                                      scalar2=None, op0=ALU.mult),
                              r=["acc", "sm52"], w=["attq"])
                for qt in range(2):
                    t = 2 * qb_ + qt
                    P.dma("sp", ATT[t * 128:(t + 1) * 128, h0 * 128:(h0 + 2) * 128], attq[:, qt, :], r=["attq"], wacc=["ATT"])
        for t in range(NT):
            xb = t % 2
            rs = slice(t * 128, (t + 1) * 128)
            if l == 0:
                P.dma("sp", xt[xb][:], x_in[rs, :], w=["xt0"])
            else:
                P.dma("sp", xt[xb][:], XR[rs, :], r=[f"XR{t}"], w=["xt0"])
            P.dma("sp", att[:], ATT[rs, :], r=["ATT"], w=["att"])
            tail(l, t, xb, att, "att")

    def expert_phase(l):
        w1r = moe_w1.rearrange("l e (p j) n -> (l e p) (j n)", j=8)
        w3r = moe_w3.rearrange("l e (p j) n -> (l e p) (j n)", j=8)
        w2r = moe_w2.rearrange("l e (p j) n -> (l e p) (j n)", j=4)
        s0f = stg(0)[:, :]
        s1f = stg(1)[:, :]
        s2f = WOUT[:, :, :].rearrange("p a b -> p (a b)").bitcast(F32)
        s0 = stg(0)[:, :].rearrange("p (k n) -> p k n", k=8)
        s1 = stg(1)[:, :].rearrange("p (k n) -> p k n", k=8)
        s2 = WOUT[:, :, :].rearrange("p a b -> p (a b)").bitcast(F32).rearrange("p (k n) -> p k n", k=4)
        nblk = DBG.get("nblk", NBLK)

        def stage_a(b):
            eb_ = b % 2
            tb = tabt[eb_]
            ix = idxi[eb_]
            P.dma("sp", tb[:], TAB[b * 128:(b + 1) * 128, :], r=["TAB"], w=[f"tabt{eb_}"])
            P.dve(lambda e, tb=tb, ix=ix: e.tensor_copy(out=ix[:], in_=tb[:, 0:2]), r=[f"tabt{eb_}"], w=[f"idxi{eb_}"])
            P.op("pool", lambda e, ix=ix, eb_=eb_: e.indirect_dma_start(
                out=xg[eb_][:, :], out_offset=None, in_=H2,
                in_offset=bass.IndirectOffsetOnAxis(ap=ix[:, 0:1], axis=0)),
                ["H2", f"idxi{eb_}"], [f"xg{eb_}"], dma=True)
            if not DBG.get("nowdma"):
                for sv, wr_, tg in ((s0f, w1r, "R2_0"), (s1f, w3r, "R2_1"), (s2f, w2r, "WOUTt")):
                    P.op("pool", lambda e, b=b, sv=sv, wr_=wr_: e.indirect_dma_start(
                        out=sv, out_offset=None, in_=wr_,
                        in_offset=bass.IndirectOffsetOnAxis(ap=widx1i[:, 0, b:b + 1], axis=0)),
                        ["widx1i"], [tg], dma=True)

        def stage_cast(b):
            eb_ = b % 2
            w1v, w3v, w2v = ew(eb_, 0), ew(eb_, 1), ew(eb_, 2)
            wt = [f"R1_{eb_}_{i}" for i in range(3)]
            P.act(lambda e, w1v=w1v: e.copy(out=w1v, in_=s0), r=["R2_0"], w=[wt[0]])
            P.dve(lambda e, w3v=w3v: e.tensor_copy(out=w3v, in_=s1), r=["R2_1"], w=[wt[1]])
            P.act(lambda e, w2v=w2v: e.copy(out=w2v, in_=s2), r=["WOUTt"], w=[wt[2]])

        def stage_b(b):
            eb_ = b % 2
            w1v, w3v, w2v = ew(eb_, 0), ew(eb_, 1), ew(eb_, 2)
            wt = [f"R1_{eb_}_{i}" for i in range(3)]
            tb = tabt[eb_]
            ix = idxi[eb_]
            i = tcount[0] % 2
            tcount[0] += 1
            for k in range(8):
                tp(pT[i][:, k, :], xg[eb_][:, :].rearrange("t (p j) -> t j p", j=8)[:, k, :], identb[:],
                   r=[f"xg{eb_}", "identb"], w=[f"pT{i}"])
            P.dve(lambda e, i=i, eb_=eb_: e.tensor_copy(out=xgT[eb_][:, :, :], in_=pT[i][:]), r=[f"pT{i}"], w=[f"xgT{eb_}"])
            for hc in range(0 if DBG.get("nomm") else 4):
                for k in range(8):
                    mm(pM[0][:, 0:128], w1v[:, k, :].rearrange("p (m f) -> p f m", f=4)[:, hc, :], xgT[eb_][:, k, :], start=(k == 0), stop=(k == 7),
                       r=[f"xgT{eb_}", wt[0]], w=["pM0"])
                for k in range(8):
                    mm(pM[1][:, 0:128], w3v[:, k, :].rearrange("p (m f) -> p f m", f=4)[:, hc, :], xgT[eb_][:, k, :], start=(k == 0), stop=(k == 7),
                       r=[f"xgT{eb_}", wt[1]], w=["pM1"])
                P.act(lambda e: e.activation(out=s1t[:, :], in_=pM[0][:, 0:128], func=AF.Silu), r=["pM0"], w=["s1t"])
                P.dve(lambda e, hc=hc, eb_=eb_: e.tensor_tensor(out=actT[eb_][:, hc, :], in0=pM[1][:, 0:128], in1=s1t[:, :],
                                                                op=ALU.mult), r=["pM1", "s1t"], w=[f"actT{eb_}"])
            ytag = ("m1", "junk")[eb_]
            for half in range(2):
                for hc in range(4):
                    mm(pS[half][:, :], actT[eb_][:, hc, :], w2v[:, hc, half * 512:(half + 1) * 512],
                       start=(hc == 0), stop=(hc == 3), r=[f"actT{eb_}", wt[2]], w=[f"pS{half}"])
                if half == 0:
                    P.act(lambda e, eb_=eb_, tb=tb: e.activation(out=yo[eb_][:, 0:512], in_=pS[0][:, :], func=AF.Copy,
                                                                 scale=tb[:, 2:3]),
                          r=["pS0", f"tabt{eb_}"], w=[ytag])
                else:
                    P.dve(lambda e, eb_=eb_, tb=tb: e.tensor_scalar(out=yo[eb_][:, 512:1024], in0=pS[1][:, :],
                                                                     scalar1=tb[:, 2:3], scalar2=None, op0=ALU.mult),
                          r=["pS1", f"tabt{eb_}"], w=[ytag])
            P.op("pool", lambda e, eb_=eb_, ix=ix: e.indirect_dma_start(
                out=MO, out_offset=bass.IndirectOffsetOnAxis(ap=ix[:, 1:2], axis=0), in_=yo[eb_][:], in_offset=None),
                [ytag, f"idxi{eb_}"], None, dma=True, wacc=["MO"])

        stage_a(0)
        stage_cast(0)
        for b in range(nblk):
            if b + 1 < nblk:
                stage_a(b + 1)
            stage_b(b)
            if b + 1 < nblk:
                stage_cast(b + 1)

    for l in range(n_layers):
        layer_start(l)
        if DBG.get("stage") == "A":
            for r_ in range(6):
                P.dma("sp", out[r_ * 128:(r_ + 1) * 128, :], rows[:, r_, :], r=[f"rows{r_}"], w=[f"out{r_}"])
            break
        if l % 2 == 0:
            even_layer(l)
        else:
            odd_layer(l)
        if not DBG.get("noexp"):
            route_finalize(l)
            expert_phase(l)
    if DBG.get("nofinal"):
        P.emit()
        es.close()
        return nc
    P.pool(lambda e: e.tensor_copy(out=rows[:, 6, :], in_=rows[:, 5, :]), r=["rows5"], w=["rows6"])
    for t in range(NT):
        xb = t % 2
        load_x(1, t, xb)
        P.dma("sp", out[t * 128:(t + 1) * 128, :], xt[xb][:], r=["xt0"], w=[f"out{t}"])
    P.emit()
    es.close()
    return nc


def _rel_bucket_np(d):
    d = np.maximum(d, 0)
    logd = np.log(np.maximum(d, 1).astype(np.float32) / 16) / math.log(128 / 16)
    far = np.minimum(16 + (logd * 16).astype(np.int32), 31)
    return np.where(d < 16, d, far)


def _constants():
    k = np.arange(128)[:, None]
    q = np.arange(128)[None, :]
    c = {}
    c["ident"] = np.eye(128, dtype=np.float32)
    same = (k // 64) == (q // 64)
    c["tri"] = (same & (k <= q)).astype(np.float32)
    c["bones"] = same.astype(np.float32)
    c["cind"] = (np.arange(128)[:, None] // 64 == np.arange(2)[None, :]).astype(np.float32)
    c["stri"] = (k < q).astype(np.float32)
    mask0 = np.zeros((128, 256), np.float32)
    mask0[:, 0:128] = np.where(q >= k, 0.0, NEG)
    mask0[:, 128:256] = np.where(q < k, 0.0, NEG)
    c["mask0"] = mask0
    r = np.arange(TABROWS)
    tab = np.zeros((TABROWS, 4), np.float32)
    tab[:, 1] = S + (r % 128)
    c["tabinit"] = tab
    rc_ = np.zeros((128, 172), np.float32)
    rc_[:, 0:64] = (np.arange(64) * 128)[None, :]
    rc_[:, 64:160] = (np.arange(96) * 128)[None, :]
    rc_[:, 160:168] = np.arange(8)[None, :] * 128 + np.arange(128)[:, None]
    rc_[:, 168:172] = np.arange(4)[None, :] * 128 + np.arange(128)[:, None]
    c["rcst"] = rc_
    pid = np.zeros((128, 2), np.float32)
    pid[:, 0] = np.arange(128)
    pid[:, 1] = 32 * CAP + np.arange(128)
    c["pidx"] = pid
    d0 = q - k
    d1 = 128 + q - k
    c["_b0"] = _rel_bucket_np(d0)
    c["_b1"] = _rel_bucket_np(d1)
    return c


_CACHE = {}


def kernel(x, c, rel_bias, ada_w, ada_b, norm1_w, norm2_w, even_w_in, even_w_out, a_q_norm, a_k_norm, a_sinks,
           b_gate_up, b_gate_bias, b_out_norm, odd_w_in, odd_w_out, c_q_norm, c_k_norm, moe_w_group, moe_b_group,
           moe_w_expert, moe_b_expert, moe_w1, moe_w3, moe_w2, _n_layers=4, _cores=None, _dbg=None):
    f = lambda a: np.ascontiguousarray(np.asarray(a, dtype=np.float32))
    cst = _constants()
    rel_bias = f(rel_bias)
    relT = np.empty((8, 128, 256), np.float32)
    relT[:, :, 0:128] = np.transpose(rel_bias[cst["_b0"]], (2, 0, 1))
    relT[:, :, 128:256] = np.transpose(rel_bias[cst["_b1"]], (2, 0, 1))
    shared = {
        "rel_bias": rel_bias, "relT": relT, "ada_w": f(ada_w[:_n_layers]), "ada_b": f(ada_b), "norm1_w": f(norm1_w),
        "norm2_w": f(norm2_w), "even_w_in": f(even_w_in), "even_w_out": f(even_w_out), "a_q_norm": f(a_q_norm),
        "a_k_norm": f(a_k_norm), "a_sinks": f(a_sinks), "b_gate_up": f(b_gate_up), "b_gate_bias": f(b_gate_bias),
        "b_out_norm": f(b_out_norm), "odd_w_in": f(odd_w_in), "odd_w_out": f(odd_w_out), "c_q_norm": f(c_q_norm),
        "c_k_norm": f(c_k_norm),
        "moe_wr": np.ascontiguousarray(np.concatenate([f(moe_w_group), f(moe_w_expert)], axis=-1)),
        "moe_br": np.ascontiguousarray(np.concatenate([f(moe_b_group), f(moe_b_expert)], axis=-1)),
        "moe_w1": f(moe_w1[:_n_layers]), "moe_w3": f(moe_w3[:_n_layers]), "moe_w2": f(moe_w2[:_n_layers]),
    }
    for k_ in ("ident", "tri", "bones", "cind", "stri", "mask0", "tabinit", "rcst", "pidx"):
        shared[k_] = cst[k_]
    x = f(x)
    c = f(c)
    cores = list(range(8)) if _cores is None else _cores
    in_maps = []
    for b in cores:
        m = dict(shared)
        m["x"] = x[b]
        m["cT"] = np.ascontiguousarray(c[b].reshape(8, 128).T)
        in_maps.append(m)
    key = (_n_layers, str(_dbg))
    if key not in _CACHE:
        _CACHE[key] = build_program(_n_layers, _dbg)
    nc = _CACHE[key]
    res = run_bass_kernel_spmd(nc, in_maps, core_ids=list(range(len(cores))))
    outs = [r["out"] for r in res.results]
    return np.stack(outs, axis=0).astype(np.float32)
```

```python
import math
from contextlib import ExitStack

import numpy as np
import concourse.bass as bass
import concourse.mybir as mybir
from concourse.bass_utils import run_bass_kernel_spmd

F32 = mybir.dt.float32
BF16 = mybir.dt.bfloat16
I32 = mybir.dt.int32
AF = mybir.ActivationFunctionType
ALU = mybir.AluOpType
AX = mybir.AxisListType

S = 4096
D = 1024
NT = 32
CAP = 384
NB = CAP // 128
NEG = -30000.0
BIG = 10000.0
MOH = S + 128
NBLK = 96
TABROWS = NBLK * 128
EPS = 1e-6

COMPUTE = ("pe", "act", "dve", "pool")
DMAQ_K = 12
SEM_EPOCH = 6000
DMA_EPOCH = 400


class Buf:
    __slots__ = ("name", "w", "r")

    def __init__(self, name):
        self.name = name
        self.w = []
        self.r = []


class Op:
    __slots__ = ("eng", "fn", "seq", "dma", "deps", "signal", "sig_idx", "dma_idx")

    def __init__(self, eng, fn, dma):
        self.eng = eng
        self.fn = fn
        self.dma = dma
        self.deps = []
        self.signal = False
        self.sig_idx = None
        self.dma_idx = None


def _compress(lst):
    keep = []
    cnt = {}
    for x in reversed(lst):
        key = (x.eng, x.dma)
        c = cnt.get(key, 0)
        lim = DMAQ_K if x.dma else 1
        if c < lim:
            keep.append(x)
            cnt[key] = c + 1
    keep.reverse()
    return keep


class _Rec:
    def __init__(self):
        self.calls = []

    def __getattr__(self, name):
        def f(*a, **k):
            self.calls.append((name, a, k))
            return self
        return f


class _LazyReg:
    def __init__(self, v):
        self.v = v


_REGCACHE = {}


def _freeze(fn):
    rec = _Rec()
    fn(rec)
    assert len(rec.calls) == 1, rec.calls
    name, a, k = rec.calls[0]
    lazy = [kk for kk, vv in k.items() if isinstance(vv, _LazyReg)]
    if not lazy:
        return lambda e: getattr(e, name)(*a, **k)

    def replay(e):
        k2 = dict(k)
        for kk in lazy:
            key = (id(e), k[kk].v)
            if key not in _REGCACHE:
                _REGCACHE[key] = e.to_reg(k[kk].v)
            k2[kk] = _REGCACHE[key]
        return getattr(e, name)(*a, **k2)
    return replay


class Prog:
    def __init__(self, nc):
        self.nc = nc
        self.ops = {e: [] for e in ("pe", "act", "dve", "pool", "sp")}
        self.ndma = {e: 0 for e in self.ops}
        self.known = {e: {f: -1 for f in self.ops} for e in self.ops}
        self.kd_upto = {e: {f: -1 for f in self.ops} for e in self.ops}
        self.kd_set = {e: {f: set() for f in self.ops} for e in self.ops}
        self.bufs = {}

    def buf(self, name):
        b = self.bufs.get(name)
        if b is None:
            b = Buf(name)
            self.bufs[name] = b
        return b

    def _norm(self, lst):
        out = []
        for x in lst or []:
            if isinstance(x, (list, tuple)):
                out.extend(self._norm(x))
            elif isinstance(x, str):
                out.append(self.buf(x))
            else:
                out.append(x)
        return out

    def _satisfied(self, E, d):
        if d.dma:
            return d.dma_idx <= self.kd_upto[E][d.eng] or d.dma_idx in self.kd_set[E][d.eng]
        return d.seq <= self.known[E][d.eng]

    def _learn(self, E, d):
        if d.dma:
            F = d.eng
            s = self.kd_set[E][F]
            s.add(d.dma_idx)
            u = max(self.kd_upto[E][F], d.dma_idx - DMAQ_K)
            while (u + 1) in s:
                u += 1
            self.kd_upto[E][F] = u
            if len(s) > 64:
                self.kd_set[E][F] = {x for x in s if x > u}
        elif d.seq > self.known[E][d.eng]:
            self.known[E][d.eng] = d.seq

    def op(self, eng, fn, reads=None, writes=None, dma=False, wacc=None):
        reads = self._norm(reads)
        writes = self._norm(writes)
        wacc = self._norm(wacc)
        psr = [b for b in reads if b.name[:2] in ("pT", "pM", "pS", "pO")]
        if psr:
            reads = [b for b in reads if b not in psr]
            writes = writes + [b for b in psr if b not in writes]
        if fn is not None:
            fn = _freeze(fn)
        o = Op(eng, fn, dma)
        o.seq = len(self.ops[eng])
        if dma:
            o.dma_idx = self.ndma[eng]
            self.ndma[eng] += 1
        raw = set()
        cand = []
        for b in reads:
            for d in b.w:
                raw.add(id(d))
                cand.append(d)
        for b in writes:
            cand.extend(b.w)
            cand.extend(b.r)
        for b in wacc:
            cand.extend(b.r)
        seen = set()
        uniq = []
        for d in cand:
            if id(d) not in seen:
                seen.add(id(d))
                uniq.append(d)
        uniq.sort(key=lambda d: -(d.dma_idx if d.dma else d.seq))
        for d in uniq:
            if (not d.dma) and (not dma) and d.eng == eng and id(d) not in raw:
                continue
            if self._satisfied(eng, d):
                continue
            o.deps.append(d)
            d.signal = True
            self._learn(eng, d)
        for b in writes:
            b.w = [o]
            b.r = []
        for b in wacc:
            b.w.append(o)
            if len(b.w) > 40:
                b.w = _compress(b.w)
        for b in reads:
            if fn is None:
                break
            if not dma:
                b.r = [x for x in b.r if x.dma or x.eng != eng]
            b.r.append(o)
            if len(b.r) > 40:
                b.r = _compress(b.r)
        self.ops[eng].append(o)
        return o

    def pe(self, fn, r=None, w=None):
        return self.op("pe", fn, r, w)

    def act(self, fn, r=None, w=None):
        return self.op("act", fn, r, w)

    def dve(self, fn, r=None, w=None):
        return self.op("dve", fn, r, w)

    def pool(self, fn, r=None, w=None):
        return self.op("pool", fn, r, w)

    def barrier(self, fn):
        allb = list(self.bufs.values())
        self.op("dve", fn, allb, allb + [self.buf("FENCE")])
        for e in ("pe", "act", "pool", "sp"):
            self.op(e, None, [self.buf("FENCE")], None)

    def dma(self, eng, out, in_, r=None, w=None, wacc=None):
        return self.op(eng, lambda e: e.dma_start(out=out, in_=in_), r, w, dma=True, wacc=wacc)

    def emit(self):
        nc = self.nc
        nsig = {}
        for e in COMPUTE:
            c = 0
            for o in self.ops[e]:
                if o.signal and not o.dma:
                    o.sig_idx = c
                    c += 1
            nsig[e] = c
        es = ExitStack()
        sems = {}
        for e in COMPUTE:
            n_ep = max(1, (nsig[e] + SEM_EPOCH - 1) // SEM_EPOCH)
            sems[e] = [es.enter_context(nc.semaphore(f"tl_{e}_{i}")) for i in range(n_ep)]
        dsems = {}
        per_ep = DMA_EPOCH * DMAQ_K
        for e in self.ops:
            if self.ndma[e] == 0:
                continue
            n_ep = (self.ndma[e] + per_ep - 1) // per_ep
            dsems[e] = [[es.enter_context(nc.semaphore(f"dq_{e}_{i}_{k}")) for k in range(DMAQ_K)]
                        for i in range(n_ep)]

        def dma_sem(e, idx):
            return dsems[e][idx // per_ep][idx % DMAQ_K], 16 * ((idx % per_ep) // DMAQ_K + 1)

        def wait_for(engobj, d):
            if d.dma:
                s, v = dma_sem(d.eng, d.dma_idx)
            else:
                s = sems[d.eng][d.sig_idx // SEM_EPOCH]
                v = d.sig_idx % SEM_EPOCH + 1
            engobj.wait_ge(s, v)

        prog = self

        def run_engine(ename, engobj):
            for o in prog.ops[ename]:
                for d in o.deps:
                    wait_for(engobj, d)
                if o.dma:
                    if o.dma_idx >= DMAQ_K:
                        s, v = dma_sem(ename, o.dma_idx - DMAQ_K)
                        engobj.wait_ge(s, v)
                    inst = o.fn(engobj)
                    s, v = dma_sem(ename, o.dma_idx)
                    inst.then_inc(s, 16)
                elif o.fn is not None:
                    inst = o.fn(engobj)
                    if o.signal:
                        inst.then_inc(sems[ename][o.sig_idx // SEM_EPOCH], 1)
            n = prog.ndma[ename]
            for idx in range(max(0, n - DMAQ_K), n):
                s, v = dma_sem(ename, idx)
                engobj.wait_ge(s, v)

        with nc.Block() as block:
            @block.tensor
            def _(e):
                run_engine("pe", e)

            @block.scalar
            def _(e):
                run_engine("act", e)

            @block.vector
            def _(e):
                run_engine("dve", e)

            @block.gpsimd
            def _(e):
                run_engine("pool", e)

            @block.sync
            def _(e):
                run_engine("sp", e)
        es.close()


def build_program(n_layers=4, dbg=None):
    DBG = dbg or {}
    _REGCACHE.clear()
    nc = bass.Bass("TRN2", target_bir_lowering=False)
    P = Prog(nc)
    es = ExitStack()

    def din(name, shape, dt=F32):
        return nc.dram_tensor(name, list(shape), dt, kind="ExternalInput").ap()

    def dscr(name, shape, dt=F32):
        return nc.dram_tensor(name, list(shape), dt, kind="Internal").ap()

    def sb(name, shape, dt=F32):
        return es.enter_context(nc.sbuf_tensor("s_" + name, list(shape), dt))

    def ps(name, shape, dt=F32):
        return es.enter_context(nc.psum_tensor("p_" + name, list(shape), dt))

    x_in = din("x", [S, D])
    cT_in = din("cT", [128, 8])
    rel_bias = din("rel_bias", [32, 8])
    relT_in = din("relT", [8, 128, 256])
    mask0_in = din("mask0", [128, 256])
    ident_in = din("ident", [128, 128])
    tri_in = din("tri", [128, 128])
    bones_in = din("bones", [128, 128])
    cind_in = din("cind", [128, 2])
    stri_in = din("stri", [128, 128])
    tabinit_in = din("tabinit", [TABROWS, 4])
    rcst_in = din("rcst", [128, 172])
    pidx_in = din("pidx", [128, 2])
    NLW = n_layers
    ada_w = din("ada_w", [NLW, D, 6 * D])
    ada_b = din("ada_b", [4, 6 * D])
    norm1_w = din("norm1_w", [4, D])
    norm2_w = din("norm2_w", [4, D])
    even_w_in = din("even_w_in", [2, D, 2320])
    even_w_out = din("even_w_out", [2, D, D])
    a_q_norm = din("a_q_norm", [2, 64])
    a_k_norm = din("a_k_norm", [2, 64])
    a_sinks = din("a_sinks", [2, 8])
    b_gate_up = din("b_gate_up", [2, 16, 256])
    b_gate_bias = din("b_gate_bias", [2, 256])
    b_out_norm = din("b_out_norm", [2, 128])
    odd_w_in = din("odd_w_in", [2, D, 3072])
    odd_w_out = din("odd_w_out", [2, D, D])
    c_q_norm = din("c_q_norm", [2, 128])
    c_k_norm = din("c_k_norm", [2, 128])
    moe_wr = din("moe_wr", [4, D, 36])
    moe_br = din("moe_br", [4, 36])
    moe_w1 = din("moe_w1", [NLW, 32, D, 512])
    moe_w3 = din("moe_w3", [NLW, 32, D, 512])
    moe_w2 = din("moe_w2", [NLW, 32, 512, D])
    out = nc.dram_tensor("out", [S, D], F32, kind="ExternalOutput").ap()

    XR = dscr("XR", [S, D])
    H2 = dscr("H2", [S, D], BF16)
    TAB = dscr("TAB", [TABROWS, 4])
    MO = dscr("MO", [2 * MOH, D])
    ATT = dscr("ATT", [S, D], BF16)
    QT = dscr("QT", [8, 128, S], BF16)
    KT = dscr("KT", [8, 128, S], BF16)
    VA = dscr("VA", [S, 8, 129], BF16)

    R1 = sb("R1", [128, 24576], BF16)
    R2 = sb("R2", [128, 8448], F32)
    WOUT = sb("WOUT", [128, 8, 1024], BF16)
    rows = sb("rows", [128, 7, 1024])
    BS = sb("BS", [128, 8, 256])
    BM1 = sb("BM1", [128, 8, 128])
    relfar = sb("relfar", [128, 8])
    identf = sb("identf", [128, 128])
    identb = sb("identb", [128, 128], BF16)
    trif = sb("trif", [128, 128])
    bonesf = sb("bonesf", [128, 128])
    cindf = sb("cindf", [128, 2])
    strib = sb("strib", [128, 128], BF16)
    onesb = sb("onesb", [128, 128], BF16)
    bonesb = sb("bonesb", [128, 128], BF16)
    rcst = sb("rcst", [128, 172])
    pidx = sb("pidx", [128, 2])
    cTs = sb("cTs", [128, 8])
    xt0_ = sb("xt0", [128, D])
    xt = [xt0_, xt0_]
    m1 = sb("m1", [128, D])
    junk = sb("junk", [128, D])
    m2 = junk
    condbc = junk[:, :].rearrange("p (k n) -> p k n", k=8)
    tmpf = sb("tmpf", [128, D])
    hb = sb("hb", [128, D], BF16)
    hT = sb("hT", [128, 8, 128], BF16)
    proj = sb("proj", [128, 2320])
    att = sb("att", [128, D], BF16)
    h2b = hb
    sm = sb("sm", [128, 64])
    wrow = sb("wrow", [128, 640])
    WR = sb("WR", [128, 8, 36], BF16)
    brow = sb("brow", [128, 36])
    adab = m1[:, 0:512]
    EV = sb("EV", [128, 7888])

    class Carver:
        def __init__(self):
            self.off = 0

        def get(self, shape, dt=F32):
            np_ = shape[0]
            n = 1
            for d_ in shape[1:]:
                n *= d_
            sz = 2 if dt == BF16 else 4
            nb = (n * sz + 3) // 4 * 4
            c0 = self.off // 4
            self.off += nb
            assert self.off <= 7888 * 4, self.off
            v = EV[0:np_, c0:c0 + nb // 4]
            if dt != F32:
                v = v.bitcast(dt)[:, 0:n]
            if len(shape) == 3:
                v = v.rearrange("p (a b) -> p a b", a=shape[1])
            elif len(shape) == 4:
                v = v.rearrange("p (a b c) -> p a b c", a=shape[1], b=shape[2])
            return v

    cv = Carver()
    qkb = cv.get([128, 512], BF16)
    kdup = cv.get([128, 2, 128], BF16)
    qkT = [cv.get([128, 6, 128], BF16) for i in range(2)]
    vaS = [cv.get([128, 2, 65], BF16) for i in range(2)]
    sc = cv.get([128, 256])
    pexp = [cv.get([128, 256], BF16) for i in range(2)]
    abT = cv.get([32, 128], BF16)
    abp = cv.get([128, 32], BF16)
    gnb = cv.get([128, 256], BF16)
    zt = cv.get([128, 256])
    gneg = cv.get([128, 256])
    bsb = zt
    eb = cv.get([128, 256])
    enb = cv.get([128, 256])
    ekd = cv.get([128, 256])
    qd = cv.get([128, 256], BF16)
    kd = cv.get([128, 256], BF16)
    ku = cv.get([128, 256], BF16)
    vbb = cv.get([128, 512], BF16)
    sgn = cv.get([128, 512])
    dec = cv.get([64, 8])
    qdT = cv.get([64, 4, 128], BF16)
    qdm = cv.get([64, 4, 2, 128], BF16)
    kdT = cv.get([64, 4, 128], BF16)
    attT = cv.get([128, 128], BF16)
    St0 = cv.get([64, 4, 128])
    St1 = cv.get([64, 4, 128])
    St0b = cv.get([64, 4, 128], BF16)
    St1b = cv.get([64, 4, 128], BF16)
    gbias = cv.get([128, 256])
    onorm1 = cv.get([128, 128])
    gupf = cv.get([32, 256])
    gupb = cv.get([32, 256], BF16)
    esink = cv.get([128, 8])
    ev_even = cv.off
    cv = Carver()
    qnb = cv.get([128, 2048], BF16)
    vast = cv.get([128, 8, 129], BF16)
    kmsum = cv.get([128, 8, 32])
    kmean = cv.get([128, 8, 16], BF16)
    qTt = cv.get([128, 2, 256], BF16)
    scs = cv.get([128, 2, 16])
    sel = cv.get([128, 2, 2, 16])
    pT2 = [cv.get([128, 256], BF16) for i in range(4)]
    acc = cv.get([128, 2, 2, 129])
    attq = cv.get([128, 2, 256], BF16)
    sco_ = [cv.get([128, 128]) for i in range(2)]
    cv = Carver()
    xg = [cv.get([128, D], BF16) for i in range(3)]
    xgT = [cv.get([128, 8, 128], BF16) for i in range(2)]
    s1t4 = cv.get([128, 4, 128])
    actT = [cv.get([128, 4, 128], BF16) for i in range(2)]
    rtab = cv.get([128, 6, 32])
    blkE = cv.get([128, 96])
    widx1f = cv.get([128, 8, 96])
    widx1i = cv.get([128, 8, 96], I32)
    widx2f = cv.get([128, 4, 96])
    widx2i = cv.get([128, 4, 96], I32)
    M12all = R2[:, 0:2048].rearrange("p (t k e) -> p t k e", t=32, k=2)
    slall = R2[:, 2048:2112].rearrange("p (t k) -> p t k", t=32)
    recall = R2[:, 2112:2368].rearrange("p (t k c) -> p t k c", t=32, k=2)
    top8 = sb("top8", [128, 8])
    lg = sb("lg", [128, 36])
    msk = sb("msk", [128, 32])
    M12 = sb("M12", [128, 2, 32])
    Mb = sb("Mb", [128, 32], BF16)
    cbase = sb("cbase", [128, 32])
    Cc = sb("Cc", [128, 32])
    prod = sb("prod", [128, 2, 32])
    desti = [sb(f"desti{i}", [128, 2], I32) for i in range(2)]
    tabt = [cv.get([128, 4]) for i in range(3)]
    idxi = [cv.get([128, 2], I32) for i in range(3)]
    yo = [m1, junk]
    print("SBUF bytes remaining/partition:", nc.sbuf_bytes_remaining)

    pT = [ps(f"pT{i}", [128, 8, 128], BF16) for i in range(2)]
    pM = [ps(f"pM{i}", [128, 512]) for i in range(2)]
    pS = [ps(f"pS{i}", [128, 512]) for i in range(2)]
    pO = [ps(f"pO{i}", [128, 512]) for i in range(2)]

    R1w = R1[:, :].rearrange("p (k n) -> p k n", k=8)

    def ew(eb_, which):
        base = eb_ * 12288
        if which == 0:
            return R1[:, base:base + 4096].rearrange("p (k n) -> p k n", k=8)
        if which == 1:
            return R1[:, base + 4096:base + 8192].rearrange("p (k n) -> p k n", k=8)
        return R1[:, base + 8192:base + 12288].rearrange("p (k n) -> p k n", k=4)

    def stg(i):
        return R2[:, i * 4096:(i + 1) * 4096]

    WINT = [f"R1_{a}_{b}" for a in range(2) for b in range(3)]

    def bc(ap):
        return ap.partition_broadcast(128).squeeze(1)

    def fence(tags, eng="dve"):
        P.op(eng, lambda e: e.memset(sm[:, 62:63], 0.0), tags, tags + ["sm62"])

    R2b = R2[:, :].bitcast(BF16)
    KTs = R2b[:, 0:8192].rearrange("p (h t) -> p h t", h=2)
    VAs = R2b[:, 8192:8192 + 8256].rearrange("p (t h c) -> p t h c", t=32, h=2)
    VAs2 = R2b[:, 8192:8192 + 8256].rearrange("p (t c) -> p t c", t=32)

    def mm(out_, lhsT, rhs, start=True, stop=True, r=None, w=None):
        P.pe(lambda e: e.matmul(out_, lhsT=lhsT, rhs=rhs, start=start, stop=stop), r=r, w=w)

    def tp(out_, in_, ident, r=None, w=None):
        P.pe(lambda e: e.transpose(out=out_, in_=in_, identity=ident), r=r, w=w)

    P.dma("sp", identf[:], ident_in, w=["identf"])
    P.dma("sp", trif[:], tri_in, w=["trif"])
    P.dma("sp", bonesf[:], bones_in, w=["bonesf"])
    P.dma("sp", cindf[:], cind_in, w=["cindf"])
    P.dma("sp", rcst[:], rcst_in, w=["rcst"])
    P.dma("sp", pidx[:], pidx_in, w=["pidx"])
    P.dma("sp", cTs[:], cT_in, w=["cTs"])
    P.dma("sp", tmpf[:, 0:128], stri_in, w=["tmpf"])
    P.dve(lambda e: e.tensor_copy(out=strib[:], in_=tmpf[:, 0:128]), r=["tmpf"], w=["strib"])
    P.dve(lambda e: e.tensor_copy(out=identb[:], in_=identf[:]), r=["identf"], w=["identb"])
    P.dve(lambda e: e.memset(onesb[:], 1.0), w=["onesb"])
    P.dve(lambda e: e.tensor_copy(out=bonesb[:], in_=bonesf[:]), r=["bonesf"], w=["bonesb"])
    P.dma("sp", BS[:], relT_in.rearrange("h k q -> k h q"), w=["BS"])
    P.dma("sp", m1[:, 0:256], mask0_in, w=["m1"])
    P.dve(lambda e: e.tensor_copy(out=BM1[:], in_=BS[:, :, 128:256]), r=["BS"], w=["BM"])
    for h in range(8):
        P.dve(lambda e, h=h: e.tensor_tensor(out=BS[:, h, :], in0=BS[:, h, :], in1=m1[:, 0:256], op=ALU.add),
              r=["BS", "m1"], w=["BS"])
    P.dma("sp", relfar[:], bc(rel_bias[31:32, :]), w=["relfar"])
    P.act(lambda e: e.activation(out=cTs[:], in_=cTs[:], func=AF.Silu), r=["cTs"], w=["cTs"])

    def rstd_from_ssq(ssq_ap, n, inv_n, tag):
        P.dve(lambda e: e.tensor_scalar(out=ssq_ap, in0=ssq_ap, scalar1=inv_n, scalar2=EPS, op0=ALU.mult, op1=ALU.add),
              r=[tag], w=[tag])
        P.act(lambda e: e.activation(out=ssq_ap, in_=ssq_ap, func=AF.Sqrt), r=[tag], w=[tag])
        P.dve(lambda e: e.reciprocal(out=ssq_ap, in_=ssq_ap), r=[tag], w=[tag])

    def load_weight_bf16(dst_view, src_ap, nk, ncols, dst_tags, engs=("act", "dve", "pool")):
        srcv = src_ap.rearrange("(k p) n -> p k n", p=128)
        cpp = 4096 // nk
        c0 = 0
        i = 0
        while c0 < ncols:
            cw = min(cpp, ncols - c0)
            part = i % 2
            sv = stg(part)[:, 0:nk * cw].rearrange("p (k n) -> p k n", k=nk)
            P.dma("sp", sv, srcv[:, :, c0:c0 + cw], w=[f"R2_{part}"])
            en = engs[i % len(engs)]
            P.op(en, lambda e, sv=sv, c0=c0, cw=cw, en=en: (e.copy if en == "act" else e.tensor_copy)(
                out=dst_view[:, :, c0:c0 + cw], in_=sv),
                 [f"R2_{part}"], None, wacc=dst_tags)
            c0 += cw
            i += 1

    def layer_start(l):
        even = (l % 2 == 0)
        j = l // 2
        P.barrier(lambda e: e.memset(sm[:, 62:63], 0.0))
        if l > 0:
            P.pool(lambda e: e.tensor_copy(out=rows[:, 6, :], in_=rows[:, 5, :]), r=["rows5"], w=["rows6"])
        P.dve(lambda e: e.memset(tmpf[:, 0:128], 1.0), w=["tmpf"])
        for k in range(8):
            P.dve(lambda e, k=k: e.tensor_scalar(out=condbc[:, k, :], in0=tmpf[:, 0:128], scalar1=cTs[:, k:k + 1],
                                                 scalar2=None, op0=ALU.mult), r=["tmpf", "cTs"], w=["junk"])
        aw = ada_w[l].rearrange("(k p) n -> p k n", p=128)
        for cch in range(12):
            part = cch % 2
            sv = stg(part)[:, :].rearrange("p (k n) -> p k n", k=8)
            P.dma("sp", sv, aw[:, :, cch * 512:(cch + 1) * 512], w=[f"R2_{part}"])
            P.dma("sp", adab[:], bc(ada_b[l:l + 1, cch * 512:(cch + 1) * 512]), w=["m1"])
            pm = pM[cch % 2]
            for k in range(8):
                mm(pm[:, :], condbc[:, k, :], sv[:, k, :], start=(k == 0), stop=(k == 7),
                   r=["junk", f"R2_{part}"], w=[f"pM{cch % 2}"])
            rr = cch // 2
            hf = cch % 2
            P.dve(lambda e, pm=pm, rr=rr, hf=hf: e.tensor_tensor(out=rows[:, rr, hf * 512:(hf + 1) * 512], in0=pm[:, :],
                                                                  in1=adab[:], op=ALU.add),
                  r=[f"pM{cch % 2}", "m1"], w=[f"rows{rr}"])
        for rr, nw in ((1, norm1_w), (4, norm2_w)):
            P.dma("sp", tmpf[:], bc(nw[l:l + 1, :]), w=["tmpf"])
            P.dve(lambda e, rr=rr: e.scalar_tensor_tensor(out=rows[:, rr, :], in0=rows[:, rr, :], scalar=1.0, in1=tmpf[:],
                                                          op0=ALU.add, op1=ALU.mult),
                  r=[f"rows{rr}", "tmpf"], w=[f"rows{rr}"])
        P.dma("sp", tmpf[:, 0:288].rearrange("p (k n) -> p k n", k=8), moe_wr[l].rearrange("(k p) n -> p k n", p=128),
              w=["tmpf"])
        P.dve(lambda e: e.tensor_copy(out=WR[:], in_=tmpf[:, 0:288].rearrange("p (k n) -> p k n", k=8)),
              r=["tmpf"], w=["WR"])
        P.dma("sp", brow[:], bc(moe_br[l:l + 1, :]), w=["brow"])
        P.dve(lambda e: e.memset(cbase[:], 0.0), w=["cbase"])
        P.dma("sp", TAB, tabinit_in, w=["TAB"])
        win = even_w_in[j] if even else odd_w_in[j]
        ncols = 2320 if even else 3072
        wo = even_w_out[j] if even else odd_w_out[j]
        load_weight_bf16(R1w[:, :, 0:ncols], win, 8, ncols, WINT)
        load_weight_bf16(WOUT[:, :, :], wo, 8, 1024, ["WOUTt"])
        if even:
            P.dma("sp", tmpf[:, 0:64], bc(a_q_norm[j:j + 1, :]), w=["tmpf"])
            P.dma("sp", tmpf[:, 64:128], bc(a_k_norm[j:j + 1, :]), w=["tmpf2"])
            for i in range(10):
                src = tmpf[:, 0:64] if i < 8 else tmpf[:, 64:128]
                P.dve(lambda e, i=i, src=src: e.tensor_copy(out=wrow[:, i * 64:(i + 1) * 64], in_=src),
                      r=["tmpf", "tmpf2"], w=["wrow"])
            P.dma("sp", gbias[:], bc(b_gate_bias[j:j + 1, :]), w=["gbias"])
            P.dve(lambda e: e.memset(gupf[:, :], 0.0), w=["gupf"])
            P.dma("sp", gupf[0:16, :], b_gate_up[j], w=["gupf"])
            P.dve(lambda e: e.tensor_copy(out=gupb[:, :], in_=gupf[:, :]), r=["gupf"], w=["gupb"])
            P.dve(lambda e: e.memset(abp[:, :], 0.0), w=["abp"])
            P.dma("sp", onorm1[:, :], bc(b_out_norm[j:j + 1, :]), w=["onorm4"])
            P.dma("sp", esink[:], bc(a_sinks[j:j + 1, :]), w=["esink"])
            P.act(lambda e: e.activation(out=esink[:], in_=esink[:], func=AF.Exp), r=["esink"], w=["esink"])
            P.dve(lambda e: e.memset(St0[:], 0.0), w=["St0"])
            P.dve(lambda e: e.memset(qdm[:], 0.0), w=["qdm"])
            for i in range(2):
                P.dve(lambda e, i=i: e.memset(vaS[i][:], 1.0), w=[f"vaS{i}"])
        else:
            P.dma("sp", wrow[:, 0:128], bc(c_q_norm[j:j + 1, :]), w=["wrow"])
            P.dma("sp", wrow[:, 128:256], bc(c_k_norm[j:j + 1, :]), w=["wrow2"])
            P.dve(lambda e: e.memset(vast[:], 1.0), w=["vast"])

    def load_x(l, t, xb):
        xs = xt[xb]
        tg = "xt0"
        rs = slice(t * 128, (t + 1) * 128)
        if l == 0:
            P.dma("sp", xs[:], x_in[rs, :], w=[tg])
        else:
            P.dma("sp", xs[:], XR[rs, :], r=[f"XR{t}"], w=[tg])
            P.dma("sp", m1[:], MO[rs, :], r=["MO"], w=["m1"])
            P.dma("sp", m2[:], MO[MOH + t * 128:MOH + (t + 1) * 128, :], r=["MO"], w=["junk"])
            P.pool(lambda e: e.tensor_tensor(out=m1[:], in0=m1[:], in1=m2[:], op=ALU.add), r=["m1", "junk"], w=["m1"])
            P.pool(lambda e: e.tensor_tensor(out=m1[:], in0=m1[:], in1=rows[:, 6, :], op=ALU.mult),
                   r=["m1", "rows6"], w=["m1"])
            P.dve(lambda e: e.tensor_tensor(out=xs[:], in0=xs[:], in1=m1[:], op=ALU.add), r=[tg, "m1"], w=[tg])

    def norm_mod(xs, xtag, grow, srow, outb, otag):
        P.act(lambda e: e.activation(out=junk[:], in_=xs[:], func=AF.Square, accum_out=sm[:, 0:1]),
              r=[xtag], w=["junk", "sm0"])
        rstd_from_ssq(sm[:, 0:1], 1, 1.0 / D, "sm0")
        P.dve(lambda e: e.scalar_tensor_tensor(out=tmpf[:], in0=xs[:], scalar=sm[:, 0:1], in1=rows[:, grow, :],
                                               op0=ALU.mult, op1=ALU.mult),
              r=[xtag, "sm0", f"rows{grow}"], w=["tmpf"])
        P.pool(lambda e: e.tensor_tensor(out=outb[:], in0=tmpf[:], in1=rows[:, srow, :], op=ALU.add),
               r=["tmpf", f"rows{srow}"], w=[otag])

    tcount = [0]

    def transpose8(src, stag, dst, dtag):
        i = tcount[0] % 2
        tcount[0] += 1
        for k in range(8):
            tp(pT[i][:, k, :], src[:, k * 128:(k + 1) * 128], identb[:], r=[stag, "identb"], w=[f"pT{i}"])
        P.act(lambda e: e.copy(out=dst[:], in_=pT[i][:]), r=[f"pT{i}"], w=[dtag])

    mcount = [0]

    def project(srcT, stag, wview, wtag, ncols, dst, dtag):
        c0 = 0
        while c0 < ncols:
            cw = min(512, ncols - c0)
            i = mcount[0] % 2
            mcount[0] += 1
            for k in range(8):
                mm(pM[i][:, 0:cw], srcT[:, k, :], wview[:, k, c0:c0 + cw], start=(k == 0), stop=(k == 7),
                   r=[stag, wtag], w=[f"pM{i}"])
            if i == 0:
                P.act(lambda e, i=i, c0=c0, cw=cw: e.copy(out=dst[:, c0:c0 + cw], in_=pM[i][:, 0:cw]),
                      r=[f"pM{i}"], w=[dtag])
            else:
                P.dve(lambda e, i=i, c0=c0, cw=cw: e.tensor_copy(out=dst[:, c0:c0 + cw], in_=pM[i][:, 0:cw]),
                      r=[f"pM{i}"], w=[dtag])
            c0 += cw

    def tail(l, t, xb, attsrc, atag):
        xs = xt[xb]
        tg = "xt0"
        rs = slice(t * 128, (t + 1) * 128)
        transpose8(attsrc, atag, hT, "hT")
        for c in range(2):
            i = mcount[0] % 2
            mcount[0] += 1
            for k in range(8):
                mm(pM[i][:, :], hT[:, k, :], WOUT[:, k, c * 512:(c + 1) * 512], start=(k == 0), stop=(k == 7),
                   r=["hT", "WOUTt"], w=[f"pM{i}"])
            P.dve(lambda e, i=i, c=c: e.tensor_tensor(out=tmpf[:, c * 512:(c + 1) * 512], in0=pM[i][:, :],
                                                      in1=rows[:, 2, c * 512:(c + 1) * 512], op=ALU.mult),
                  r=[f"pM{i}", "rows2"], w=["tmpf"])
        P.pool(lambda e: e.tensor_tensor(out=xs[:], in0=xs[:], in1=tmpf[:], op=ALU.add), r=[tg, "tmpf"], w=[tg])
        P.dma("sp", XR[rs, :], xs[:], r=[tg], w=[f"XR{t}"])
        norm_mod(xs, tg, 4, 3, h2b, "hb")
        P.dma("sp", H2[rs, :], h2b[:], r=["hb"], wacc=["H2"])
        transpose8(h2b, "hb", hT, "hT")
        i = mcount[0] % 2
        mcount[0] += 1
        for k in range(8):
            mm(pM[i][:, 0:36], hT[:, k, :], WR[:, k, :], start=(k == 0), stop=(k == 7), r=["hT", "WR"], w=[f"pM{i}"])
        P.dve(lambda e: e.tensor_tensor(out=lg[:], in0=pM[i][:, 0:36], in1=brow[:], op=ALU.add),
              r=[f"pM{i}", "brow"], w=["lg"])
        RT = ["rt"]
        V = P.dve
        V(lambda e: e.tensor_reduce(out=sm[:, 8:9], in_=lg[:, 0:4], axis=AX.X, op=ALU.max), r=["lg"], w=RT)
        V(lambda e: e.tensor_scalar(out=sm[:, 12:16], in0=lg[:, 0:4], scalar1=sm[:, 8:9], scalar2=None, op0=ALU.subtract),
          r=["lg"] + RT, w=RT)
        P.act(lambda e: e.activation(out=sm[:, 16:20], in_=sm[:, 12:16], func=AF.Exp, accum_out=sm[:, 9:10]), r=RT, w=RT)
        V(lambda e: e.reciprocal(out=sm[:, 9:10], in_=sm[:, 9:10]), r=RT, w=RT)
        V(lambda e: e.tensor_scalar(out=sm[:, 20:24], in0=sm[:, 12:16], scalar1=0.0, scalar2=None, op0=ALU.is_ge),
          r=RT, w=RT)
        V(lambda e: e.tensor_scalar(out=sm[:, 20:24], in0=sm[:, 20:24], scalar1=BIG, scalar2=-BIG, op0=ALU.mult,
                                    op1=ALU.add), r=RT, w=RT)
        V(lambda e: e.tensor_tensor(out=msk[:, :].rearrange("p (g e) -> p g e", g=4),
                                    in0=lg[:, 4:36].rearrange("p (g e) -> p g e", g=4),
                                    in1=sm[:, 20:24].unsqueeze(2).to_broadcast([128, 4, 8]), op=ALU.add),
          r=["lg"] + RT, w=["msk"])
        V(lambda e: e.max(out=top8[:], in_=msk[:]), r=["msk"], w=["top8"])
        V(lambda e: e.tensor_scalar(out=M12[:, 0, :], in0=msk[:], scalar1=top8[:, 0:1], scalar2=None, op0=ALU.is_equal),
          r=["msk", "top8"], w=["M12"])
        V(lambda e: e.tensor_scalar(out=M12[:, 1, :], in0=msk[:], scalar1=top8[:, 1:2], scalar2=None, op0=ALU.is_equal),
          r=["msk", "top8"], w=["M12"])
        V(lambda e: e.tensor_tensor(out=sm[:, 10:11], in0=top8[:, 1:2], in1=top8[:, 0:1], op=ALU.subtract),
          r=["top8"], w=RT)
        P.act(lambda e: e.activation(out=sm[:, 10:11], in_=sm[:, 10:11], func=AF.Exp), r=RT, w=RT)
        V(lambda e: e.tensor_scalar(out=sm[:, 10:11], in0=sm[:, 10:11], scalar1=1.0, scalar2=None, op0=ALU.add), r=RT, w=RT)
        V(lambda e: e.reciprocal(out=sm[:, 10:11], in_=sm[:, 10:11]), r=RT, w=RT)
        rc = recall[:, t, :, :]
        rtag = "R2_0"
        V(lambda e: e.tensor_tensor(out=rc[:, 0, 2:3], in0=sm[:, 10:11], in1=sm[:, 9:10], op=ALU.mult), r=RT, w=[rtag])
        V(lambda e: e.tensor_tensor(out=rc[:, 1, 2:3], in0=sm[:, 9:10], in1=rc[:, 0, 2:3], op=ALU.subtract),
          r=RT + [rtag], w=[rtag])
        V(lambda e: e.tensor_scalar(out=rc[:, 0, 0:2], in0=pidx[:, 0:1].to_broadcast([128, 2]), scalar1=float(t * 128),
                                    scalar2=None, op0=ALU.add), r=["pidx"], w=[rtag])
        V(lambda e: e.tensor_scalar(out=rc[:, 1, 0:1], in0=pidx[:, 0:1], scalar1=float(t * 128), scalar2=None,
                                    op0=ALU.add), r=["pidx"], w=[rtag])
        V(lambda e: e.tensor_scalar(out=rc[:, 1, 1:2], in0=pidx[:, 0:1], scalar1=float(t * 128 + MOH), scalar2=None,
                                    op0=ALU.add), r=["pidx"], w=[rtag])
        V(lambda e: e.memset(rc[:, :, 3:4], 0.0), w=[rtag])
        V(lambda e: e.tensor_tensor(out=Mb[:], in0=M12[:, 0, :], in1=M12[:, 1, :], op=ALU.add), r=["M12"], w=["Mb"])
        V(lambda e: e.tensor_copy(out=M12all[:, t, :, :], in_=M12[:]), r=["M12"], w=[rtag])
        j_ = mcount[0] % 2
        mcount[0] += 1
        mm(pM[j_][:, 0:32], strib[:], Mb[:], r=["strib", "Mb"], w=[f"pM{j_}"])
        mm(pM[j_][:, 64:96], onesb[:], Mb[:], r=["onesb", "Mb"], w=[f"pM{j_}"])
        V(lambda e: e.tensor_tensor(out=Cc[:], in0=pM[j_][:, 0:32], in1=cbase[:], op=ALU.add),
          r=[f"pM{j_}", "cbase"], w=["Cc"])
        V(lambda e: e.tensor_tensor(out=cbase[:], in0=pM[j_][:, 64:96], in1=cbase[:], op=ALU.add),
          r=[f"pM{j_}", "cbase"], w=["cbase"])
        V(lambda e: e.tensor_tensor(out=prod[:], in0=M12[:], in1=Cc[:, :].unsqueeze(1).to_broadcast([128, 2, 32]),
                                    op=ALU.mult), r=["M12", "Cc"], w=["prod"])
        V(lambda e: e.tensor_reduce(out=slall[:, t, :], in_=prod[:], axis=AX.X, op=ALU.add), r=["prod"], w=[rtag])

    def route_finalize(l):
        P.barrier(lambda e: e.memset(sm[:, 62:63], 0.0))
        V = P.dve
        FT = ["rfin"]
        for half in range(2):
            es_ = slice(half * 16, (half + 1) * 16)
            t3 = tmpf[:, :].rearrange("p (a b) -> p a b", a=16)
            V(lambda e: e.tensor_tensor(out=t3, in0=cbase[:, es_].unsqueeze(2).to_broadcast([128, 16, 64]),
                                        in1=rcst[:, 0:64].unsqueeze(1).to_broadcast([128, 16, 64]), op=ALU.is_gt),
              r=["cbase", "rcst"], w=["tmpf"])
            V(lambda e: e.tensor_reduce(out=rtab[:, 0, es_], in_=t3, axis=AX.X, op=ALU.add), r=["tmpf"], w=FT)
        V(lambda e: e.tensor_scalar(out=rtab[:, 1, :], in0=rtab[:, 0, :], scalar1=128.0, scalar2=None, op0=ALU.mult),
          r=FT, w=FT)
        V(lambda e: e.memset(rtab[:, 5, :], 0.0), w=FT)
        V(lambda e: e.tensor_tensor_scan(out=rtab[:, 2, :], data0=rtab[:, 1, :], data1=rtab[:, 5, :], initial=0.0,
                                         op0=ALU.add, op1=ALU.add), r=FT, w=FT)
        V(lambda e: e.tensor_tensor(out=rtab[:, 3, :], in0=rtab[:, 2, :], in1=rtab[:, 1, :], op=ALU.subtract),
          r=FT, w=FT)
        V(lambda e: e.memset(blkE[:, :], 0.0), w=["blkE"])
        for ex in range(32):
            V(lambda e, ex=ex: e.scalar_tensor_tensor(out=blkE[:, :], in0=rcst[:, 64:160], scalar=rtab[:, 2, ex:ex + 1],
                                                      in1=blkE[:, :], op0=ALU.is_ge, op1=ALU.add),
              r=FT + ["rcst", "blkE"], w=["blkE"])
        V(lambda e: e.tensor_scalar(out=blkE[:, :], in0=blkE[:, :], scalar1=31.0, scalar2=float(32 * l), op0=ALU.min,
                                    op1=ALU.add), r=["blkE"], w=["blkE"])
        V(lambda e: e.tensor_scalar(out=widx1f[:, 0, :], in0=blkE[:, :], scalar1=128.0, scalar2=pidx[:, 0:1],
                                    op0=ALU.mult, op1=ALU.add), r=["blkE", "pidx"], w=["widx1f"])
        V(lambda e: e.memset(widx2f[:, 0, 0:1], 0.0), w=["widx2f"])
        V(lambda e: e.tensor_tensor(out=widx2f[:, 0, 1:96], in0=blkE[:, 1:96], in1=blkE[:, 0:95], op=ALU.is_equal),
          r=["blkE"], w=["widx2f"])
        V(lambda e: e.scalar_tensor_tensor(out=widx1f[:, 0, :], in0=widx2f[:, 0, :], scalar=268435456.0, in1=widx1f[:, 0, :],
                                           op0=ALU.mult, op1=ALU.add), r=["widx1f", "widx2f"], w=["widx1f"])
        V(lambda e: e.tensor_copy(out=widx1i[:, 0, :], in_=widx1f[:, 0, :]), r=["widx1f"], w=["widx1i"])
        for t in range(NT):
            rb_ = t % 2
            di = desti[rb_]
            dtag = f"desti{rb_}"
            V(lambda e, t=t: e.tensor_tensor(out=prod[:], in0=M12all[:, t, :, :],
                                             in1=rtab[:, 3, :].unsqueeze(1).to_broadcast([128, 2, 32]), op=ALU.mult),
              r=["R2_0"] + FT, w=["prod"])
            V(lambda e: e.tensor_reduce(out=sm[:, 26:28], in_=prod[:], axis=AX.X, op=ALU.add), r=["prod"], w=["rt"])
            V(lambda e, t=t: e.tensor_tensor(out=sm[:, 26:28], in0=sm[:, 26:28], in1=slall[:, t, :], op=ALU.add),
              r=["rt", "R2_0"], w=["rt"])
            V(lambda e, di=di: e.tensor_copy(out=di[:], in_=sm[:, 26:28]), r=["rt"], w=[dtag])
            for k in range(2):
                P.op("pool", lambda e, k=k, di=di, t=t: e.indirect_dma_start(
                    out=TAB, out_offset=bass.IndirectOffsetOnAxis(ap=di[:, k:k + 1], axis=0), in_=recall[:, t, k, :],
                    in_offset=None), ["R2_0", dtag], None, dma=True, wacc=["TAB"])

    def even_layer(l):
        for t in range(DBG.get("tiles", NT)):
            xb = t % 2
            cur = t % 2
            prv = 1 - cur
            load_x(l, t, xb)
            norm_mod(xt[xb], "xt0", 1, 0, hb, "hb")
            transpose8(hb, "hb", hT, "hT")
            project(hT, "hT", R1w, WINT, 2320, proj, "proj")
            if DBG.get("cut") == 1:
                P.dma("sp", out[t * 128:(t + 1) * 128, :], proj[:, 0:1024], r=["proj"], w=[f"out{t}"])
                continue
            P.dve(lambda e: e.tensor_tensor(out=tmpf[:, 0:640], in0=proj[:, 0:640], in1=proj[:, 0:640], op=ALU.mult),
                  r=["proj"], w=["tmpf"])
            P.dve(lambda e: e.tensor_reduce(out=sm[:, 32:42], in_=tmpf[:, 0:640].rearrange("p (g d) -> p g d", g=10),
                                            axis=AX.X, op=ALU.add), r=["tmpf"], w=["sm32"])
            rstd_from_ssq(sm[:, 32:42], 10, 1.0 / 64, "sm32")
            P.dve(lambda e: e.tensor_tensor(out=tmpf[:, 0:640].rearrange("p (g d) -> p g d", g=10),
                                            in0=proj[:, 0:640].rearrange("p (g d) -> p g d", g=10),
                                            in1=sm[:, 32:42].unsqueeze(2).to_broadcast([128, 10, 64]), op=ALU.mult),
                  r=["proj", "sm32"], w=["tmpf"])
            P.pool(lambda e: e.tensor_tensor(out=qkb[:], in0=tmpf[:, 0:512], in1=wrow[:, 0:512], op=ALU.mult),
                   r=["tmpf", "wrow"], w=["qkb"])
            for hf in range(2):
                P.pool(lambda e, hf=hf: e.tensor_tensor(out=kdup[:, :, hf * 64:(hf + 1) * 64],
                                                        in0=tmpf[:, 512:640].rearrange("p (g d) -> p g d", g=2),
                                                        in1=wrow[:, 512:640].rearrange("p (g d) -> p g d", g=2),
                                                        op=ALU.mult), r=["tmpf", "wrow"], w=["kdup"])
            P.act(lambda e: e.copy(out=vaS[cur][:, :, 0:64], in_=proj[:, 640:768].rearrange("p (g d) -> p g d", g=2)),
                  r=["proj"], w=[f"vaS{cur}"])
            i = tcount[0] % 2
            tcount[0] += 1
            for pr in range(4):
                tp(pT[i][:, pr, :], qkb[:, pr * 128:(pr + 1) * 128], identb[:], r=["qkb", "identb"], w=[f"pT{i}"])
            for g in range(2):
                tp(pT[i][:, 4 + g, :], kdup[:, g, :], identb[:], r=["kdup", "identb"], w=[f"pT{i}"])
            P.act(lambda e, i=i: e.copy(out=qkT[cur][:, :, :], in_=pT[i][:, 0:6, :]), r=[f"pT{i}"], w=[f"qkT{cur}"])
            if DBG.get("cut") == 2:
                P.dve(lambda e: e.tensor_copy(out=tmpf[:, 0:768], in_=qkT[cur][:, :, :].rearrange("p a b -> p (a b)")), r=[f"qkT{cur}"], w=["tmpf"])
                P.dma("sp", out[t * 128:(t + 1) * 128, 0:768], tmpf[:, 0:768], r=["tmpf"], w=[f"out{t}"])
                continue
            for hg in range(2):
                po = pO[hg]
                for hh in range(4):
                    h = hg * 4 + hh
                    g = h // 4
                    pr = h // 2
                    b0 = (h % 2) * 64
                    psx = pS[h % 2]
                    mm(psx[:, 0:128], qkT[cur][b0:b0 + 64, 4 + g, :], qkT[cur][b0:b0 + 64, pr, :],
                       r=[f"qkT{cur}"], w=[f"pS{h % 2}"])
                    if t > 0:
                        mm(psx[:, 128:256], qkT[prv][b0:b0 + 64, 4 + g, :], qkT[cur][b0:b0 + 64, pr, :],
                           r=[f"qkT{cur}", f"qkT{prv}"], w=[f"pS{h % 2}"])
                    P.dve(lambda e, psx=psx, h=h: e.scalar_tensor_tensor(out=sc[:], in0=psx[:, 0:256], scalar=0.125,
                                                                         in1=BS[:, h, :], op0=ALU.mult, op1=ALU.add),
                          r=[f"pS{h % 2}", "BS"], w=["sc"])
                    pe_ = pexp[h % 2]
                    P.act(lambda e, pe_=pe_: e.activation(out=pe_[:], in_=sc[:], func=AF.Exp), r=["sc"], w=[f"pexp{h % 2}"])
                    mm(po[:, hh * 65:(hh + 1) * 65], pe_[:, 0:128], vaS[cur][:, g, :], start=True, stop=(t == 0),
                       r=[f"pexp{h % 2}", f"vaS{cur}"], w=[f"pO{hg}"])
                    if t > 0:
                        mm(po[:, hh * 65:(hh + 1) * 65], pe_[:, 128:256], vaS[prv][:, g, :], start=False, stop=True,
                           r=[f"pexp{h % 2}", f"vaS{prv}"], w=[f"pO{hg}"])
                pov = po[:, 0:260].rearrange("p (h c) -> p h c", h=4)
                P.dve(lambda e, pov=pov, hg=hg: e.tensor_tensor(out=sm[:, 44:48].unsqueeze(2), in0=pov[:, :, 64:65],
                                                                in1=esink[:, hg * 4:(hg + 1) * 4].unsqueeze(2), op=ALU.add),
                      r=[f"pO{hg}", "esink"], w=["sm44"])
                P.dve(lambda e: e.reciprocal(out=sm[:, 44:48], in_=sm[:, 44:48]), r=["sm44"], w=["sm44"])
                P.dve(lambda e, pov=pov, hg=hg: e.tensor_tensor(
                    out=att[:, hg * 256:(hg + 1) * 256].rearrange("p (h d) -> p h d", h=4), in0=pov[:, :, 0:64],
                    in1=sm[:, 44:48].unsqueeze(2).to_broadcast([128, 4, 64]), op=ALU.mult),
                    r=[f"pO{hg}", "sm44"], w=["att"])
            if DBG.get("cut") == 31:
                P.dve(lambda e: e.tensor_copy(out=tmpf[:, 0:256], in_=sc[:, :]), r=["sc"], w=["tmpf"])
                P.dve(lambda e: e.tensor_copy(out=tmpf[:, 256:516], in_=pO[1][:, 0:260]), r=["pO1"], w=["tmpf"])
                P.dve(lambda e: e.tensor_copy(out=tmpf[:, 520:528], in_=esink[:, :]), r=["esink"], w=["tmpf"])
                P.dve(lambda e: e.tensor_copy(out=tmpf[:, 528:532], in_=sm[:, 44:48]), r=["sm44"], w=["tmpf"])
                P.dve(lambda e: e.tensor_copy(out=tmpf[:, 532:788], in_=pexp[1][:, :]), r=["pexp1"], w=["tmpf"])
                P.dve(lambda e: e.tensor_copy(out=tmpf[:, 788:918], in_=vaS[cur][:, :, :].rearrange("p a b -> p (a b)")), r=[f"vaS{cur}"], w=["tmpf"])
                P.dma("sp", out[t * 128:(t + 1) * 128, :], tmpf[:, :], r=["tmpf"], w=[f"out{t}"])
                continue
            if DBG.get("cut") == 3:
                P.dve(lambda e: e.tensor_copy(out=tmpf[:, 0:512], in_=att[:, 0:512]), r=["att"], w=["tmpf"])
                P.dma("sp", out[t * 128:(t + 1) * 128, 0:512], tmpf[:, 0:512], r=["tmpf"], w=[f"out{t}"])
                continue
            P.dve(lambda e: e.tensor_copy(out=abp[:, 0:16], in_=proj[:, 2304:2320]), r=["proj"], w=["abp"])
            i = tcount[0] % 2
            tcount[0] += 1
            tp(pT[i][0:32, 0, :], abp[:, :], identb[:], r=["abp", "identb"], w=[f"pT{i}"])
            P.act(lambda e: e.copy(out=abT[:, :], in_=pT[i][0:32, 0, :]), r=[f"pT{i}"], w=["abT"])
            mm(pS[1][:, 0:256], abT[:, :], gupb[:, :], r=["abT", "gupb"], w=["pS1"])
            P.dve(lambda e: e.tensor_tensor(out=zt[:], in0=pS[1][:, 0:256], in1=gbias[:], op=ALU.add),
                  r=["pS1", "gbias"], w=["zt"])
            if DBG.get("cut") == 41:
                P.dve(lambda e: e.tensor_copy(out=tmpf[:, 0:256], in_=zt[:, :]), r=["zt"], w=["tmpf"])
                P.dma("sp", out[t * 128:(t + 1) * 128, 0:256], tmpf[:, 0:256], r=["tmpf"], w=[f"out{t}"])
                continue
            P.act(lambda e: e.activation(out=zt[:], in_=zt[:], func=AF.Exp, scale=-1.0), r=["zt"], w=["zt"])
            P.dve(lambda e: e.tensor_scalar(out=zt[:], in0=zt[:], scalar1=1.0, scalar2=None, op0=ALU.add), r=["zt"], w=["zt"])
            P.act(lambda e: e.activation(out=zt[:], in_=zt[:], func=AF.Ln), r=["zt"], w=["zt"])
            P.dve(lambda e: e.tensor_scalar(out=gneg[:], in0=zt[:], scalar1=-1.0 / 16.0, scalar2=None, op0=ALU.mult),
                  r=["zt"], w=["gneg"])
            if DBG.get("cut") == 42:
                P.dve(lambda e: e.tensor_copy(out=tmpf[:, 0:256], in_=gneg[:, :]), r=["gneg"], w=["tmpf"])
                P.dma("sp", out[t * 128:(t + 1) * 128, 0:256], tmpf[:, 0:256], r=["tmpf"], w=[f"out{t}"])
                continue
            mm(pS[0][:, 0:256], trif[:], gneg[:], r=["trif", "gneg"], w=["pS0"])
            mm(pS[0][:, 256:512], bonesf[:], gneg[:], r=["bonesf", "gneg"], w=["pS0"])
            P.act(lambda e: e.copy(out=bsb[:], in_=pS[0][:, 0:256]), r=["pS0"], w=["zt"])
            P.act(lambda e: e.activation(out=eb[:], in_=pS[0][:, 0:256], func=AF.Exp), r=["pS0"], w=["eb"])
            P.act(lambda e: e.activation(out=enb[:], in_=pS[0][:, 0:256], func=AF.Exp, scale=-1.0), r=["pS0"], w=["enb"])
            P.dve(lambda e: e.tensor_tensor(out=ekd[:], in0=pS[0][:, 256:512], in1=bsb[:], op=ALU.subtract),
                  r=["pS0", "zt"], w=["ekd"])
            P.act(lambda e: e.activation(out=ekd[:], in_=ekd[:], func=AF.Exp), r=["ekd"], w=["ekd"])
            if DBG.get("cut") == 43:
                P.dve(lambda e: e.tensor_copy(out=tmpf[:, 0:256], in_=ekd[:, :]), r=["ekd"], w=["tmpf"])
                P.dma("sp", out[t * 128:(t + 1) * 128, 0:256], tmpf[:, 0:256], r=["tmpf"], w=[f"out{t}"])
                continue
            P.dve(lambda e: e.scalar_tensor_tensor(out=qd[:], in0=proj[:, 768:1024], scalar=0.125, in1=eb[:],
                                                   op0=ALU.mult, op1=ALU.mult), r=["proj", "eb"], w=["qd"])
            P.pool(lambda e: e.tensor_tensor(out=kd[:], in0=proj[:, 1024:1280], in1=enb[:], op=ALU.mult),
                   r=["proj", "enb"], w=["kd"])
            P.pool(lambda e: e.tensor_tensor(out=ku[:], in0=proj[:, 1024:1280], in1=ekd[:], op=ALU.mult),
                   r=["proj", "ekd"], w=["ku"])
            P.pool(lambda e: e.tensor_copy(out=vbb[:], in_=proj[:, 1280:1792]), r=["proj"], w=["vbb"])
            P.act(lambda e: e.activation(out=sgn[:], in_=proj[:, 1792:2304], func=AF.Silu), r=["proj"], w=["sgn"])
            P.dve(lambda e: e.tensor_tensor(out=sgn[:, :].rearrange("p (h d) -> p h d", h=4),
                                             in0=sgn[:, :].rearrange("p (h d) -> p h d", h=4),
                                             in1=onorm1[:, :].unsqueeze(1).to_broadcast([128, 4, 128]), op=ALU.mult),
                   r=["sgn", "onorm4"], w=["sgn"])
            P.dve(lambda e: e.tensor_copy(out=gnb[:, :], in_=gneg[:, :]), r=["gneg"], w=["gnb"])
            for hh in range(4):
                mm(pS[1][0:64, hh * 128:(hh + 1) * 128], gnb[:, hh * 64:(hh + 1) * 64], bonesb[:, :], r=["gnb", "bonesb"], w=["pS1"])
            P.act(lambda e: e.activation(out=dec[:, :].rearrange("p (h c) -> p h c", h=4),
                                         in_=pS[1][0:64, :].rearrange("p (h c r) -> p h c r", h=4, c=2)[:, :, :, 0],
                                         func=AF.Exp), r=["pS1"], w=["dec"])
            if DBG.get("cut") == 4:
                P.dve(lambda e: e.tensor_copy(out=tmpf[:, 0:256], in_=qd[:, :]), r=["qd"], w=["tmpf"])
                P.dve(lambda e: e.tensor_copy(out=tmpf[:, 256:512], in_=ku[:, :]), r=["ku"], w=["tmpf"])
                P.dve(lambda e: e.tensor_copy(out=tmpf[0:64, 512:520], in_=dec[:, :]), r=["dec"], w=["tmpf"])
                P.dma("sp", out[t * 128:(t + 1) * 128, 0:520], tmpf[:, 0:520], r=["tmpf"], w=[f"out{t}"])
                continue
            i = tcount[0] % 2
            tcount[0] += 1
            for hh in range(4):
                tp(pT[i][0:64, hh, :], qd[:, hh * 64:(hh + 1) * 64], identb[:], r=["qd", "identb"], w=[f"pT{i}"])
                tp(pT[i][0:64, 4 + hh, :], kd[:, hh * 64:(hh + 1) * 64], identb[:], r=["kd", "identb"], w=[f"pT{i}"])
            P.act(lambda e, i=i: e.copy(out=qdT[:], in_=pT[i][0:64, 0:4, :]), r=[f"pT{i}"], w=["qdT"])
            P.act(lambda e, i=i: e.copy(out=kdT[:], in_=pT[i][0:64, 4:8, :]), r=[f"pT{i}"], w=["kdT"])
            P.dve(lambda e, i=i: e.tensor_copy(out=qdm[:, :, 0, 0:64], in_=pT[i][0:64, 0:4, 0:64]), r=[f"pT{i}"], w=["qdm"])
            P.dve(lambda e, i=i: e.tensor_copy(out=qdm[:, :, 1, 64:128], in_=pT[i][0:64, 0:4, 64:128]),
                  r=[f"pT{i}"], w=["qdm"])
            P.act(lambda e: e.copy(out=St0b[:], in_=St0[:]), r=["St0"], w=["St0b"])
            for hh in range(4):
                vs = slice(hh * 128, (hh + 1) * 128)
                ks = slice(hh * 64, (hh + 1) * 64)
                pa = pS[hh % 2]
                mm(pa[:, 0:128], kdT[:, hh, :], qdT[:, hh, :], r=["kdT", "qdT"], w=[f"pS{hh % 2}"])
                P.dve(lambda e, pa=pa: e.tensor_tensor(out=attT[:], in0=pa[:, 0:128], in1=trif[:], op=ALU.mult),
                      r=[f"pS{hh % 2}", "trif"], w=["attT"])
                mm(pO[0][0:64, vs], ku[0:64, ks], vbb[0:64, vs], r=["ku", "vbb"], w=["pO0"])
                mm(pO[1][0:64, vs], ku[64:128, ks], vbb[64:128, vs], r=["ku", "vbb"], w=["pO1"])
                P.dve(lambda e, hh=hh: e.scalar_tensor_tensor(out=St1[:, hh, :], in0=St0[:, hh, :],
                                                              scalar=dec[:, 2 * hh:2 * hh + 1], in1=pO[0][0:64, vs],
                                                              op0=ALU.mult, op1=ALU.add),
                      r=["St0", "dec", "pO0"], w=["St1"])
                P.act(lambda e, hh=hh: e.copy(out=St1b[:, hh, :], in_=St1[:, hh, :]), r=["St1"], w=["St1b"])
                mm(pa[:, 256:384], attT[:, :], vbb[:, vs], start=True, stop=False, r=["attT", "vbb"], w=[f"pS{hh % 2}"])
                mm(pa[:, 256:384], qdm[:, hh, 0, :], St0b[:, hh, :], start=False, stop=False, r=["qdm", "St0b"],
                   w=[f"pS{hh % 2}"])
                mm(pa[:, 256:384], qdm[:, hh, 1, :], St1b[:, hh, :], start=False, stop=True, r=["qdm", "St1b"],
                   w=[f"pS{hh % 2}"])
                P.dve(lambda e, hh=hh: e.scalar_tensor_tensor(out=St0[:, hh, :], in0=St1[:, hh, :],
                                                              scalar=dec[:, 2 * hh + 1:2 * hh + 2],
                                                              in1=pO[1][0:64, vs], op0=ALU.mult, op1=ALU.add),
                      r=["St1", "dec", "pO1"], w=["St0"])
                P.act(lambda e, pa=pa: e.activation(out=junk[:, 0:128], in_=pa[:, 256:384], func=AF.Square,
                                                    accum_out=sm[:, 50:51]), r=[f"pS{hh % 2}"], w=["junk", "sm50"])
                rstd_from_ssq(sm[:, 50:51], 1, 1.0 / 128, "sm50")
                P.dve(lambda e, pa=pa, hh=hh: e.scalar_tensor_tensor(out=att[:, 512 + hh * 128:512 + (hh + 1) * 128],
                                                                     in0=pa[:, 256:384], scalar=sm[:, 50:51],
                                                                     in1=sgn[:, hh * 128:(hh + 1) * 128], op0=ALU.mult,
                                                                     op1=ALU.mult),
                      r=[f"pS{hh % 2}", "sm50", "sgn"], w=["att"])
            if DBG.get("notail"):
                P.dve(lambda e: e.tensor_copy(out=tmpf[:], in_=att[:]), r=["att"], w=["tmpf"])
                P.dma("sp", out[t * 128:(t + 1) * 128, :], tmpf[:], r=["tmpf"], w=[f"out{t}"])
                continue
            tail(l, t, xb, att, "att")
            if DBG.get("dumph2"):
                P.dve(lambda e: e.tensor_copy(out=tmpf[:], in_=hb[:]), r=["hb"], w=["tmpf"])
                P.dma("sp", out[t * 128:(t + 1) * 128, :], tmpf[:], r=["tmpf"], w=[f"out{t}"])

    def odd_layer(l):
        P.dve(lambda e: e.memset(kmsum[:], 0.0), w=["kmsum"])
        for t in range(NT):
            xb = t % 2
            rs = slice(t * 128, (t + 1) * 128)
            load_x(l, t, xb)
            if l > 0:
                P.dma("sp", XR[rs, :], xt[xb][:], r=["xt0"], w=[f"XR{t}"])
            norm_mod(xt[xb], "xt0", 1, 0, hb, "hb")
            transpose8(hb, "hb", hT, "hT")
            project(hT, "hT", R1w, WINT, 2048, proj, "proj")
            for half in range(2):
                src = proj[:, half * 1024:(half + 1) * 1024]
                P.dve(lambda e, src=src: e.tensor_tensor(out=tmpf[:], in0=src, in1=src, op=ALU.mult), r=["proj"], w=["tmpf"])
                P.dve(lambda e: e.tensor_reduce(out=sm[:, 32:40], in_=tmpf[:, :].rearrange("p (g d) -> p g d", g=8),
                                                axis=AX.X, op=ALU.add), r=["tmpf"], w=["sm32"])
                rstd_from_ssq(sm[:, 32:40], 8, 1.0 / 128, "sm32")
                P.dve(lambda e, src=src: e.tensor_tensor(out=tmpf[:, :].rearrange("p (g d) -> p g d", g=8),
                                                         in0=src.rearrange("p (g d) -> p g d", g=8),
                                                         in1=sm[:, 32:40].unsqueeze(2).to_broadcast([128, 8, 128]),
                                                         op=ALU.mult), r=["proj", "sm32"], w=["tmpf"])
                P.pool(lambda e, half=half: e.tensor_tensor(
                    out=qnb[:, half * 1024:(half + 1) * 1024].rearrange("p (g d) -> p g d", g=8),
                    in0=tmpf[:, :].rearrange("p (g d) -> p g d", g=8),
                    in1=wrow[:, half * 128:(half + 1) * 128].unsqueeze(1).to_broadcast([128, 8, 128]), op=ALU.mult),
                    r=["tmpf", "wrow", "wrow2"], w=["qnb"])
            project(hT, "hT", R1w[:, :, 2048:3072], WINT, 1024, proj, "proj")
            P.act(lambda e: e.copy(out=vast[:, :, 0:128], in_=proj[:, 0:1024].rearrange("p (g d) -> p g d", g=8)),
                  r=["proj"], w=["vast"])
            P.dma("sp", VA[rs, :, :].rearrange("t h c -> t (h c)"), vast[:, :, :].rearrange("p h c -> p (h c)"), r=["vast"], wacc=["VA"])
            for half in range(2):
                dstD = QT if half == 0 else KT
                transpose8(qnb[:, half * 1024:(half + 1) * 1024], "qnb", hT, "hT")
                P.dma("sp", dstD[:, :, rs].rearrange("h d t -> d h t"), hT[:], r=["hT"], wacc=["QT" if half == 0 else "KT"])
                if half == 1:
                    P.dve(lambda e, t=t: e.tensor_reduce(out=kmsum[:, :, t], in_=hT[:], axis=AX.X, op=ALU.add),
                          r=["hT"], w=["kmsum"])
        P.dve(lambda e: e.tensor_tensor(out=tmpf[:, 0:128].rearrange("p (h b) -> p h b", h=8), in0=kmsum[:, :, :].rearrange("p h (b two) -> p h b two", two=2)[:, :, :, 0],
                                        in1=kmsum[:, :, :].rearrange("p h (b two) -> p h b two", two=2)[:, :, :, 1], op=ALU.add), r=["kmsum"], w=["tmpf"])
        P.dve(lambda e: e.tensor_scalar(out=kmean[:], in0=tmpf[:, 0:128].rearrange("p (h b) -> p h b", h=8),
                                        scalar1=1.0 / 256, scalar2=None, op0=ALU.mult), r=["tmpf"], w=["kmean"])
        pcount = [0]
        ocount = [0]
        scount = [0]
        pTf = [pT[i][:, :, :].rearrange("p a b -> p (a b)").bitcast(F32) for i in range(2)]
        sbanks = ((pS[0], "pS0"), (pS[1], "pS1"), (pTf[0], "pT0"), (pTf[1], "pT1"))
        for hgp in range(4):
            h0 = hgp * 2
            P.dma("sp", KTs, KT[h0:h0 + 2, :, :].rearrange("h d t -> d h t"), r=["KT"], w=["R2_0"])
            P.dma("sp", VAs2, VA[:, h0:h0 + 2, :].rearrange("(t p) h c -> p t (h c)", p=128), r=["VA"], w=["R2_1"])
            for qb_ in range(16):
                qs = slice(qb_ * 256, (qb_ + 1) * 256)
                P.dma("sp", qTt[:], QT[h0:h0 + 2, :, qs].rearrange("h d t -> d h t"), r=["QT"], w=["qTt"])
                for hh in range(2):
                    h = h0 + hh
                    for qt in range(2):
                        mm(pM[0][:, qt * 16:(qt + 1) * 16], qTt[:, hh, qt * 128:(qt + 1) * 128], kmean[:, h, :],
                           r=["qTt", "kmean"], w=["pM0"])
                    P.dve(lambda e: e.memset(scs[:], NEG), w=["scs"])
                    if qb_ > 0:
                        P.dve(lambda e, qb_=qb_: e.tensor_copy(
                            out=scs[:, :, 0:qb_], in_=pM[0][:, 0:32].rearrange("p (q b) -> p q b", q=2)[:, :, 0:qb_]),
                            r=["pM0"], w=["scs"])
                    for qt in range(2):
                        P.dve(lambda e, qt=qt: e.max(out=top8[:], in_=scs[:, qt, :]), r=["scs"], w=["top8"])
                        P.dve(lambda e, qt=qt, hh=hh: e.tensor_scalar(out=sel[:, hh, qt, :], in0=scs[:, qt, :],
                                                                      scalar1=top8[:, 2:3], scalar2=None, op0=ALU.is_ge),
                              r=["scs", "top8"], w=["sel"])
                    nkt = 2 * qb_ + 2
                    for qt in range(2):
                        qtile = 2 * qb_ + qt
                        qsl = slice(qt * 128, (qt + 1) * 128)
                        po = pO[0]
                        first = True
                        for kt in range(2 * qb_, qtile + 1):
                            dl = qtile - kt
                            psx = pS[pcount[0] % 2]
                            ptag = f"pS{pcount[0] % 2}"
                            pb = pT2[pcount[0] % 4]
                            pbt = f"pT2_{pcount[0] % 4}"
                            pcount[0] += 1
                            mm(psx[:, 0:128], KTs[:, hh, kt * 128:(kt + 1) * 128], qTt[:, hh, qsl], r=["R2_0", "qTt"], w=[ptag])
                            P.dve(lambda e, psx=psx, h=h, dl=dl: e.scalar_tensor_tensor(
                                out=sco_[(pcount[0] - 1) % 2][:, 0:128], in0=psx[:, 0:128], scalar=128 ** -0.5,
                                in1=(BS[:, h, 0:128] if dl == 0 else BM1[:, h, :]), op0=ALU.mult, op1=ALU.add), r=[ptag, "BM", "BS"], w=[f"sco{(pcount[0] - 1) % 2}"])
                            P.act(lambda e, pb=pb: e.activation(out=pb[:, 0:128], in_=sco_[(pcount[0] - 1) % 2][:, 0:128], func=AF.Exp),
                                  r=[f"sco{(pcount[0] - 1) % 2}"], w=[pbt])
                            mm(po[:, qt * 129:(qt + 1) * 129], pb[:, 0:128], VAs[:, kt, hh, :], start=first,
                               stop=(kt == qtile), r=[pbt, "R2_1"], w=["pO0"])
                            first = False
                    P.dve(lambda e, hh=hh: e.tensor_copy(out=acc[:, hh, :, :],
                                                         in_=pO[0][:, 0:258].rearrange("p (q c) -> p q c", q=2)),
                          r=["pO0"], w=["acc"])
                    def stage1(jb, hh=hh, h=h, qb_=qb_):
                        pbs = []
                        for kk in range(2):
                            kt = 2 * jb + kk
                            psx, ptag = sbanks[scount[0] % 4]
                            scount[0] += 1
                            pb = pT2[pcount[0] % 4]
                            pbt = f"pT2_{pcount[0] % 4}"
                            sci = pcount[0] % 2
                            pcount[0] += 1
                            mm(psx[:, 0:256], KTs[:, hh, kt * 128:(kt + 1) * 128], qTt[:, hh, :], r=["R2_0", "qTt"], w=[ptag])
                            if kt == 2 * qb_ - 1:
                                P.dve(lambda e, psx=psx, h=h, sci=sci: e.scalar_tensor_tensor(
                                    out=sco_[sci][:, 0:128], in0=psx[:, 0:128], scalar=128 ** -0.5, in1=BM1[:, h, :],
                                    op0=ALU.mult, op1=ALU.add), r=[ptag, "BM"], w=[f"sco{sci}"])
                                P.act(lambda e, pb=pb, sci=sci: e.activation(out=pb[:, 0:128], in_=sco_[sci][:, 0:128], func=AF.Exp),
                                      r=[f"sco{sci}"], w=[pbt])
                                P.act(lambda e, pb=pb, psx=psx, h=h: e.activation(out=pb[:, 128:256], in_=psx[:, 128:256],
                                                                                   func=AF.Exp, bias=relfar[:, h:h + 1],
                                                                                   scale=128 ** -0.5),
                                      r=[ptag, "relfar"], w=[pbt])
                            else:
                                P.act(lambda e, pb=pb, psx=psx, h=h: e.activation(out=pb[:, 0:256], in_=psx[:, 0:256],
                                                                                   func=AF.Exp, bias=relfar[:, h:h + 1],
                                                                                   scale=128 ** -0.5),
                                      r=[ptag, "relfar"], w=[pbt])
                            pbs.append((pb, pbt, kt))
                        return pbs

                    def stage2(jb, pbs, hh=hh):
                        po, potag = ((pO[1], "pO1"), (pM[1], "pM1"))[ocount[0] % 2]
                        ocount[0] += 1
                        for qt in range(2):
                            for kk, (pb, pbt, kt) in enumerate(pbs):
                                mm(po[:, qt * 129:(qt + 1) * 129], pb[:, qt * 128:(qt + 1) * 128], VAs[:, kt, hh, :],
                                   start=(kk == 0), stop=(kk == 1), r=[pbt, "R2_1"], w=[potag])
                        for qt in range(2):
                            P.dve(lambda e, qt=qt, hh=hh, jb=jb, po=po: e.scalar_tensor_tensor(
                                out=acc[:, hh, qt, :], in0=po[:, qt * 129:(qt + 1) * 129], scalar=sel[:, hh, qt, jb:jb + 1],
                                in1=acc[:, hh, qt, :], op0=ALU.mult, op1=ALU.add), r=[potag, "sel", "acc"], w=["acc"])

                    if qb_ > 0:
                        cur_pbs = stage1(0)
                        for jb in range(qb_):
                            nxt = stage1(jb + 1) if jb + 1 < qb_ else None
                            stage2(jb, cur_pbs)
                            cur_pbs = nxt
                    for qt in range(2):
                        P.dve(lambda e, qt=qt, hh=hh: e.reciprocal(out=sm[:, 52:53], in_=acc[:, hh, qt, 128:129]),
                              r=["acc"], w=["sm52"])
                        P.dve(lambda e, qt=qt, hh=hh: e.tensor_scalar(out=attq[:, qt, hh * 128:(hh + 1) * 128],
                                                                      in0=acc[:, hh, qt, 0:128], scalar1=sm[:, 52:53],
                                                                      scalar2=None, op0=ALU.mult),
                              r=["acc", "sm52"], w=["attq"])
                for qt in range(2):
                    t = 2 * qb_ + qt
                    P.dma("sp", ATT[t * 128:(t + 1) * 128, h0 * 128:(h0 + 2) * 128], attq[:, qt, :], r=["attq"], wacc=["ATT"])
        for t in range(NT):
            xb = t % 2
            rs = slice(t * 128, (t + 1) * 128)
            if l == 0:
                P.dma("sp", xt[xb][:], x_in[rs, :], w=["xt0"])
            else:
                P.dma("sp", xt[xb][:], XR[rs, :], r=[f"XR{t}"], w=["xt0"])
            P.dma("sp", att[:], ATT[rs, :], r=["ATT"], w=["att"])
            tail(l, t, xb, att, "att")

    def expert_phase(l):
        w1r = moe_w1.rearrange("l e (p j) n -> (l e p) (j n)", j=8)
        w3r = moe_w3.rearrange("l e (p j) n -> (l e p) (j n)", j=8)
        w2r = moe_w2.rearrange("l e (p j) n -> (l e p) (j n)", j=4)
        s0f = stg(0)[:, :]
        s1f = stg(1)[:, :]
        s2f = WOUT[:, :, :].rearrange("p a b -> p (a b)").bitcast(F32)
        s0 = stg(0)[:, :].rearrange("p (k n) -> p k n", k=8)
        s1 = stg(1)[:, :].rearrange("p (k n) -> p k n", k=8)
        s2 = WOUT[:, :, :].rearrange("p a b -> p (a b)").bitcast(F32).rearrange("p (k n) -> p k n", k=4)
        nblk = DBG.get("nblk", NBLK)

        def stage_tok(b):
            tb_ = b % 3
            tb = tabt[tb_]
            ix = idxi[tb_]
            P.dma("sp", tb[:], TAB[b * 128:(b + 1) * 128, :], r=["TAB"], w=[f"tabt{tb_}"])
            P.dve(lambda e, tb=tb, ix=ix: e.tensor_copy(out=ix[:], in_=tb[:, 0:2]), r=[f"tabt{tb_}"], w=[f"idxi{tb_}"])
            P.op("pool", lambda e, ix=ix, tb_=tb_: e.indirect_dma_start(
                out=xg[tb_][:, :], out_offset=None, in_=H2,
                in_offset=bass.IndirectOffsetOnAxis(ap=ix[:, 0:1], axis=0)),
                ["H2", f"idxi{tb_}"], [f"xg{tb_}"], dma=True)

        def stage_w(b):
            if not DBG.get("nowdma"):
                for sv, wr_, tg in ((s0f, w1r, "R2_0"), (s1f, w3r, "R2_1"), (s2f, w2r, "WOUTt")):
                    P.op("pool", lambda e, b=b, sv=sv, wr_=wr_: e.indirect_dma_start(
                        out=sv, out_offset=None, in_=wr_,
                        in_offset=bass.IndirectOffsetOnAxis(ap=widx1i[:, 0, b:b + 1], axis=0),
                        bounds_check=_LazyReg(NLW * 32 * 128 - 1), oob_is_err=False),
                        ["widx1i"], [tg], dma=True)

        def stage_cast(b):
            eb_ = b % 2
            w1v, w3v, w2v = ew(eb_, 0), ew(eb_, 1), ew(eb_, 2)
            wt = [f"R1_{eb_}_{i}" for i in range(3)]
            P.act(lambda e, w1v=w1v: e.copy(out=w1v, in_=s0), r=["R2_0"], w=[wt[0]])
            P.dve(lambda e, w3v=w3v: e.tensor_copy(out=w3v, in_=s1), r=["R2_1"], w=[wt[1]])
            P.act(lambda e, w2v=w2v: e.copy(out=w2v, in_=s2), r=["WOUTt"], w=[wt[2]])

        def stage_b1(b):
            eb_ = b % 2
            tb_ = b % 3
            i = tcount[0] % 2
            tcount[0] += 1
            for k in range(8):
                tp(pT[i][:, k, :], xg[tb_][:, :].rearrange("t (p j) -> t j p", j=8)[:, k, :], identb[:],
                   r=[f"xg{tb_}", "identb"], w=[f"pT{i}"])
            P.dve(lambda e, i=i, eb_=eb_: e.tensor_copy(out=xgT[eb_][:, :, :], in_=pT[i][:]), r=[f"pT{i}"], w=[f"xgT{eb_}"])

        def stage_b(b):
            eb_ = b % 2
            tb_ = b % 3
            w1v, w3v, w2v = ew(eb_, 0), ew(eb_, 1), ew(eb_, 2)
            wt = [f"R1_{eb_}_{i}" for i in range(3)]
            tb = tabt[tb_]
            ix = idxi[tb_]
            hbanks = ((pM[0], "pM0"), (pM[1], "pM1"), (pO[0], "pO0"), (pO[1], "pO1"))
            for hc in range(4):
                hb_, htag = hbanks[hc]
                for k in range(8):
                    mm(hb_[:, 0:128], w1v[:, k, :].rearrange("p (m f) -> p f m", f=4)[:, hc, :], xgT[eb_][:, k, :],
                       start=(k == 0), stop=(k == 7), r=[f"xgT{eb_}", wt[0]], w=[htag])
                for k in range(8):
                    mm(hb_[:, 128:256], w3v[:, k, :].rearrange("p (m f) -> p f m", f=4)[:, hc, :], xgT[eb_][:, k, :],
                       start=(k == 0), stop=(k == 7), r=[f"xgT{eb_}", wt[1]], w=[htag])
            for hc in range(4):
                hb_, htag = hbanks[hc]
                P.act(lambda e, hb_=hb_, hc=hc: e.activation(out=s1t4[:, hc, :], in_=hb_[:, 0:128], func=AF.Silu),
                      r=[htag], w=[f"s1t{hc}"])
                P.dve(lambda e, hc=hc, eb_=eb_, hb_=hb_: e.tensor_tensor(out=actT[eb_][:, hc, :], in0=hb_[:, 128:256],
                                                                         in1=s1t4[:, hc, :], op=ALU.mult),
                      r=[htag, f"s1t{hc}"], w=[f"actT{eb_}"])
            ytag = ("m1", "junk")[eb_]
            for half in range(2):
                for hc in range(4):
                    mm(pS[half][:, :], actT[eb_][:, hc, :], w2v[:, hc, half * 512:(half + 1) * 512],
                       start=(hc == 0), stop=(hc == 3), r=[f"actT{eb_}", wt[2]], w=[f"pS{half}"])
                if half == 0:
                    P.act(lambda e, eb_=eb_, tb=tb: e.activation(out=yo[eb_][:, 0:512], in_=pS[0][:, :], func=AF.Copy,
                                                                 scale=tb[:, 2:3]),
                          r=["pS0", f"tabt{tb_}"], w=[ytag])
                else:
                    P.dve(lambda e, eb_=eb_, tb=tb: e.tensor_scalar(out=yo[eb_][:, 512:1024], in0=pS[1][:, :],
                                                                     scalar1=tb[:, 2:3], scalar2=None, op0=ALU.mult),
                          r=["pS1", f"tabt{tb_}"], w=[ytag])
            P.op("pool", lambda e, eb_=eb_, ix=ix: e.indirect_dma_start(
                out=MO, out_offset=bass.IndirectOffsetOnAxis(ap=ix[:, 1:2], axis=0), in_=yo[eb_][:], in_offset=None),
                [ytag, f"idxi{tb_}"], None, dma=True, wacc=["MO"])

        stage_tok(0)
        stage_w(0)
        stage_cast(0)
        if nblk > 1:
            stage_tok(1)
            stage_w(1)
        for b in range(nblk):
            stage_b1(b)
            if b + 1 < nblk:
                stage_cast(b + 1)
            if b + 2 < nblk:
                stage_tok(b + 2)
                stage_w(b + 2)
            stage_b(b)

    for l in range(n_layers):
        layer_start(l)
        if DBG.get("stage") == "A":
            for r_ in range(6):
                P.dma("sp", out[r_ * 128:(r_ + 1) * 128, :], rows[:, r_, :], r=[f"rows{r_}"], w=[f"out{r_}"])
            break
        if l % 2 == 0:
            even_layer(l)
        else:
            odd_layer(l)
        if not DBG.get("noexp"):
            route_finalize(l)
            expert_phase(l)
    if DBG.get("nofinal"):
        P.emit()
        es.close()
        return nc
    P.pool(lambda e: e.tensor_copy(out=rows[:, 6, :], in_=rows[:, 5, :]), r=["rows5"], w=["rows6"])
    for t in range(NT):
        xb = t % 2
        load_x(1, t, xb)
        P.dma("sp", out[t * 128:(t + 1) * 128, :], xt[xb][:], r=["xt0"], w=[f"out{t}"])
    P.emit()
    es.close()
    return nc


def _rel_bucket_np(d):
    d = np.maximum(d, 0)
    logd = np.log(np.maximum(d, 1).astype(np.float32) / 16) / math.log(128 / 16)
    far = np.minimum(16 + (logd * 16).astype(np.int32), 31)
    return np.where(d < 16, d, far)


def _constants():
    k = np.arange(128)[:, None]
    q = np.arange(128)[None, :]
    c = {}
    c["ident"] = np.eye(128, dtype=np.float32)
    same = (k // 64) == (q // 64)
    c["tri"] = (same & (k <= q)).astype(np.float32)
    c["bones"] = same.astype(np.float32)
    c["cind"] = (np.arange(128)[:, None] // 64 == np.arange(2)[None, :]).astype(np.float32)
    c["stri"] = (k < q).astype(np.float32)
    mask0 = np.zeros((128, 256), np.float32)
    mask0[:, 0:128] = np.where(q >= k, 0.0, NEG)
    mask0[:, 128:256] = np.where(q < k, 0.0, NEG)
    c["mask0"] = mask0
    r = np.arange(TABROWS)
    tab = np.zeros((TABROWS, 4), np.float32)
    tab[:, 1] = S + (r % 128)
    c["tabinit"] = tab
    rc_ = np.zeros((128, 172), np.float32)
    rc_[:, 0:64] = (np.arange(64) * 128)[None, :]
    rc_[:, 64:160] = (np.arange(96) * 128)[None, :]
    rc_[:, 160:168] = np.arange(8)[None, :] * 128 + np.arange(128)[:, None]
    rc_[:, 168:172] = np.arange(4)[None, :] * 128 + np.arange(128)[:, None]
    c["rcst"] = rc_
    pid = np.zeros((128, 2), np.float32)
    pid[:, 0] = np.arange(128)
    pid[:, 1] = 32 * CAP + np.arange(128)
    c["pidx"] = pid
    d0 = q - k
    d1 = 128 + q - k
    c["_b0"] = _rel_bucket_np(d0)
    c["_b1"] = _rel_bucket_np(d1)
    return c


_CACHE = {}


def kernel(x, c, rel_bias, ada_w, ada_b, norm1_w, norm2_w, even_w_in, even_w_out, a_q_norm, a_k_norm, a_sinks,
           b_gate_up, b_gate_bias, b_out_norm, odd_w_in, odd_w_out, c_q_norm, c_k_norm, moe_w_group, moe_b_group,
           moe_w_expert, moe_b_expert, moe_w1, moe_w3, moe_w2, _n_layers=4, _cores=None, _dbg=None):
    f = lambda a: np.ascontiguousarray(np.asarray(a, dtype=np.float32))
    cst = _constants()
    rel_bias = f(rel_bias)
    relT = np.empty((8, 128, 256), np.float32)
    relT[:, :, 0:128] = np.transpose(rel_bias[cst["_b0"]], (2, 0, 1))
    relT[:, :, 128:256] = np.transpose(rel_bias[cst["_b1"]], (2, 0, 1))
    shared = {
        "rel_bias": rel_bias, "relT": relT, "ada_w": f(ada_w[:_n_layers]), "ada_b": f(ada_b), "norm1_w": f(norm1_w),
        "norm2_w": f(norm2_w), "even_w_in": f(even_w_in), "even_w_out": f(even_w_out), "a_q_norm": f(a_q_norm),
        "a_k_norm": f(a_k_norm), "a_sinks": f(a_sinks), "b_gate_up": f(b_gate_up), "b_gate_bias": f(b_gate_bias),
        "b_out_norm": f(b_out_norm), "odd_w_in": f(odd_w_in), "odd_w_out": f(odd_w_out), "c_q_norm": f(c_q_norm),
        "c_k_norm": f(c_k_norm),
        "moe_wr": np.ascontiguousarray(np.concatenate([f(moe_w_group), f(moe_w_expert)], axis=-1)),
        "moe_br": np.ascontiguousarray(np.concatenate([f(moe_b_group), f(moe_b_expert)], axis=-1)),
        "moe_w1": f(moe_w1[:_n_layers]), "moe_w3": f(moe_w3[:_n_layers]), "moe_w2": f(moe_w2[:_n_layers]),
    }
    for k_ in ("ident", "tri", "bones", "cind", "stri", "mask0", "tabinit", "rcst", "pidx"):
        shared[k_] = cst[k_]
    x = f(x)
    c = f(c)
    cores = list(range(8)) if _cores is None else _cores
    in_maps = []
    for b in cores:
        m = dict(shared)
        m["x"] = x[b]
        m["cT"] = np.ascontiguousarray(c[b].reshape(8, 128).T)
        in_maps.append(m)
    key = (_n_layers, str(_dbg))
    if key not in _CACHE:
        _CACHE[key] = build_program(_n_layers, _dbg)
    nc = _CACHE[key]
    res = run_bass_kernel_spmd(nc, in_maps, core_ids=list(range(len(cores))))
    outs = [r["out"] for r in res.results]
    return np.stack(outs, axis=0).astype(np.float32)
```

```python
import math
from contextlib import ExitStack

import numpy as np
import concourse.bass as bass
import concourse.mybir as mybir
from concourse.bass_utils import run_bass_kernel_spmd

F32 = mybir.dt.float32
BF16 = mybir.dt.bfloat16
I32 = mybir.dt.int32
AF = mybir.ActivationFunctionType
ALU = mybir.AluOpType
AX = mybir.AxisListType

S = 4096
D = 1024
NT = 32
CAP = 384
NB = CAP // 128
NEG = -30000.0
BIG = 10000.0
MOH = S + 128
NBLK = 96
TABROWS = NBLK * 128
EPS = 1e-6

COMPUTE = ("pe", "act", "dve", "pool")
DMAQ_K = 12
SEM_EPOCH = 6000
DMA_EPOCH = 400


class Buf:
    __slots__ = ("name", "w", "r")

    def __init__(self, name):
        self.name = name
        self.w = []
        self.r = []


class Op:
    __slots__ = ("eng", "fn", "seq", "dma", "deps", "signal", "sig_idx", "dma_idx")

    def __init__(self, eng, fn, dma):
        self.eng = eng
        self.fn = fn
        self.dma = dma
        self.deps = []
        self.signal = False
        self.sig_idx = None
        self.dma_idx = None


def _compress(lst):
    keep = []
    cnt = {}
    for x in reversed(lst):
        key = (x.eng, x.dma)
        c = cnt.get(key, 0)
        lim = DMAQ_K if x.dma else 1
        if c < lim:
            keep.append(x)
            cnt[key] = c + 1
    keep.reverse()
    return keep


class _Rec:
    def __init__(self):
        self.calls = []

    def __getattr__(self, name):
        def f(*a, **k):
            self.calls.append((name, a, k))
            return self
        return f


class _LazyReg:
    def __init__(self, v):
        self.v = v


_REGCACHE = {}


def _freeze(fn):
    rec = _Rec()
    fn(rec)
    assert len(rec.calls) == 1, rec.calls
    name, a, k = rec.calls[0]
    lazy = [kk for kk, vv in k.items() if isinstance(vv, _LazyReg)]
    if not lazy:
        return lambda e: getattr(e, name)(*a, **k)

    def replay(e):
        k2 = dict(k)
        for kk in lazy:
            key = (id(e), k[kk].v)
            if key not in _REGCACHE:
                _REGCACHE[key] = e.to_reg(k[kk].v)
            k2[kk] = _REGCACHE[key]
        return getattr(e, name)(*a, **k2)
    return replay


class Prog:
    def __init__(self, nc):
        self.nc = nc
        self.ops = {e: [] for e in ("pe", "act", "dve", "pool", "sp")}
        self.ndma = {e: 0 for e in self.ops}
        self.known = {e: {f: -1 for f in self.ops} for e in self.ops}
        self.kd_upto = {e: {f: -1 for f in self.ops} for e in self.ops}
        self.kd_set = {e: {f: set() for f in self.ops} for e in self.ops}
        self.bufs = {}

    def buf(self, name):
        b = self.bufs.get(name)
        if b is None:
            b = Buf(name)
            self.bufs[name] = b
        return b

    def _norm(self, lst):
        out = []
        for x in lst or []:
            if isinstance(x, (list, tuple)):
                out.extend(self._norm(x))
            elif isinstance(x, str):
                out.append(self.buf(x))
            else:
                out.append(x)
        return out

    def _satisfied(self, E, d):
        if d.dma:
            return d.dma_idx <= self.kd_upto[E][d.eng] or d.dma_idx in self.kd_set[E][d.eng]
        return d.seq <= self.known[E][d.eng]

    def _learn(self, E, d):
        if d.dma:
            F = d.eng
            s = self.kd_set[E][F]
            s.add(d.dma_idx)
            u = max(self.kd_upto[E][F], d.dma_idx - DMAQ_K)
            while (u + 1) in s:
                u += 1
            self.kd_upto[E][F] = u
            if len(s) > 64:
                self.kd_set[E][F] = {x for x in s if x > u}
        elif d.seq > self.known[E][d.eng]:
            self.known[E][d.eng] = d.seq

    def op(self, eng, fn, reads=None, writes=None, dma=False, wacc=None):
        reads = self._norm(reads)
        writes = self._norm(writes)
        wacc = self._norm(wacc)
        psr = [b for b in reads if b.name[:2] in ("pT", "pM", "pS", "pO")]
        if psr:
            reads = [b for b in reads if b not in psr]
            writes = writes + [b for b in psr if b not in writes]
        if fn is not None:
            fn = _freeze(fn)
        o = Op(eng, fn, dma)
        o.seq = len(self.ops[eng])
        if dma:
            o.dma_idx = self.ndma[eng]
            self.ndma[eng] += 1
        raw = set()
        cand = []
        for b in reads:
            for d in b.w:
                raw.add(id(d))
                cand.append(d)
        for b in writes:
            cand.extend(b.w)
            cand.extend(b.r)
        for b in wacc:
            cand.extend(b.r)
        seen = set()
        uniq = []
        for d in cand:
            if id(d) not in seen:
                seen.add(id(d))
                uniq.append(d)
        uniq.sort(key=lambda d: -(d.dma_idx if d.dma else d.seq))
        for d in uniq:
            if (not d.dma) and (not dma) and d.eng == eng and id(d) not in raw:
                continue
            if self._satisfied(eng, d):
                continue
            o.deps.append(d)
            d.signal = True
            self._learn(eng, d)
        for b in writes:
            b.w = [o]
            b.r = []
        for b in wacc:
            b.w.append(o)
            if len(b.w) > 40:
                b.w = _compress(b.w)
        for b in reads:
            if fn is None:
                break
            if not dma:
                b.r = [x for x in b.r if x.dma or x.eng != eng]
            b.r.append(o)
            if len(b.r) > 40:
                b.r = _compress(b.r)
        self.ops[eng].append(o)
        return o

    def pe(self, fn, r=None, w=None):
        return self.op("pe", fn, r, w)

    def act(self, fn, r=None, w=None):
        return self.op("act", fn, r, w)

    def dve(self, fn, r=None, w=None):
        return self.op("dve", fn, r, w)

    def pool(self, fn, r=None, w=None):
        return self.op("pool", fn, r, w)

    def barrier(self, fn):
        allb = list(self.bufs.values())
        self.op("dve", fn, allb, allb + [self.buf("FENCE")])
        for e in ("pe", "act", "pool", "sp"):
            self.op(e, None, [self.buf("FENCE")], None)

    def dma(self, eng, out, in_, r=None, w=None, wacc=None):
        return self.op(eng, lambda e: e.dma_start(out=out, in_=in_), r, w, dma=True, wacc=wacc)

    def emit(self):
        nc = self.nc
        nsig = {}
        for e in COMPUTE:
            c = 0
            for o in self.ops[e]:
                if o.signal and not o.dma:
                    o.sig_idx = c
                    c += 1
            nsig[e] = c
        es = ExitStack()
        sems = {}
        for e in COMPUTE:
            n_ep = max(1, (nsig[e] + SEM_EPOCH - 1) // SEM_EPOCH)
            sems[e] = [es.enter_context(nc.semaphore(f"tl_{e}_{i}")) for i in range(n_ep)]
        dsems = {}
        per_ep = DMA_EPOCH * DMAQ_K
        for e in self.ops:
            if self.ndma[e] == 0:
                continue
            n_ep = (self.ndma[e] + per_ep - 1) // per_ep
            dsems[e] = [[es.enter_context(nc.semaphore(f"dq_{e}_{i}_{k}")) for k in range(DMAQ_K)]
                        for i in range(n_ep)]

        def dma_sem(e, idx):
            return dsems[e][idx // per_ep][idx % DMAQ_K], 16 * ((idx % per_ep) // DMAQ_K + 1)

        def wait_for(engobj, d):
            if d.dma:
                s, v = dma_sem(d.eng, d.dma_idx)
            else:
                s = sems[d.eng][d.sig_idx // SEM_EPOCH]
                v = d.sig_idx % SEM_EPOCH + 1
            engobj.wait_ge(s, v)

        prog = self

        def run_engine(ename, engobj):
            for o in prog.ops[ename]:
                for d in o.deps:
                    wait_for(engobj, d)
                if o.dma:
                    if o.dma_idx >= DMAQ_K:
                        s, v = dma_sem(ename, o.dma_idx - DMAQ_K)
                        engobj.wait_ge(s, v)
                    inst = o.fn(engobj)
                    s, v = dma_sem(ename, o.dma_idx)
                    inst.then_inc(s, 16)
                elif o.fn is not None:
                    inst = o.fn(engobj)
                    if o.signal:
                        inst.then_inc(sems[ename][o.sig_idx // SEM_EPOCH], 1)
            n = prog.ndma[ename]
            for idx in range(max(0, n - DMAQ_K), n):
                s, v = dma_sem(ename, idx)
                engobj.wait_ge(s, v)

        with nc.Block() as block:
            @block.tensor
            def _(e):
                run_engine("pe", e)

            @block.scalar
            def _(e):
                run_engine("act", e)

            @block.vector
            def _(e):
                run_engine("dve", e)

            @block.gpsimd
            def _(e):
                run_engine("pool", e)

            @block.sync
            def _(e):
                run_engine("sp", e)
        es.close()


def build_program(n_layers=4, dbg=None):
    DBG = dbg or {}
    _REGCACHE.clear()
    nc = bass.Bass("TRN2", target_bir_lowering=False)
    P = Prog(nc)
    es = ExitStack()

    def din(name, shape, dt=F32):
        return nc.dram_tensor(name, list(shape), dt, kind="ExternalInput").ap()

    def dscr(name, shape, dt=F32):
        return nc.dram_tensor(name, list(shape), dt, kind="Internal").ap()

    def sb(name, shape, dt=F32):
        return es.enter_context(nc.sbuf_tensor("s_" + name, list(shape), dt))

    def ps(name, shape, dt=F32):
        return es.enter_context(nc.psum_tensor("p_" + name, list(shape), dt))

    x_in = din("x", [S, D])
    cT_in = din("cT", [128, 8])
    rel_bias = din("rel_bias", [32, 8])
    relT_in = din("relT", [8, 128, 256])
    mask0_in = din("mask0", [128, 256])
    ident_in = din("ident", [128, 128])
    tri_in = din("tri", [128, 128])
    bones_in = din("bones", [128, 128])
    cind_in = din("cind", [128, 2])
    stri_in = din("stri", [128, 128])
    tabinit_in = din("tabinit", [TABROWS, 4])
    rcst_in = din("rcst", [128, 172])
    pidx_in = din("pidx", [128, 2])
    NLW = n_layers
    ada_w = din("ada_w", [NLW, D, 6 * D])
    ada_b = din("ada_b", [4, 6 * D])
    norm1_w = din("norm1_w", [4, D])
    norm2_w = din("norm2_w", [4, D])
    even_w_in = din("even_w_in", [2, D, 2320])
    even_w_out = din("even_w_out", [2, D, D])
    a_q_norm = din("a_q_norm", [2, 64])
    a_k_norm = din("a_k_norm", [2, 64])
    a_sinks = din("a_sinks", [2, 8])
    b_gate_up = din("b_gate_up", [2, 16, 256])
    b_gate_bias = din("b_gate_bias", [2, 256])
    b_out_norm = din("b_out_norm", [2, 128])
    odd_w_in = din("odd_w_in", [2, D, 3072])
    odd_w_out = din("odd_w_out", [2, D, D])
    c_q_norm = din("c_q_norm", [2, 128])
    c_k_norm = din("c_k_norm", [2, 128])
    moe_wr = din("moe_wr", [4, D, 36])
    moe_br = din("moe_br", [4, 36])
    moe_w1 = din("moe_w1", [NLW, 32, D, 512])
    moe_w3 = din("moe_w3", [NLW, 32, D, 512])
    moe_w2 = din("moe_w2", [NLW, 32, 512, D])
    out = nc.dram_tensor("out", [S, D], F32, kind="ExternalOutput").ap()

    XR = dscr("XR", [S, D])
    H2 = dscr("H2", [S, D], BF16)
    TAB = dscr("TAB", [TABROWS, 4])
    MO = dscr("MO", [2 * MOH, D])
    ATT = dscr("ATT", [S, D], BF16)
    QT = dscr("QT", [8, 128, S], BF16)
    KT = dscr("KT", [8, 128, S], BF16)
    VA = dscr("VA", [S, 8, 129], BF16)

    R1 = sb("R1", [128, 24576], BF16)
    R2 = sb("R2", [128, 8448], F32)
    WOUT = sb("WOUT", [128, 8, 1024], BF16)
    rows = sb("rows", [128, 7, 1024])
    BS = sb("BS", [128, 8, 256])
    BM1 = sb("BM1", [128, 8, 128])
    relfar = sb("relfar", [128, 8])
    identf = sb("identf", [128, 128])
    identb = sb("identb", [128, 128], BF16)
    trif = sb("trif", [128, 128])
    bonesf = sb("bonesf", [128, 128])
    cindf = sb("cindf", [128, 2])
    strib = sb("strib", [128, 128], BF16)
    onesb = sb("onesb", [128, 128], BF16)
    bonesb = sb("bonesb", [128, 128], BF16)
    rcst = sb("rcst", [128, 172])
    pidx = sb("pidx", [128, 2])
    cTs = sb("cTs", [128, 8])
    xt0_ = sb("xt0", [128, D])
    xt = [xt0_, xt0_]
    m1 = sb("m1", [128, D])
    junk = sb("junk", [128, D])
    m2 = junk
    condbc = junk[:, :].rearrange("p (k n) -> p k n", k=8)
    tmpf = sb("tmpf", [128, D])
    hb = sb("hb", [128, D], BF16)
    hT = sb("hT", [128, 8, 128], BF16)
    proj = sb("proj", [128, 2320])
    att = sb("att", [128, D], BF16)
    h2b = hb
    sm = sb("sm", [128, 64])
    wrow = sb("wrow", [128, 640])
    WR = sb("WR", [128, 8, 36], BF16)
    brow = sb("brow", [128, 36])
    adab = m1[:, 0:512]
    EV = sb("EV", [128, 7888])

    class Carver:
        def __init__(self):
            self.off = 0

        def get(self, shape, dt=F32):
            np_ = shape[0]
            n = 1
            for d_ in shape[1:]:
                n *= d_
            sz = 2 if dt == BF16 else 4
            nb = (n * sz + 3) // 4 * 4
            c0 = self.off // 4
            self.off += nb
            assert self.off <= 7888 * 4, self.off
            v = EV[0:np_, c0:c0 + nb // 4]
            if dt != F32:
                v = v.bitcast(dt)[:, 0:n]
            if len(shape) == 3:
                v = v.rearrange("p (a b) -> p a b", a=shape[1])
            elif len(shape) == 4:
                v = v.rearrange("p (a b c) -> p a b c", a=shape[1], b=shape[2])
            return v

    cv = Carver()
    qkb = cv.get([128, 512], BF16)
    kdup = cv.get([128, 2, 128], BF16)
    qkT = [cv.get([128, 6, 128], BF16) for i in range(2)]
    vaS = [cv.get([128, 2, 65], BF16) for i in range(2)]
    sc = cv.get([128, 256])
    pexp = [cv.get([128, 256], BF16) for i in range(2)]
    abT = cv.get([32, 128], BF16)
    abp = cv.get([128, 32], BF16)
    gnb = cv.get([128, 256], BF16)
    zt = cv.get([128, 256])
    gneg = cv.get([128, 256])
    bsb = zt
    eb = cv.get([128, 256])
    enb = cv.get([128, 256])
    ekd = cv.get([128, 256])
    qd = cv.get([128, 256], BF16)
    kd = cv.get([128, 256], BF16)
    ku = cv.get([128, 256], BF16)
    vbb = cv.get([128, 512], BF16)
    sgn = cv.get([128, 512])
    dec = cv.get([64, 8])
    qdT = cv.get([64, 4, 128], BF16)
    qdm = cv.get([64, 4, 2, 128], BF16)
    kdT = cv.get([64, 4, 128], BF16)
    attT = cv.get([128, 128], BF16)
    St0 = cv.get([64, 4, 128])
    St1 = cv.get([64, 4, 128])
    St0b = cv.get([64, 4, 128], BF16)
    St1b = cv.get([64, 4, 128], BF16)
    gbias = cv.get([128, 256])
    onorm1 = cv.get([128, 128])
    gupf = cv.get([32, 256])
    gupb = cv.get([32, 256], BF16)
    esink = cv.get([128, 8])
    ev_even = cv.off
    cv = Carver()
    qnb = cv.get([128, 2048], BF16)
    vast = cv.get([128, 8, 129], BF16)
    kmsum = cv.get([128, 8, 32])
    kmean = cv.get([128, 8, 16], BF16)
    qTt = cv.get([128, 2, 256], BF16)
    scs = cv.get([128, 2, 16])
    sel = cv.get([128, 2, 2, 16])
    pT2 = [cv.get([128, 256], BF16) for i in range(4)]
    acc = cv.get([128, 2, 2, 129])
    attq = cv.get([128, 2, 256], BF16)
    sco_ = [cv.get([128, 128]) for i in range(2)]
    cv = Carver()
    xg = [cv.get([128, D], BF16) for i in range(3)]
    xgT = [cv.get([128, 8, 128], BF16) for i in range(2)]
    s1t4 = cv.get([128, 4, 128])
    actT = [cv.get([128, 4, 128], BF16) for i in range(2)]
    rtab = cv.get([128, 6, 32])
    blkE = cv.get([128, 96])
    widx1f = cv.get([128, 8, 96])
    widx1i = cv.get([128, 8, 96], I32)
    widx2f = cv.get([128, 4, 96])
    widx2i = cv.get([128, 4, 96], I32)
    M12all = R2[:, 0:2048].rearrange("p (t k e) -> p t k e", t=32, k=2)
    slall = R2[:, 2048:2112].rearrange("p (t k) -> p t k", t=32)
    recall = R2[:, 2112:2368].rearrange("p (t k c) -> p t k c", t=32, k=2)
    top8 = sb("top8", [128, 8])
    lg = sb("lg", [128, 36])
    msk = sb("msk", [128, 32])
    M12 = sb("M12", [128, 2, 32])
    Mb = sb("Mb", [128, 32], BF16)
    cbase = sb("cbase", [128, 32])
    Cc = sb("Cc", [128, 32])
    prod = sb("prod", [128, 2, 32])
    desti = [sb(f"desti{i}", [128, 2], I32) for i in range(2)]
    tabt = [cv.get([128, 4]) for i in range(3)]
    idxi = [cv.get([128, 2], I32) for i in range(3)]
    yo = [m1, junk]
    print("SBUF bytes remaining/partition:", nc.sbuf_bytes_remaining)

    pT = [ps(f"pT{i}", [128, 8, 128], BF16) for i in range(2)]
    pM = [ps(f"pM{i}", [128, 512]) for i in range(2)]
    pS = [ps(f"pS{i}", [128, 512]) for i in range(2)]
    pO = [ps(f"pO{i}", [128, 512]) for i in range(2)]

    R1w = R1[:, :].rearrange("p (k n) -> p k n", k=8)

    def ew(eb_, which):
        base = eb_ * 12288
        if which == 0:
            return R1[:, base:base + 4096].rearrange("p (k n) -> p k n", k=8)
        if which == 1:
            return R1[:, base + 4096:base + 8192].rearrange("p (k n) -> p k n", k=8)
        return R1[:, base + 8192:base + 12288].rearrange("p (k n) -> p k n", k=4)

    def stg(i):
        return R2[:, i * 4096:(i + 1) * 4096]

    WINT = [f"R1_{a}_{b}" for a in range(2) for b in range(3)]

    def bc(ap):
        return ap.partition_broadcast(128).squeeze(1)

    def fence(tags, eng="dve"):
        P.op(eng, lambda e: e.memset(sm[:, 62:63], 0.0), tags, tags + ["sm62"])

    R2b = R2[:, :].bitcast(BF16)
    KTs = R2b[:, 0:8192].rearrange("p (h t) -> p h t", h=2)
    VAs = R2b[:, 8192:8192 + 8256].rearrange("p (t h c) -> p t h c", t=32, h=2)
    VAs2 = R2b[:, 8192:8192 + 8256].rearrange("p (t c) -> p t c", t=32)

    def mm(out_, lhsT, rhs, start=True, stop=True, r=None, w=None):
        P.pe(lambda e: e.matmul(out_, lhsT=lhsT, rhs=rhs, start=start, stop=stop), r=r, w=w)

    def tp(out_, in_, ident, r=None, w=None):
        P.pe(lambda e: e.transpose(out=out_, in_=in_, identity=ident), r=r, w=w)

    P.dma("sp", identf[:], ident_in, w=["identf"])
    P.dma("sp", trif[:], tri_in, w=["trif"])
    P.dma("sp", bonesf[:], bones_in, w=["bonesf"])
    P.dma("sp", cindf[:], cind_in, w=["cindf"])
    P.dma("sp", rcst[:], rcst_in, w=["rcst"])
    P.dma("sp", pidx[:], pidx_in, w=["pidx"])
    P.dma("sp", cTs[:], cT_in, w=["cTs"])
    P.dma("sp", tmpf[:, 0:128], stri_in, w=["tmpf"])
    P.dve(lambda e: e.tensor_copy(out=strib[:], in_=tmpf[:, 0:128]), r=["tmpf"], w=["strib"])
    P.dve(lambda e: e.tensor_copy(out=identb[:], in_=identf[:]), r=["identf"], w=["identb"])
    P.dve(lambda e: e.memset(onesb[:], 1.0), w=["onesb"])
    P.dve(lambda e: e.tensor_copy(out=bonesb[:], in_=bonesf[:]), r=["bonesf"], w=["bonesb"])
    P.dma("sp", BS[:], relT_in.rearrange("h k q -> k h q"), w=["BS"])
    P.dma("sp", m1[:, 0:256], mask0_in, w=["m1"])
    P.dve(lambda e: e.tensor_copy(out=BM1[:], in_=BS[:, :, 128:256]), r=["BS"], w=["BM"])
    for h in range(8):
        P.dve(lambda e, h=h: e.tensor_tensor(out=BS[:, h, :], in0=BS[:, h, :], in1=m1[:, 0:256], op=ALU.add),
              r=["BS", "m1"], w=["BS"])
    P.dma("sp", relfar[:], bc(rel_bias[31:32, :]), w=["relfar"])
    P.act(lambda e: e.activation(out=cTs[:], in_=cTs[:], func=AF.Silu), r=["cTs"], w=["cTs"])

    def rstd_from_ssq(ssq_ap, n, inv_n, tag):
        P.dve(lambda e: e.tensor_scalar(out=ssq_ap, in0=ssq_ap, scalar1=inv_n, scalar2=EPS, op0=ALU.mult, op1=ALU.add),
              r=[tag], w=[tag])
        P.act(lambda e: e.activation(out=ssq_ap, in_=ssq_ap, func=AF.Sqrt), r=[tag], w=[tag])
        P.dve(lambda e: e.reciprocal(out=ssq_ap, in_=ssq_ap), r=[tag], w=[tag])

    def load_weight_bf16(dst_view, src_ap, nk, ncols, dst_tags, engs=("act", "dve", "pool")):
        srcv = src_ap.rearrange("(k p) n -> p k n", p=128)
        cpp = 4096 // nk
        c0 = 0
        i = 0
        while c0 < ncols:
            cw = min(cpp, ncols - c0)
            part = i % 2
            sv = stg(part)[:, 0:nk * cw].rearrange("p (k n) -> p k n", k=nk)
            P.dma("sp", sv, srcv[:, :, c0:c0 + cw], w=[f"R2_{part}"])
            en = engs[i % len(engs)]
            P.op(en, lambda e, sv=sv, c0=c0, cw=cw, en=en: (e.copy if en == "act" else e.tensor_copy)(
                out=dst_view[:, :, c0:c0 + cw], in_=sv),
                 [f"R2_{part}"], None, wacc=dst_tags)
            c0 += cw
            i += 1

    def layer_start(l):
        even = (l % 2 == 0)
        j = l // 2
        P.barrier(lambda e: e.memset(sm[:, 62:63], 0.0))
        if l > 0:
            P.pool(lambda e: e.tensor_copy(out=rows[:, 6, :], in_=rows[:, 5, :]), r=["rows5"], w=["rows6"])
        P.dve(lambda e: e.memset(tmpf[:, 0:128], 1.0), w=["tmpf"])
        for k in range(8):
            P.dve(lambda e, k=k: e.tensor_scalar(out=condbc[:, k, :], in0=tmpf[:, 0:128], scalar1=cTs[:, k:k + 1],
                                                 scalar2=None, op0=ALU.mult), r=["tmpf", "cTs"], w=["junk"])
        aw = ada_w[l].rearrange("(k p) n -> p k n", p=128)
        for cch in range(12):
            part = cch % 2
            sv = stg(part)[:, :].rearrange("p (k n) -> p k n", k=8)
            P.dma("sp", sv, aw[:, :, cch * 512:(cch + 1) * 512], w=[f"R2_{part}"])
            P.dma("sp", adab[:], bc(ada_b[l:l + 1, cch * 512:(cch + 1) * 512]), w=["m1"])
            pm = pM[cch % 2]
            for k in range(8):
                mm(pm[:, :], condbc[:, k, :], sv[:, k, :], start=(k == 0), stop=(k == 7),
                   r=["junk", f"R2_{part}"], w=[f"pM{cch % 2}"])
            rr = cch // 2
            hf = cch % 2
            P.dve(lambda e, pm=pm, rr=rr, hf=hf: e.tensor_tensor(out=rows[:, rr, hf * 512:(hf + 1) * 512], in0=pm[:, :],
                                                                  in1=adab[:], op=ALU.add),
                  r=[f"pM{cch % 2}", "m1"], w=[f"rows{rr}"])
        for rr, nw in ((1, norm1_w), (4, norm2_w)):
            P.dma("sp", tmpf[:], bc(nw[l:l + 1, :]), w=["tmpf"])
            P.dve(lambda e, rr=rr: e.scalar_tensor_tensor(out=rows[:, rr, :], in0=rows[:, rr, :], scalar=1.0, in1=tmpf[:],
                                                          op0=ALU.add, op1=ALU.mult),
                  r=[f"rows{rr}", "tmpf"], w=[f"rows{rr}"])
        P.dma("sp", tmpf[:, 0:288].rearrange("p (k n) -> p k n", k=8), moe_wr[l].rearrange("(k p) n -> p k n", p=128),
              w=["tmpf"])
        P.dve(lambda e: e.tensor_copy(out=WR[:], in_=tmpf[:, 0:288].rearrange("p (k n) -> p k n", k=8)),
              r=["tmpf"], w=["WR"])
        P.dma("sp", brow[:], bc(moe_br[l:l + 1, :]), w=["brow"])
        P.dve(lambda e: e.memset(cbase[:], 0.0), w=["cbase"])
        P.dma("sp", TAB, tabinit_in, w=["TAB"])
        win = even_w_in[j] if even else odd_w_in[j]
        ncols = 2320 if even else 3072
        wo = even_w_out[j] if even else odd_w_out[j]
        load_weight_bf16(R1w[:, :, 0:ncols], win, 8, ncols, WINT)
        load_weight_bf16(WOUT[:, :, :], wo, 8, 1024, ["WOUTt"])
        if even:
            P.dma("sp", tmpf[:, 0:64], bc(a_q_norm[j:j + 1, :]), w=["tmpf"])
            P.dma("sp", tmpf[:, 64:128], bc(a_k_norm[j:j + 1, :]), w=["tmpf2"])
            for i in range(10):
                src = tmpf[:, 0:64] if i < 8 else tmpf[:, 64:128]
                P.dve(lambda e, i=i, src=src: e.tensor_copy(out=wrow[:, i * 64:(i + 1) * 64], in_=src),
                      r=["tmpf", "tmpf2"], w=["wrow"])
            P.dma("sp", gbias[:], bc(b_gate_bias[j:j + 1, :]), w=["gbias"])
            P.dve(lambda e: e.memset(gupf[:, :], 0.0), w=["gupf"])
            P.dma("sp", gupf[0:16, :], b_gate_up[j], w=["gupf"])
            P.dve(lambda e: e.tensor_copy(out=gupb[:, :], in_=gupf[:, :]), r=["gupf"], w=["gupb"])
            P.dve(lambda e: e.memset(abp[:, :], 0.0), w=["abp"])
            P.dma("sp", onorm1[:, :], bc(b_out_norm[j:j + 1, :]), w=["onorm4"])
            P.dma("sp", esink[:], bc(a_sinks[j:j + 1, :]), w=["esink"])
            P.act(lambda e: e.activation(out=esink[:], in_=esink[:], func=AF.Exp), r=["esink"], w=["esink"])
            P.dve(lambda e: e.memset(St0[:], 0.0), w=["St0"])
            P.dve(lambda e: e.memset(qdm[:], 0.0), w=["qdm"])
            for i in range(2):
                P.dve(lambda e, i=i: e.memset(vaS[i][:], 1.0), w=[f"vaS{i}"])
        else:
            P.dma("sp", wrow[:, 0:128], bc(c_q_norm[j:j + 1, :]), w=["wrow"])
            P.dma("sp", wrow[:, 128:256], bc(c_k_norm[j:j + 1, :]), w=["wrow2"])
            P.dve(lambda e: e.memset(vast[:], 1.0), w=["vast"])

    def load_x(l, t, xb):
        xs = xt[xb]
        tg = "xt0"
        rs = slice(t * 128, (t + 1) * 128)
        if l == 0:
            P.dma("sp", xs[:], x_in[rs, :], w=[tg])
        else:
            P.dma("sp", xs[:], XR[rs, :], r=[f"XR{t}"], w=[tg])
            P.dma("sp", m1[:], MO[rs, :], r=["MO"], w=["m1"])
            P.dma("sp", m2[:], MO[MOH + t * 128:MOH + (t + 1) * 128, :], r=["MO"], w=["junk"])
            P.pool(lambda e: e.tensor_tensor(out=m1[:], in0=m1[:], in1=m2[:], op=ALU.add), r=["m1", "junk"], w=["m1"])
            P.pool(lambda e: e.tensor_tensor(out=m1[:], in0=m1[:], in1=rows[:, 6, :], op=ALU.mult),
                   r=["m1", "rows6"], w=["m1"])
            P.dve(lambda e: e.tensor_tensor(out=xs[:], in0=xs[:], in1=m1[:], op=ALU.add), r=[tg, "m1"], w=[tg])

    def norm_mod(xs, xtag, grow, srow, outb, otag):
        P.act(lambda e: e.activation(out=junk[:], in_=xs[:], func=AF.Square, accum_out=sm[:, 0:1]),
              r=[xtag], w=["junk", "sm0"])
        rstd_from_ssq(sm[:, 0:1], 1, 1.0 / D, "sm0")
        P.dve(lambda e: e.scalar_tensor_tensor(out=tmpf[:], in0=xs[:], scalar=sm[:, 0:1], in1=rows[:, grow, :],
                                               op0=ALU.mult, op1=ALU.mult),
              r=[xtag, "sm0", f"rows{grow}"], w=["tmpf"])
        P.pool(lambda e: e.tensor_tensor(out=outb[:], in0=tmpf[:], in1=rows[:, srow, :], op=ALU.add),
               r=["tmpf", f"rows{srow}"], w=[otag])

    tcount = [0]

    def transpose8(src, stag, dst, dtag):
        i = tcount[0] % 2
        tcount[0] += 1
        for k in range(8):
            tp(pT[i][:, k, :], src[:, k * 128:(k + 1) * 128], identb[:], r=[stag, "identb"], w=[f"pT{i}"])
        P.act(lambda e: e.copy(out=dst[:], in_=pT[i][:]), r=[f"pT{i}"], w=[dtag])

    mcount = [0]

    def project(srcT, stag, wview, wtag, ncols, dst, dtag):
        c0 = 0
        while c0 < ncols:
            cw = min(512, ncols - c0)
            i = mcount[0] % 2
            mcount[0] += 1
            for k in range(8):
                mm(pM[i][:, 0:cw], srcT[:, k, :], wview[:, k, c0:c0 + cw], start=(k == 0), stop=(k == 7),
                   r=[stag, wtag], w=[f"pM{i}"])
            if i == 0:
                P.act(lambda e, i=i, c0=c0, cw=cw: e.copy(out=dst[:, c0:c0 + cw], in_=pM[i][:, 0:cw]),
                      r=[f"pM{i}"], w=[dtag])
            else:
                P.dve(lambda e, i=i, c0=c0, cw=cw: e.tensor_copy(out=dst[:, c0:c0 + cw], in_=pM[i][:, 0:cw]),
                      r=[f"pM{i}"], w=[dtag])
            c0 += cw

    def tail(l, t, xb, attsrc, atag):
        xs = xt[xb]
        tg = "xt0"
        rs = slice(t * 128, (t + 1) * 128)
        transpose8(attsrc, atag, hT, "hT")
        for c in range(2):
            i = mcount[0] % 2
            mcount[0] += 1
            for k in range(8):
                mm(pM[i][:, :], hT[:, k, :], WOUT[:, k, c * 512:(c + 1) * 512], start=(k == 0), stop=(k == 7),
                   r=["hT", "WOUTt"], w=[f"pM{i}"])
            P.dve(lambda e, i=i, c=c: e.tensor_tensor(out=tmpf[:, c * 512:(c + 1) * 512], in0=pM[i][:, :],
                                                      in1=rows[:, 2, c * 512:(c + 1) * 512], op=ALU.mult),
                  r=[f"pM{i}", "rows2"], w=["tmpf"])
        P.pool(lambda e: e.tensor_tensor(out=xs[:], in0=xs[:], in1=tmpf[:], op=ALU.add), r=[tg, "tmpf"], w=[tg])
        P.dma("sp", XR[rs, :], xs[:], r=[tg], w=[f"XR{t}"])
        norm_mod(xs, tg, 4, 3, h2b, "hb")
        P.dma("sp", H2[rs, :], h2b[:], r=["hb"], wacc=["H2"])
        transpose8(h2b, "hb", hT, "hT")
        i = mcount[0] % 2
        mcount[0] += 1
        for k in range(8):
            mm(pM[i][:, 0:36], hT[:, k, :], WR[:, k, :], start=(k == 0), stop=(k == 7), r=["hT", "WR"], w=[f"pM{i}"])
        P.dve(lambda e: e.tensor_tensor(out=lg[:], in0=pM[i][:, 0:36], in1=brow[:], op=ALU.add),
              r=[f"pM{i}", "brow"], w=["lg"])
        RT = ["rt"]
        V = P.dve
        V(lambda e: e.tensor_reduce(out=sm[:, 8:9], in_=lg[:, 0:4], axis=AX.X, op=ALU.max), r=["lg"], w=RT)
        V(lambda e: e.tensor_scalar(out=sm[:, 12:16], in0=lg[:, 0:4], scalar1=sm[:, 8:9], scalar2=None, op0=ALU.subtract),
          r=["lg"] + RT, w=RT)
        P.act(lambda e: e.activation(out=sm[:, 16:20], in_=sm[:, 12:16], func=AF.Exp, accum_out=sm[:, 9:10]), r=RT, w=RT)
        V(lambda e: e.reciprocal(out=sm[:, 9:10], in_=sm[:, 9:10]), r=RT, w=RT)
        V(lambda e: e.tensor_scalar(out=sm[:, 20:24], in0=sm[:, 12:16], scalar1=0.0, scalar2=None, op0=ALU.is_ge),
          r=RT, w=RT)
        V(lambda e: e.tensor_scalar(out=sm[:, 20:24], in0=sm[:, 20:24], scalar1=BIG, scalar2=-BIG, op0=ALU.mult,
                                    op1=ALU.add), r=RT, w=RT)
        V(lambda e: e.tensor_tensor(out=msk[:, :].rearrange("p (g e) -> p g e", g=4),
                                    in0=lg[:, 4:36].rearrange("p (g e) -> p g e", g=4),
                                    in1=sm[:, 20:24].unsqueeze(2).to_broadcast([128, 4, 8]), op=ALU.add),
          r=["lg"] + RT, w=["msk"])
        V(lambda e: e.max(out=top8[:], in_=msk[:]), r=["msk"], w=["top8"])
        V(lambda e: e.tensor_scalar(out=M12[:, 0, :], in0=msk[:], scalar1=top8[:, 0:1], scalar2=None, op0=ALU.is_equal),
          r=["msk", "top8"], w=["M12"])
        V(lambda e: e.tensor_scalar(out=M12[:, 1, :], in0=msk[:], scalar1=top8[:, 1:2], scalar2=None, op0=ALU.is_equal),
          r=["msk", "top8"], w=["M12"])
        V(lambda e: e.tensor_tensor(out=sm[:, 10:11], in0=top8[:, 1:2], in1=top8[:, 0:1], op=ALU.subtract),
          r=["top8"], w=RT)
        P.act(lambda e: e.activation(out=sm[:, 10:11], in_=sm[:, 10:11], func=AF.Exp), r=RT, w=RT)
        V(lambda e: e.tensor_scalar(out=sm[:, 10:11], in0=sm[:, 10:11], scalar1=1.0, scalar2=None, op0=ALU.add), r=RT, w=RT)
        V(lambda e: e.reciprocal(out=sm[:, 10:11], in_=sm[:, 10:11]), r=RT, w=RT)
        rc = recall[:, t, :, :]
        rtag = "R2_0"
        V(lambda e: e.tensor_tensor(out=rc[:, 0, 2:3], in0=sm[:, 10:11], in1=sm[:, 9:10], op=ALU.mult), r=RT, w=[rtag])
        V(lambda e: e.tensor_tensor(out=rc[:, 1, 2:3], in0=sm[:, 9:10], in1=rc[:, 0, 2:3], op=ALU.subtract),
          r=RT + [rtag], w=[rtag])
        V(lambda e: e.tensor_scalar(out=rc[:, 0, 0:2], in0=pidx[:, 0:1].to_broadcast([128, 2]), scalar1=float(t * 128),
                                    scalar2=None, op0=ALU.add), r=["pidx"], w=[rtag])
        V(lambda e: e.tensor_scalar(out=rc[:, 1, 0:1], in0=pidx[:, 0:1], scalar1=float(t * 128), scalar2=None,
                                    op0=ALU.add), r=["pidx"], w=[rtag])
        V(lambda e: e.tensor_scalar(out=rc[:, 1, 1:2], in0=pidx[:, 0:1], scalar1=float(t * 128 + MOH), scalar2=None,
                                    op0=ALU.add), r=["pidx"], w=[rtag])
        V(lambda e: e.memset(rc[:, :, 3:4], 0.0), w=[rtag])
        V(lambda e: e.tensor_tensor(out=Mb[:], in0=M12[:, 0, :], in1=M12[:, 1, :], op=ALU.add), r=["M12"], w=["Mb"])
        V(lambda e: e.tensor_copy(out=M12all[:, t, :, :], in_=M12[:]), r=["M12"], w=[rtag])
        j_ = mcount[0] % 2
        mcount[0] += 1
        mm(pM[j_][:, 0:32], strib[:], Mb[:], r=["strib", "Mb"], w=[f"pM{j_}"])
        mm(pM[j_][:, 64:96], onesb[:], Mb[:], r=["onesb", "Mb"], w=[f"pM{j_}"])
        V(lambda e: e.tensor_tensor(out=Cc[:], in0=pM[j_][:, 0:32], in1=cbase[:], op=ALU.add),
          r=[f"pM{j_}", "cbase"], w=["Cc"])
        V(lambda e: e.tensor_tensor(out=cbase[:], in0=pM[j_][:, 64:96], in1=cbase[:], op=ALU.add),
          r=[f"pM{j_}", "cbase"], w=["cbase"])
        V(lambda e: e.tensor_tensor(out=prod[:], in0=M12[:], in1=Cc[:, :].unsqueeze(1).to_broadcast([128, 2, 32]),
                                    op=ALU.mult), r=["M12", "Cc"], w=["prod"])
        V(lambda e: e.tensor_reduce(out=slall[:, t, :], in_=prod[:], axis=AX.X, op=ALU.add), r=["prod"], w=[rtag])

    def route_finalize(l):
        P.barrier(lambda e: e.memset(sm[:, 62:63], 0.0))
        V = P.dve
        FT = ["rfin"]
        for half in range(2):
            es_ = slice(half * 16, (half + 1) * 16)
            t3 = tmpf[:, :].rearrange("p (a b) -> p a b", a=16)
            V(lambda e: e.tensor_tensor(out=t3, in0=cbase[:, es_].unsqueeze(2).to_broadcast([128, 16, 64]),
                                        in1=rcst[:, 0:64].unsqueeze(1).to_broadcast([128, 16, 64]), op=ALU.is_gt),
              r=["cbase", "rcst"], w=["tmpf"])
            V(lambda e: e.tensor_reduce(out=rtab[:, 0, es_], in_=t3, axis=AX.X, op=ALU.add), r=["tmpf"], w=FT)
        V(lambda e: e.tensor_scalar(out=rtab[:, 1, :], in0=rtab[:, 0, :], scalar1=128.0, scalar2=None, op0=ALU.mult),
          r=FT, w=FT)
        V(lambda e: e.memset(rtab[:, 5, :], 0.0), w=FT)
        V(lambda e: e.tensor_tensor_scan(out=rtab[:, 2, :], data0=rtab[:, 1, :], data1=rtab[:, 5, :], initial=0.0,
                                         op0=ALU.add, op1=ALU.add), r=FT, w=FT)
        V(lambda e: e.tensor_tensor(out=rtab[:, 3, :], in0=rtab[:, 2, :], in1=rtab[:, 1, :], op=ALU.subtract),
          r=FT, w=FT)
        V(lambda e: e.memset(blkE[:, :], 0.0), w=["blkE"])
        for ex in range(32):
            V(lambda e, ex=ex: e.scalar_tensor_tensor(out=blkE[:, :], in0=rcst[:, 64:160], scalar=rtab[:, 2, ex:ex + 1],
                                                      in1=blkE[:, :], op0=ALU.is_ge, op1=ALU.add),
              r=FT + ["rcst", "blkE"], w=["blkE"])
        V(lambda e: e.tensor_scalar(out=blkE[:, :], in0=blkE[:, :], scalar1=31.0, scalar2=float(32 * l), op0=ALU.min,
                                    op1=ALU.add), r=["blkE"], w=["blkE"])
        V(lambda e: e.tensor_scalar(out=widx1f[:, 0, :], in0=blkE[:, :], scalar1=128.0, scalar2=pidx[:, 0:1],
                                    op0=ALU.mult, op1=ALU.add), r=["blkE", "pidx"], w=["widx1f"])
        V(lambda e: e.memset(widx2f[:, 0, 0:1], 0.0), w=["widx2f"])
        V(lambda e: e.tensor_tensor(out=widx2f[:, 0, 1:96], in0=blkE[:, 1:96], in1=blkE[:, 0:95], op=ALU.is_equal),
          r=["blkE"], w=["widx2f"])
        V(lambda e: e.scalar_tensor_tensor(out=widx1f[:, 0, :], in0=widx2f[:, 0, :], scalar=268435456.0, in1=widx1f[:, 0, :],
                                           op0=ALU.mult, op1=ALU.add), r=["widx1f", "widx2f"], w=["widx1f"])
        V(lambda e: e.tensor_copy(out=widx1i[:, 0, :], in_=widx1f[:, 0, :]), r=["widx1f"], w=["widx1i"])
        for t in range(NT):
            rb_ = t % 2
            di = desti[rb_]
            dtag = f"desti{rb_}"
            V(lambda e, t=t: e.tensor_tensor(out=prod[:], in0=M12all[:, t, :, :],
                                             in1=rtab[:, 3, :].unsqueeze(1).to_broadcast([128, 2, 32]), op=ALU.mult),
              r=["R2_0"] + FT, w=["prod"])
            V(lambda e: e.tensor_reduce(out=sm[:, 26:28], in_=prod[:], axis=AX.X, op=ALU.add), r=["prod"], w=["rt"])
            V(lambda e, t=t: e.tensor_tensor(out=sm[:, 26:28], in0=sm[:, 26:28], in1=slall[:, t, :], op=ALU.add),
              r=["rt", "R2_0"], w=["rt"])
            V(lambda e, di=di: e.tensor_copy(out=di[:], in_=sm[:, 26:28]), r=["rt"], w=[dtag])
            for k in range(2):
                P.op("pool", lambda e, k=k, di=di, t=t: e.indirect_dma_start(
                    out=TAB, out_offset=bass.IndirectOffsetOnAxis(ap=di[:, k:k + 1], axis=0), in_=recall[:, t, k, :],
                    in_offset=None), ["R2_0", dtag], None, dma=True, wacc=["TAB"])

    def even_layer(l):
        for t in range(DBG.get("tiles", NT)):
            xb = t % 2
            cur = t % 2
            prv = 1 - cur
            load_x(l, t, xb)
            norm_mod(xt[xb], "xt0", 1, 0, hb, "hb")
            transpose8(hb, "hb", hT, "hT")
            project(hT, "hT", R1w, WINT, 2320, proj, "proj")
            if DBG.get("cut") == 1:
                P.dma("sp", out[t * 128:(t + 1) * 128, :], proj[:, 0:1024], r=["proj"], w=[f"out{t}"])
                continue
            P.dve(lambda e: e.tensor_tensor(out=tmpf[:, 0:640], in0=proj[:, 0:640], in1=proj[:, 0:640], op=ALU.mult),
                  r=["proj"], w=["tmpf"])
            P.dve(lambda e: e.tensor_reduce(out=sm[:, 32:42], in_=tmpf[:, 0:640].rearrange("p (g d) -> p g d", g=10),
                                            axis=AX.X, op=ALU.add), r=["tmpf"], w=["sm32"])
            rstd_from_ssq(sm[:, 32:42], 10, 1.0 / 64, "sm32")
            P.dve(lambda e: e.tensor_tensor(out=tmpf[:, 0:640].rearrange("p (g d) -> p g d", g=10),
                                            in0=proj[:, 0:640].rearrange("p (g d) -> p g d", g=10),
                                            in1=sm[:, 32:42].unsqueeze(2).to_broadcast([128, 10, 64]), op=ALU.mult),
                  r=["proj", "sm32"], w=["tmpf"])
            P.pool(lambda e: e.tensor_tensor(out=qkb[:], in0=tmpf[:, 0:512], in1=wrow[:, 0:512], op=ALU.mult),
                   r=["tmpf", "wrow"], w=["qkb"])
            for hf in range(2):
                P.pool(lambda e, hf=hf: e.tensor_tensor(out=kdup[:, :, hf * 64:(hf + 1) * 64],
                                                        in0=tmpf[:, 512:640].rearrange("p (g d) -> p g d", g=2),
                                                        in1=wrow[:, 512:640].rearrange("p (g d) -> p g d", g=2),
                                                        op=ALU.mult), r=["tmpf", "wrow"], w=["kdup"])
            P.act(lambda e: e.copy(out=vaS[cur][:, :, 0:64], in_=proj[:, 640:768].rearrange("p (g d) -> p g d", g=2)),
                  r=["proj"], w=[f"vaS{cur}"])
            i = tcount[0] % 2
            tcount[0] += 1
            for pr in range(4):
                tp(pT[i][:, pr, :], qkb[:, pr * 128:(pr + 1) * 128], identb[:], r=["qkb", "identb"], w=[f"pT{i}"])
            for g in range(2):
                tp(pT[i][:, 4 + g, :], kdup[:, g, :], identb[:], r=["kdup", "identb"], w=[f"pT{i}"])
            P.act(lambda e, i=i: e.copy(out=qkT[cur][:, :, :], in_=pT[i][:, 0:6, :]), r=[f"pT{i}"], w=[f"qkT{cur}"])
            if DBG.get("cut") == 2:
                P.dve(lambda e: e.tensor_copy(out=tmpf[:, 0:768], in_=qkT[cur][:, :, :].rearrange("p a b -> p (a b)")), r=[f"qkT{cur}"], w=["tmpf"])
                P.dma("sp", out[t * 128:(t + 1) * 128, 0:768], tmpf[:, 0:768], r=["tmpf"], w=[f"out{t}"])
                continue
            for hg in range(2):
                po = pO[hg]
                for hh in range(4):
                    h = hg * 4 + hh
                    g = h // 4
                    pr = h // 2
                    b0 = (h % 2) * 64
                    psx = pS[h % 2]
                    mm(psx[:, 0:128], qkT[cur][b0:b0 + 64, 4 + g, :], qkT[cur][b0:b0 + 64, pr, :],
                       r=[f"qkT{cur}"], w=[f"pS{h % 2}"])
                    if t > 0:
                        mm(psx[:, 128:256], qkT[prv][b0:b0 + 64, 4 + g, :], qkT[cur][b0:b0 + 64, pr, :],
                           r=[f"qkT{cur}", f"qkT{prv}"], w=[f"pS{h % 2}"])
                    P.dve(lambda e, psx=psx, h=h: e.scalar_tensor_tensor(out=sc[:], in0=psx[:, 0:256], scalar=0.125,
                                                                         in1=BS[:, h, :], op0=ALU.mult, op1=ALU.add),
                          r=[f"pS{h % 2}", "BS"], w=["sc"])
                    pe_ = pexp[h % 2]
                    P.act(lambda e, pe_=pe_: e.activation(out=pe_[:], in_=sc[:], func=AF.Exp), r=["sc"], w=[f"pexp{h % 2}"])
                    mm(po[:, hh * 65:(hh + 1) * 65], pe_[:, 0:128], vaS[cur][:, g, :], start=True, stop=(t == 0),
                       r=[f"pexp{h % 2}", f"vaS{cur}"], w=[f"pO{hg}"])
                    if t > 0:
                        mm(po[:, hh * 65:(hh + 1) * 65], pe_[:, 128:256], vaS[prv][:, g, :], start=False, stop=True,
                           r=[f"pexp{h % 2}", f"vaS{prv}"], w=[f"pO{hg}"])
                pov = po[:, 0:260].rearrange("p (h c) -> p h c", h=4)
                P.dve(lambda e, pov=pov, hg=hg: e.tensor_tensor(out=sm[:, 44:48].unsqueeze(2), in0=pov[:, :, 64:65],
                                                                in1=esink[:, hg * 4:(hg + 1) * 4].unsqueeze(2), op=ALU.add),
                      r=[f"pO{hg}", "esink"], w=["sm44"])
                P.dve(lambda e: e.reciprocal(out=sm[:, 44:48], in_=sm[:, 44:48]), r=["sm44"], w=["sm44"])
                P.dve(lambda e, pov=pov, hg=hg: e.tensor_tensor(
                    out=att[:, hg * 256:(hg + 1) * 256].rearrange("p (h d) -> p h d", h=4), in0=pov[:, :, 0:64],
                    in1=sm[:, 44:48].unsqueeze(2).to_broadcast([128, 4, 64]), op=ALU.mult),
                    r=[f"pO{hg}", "sm44"], w=["att"])
            if DBG.get("cut") == 31:
                P.dve(lambda e: e.tensor_copy(out=tmpf[:, 0:256], in_=sc[:, :]), r=["sc"], w=["tmpf"])
                P.dve(lambda e: e.tensor_copy(out=tmpf[:, 256:516], in_=pO[1][:, 0:260]), r=["pO1"], w=["tmpf"])
                P.dve(lambda e: e.tensor_copy(out=tmpf[:, 520:528], in_=esink[:, :]), r=["esink"], w=["tmpf"])
                P.dve(lambda e: e.tensor_copy(out=tmpf[:, 528:532], in_=sm[:, 44:48]), r=["sm44"], w=["tmpf"])
                P.dve(lambda e: e.tensor_copy(out=tmpf[:, 532:788], in_=pexp[1][:, :]), r=["pexp1"], w=["tmpf"])
                P.dve(lambda e: e.tensor_copy(out=tmpf[:, 788:918], in_=vaS[cur][:, :, :].rearrange("p a b -> p (a b)")), r=[f"vaS{cur}"], w=["tmpf"])
                P.dma("sp", out[t * 128:(t + 1) * 128, :], tmpf[:, :], r=["tmpf"], w=[f"out{t}"])
                continue
            if DBG.get("cut") == 3:
                P.dve(lambda e: e.tensor_copy(out=tmpf[:, 0:512], in_=att[:, 0:512]), r=["att"], w=["tmpf"])
                P.dma("sp", out[t * 128:(t + 1) * 128, 0:512], tmpf[:, 0:512], r=["tmpf"], w=[f"out{t}"])
                continue
            P.dve(lambda e: e.tensor_copy(out=abp[:, 0:16], in_=proj[:, 2304:2320]), r=["proj"], w=["abp"])
            i = tcount[0] % 2
            tcount[0] += 1
            tp(pT[i][0:32, 0, :], abp[:, :], identb[:], r=["abp", "identb"], w=[f"pT{i}"])
            P.act(lambda e: e.copy(out=abT[:, :], in_=pT[i][0:32, 0, :]), r=[f"pT{i}"], w=["abT"])
            mm(pS[1][:, 0:256], abT[:, :], gupb[:, :], r=["abT", "gupb"], w=["pS1"])
            P.dve(lambda e: e.tensor_tensor(out=zt[:], in0=pS[1][:, 0:256], in1=gbias[:], op=ALU.add),
                  r=["pS1", "gbias"], w=["zt"])
            if DBG.get("cut") == 41:
                P.dve(lambda e: e.tensor_copy(out=tmpf[:, 0:256], in_=zt[:, :]), r=["zt"], w=["tmpf"])
                P.dma("sp", out[t * 128:(t + 1) * 128, 0:256], tmpf[:, 0:256], r=["tmpf"], w=[f"out{t}"])
                continue
            P.act(lambda e: e.activation(out=zt[:], in_=zt[:], func=AF.Exp, scale=-1.0), r=["zt"], w=["zt"])
            P.dve(lambda e: e.tensor_scalar(out=zt[:], in0=zt[:], scalar1=1.0, scalar2=None, op0=ALU.add), r=["zt"], w=["zt"])
            P.act(lambda e: e.activation(out=zt[:], in_=zt[:], func=AF.Ln), r=["zt"], w=["zt"])
            P.dve(lambda e: e.tensor_scalar(out=gneg[:], in0=zt[:], scalar1=-1.0 / 16.0, scalar2=None, op0=ALU.mult),
                  r=["zt"], w=["gneg"])
            if DBG.get("cut") == 42:
                P.dve(lambda e: e.tensor_copy(out=tmpf[:, 0:256], in_=gneg[:, :]), r=["gneg"], w=["tmpf"])
                P.dma("sp", out[t * 128:(t + 1) * 128, 0:256], tmpf[:, 0:256], r=["tmpf"], w=[f"out{t}"])
                continue
            mm(pS[0][:, 0:256], trif[:], gneg[:], r=["trif", "gneg"], w=["pS0"])
            mm(pS[0][:, 256:512], bonesf[:], gneg[:], r=["bonesf", "gneg"], w=["pS0"])
            P.act(lambda e: e.copy(out=bsb[:], in_=pS[0][:, 0:256]), r=["pS0"], w=["zt"])
            P.act(lambda e: e.activation(out=eb[:], in_=pS[0][:, 0:256], func=AF.Exp), r=["pS0"], w=["eb"])
            P.act(lambda e: e.activation(out=enb[:], in_=pS[0][:, 0:256], func=AF.Exp, scale=-1.0), r=["pS0"], w=["enb"])
            P.dve(lambda e: e.tensor_tensor(out=ekd[:], in0=pS[0][:, 256:512], in1=bsb[:], op=ALU.subtract),
                  r=["pS0", "zt"], w=["ekd"])
            P.act(lambda e: e.activation(out=ekd[:], in_=ekd[:], func=AF.Exp), r=["ekd"], w=["ekd"])
            if DBG.get("cut") == 43:
                P.dve(lambda e: e.tensor_copy(out=tmpf[:, 0:256], in_=ekd[:, :]), r=["ekd"], w=["tmpf"])
                P.dma("sp", out[t * 128:(t + 1) * 128, 0:256], tmpf[:, 0:256], r=["tmpf"], w=[f"out{t}"])
                continue
            P.dve(lambda e: e.scalar_tensor_tensor(out=qd[:], in0=proj[:, 768:1024], scalar=0.125, in1=eb[:],
                                                   op0=ALU.mult, op1=ALU.mult), r=["proj", "eb"], w=["qd"])
            P.pool(lambda e: e.tensor_tensor(out=kd[:], in0=proj[:, 1024:1280], in1=enb[:], op=ALU.mult),
                   r=["proj", "enb"], w=["kd"])
            P.pool(lambda e: e.tensor_tensor(out=ku[:], in0=proj[:, 1024:1280], in1=ekd[:], op=ALU.mult),
                   r=["proj", "ekd"], w=["ku"])
            P.pool(lambda e: e.tensor_copy(out=vbb[:], in_=proj[:, 1280:1792]), r=["proj"], w=["vbb"])
            P.act(lambda e: e.activation(out=sgn[:], in_=proj[:, 1792:2304], func=AF.Silu), r=["proj"], w=["sgn"])
            P.dve(lambda e: e.tensor_tensor(out=sgn[:, :].rearrange("p (h d) -> p h d", h=4),
                                             in0=sgn[:, :].rearrange("p (h d) -> p h d", h=4),
                                             in1=onorm1[:, :].unsqueeze(1).to_broadcast([128, 4, 128]), op=ALU.mult),
                   r=["sgn", "onorm4"], w=["sgn"])
            P.dve(lambda e: e.tensor_copy(out=gnb[:, :], in_=gneg[:, :]), r=["gneg"], w=["gnb"])
            for hh in range(4):
                mm(pS[1][0:64, hh * 128:(hh + 1) * 128], gnb[:, hh * 64:(hh + 1) * 64], bonesb[:, :], r=["gnb", "bonesb"], w=["pS1"])
            P.act(lambda e: e.activation(out=dec[:, :].rearrange("p (h c) -> p h c", h=4),
                                         in_=pS[1][0:64, :].rearrange("p (h c r) -> p h c r", h=4, c=2)[:, :, :, 0],
                                         func=AF.Exp), r=["pS1"], w=["dec"])
            if DBG.get("cut") == 4:
                P.dve(lambda e: e.tensor_copy(out=tmpf[:, 0:256], in_=qd[:, :]), r=["qd"], w=["tmpf"])
                P.dve(lambda e: e.tensor_copy(out=tmpf[:, 256:512], in_=ku[:, :]), r=["ku"], w=["tmpf"])
                P.dve(lambda e: e.tensor_copy(out=tmpf[0:64, 512:520], in_=dec[:, :]), r=["dec"], w=["tmpf"])
                P.dma("sp", out[t * 128:(t + 1) * 128, 0:520], tmpf[:, 0:520], r=["tmpf"], w=[f"out{t}"])
                continue
            i = tcount[0] % 2
            tcount[0] += 1
            for hh in range(4):
                tp(pT[i][0:64, hh, :], qd[:, hh * 64:(hh + 1) * 64], identb[:], r=["qd", "identb"], w=[f"pT{i}"])
                tp(pT[i][0:64, 4 + hh, :], kd[:, hh * 64:(hh + 1) * 64], identb[:], r=["kd", "identb"], w=[f"pT{i}"])
            P.act(lambda e, i=i: e.copy(out=qdT[:], in_=pT[i][0:64, 0:4, :]), r=[f"pT{i}"], w=["qdT"])
            P.act(lambda e, i=i: e.copy(out=kdT[:], in_=pT[i][0:64, 4:8, :]), r=[f"pT{i}"], w=["kdT"])
            P.dve(lambda e, i=i: e.tensor_copy(out=qdm[:, :, 0, 0:64], in_=pT[i][0:64, 0:4, 0:64]), r=[f"pT{i}"], w=["qdm"])
            P.dve(lambda e, i=i: e.tensor_copy(out=qdm[:, :, 1, 64:128], in_=pT[i][0:64, 0:4, 64:128]),
                  r=[f"pT{i}"], w=["qdm"])
            P.act(lambda e: e.copy(out=St0b[:], in_=St0[:]), r=["St0"], w=["St0b"])
            for hh in range(4):
                vs = slice(hh * 128, (hh + 1) * 128)
                ks = slice(hh * 64, (hh + 1) * 64)
                pa = pS[hh % 2]
                mm(pa[:, 0:128], kdT[:, hh, :], qdT[:, hh, :], r=["kdT", "qdT"], w=[f"pS{hh % 2}"])
                P.dve(lambda e, pa=pa: e.tensor_tensor(out=attT[:], in0=pa[:, 0:128], in1=trif[:], op=ALU.mult),
                      r=[f"pS{hh % 2}", "trif"], w=["attT"])
                mm(pO[0][0:64, vs], ku[0:64, ks], vbb[0:64, vs], r=["ku", "vbb"], w=["pO0"])
                mm(pO[1][0:64, vs], ku[64:128, ks], vbb[64:128, vs], r=["ku", "vbb"], w=["pO1"])
                P.dve(lambda e, hh=hh: e.scalar_tensor_tensor(out=St1[:, hh, :], in0=St0[:, hh, :],
                                                              scalar=dec[:, 2 * hh:2 * hh + 1], in1=pO[0][0:64, vs],
                                                              op0=ALU.mult, op1=ALU.add),
                      r=["St0", "dec", "pO0"], w=["St1"])
                P.act(lambda e, hh=hh: e.copy(out=St1b[:, hh, :], in_=St1[:, hh, :]), r=["St1"], w=["St1b"])
                mm(pa[:, 256:384], attT[:, :], vbb[:, vs], start=True, stop=False, r=["attT", "vbb"], w=[f"pS{hh % 2}"])
                mm(pa[:, 256:384], qdm[:, hh, 0, :], St0b[:, hh, :], start=False, stop=False, r=["qdm", "St0b"],
                   w=[f"pS{hh % 2}"])
                mm(pa[:, 256:384], qdm[:, hh, 1, :], St1b[:, hh, :], start=False, stop=True, r=["qdm", "St1b"],
                   w=[f"pS{hh % 2}"])
                P.dve(lambda e, hh=hh: e.scalar_tensor_tensor(out=St0[:, hh, :], in0=St1[:, hh, :],
                                                              scalar=dec[:, 2 * hh + 1:2 * hh + 2],
                                                              in1=pO[1][0:64, vs], op0=ALU.mult, op1=ALU.add),
                      r=["St1", "dec", "pO1"], w=["St0"])
                P.act(lambda e, pa=pa: e.activation(out=junk[:, 0:128], in_=pa[:, 256:384], func=AF.Square,
                                                    accum_out=sm[:, 50:51]), r=[f"pS{hh % 2}"], w=["junk", "sm50"])
                rstd_from_ssq(sm[:, 50:51], 1, 1.0 / 128, "sm50")
                P.dve(lambda e, pa=pa, hh=hh: e.scalar_tensor_tensor(out=att[:, 512 + hh * 128:512 + (hh + 1) * 128],
                                                                     in0=pa[:, 256:384], scalar=sm[:, 50:51],
                                                                     in1=sgn[:, hh * 128:(hh + 1) * 128], op0=ALU.mult,
                                                                     op1=ALU.mult),
                      r=[f"pS{hh % 2}", "sm50", "sgn"], w=["att"])
            if DBG.get("notail"):
                P.dve(lambda e: e.tensor_copy(out=tmpf[:], in_=att[:]), r=["att"], w=["tmpf"])
                P.dma("sp", out[t * 128:(t + 1) * 128, :], tmpf[:], r=["tmpf"], w=[f"out{t}"])
                continue
            tail(l, t, xb, att, "att")
            if DBG.get("dumph2"):
                P.dve(lambda e: e.tensor_copy(out=tmpf[:], in_=hb[:]), r=["hb"], w=["tmpf"])
                P.dma("sp", out[t * 128:(t + 1) * 128, :], tmpf[:], r=["tmpf"], w=[f"out{t}"])

    def odd_layer(l):
        P.dve(lambda e: e.memset(kmsum[:], 0.0), w=["kmsum"])
        for t in range(NT):
            xb = t % 2
            rs = slice(t * 128, (t + 1) * 128)
            load_x(l, t, xb)
            if l > 0:
                P.dma("sp", XR[rs, :], xt[xb][:], r=["xt0"], w=[f"XR{t}"])
            norm_mod(xt[xb], "xt0", 1, 0, hb, "hb")
            transpose8(hb, "hb", hT, "hT")
            project(hT, "hT", R1w, WINT, 2048, proj, "proj")
            for half in range(2):
                src = proj[:, half * 1024:(half + 1) * 1024]
                P.dve(lambda e, src=src: e.tensor_tensor(out=tmpf[:], in0=src, in1=src, op=ALU.mult), r=["proj"], w=["tmpf"])
                P.dve(lambda e: e.tensor_reduce(out=sm[:, 32:40], in_=tmpf[:, :].rearrange("p (g d) -> p g d", g=8),
                                                axis=AX.X, op=ALU.add), r=["tmpf"], w=["sm32"])
                rstd_from_ssq(sm[:, 32:40], 8, 1.0 / 128, "sm32")
                P.dve(lambda e, src=src: e.tensor_tensor(out=tmpf[:, :].rearrange("p (g d) -> p g d", g=8),
                                                         in0=src.rearrange("p (g d) -> p g d", g=8),
                                                         in1=sm[:, 32:40].unsqueeze(2).to_broadcast([128, 8, 128]),
                                                         op=ALU.mult), r=["proj", "sm32"], w=["tmpf"])
                P.pool(lambda e, half=half: e.tensor_tensor(
                    out=qnb[:, half * 1024:(half + 1) * 1024].rearrange("p (g d) -> p g d", g=8),
                    in0=tmpf[:, :].rearrange("p (g d) -> p g d", g=8),
                    in1=wrow[:, half * 128:(half + 1) * 128].unsqueeze(1).to_broadcast([128, 8, 128]), op=ALU.mult),
                    r=["tmpf", "wrow", "wrow2"], w=["qnb"])
            project(hT, "hT", R1w[:, :, 2048:3072], WINT, 1024, proj, "proj")
            P.act(lambda e: e.copy(out=vast[:, :, 0:128], in_=proj[:, 0:1024].rearrange("p (g d) -> p g d", g=8)),
                  r=["proj"], w=["vast"])
            P.dma("sp", VA[rs, :, :].rearrange("t h c -> t (h c)"), vast[:, :, :].rearrange("p h c -> p (h c)"), r=["vast"], wacc=["VA"])
            for half in range(2):
                dstD = QT if half == 0 else KT
                transpose8(qnb[:, half * 1024:(half + 1) * 1024], "qnb", hT, "hT")
                P.dma("sp", dstD[:, :, rs].rearrange("h d t -> d h t"), hT[:], r=["hT"], wacc=["QT" if half == 0 else "KT"])
                if half == 1:
                    P.dve(lambda e, t=t: e.tensor_reduce(out=kmsum[:, :, t], in_=hT[:], axis=AX.X, op=ALU.add),
                          r=["hT"], w=["kmsum"])
        P.dve(lambda e: e.tensor_tensor(out=tmpf[:, 0:128].rearrange("p (h b) -> p h b", h=8), in0=kmsum[:, :, :].rearrange("p h (b two) -> p h b two", two=2)[:, :, :, 0],
                                        in1=kmsum[:, :, :].rearrange("p h (b two) -> p h b two", two=2)[:, :, :, 1], op=ALU.add), r=["kmsum"], w=["tmpf"])
        P.dve(lambda e: e.tensor_scalar(out=kmean[:], in0=tmpf[:, 0:128].rearrange("p (h b) -> p h b", h=8),
                                        scalar1=1.0 / 256, scalar2=None, op0=ALU.mult), r=["tmpf"], w=["kmean"])
        pcount = [0]
        ocount = [0]
        scount = [0]
        pTf = [pT[i][:, :, :].rearrange("p a b -> p (a b)").bitcast(F32) for i in range(2)]
        sbanks = ((pS[0], "pS0"), (pS[1], "pS1"), (pTf[0], "pT0"), (pTf[1], "pT1"))
        for hgp in range(4):
            h0 = hgp * 2
            P.dma("sp", KTs, KT[h0:h0 + 2, :, :].rearrange("h d t -> d h t"), r=["KT"], w=["R2_0"])
            P.dma("sp", VAs2, VA[:, h0:h0 + 2, :].rearrange("(t p) h c -> p t (h c)", p=128), r=["VA"], w=["R2_1"])
            for qb_ in range(16):
                qs = slice(qb_ * 256, (qb_ + 1) * 256)
                P.dma("sp", qTt[:], QT[h0:h0 + 2, :, qs].rearrange("h d t -> d h t"), r=["QT"], w=["qTt"])
                for hh in range(2):
                    h = h0 + hh
                    for qt in range(2):
                        mm(pM[0][:, qt * 16:(qt + 1) * 16], qTt[:, hh, qt * 128:(qt + 1) * 128], kmean[:, h, :],
                           r=["qTt", "kmean"], w=["pM0"])
                    P.dve(lambda e: e.memset(scs[:], NEG), w=["scs"])
                    if qb_ > 0:
                        P.dve(lambda e, qb_=qb_: e.tensor_copy(
                            out=scs[:, :, 0:qb_], in_=pM[0][:, 0:32].rearrange("p (q b) -> p q b", q=2)[:, :, 0:qb_]),
                            r=["pM0"], w=["scs"])
                    for qt in range(2):
                        P.dve(lambda e, qt=qt: e.max(out=top8[:], in_=scs[:, qt, :]), r=["scs"], w=["top8"])
                        P.dve(lambda e, qt=qt, hh=hh: e.tensor_scalar(out=sel[:, hh, qt, :], in0=scs[:, qt, :],
                                                                      scalar1=top8[:, 2:3], scalar2=None, op0=ALU.is_ge),
                              r=["scs", "top8"], w=["sel"])
                    nkt = 2 * qb_ + 2
                    for qt in range(2):
                        qtile = 2 * qb_ + qt
                        qsl = slice(qt * 128, (qt + 1) * 128)
                        po = pO[0]
                        first = True
                        for kt in range(2 * qb_, qtile + 1):
                            dl = qtile - kt
                            psx = pS[pcount[0] % 2]
                            ptag = f"pS{pcount[0] % 2}"
                            pb = pT2[pcount[0] % 4]
                            pbt = f"pT2_{pcount[0] % 4}"
                            pcount[0] += 1
                            mm(psx[:, 0:128], KTs[:, hh, kt * 128:(kt + 1) * 128], qTt[:, hh, qsl], r=["R2_0", "qTt"], w=[ptag])
                            P.dve(lambda e, psx=psx, h=h, dl=dl: e.scalar_tensor_tensor(
                                out=sco_[(pcount[0] - 1) % 2][:, 0:128], in0=psx[:, 0:128], scalar=128 ** -0.5,
                                in1=(BS[:, h, 0:128] if dl == 0 else BM1[:, h, :]), op0=ALU.mult, op1=ALU.add), r=[ptag, "BM", "BS"], w=[f"sco{(pcount[0] - 1) % 2}"])
                            P.act(lambda e, pb=pb: e.activation(out=pb[:, 0:128], in_=sco_[(pcount[0] - 1) % 2][:, 0:128], func=AF.Exp),
                                  r=[f"sco{(pcount[0] - 1) % 2}"], w=[pbt])
                            mm(po[:, qt * 129:(qt + 1) * 129], pb[:, 0:128], VAs[:, kt, hh, :], start=first,
                               stop=(kt == qtile), r=[pbt, "R2_1"], w=["pO0"])
                            first = False
                    P.dve(lambda e, hh=hh: e.tensor_copy(out=acc[:, hh, :, :],
                                                         in_=pO[0][:, 0:258].rearrange("p (q c) -> p q c", q=2)),
                          r=["pO0"], w=["acc"])
                    def stage1(jb, hh=hh, h=h, qb_=qb_):
                        pbs = []
                        for kk in range(2):
                            kt = 2 * jb + kk
                            psx, ptag = sbanks[scount[0] % 4]
                            scount[0] += 1
                            pb = pT2[pcount[0] % 4]
                            pbt = f"pT2_{pcount[0] % 4}"
                            sci = pcount[0] % 2
                            pcount[0] += 1
                            mm(psx[:, 0:256], KTs[:, hh, kt * 128:(kt + 1) * 128], qTt[:, hh, :], r=["R2_0", "qTt"], w=[ptag])
                            if kt == 2 * qb_ - 1:
                                P.dve(lambda e, psx=psx, h=h, sci=sci: e.scalar_tensor_tensor(
                                    out=sco_[sci][:, 0:128], in0=psx[:, 0:128], scalar=128 ** -0.5, in1=BM1[:, h, :],
                                    op0=ALU.mult, op1=ALU.add), r=[ptag, "BM"], w=[f"sco{sci}"])
                                P.act(lambda e, pb=pb, sci=sci: e.activation(out=pb[:, 0:128], in_=sco_[sci][:, 0:128], func=AF.Exp),
                                      r=[f"sco{sci}"], w=[pbt])
                                P.act(lambda e, pb=pb, psx=psx, h=h: e.activation(out=pb[:, 128:256], in_=psx[:, 128:256],
                                                                                   func=AF.Exp, bias=relfar[:, h:h + 1],
                                                                                   scale=128 ** -0.5),
                                      r=[ptag, "relfar"], w=[pbt])
                            else:
                                P.act(lambda e, pb=pb, psx=psx, h=h: e.activation(out=pb[:, 0:256], in_=psx[:, 0:256],
                                                                                   func=AF.Exp, bias=relfar[:, h:h + 1],
                                                                                   scale=128 ** -0.5),
                                      r=[ptag, "relfar"], w=[pbt])
                            pbs.append((pb, pbt, kt))
                        return pbs

                    def stage2(jb, pbs, hh=hh):
                        po, potag = ((pO[1], "pO1"), (pM[1], "pM1"))[ocount[0] % 2]
                        ocount[0] += 1
                        for qt in range(2):
                            for kk, (pb, pbt, kt) in enumerate(pbs):
                                mm(po[:, qt * 129:(qt + 1) * 129], pb[:, qt * 128:(qt + 1) * 128], VAs[:, kt, hh, :],
                                   start=(kk == 0), stop=(kk == 1), r=[pbt, "R2_1"], w=[potag])
                        for qt in range(2):
                            P.dve(lambda e, qt=qt, hh=hh, jb=jb, po=po: e.scalar_tensor_tensor(
                                out=acc[:, hh, qt, :], in0=po[:, qt * 129:(qt + 1) * 129], scalar=sel[:, hh, qt, jb:jb + 1],
                                in1=acc[:, hh, qt, :], op0=ALU.mult, op1=ALU.add), r=[potag, "sel", "acc"], w=["acc"])

                    if qb_ > 0:
                        cur_pbs = stage1(0)
                        for jb in range(qb_):
                            nxt = stage1(jb + 1) if jb + 1 < qb_ else None
                            stage2(jb, cur_pbs)
                            cur_pbs = nxt
                    for qt in range(2):
                        P.dve(lambda e, qt=qt, hh=hh: e.reciprocal(out=sm[:, 52:53], in_=acc[:, hh, qt, 128:129]),
                              r=["acc"], w=["sm52"])
                        P.dve(lambda e, qt=qt, hh=hh: e.tensor_scalar(out=attq[:, qt, hh * 128:(hh + 1) * 128],
                                                                      in0=acc[:, hh, qt, 0:128], scalar1=sm[:, 52:53],
                                                                      scalar2=None, op0=ALU.mult),
                              r=["acc", "sm52"], w=["attq"])
                for qt in range(2):
                    t = 2 * qb_ + qt
                    P.dma("sp", ATT[t * 128:(t + 1) * 128, h0 * 128:(h0 + 2) * 128], attq[:, qt, :], r=["attq"], wacc=["ATT"])
        for t in range(NT):
            xb = t % 2
            rs = slice(t * 128, (t + 1) * 128)
            if l == 0:
                P.dma("sp", xt[xb][:], x_in[rs, :], w=["xt0"])
            else:
                P.dma("sp", xt[xb][:], XR[rs, :], r=[f"XR{t}"], w=["xt0"])
            P.dma("sp", att[:], ATT[rs, :], r=["ATT"], w=["att"])
            tail(l, t, xb, att, "att")

    def expert_phase(l):
        w1r = moe_w1.rearrange("l e (p j) n -> (l e p) (j n)", j=8)
        w3r = moe_w3.rearrange("l e (p j) n -> (l e p) (j n)", j=8)
        w2r = moe_w2.rearrange("l e (p j) n -> (l e p) (j n)", j=4)
        s0f = stg(0)[:, :]
        s1f = stg(1)[:, :]
        s2f = WOUT[:, :, :].rearrange("p a b -> p (a b)").bitcast(F32)
        s0 = stg(0)[:, :].rearrange("p (k n) -> p k n", k=8)
        s1 = stg(1)[:, :].rearrange("p (k n) -> p k n", k=8)
        s2 = WOUT[:, :, :].rearrange("p a b -> p (a b)").bitcast(F32).rearrange("p (k n) -> p k n", k=4)
        nblk = DBG.get("nblk", NBLK)

        def stage_tok(b):
            tb_ = b % 3
            tb = tabt[tb_]
            ix = idxi[tb_]
            P.dma("sp", tb[:], TAB[b * 128:(b + 1) * 128, :], r=["TAB"], w=[f"tabt{tb_}"])
            P.dve(lambda e, tb=tb, ix=ix: e.tensor_copy(out=ix[:], in_=tb[:, 0:2]), r=[f"tabt{tb_}"], w=[f"idxi{tb_}"])
            P.op("pool", lambda e, ix=ix, tb_=tb_: e.indirect_dma_start(
                out=xg[tb_][:, :], out_offset=None, in_=H2,
                in_offset=bass.IndirectOffsetOnAxis(ap=ix[:, 0:1], axis=0)),
                ["H2", f"idxi{tb_}"], [f"xg{tb_}"], dma=True)

        def stage_w(b):
            if not DBG.get("nowdma"):
                for sv, wr_, tg in ((s0f, w1r, "R2_0"), (s1f, w3r, "R2_1"), (s2f, w2r, "WOUTt")):
                    P.op("pool", lambda e, b=b, sv=sv, wr_=wr_: e.indirect_dma_start(
                        out=sv, out_offset=None, in_=wr_,
                        in_offset=bass.IndirectOffsetOnAxis(ap=widx1i[:, 0, b:b + 1], axis=0),
                        bounds_check=_LazyReg(NLW * 32 * 128 - 1), oob_is_err=False),
                        ["widx1i"], [tg], dma=True)

        def stage_cast(b):
            eb_ = b % 2
            w1v, w3v, w2v = ew(eb_, 0), ew(eb_, 1), ew(eb_, 2)
            wt = [f"R1_{eb_}_{i}" for i in range(3)]
            P.act(lambda e, w1v=w1v: e.copy(out=w1v, in_=s0), r=["R2_0"], w=[wt[0]])
            P.dve(lambda e, w3v=w3v: e.tensor_copy(out=w3v, in_=s1), r=["R2_1"], w=[wt[1]])
            P.act(lambda e, w2v=w2v: e.copy(out=w2v, in_=s2), r=["WOUTt"], w=[wt[2]])

        def stage_b1(b):
            eb_ = b % 2
            tb_ = b % 3
            i = tcount[0] % 2
            tcount[0] += 1
            for k in range(8):
                tp(pT[i][:, k, :], xg[tb_][:, :].rearrange("t (p j) -> t j p", j=8)[:, k, :], identb[:],
                   r=[f"xg{tb_}", "identb"], w=[f"pT{i}"])
            P.dve(lambda e, i=i, eb_=eb_: e.tensor_copy(out=xgT[eb_][:, :, :], in_=pT[i][:]), r=[f"pT{i}"], w=[f"xgT{eb_}"])

        def stage_b(b):
            eb_ = b % 2
            tb_ = b % 3
            w1v, w3v, w2v = ew(eb_, 0), ew(eb_, 1), ew(eb_, 2)
            wt = [f"R1_{eb_}_{i}" for i in range(3)]
            tb = tabt[tb_]
            ix = idxi[tb_]
            hbanks = ((pM[0], "pM0"), (pM[1], "pM1"), (pO[0], "pO0"), (pO[1], "pO1"))
            for hc in range(4):
                hb_, htag = hbanks[hc]
                for k in range(8):
                    mm(hb_[:, 0:128], w1v[:, k, :].rearrange("p (m f) -> p f m", f=4)[:, hc, :], xgT[eb_][:, k, :],
                       start=(k == 0), stop=(k == 7), r=[f"xgT{eb_}", wt[0]], w=[htag])
                for k in range(8):
                    mm(hb_[:, 128:256], w3v[:, k, :].rearrange("p (m f) -> p f m", f=4)[:, hc, :], xgT[eb_][:, k, :],
                       start=(k == 0), stop=(k == 7), r=[f"xgT{eb_}", wt[1]], w=[htag])
            for hc in range(4):
                hb_, htag = hbanks[hc]
                P.act(lambda e, hb_=hb_, hc=hc: e.activation(out=s1t4[:, hc, :], in_=hb_[:, 0:128], func=AF.Silu),
                      r=[htag], w=[f"s1t{hc}"])
                P.dve(lambda e, hc=hc, eb_=eb_, hb_=hb_: e.tensor_tensor(out=actT[eb_][:, hc, :], in0=hb_[:, 128:256],
                                                                         in1=s1t4[:, hc, :], op=ALU.mult),
                      r=[htag, f"s1t{hc}"], w=[f"actT{eb_}"])
            ytag = ("m1", "junk")[eb_]
            for half in range(2):
                for hc in range(4):
                    mm(pS[half][:, :], actT[eb_][:, hc, :], w2v[:, hc, half * 512:(half + 1) * 512],
                       start=(hc == 0), stop=(hc == 3), r=[f"actT{eb_}", wt[2]], w=[f"pS{half}"])
                if half == 0:
                    P.act(lambda e, eb_=eb_, tb=tb: e.activation(out=yo[eb_][:, 0:512], in_=pS[0][:, :], func=AF.Copy,
                                                                 scale=tb[:, 2:3]),
                          r=["pS0", f"tabt{tb_}"], w=[ytag])
                else:
                    P.dve(lambda e, eb_=eb_, tb=tb: e.tensor_scalar(out=yo[eb_][:, 512:1024], in0=pS[1][:, :],
                                                                     scalar1=tb[:, 2:3], scalar2=None, op0=ALU.mult),
                          r=["pS1", f"tabt{tb_}"], w=[ytag])
            P.op("pool", lambda e, eb_=eb_, ix=ix: e.indirect_dma_start(
                out=MO, out_offset=bass.IndirectOffsetOnAxis(ap=ix[:, 1:2], axis=0), in_=yo[eb_][:], in_offset=None),
                [ytag, f"idxi{tb_}"], None, dma=True, wacc=["MO"])

        stage_tok(0)
        stage_w(0)
        stage_cast(0)
        if nblk > 1:
            stage_tok(1)
            stage_w(1)
        for b in range(nblk):
            stage_b1(b)
            if b + 1 < nblk:
                stage_cast(b + 1)
            if b + 2 < nblk:
                stage_tok(b + 2)
                stage_w(b + 2)
            stage_b(b)

    for l in range(n_layers):
        layer_start(l)
        if DBG.get("stage") == "A":
            for r_ in range(6):
                P.dma("sp", out[r_ * 128:(r_ + 1) * 128, :], rows[:, r_, :], r=[f"rows{r_}"], w=[f"out{r_}"])
            break
        if l % 2 == 0:
            even_layer(l)
        else:
            odd_layer(l)
        if not DBG.get("noexp"):
            route_finalize(l)
            expert_phase(l)
    if DBG.get("nofinal"):
        P.emit()
        es.close()
        return nc
    P.pool(lambda e: e.tensor_copy(out=rows[:, 6, :], in_=rows[:, 5, :]), r=["rows5"], w=["rows6"])
    P.barrier(lambda e: e.memset(sm[:, 62:63], 0.0))
    fsets = ((xt[0][:, :], m1[:, :], junk[:, :], ("xt0", "m1", "junk")),
             (tmpf[:, :], proj[:, 0:1024], proj[:, 1024:2048], ("tmpf", "fmA", "fmB")),
             (EV[:, 0:1024], EV[:, 1024:2048], EV[:, 2048:3072], ("fmC", "fmD", "fmE")))
    for t in range(NT):
        xs, ma, mb, (tx, ta, tb2) = fsets[t % 3]
        rs = slice(t * 128, (t + 1) * 128)
        P.dma("sp", xs, XR[rs, :], r=[f"XR{t}"], w=[tx])
        P.dma("sp", ma, MO[rs, :], r=["MO"], w=[ta])
        P.dma("sp", mb, MO[MOH + t * 128:MOH + (t + 1) * 128, :], r=["MO"], w=[tb2])
        P.pool(lambda e, ma=ma, mb=mb: e.tensor_tensor(out=ma, in0=ma, in1=mb, op=ALU.add), r=[ta, tb2], w=[ta])
        P.dve(lambda e, ma=ma: e.tensor_tensor(out=ma, in0=ma, in1=rows[:, 6, :], op=ALU.mult), r=[ta, "rows6"], w=[ta])
        P.dve(lambda e, xs=xs, ma=ma: e.tensor_tensor(out=xs, in0=xs, in1=ma, op=ALU.add), r=[tx, ta], w=[tx])
        P.dma("sp", out[rs, :], xs, r=[tx], w=[f"out{t}"])
    P.emit()
    es.close()
    return nc


def _rel_bucket_np(d):
    d = np.maximum(d, 0)
    logd = np.log(np.maximum(d, 1).astype(np.float32) / 16) / math.log(128 / 16)
    far = np.minimum(16 + (logd * 16).astype(np.int32), 31)
    return np.where(d < 16, d, far)


def _constants():
    k = np.arange(128)[:, None]
    q = np.arange(128)[None, :]
    c = {}
    c["ident"] = np.eye(128, dtype=np.float32)
    same = (k // 64) == (q // 64)
    c["tri"] = (same & (k <= q)).astype(np.float32)
    c["bones"] = same.astype(np.float32)
    c["cind"] = (np.arange(128)[:, None] // 64 == np.arange(2)[None, :]).astype(np.float32)
    c["stri"] = (k < q).astype(np.float32)
    mask0 = np.zeros((128, 256), np.float32)
    mask0[:, 0:128] = np.where(q >= k, 0.0, NEG)
    mask0[:, 128:256] = np.where(q < k, 0.0, NEG)
    c["mask0"] = mask0
    r = np.arange(TABROWS)
    tab = np.zeros((TABROWS, 4), np.float32)
    tab[:, 1] = S + (r % 128)
    c["tabinit"] = tab
    rc_ = np.zeros((128, 172), np.float32)
    rc_[:, 0:64] = (np.arange(64) * 128)[None, :]
    rc_[:, 64:160] = (np.arange(96) * 128)[None, :]
    rc_[:, 160:168] = np.arange(8)[None, :] * 128 + np.arange(128)[:, None]
    rc_[:, 168:172] = np.arange(4)[None, :] * 128 + np.arange(128)[:, None]
    c["rcst"] = rc_
    pid = np.zeros((128, 2), np.float32)
    pid[:, 0] = np.arange(128)
    pid[:, 1] = 32 * CAP + np.arange(128)
    c["pidx"] = pid
    d0 = q - k
    d1 = 128 + q - k
    c["_b0"] = _rel_bucket_np(d0)
    c["_b1"] = _rel_bucket_np(d1)
    return c


_CACHE = {}


def kernel(x, c, rel_bias, ada_w, ada_b, norm1_w, norm2_w, even_w_in, even_w_out, a_q_norm, a_k_norm, a_sinks,
           b_gate_up, b_gate_bias, b_out_norm, odd_w_in, odd_w_out, c_q_norm, c_k_norm, moe_w_group, moe_b_group,
           moe_w_expert, moe_b_expert, moe_w1, moe_w3, moe_w2, _n_layers=4, _cores=None, _dbg=None):
    f = lambda a: np.ascontiguousarray(np.asarray(a, dtype=np.float32))
    cst = _constants()
    rel_bias = f(rel_bias)
    relT = np.empty((8, 128, 256), np.float32)
    relT[:, :, 0:128] = np.transpose(rel_bias[cst["_b0"]], (2, 0, 1))
    relT[:, :, 128:256] = np.transpose(rel_bias[cst["_b1"]], (2, 0, 1))
    shared = {
        "rel_bias": rel_bias, "relT": relT, "ada_w": f(ada_w[:_n_layers]), "ada_b": f(ada_b), "norm1_w": f(norm1_w),
        "norm2_w": f(norm2_w), "even_w_in": f(even_w_in), "even_w_out": f(even_w_out), "a_q_norm": f(a_q_norm),
        "a_k_norm": f(a_k_norm), "a_sinks": f(a_sinks), "b_gate_up": f(b_gate_up), "b_gate_bias": f(b_gate_bias),
        "b_out_norm": f(b_out_norm), "odd_w_in": f(odd_w_in), "odd_w_out": f(odd_w_out), "c_q_norm": f(c_q_norm),
        "c_k_norm": f(c_k_norm),
        "moe_wr": np.ascontiguousarray(np.concatenate([f(moe_w_group), f(moe_w_expert)], axis=-1)),
        "moe_br": np.ascontiguousarray(np.concatenate([f(moe_b_group), f(moe_b_expert)], axis=-1)),
        "moe_w1": f(moe_w1[:_n_layers]), "moe_w3": f(moe_w3[:_n_layers]), "moe_w2": f(moe_w2[:_n_layers]),
    }
    for k_ in ("ident", "tri", "bones", "cind", "stri", "mask0", "tabinit", "rcst", "pidx"):
        shared[k_] = cst[k_]
    x = f(x)
    c = f(c)
    cores = list(range(8)) if _cores is None else _cores
    in_maps = []
    for b in cores:
        m = dict(shared)
        m["x"] = x[b]
        m["cT"] = np.ascontiguousarray(c[b].reshape(8, 128).T)
        in_maps.append(m)
    key = (_n_layers, str(_dbg))
    if key not in _CACHE:
        _CACHE[key] = build_program(_n_layers, _dbg)
    nc = _CACHE[key]
    res = run_bass_kernel_spmd(nc, in_maps, core_ids=list(range(len(cores))))
    outs = [r["out"] for r in res.results]
    return np.stack(outs, axis=0).astype(np.float32)
```

```python
import math
from contextlib import ExitStack

import numpy as np
import concourse.bass as bass
import concourse.mybir as mybir
from concourse.bass_utils import run_bass_kernel_spmd

F32 = mybir.dt.float32
BF16 = mybir.dt.bfloat16
I32 = mybir.dt.int32
AF = mybir.ActivationFunctionType
ALU = mybir.AluOpType
AX = mybir.AxisListType

S = 4096
D = 1024
NT = 32
CAP = 384
NB = CAP // 128
NEG = -30000.0
BIG = 10000.0
MOH = S + 128
NBLK = 96
TABROWS = NBLK * 128
EPS = 1e-6

COMPUTE = ("pe", "act", "dve", "pool")
DMAQ_K = 12
SEM_EPOCH = 6000
DMA_EPOCH = 400


class Buf:
    __slots__ = ("name", "w", "r")

    def __init__(self, name):
        self.name = name
        self.w = []
        self.r = []


class Op:
    __slots__ = ("eng", "fn", "seq", "dma", "deps", "signal", "sig_idx", "dma_idx")

    def __init__(self, eng, fn, dma):
        self.eng = eng
        self.fn = fn
        self.dma = dma
        self.deps = []
        self.signal = False
        self.sig_idx = None
        self.dma_idx = None


def _compress(lst):
    keep = []
    cnt = {}
    for x in reversed(lst):
        key = (x.eng, x.dma)
        c = cnt.get(key, 0)
        lim = DMAQ_K if x.dma else 1
        if c < lim:
            keep.append(x)
            cnt[key] = c + 1
    keep.reverse()
    return keep


class _Rec:
    def __init__(self):
        self.calls = []

    def __getattr__(self, name):
        def f(*a, **k):
            self.calls.append((name, a, k))
            return self
        return f


class _LazyReg:
    def __init__(self, v):
        self.v = v


_REGCACHE = {}


def _freeze(fn):
    rec = _Rec()
    fn(rec)
    assert len(rec.calls) == 1, rec.calls
    name, a, k = rec.calls[0]
    lazy = [kk for kk, vv in k.items() if isinstance(vv, _LazyReg)]
    if not lazy:
        return lambda e: getattr(e, name)(*a, **k)

    def replay(e):
        k2 = dict(k)
        for kk in lazy:
            key = (id(e), k[kk].v)
            if key not in _REGCACHE:
                _REGCACHE[key] = e.to_reg(k[kk].v)
            k2[kk] = _REGCACHE[key]
        return getattr(e, name)(*a, **k2)
    return replay


class Prog:
    def __init__(self, nc):
        self.nc = nc
        self.ops = {e: [] for e in ("pe", "act", "dve", "pool", "sp")}
        self.ndma = {e: 0 for e in self.ops}
        self.known = {e: {f: -1 for f in self.ops} for e in self.ops}
        self.kd_upto = {e: {f: -1 for f in self.ops} for e in self.ops}
        self.kd_set = {e: {f: set() for f in self.ops} for e in self.ops}
        self.bufs = {}

    def buf(self, name):
        b = self.bufs.get(name)
        if b is None:
            b = Buf(name)
            self.bufs[name] = b
        return b

    def _norm(self, lst):
        out = []
        for x in lst or []:
            if isinstance(x, (list, tuple)):
                out.extend(self._norm(x))
            elif isinstance(x, str):
                out.append(self.buf(x))
            else:
                out.append(x)
        return out

    def _satisfied(self, E, d):
        if d.dma:
            return d.dma_idx <= self.kd_upto[E][d.eng] or d.dma_idx in self.kd_set[E][d.eng]
        return d.seq <= self.known[E][d.eng]

    def _learn(self, E, d):
        if d.dma:
            F = d.eng
            s = self.kd_set[E][F]
            s.add(d.dma_idx)
            u = max(self.kd_upto[E][F], d.dma_idx - DMAQ_K)
            while (u + 1) in s:
                u += 1
            self.kd_upto[E][F] = u
            if len(s) > 64:
                self.kd_set[E][F] = {x for x in s if x > u}
        elif d.seq > self.known[E][d.eng]:
            self.known[E][d.eng] = d.seq

    def op(self, eng, fn, reads=None, writes=None, dma=False, wacc=None):
        reads = self._norm(reads)
        writes = self._norm(writes)
        wacc = self._norm(wacc)
        psr = [b for b in reads if b.name[:2] in ("pT", "pM", "pS", "pO")]
        if psr:
            reads = [b for b in reads if b not in psr]
            writes = writes + [b for b in psr if b not in writes]
        if fn is not None:
            fn = _freeze(fn)
        o = Op(eng, fn, dma)
        o.seq = len(self.ops[eng])
        if dma:
            o.dma_idx = self.ndma[eng]
            self.ndma[eng] += 1
        raw = set()
        cand = []
        for b in reads:
            for d in b.w:
                raw.add(id(d))
                cand.append(d)
        for b in writes:
            cand.extend(b.w)
            cand.extend(b.r)
        for b in wacc:
            cand.extend(b.r)
        seen = set()
        uniq = []
        for d in cand:
            if id(d) not in seen:
                seen.add(id(d))
                uniq.append(d)
        uniq.sort(key=lambda d: -(d.dma_idx if d.dma else d.seq))
        for d in uniq:
            if (not d.dma) and (not dma) and d.eng == eng and id(d) not in raw:
                continue
            if self._satisfied(eng, d):
                continue
            o.deps.append(d)
            d.signal = True
            self._learn(eng, d)
        for b in writes:
            b.w = [o]
            b.r = []
        for b in wacc:
            b.w.append(o)
            if len(b.w) > 40:
                b.w = _compress(b.w)
        for b in reads:
            if fn is None:
                break
            if not dma:
                b.r = [x for x in b.r if x.dma or x.eng != eng]
            b.r.append(o)
            if len(b.r) > 40:
                b.r = _compress(b.r)
        self.ops[eng].append(o)
        return o

    def pe(self, fn, r=None, w=None):
        return self.op("pe", fn, r, w)

    def act(self, fn, r=None, w=None):
        return self.op("act", fn, r, w)

    def dve(self, fn, r=None, w=None):
        return self.op("dve", fn, r, w)

    def pool(self, fn, r=None, w=None):
        return self.op("pool", fn, r, w)

    def barrier(self, fn):
        allb = list(self.bufs.values())
        self.op("dve", fn, allb, allb + [self.buf("FENCE")])
        for e in ("pe", "act", "pool", "sp"):
            self.op(e, None, [self.buf("FENCE")], None)

    def dma(self, eng, out, in_, r=None, w=None, wacc=None):
        return self.op(eng, lambda e: e.dma_start(out=out, in_=in_), r, w, dma=True, wacc=wacc)

    def emit(self):
        nc = self.nc
        nsig = {}
        for e in COMPUTE:
            c = 0
            for o in self.ops[e]:
                if o.signal and not o.dma:
                    o.sig_idx = c
                    c += 1
            nsig[e] = c
        es = ExitStack()
        sems = {}
        for e in COMPUTE:
            n_ep = max(1, (nsig[e] + SEM_EPOCH - 1) // SEM_EPOCH)
            sems[e] = [es.enter_context(nc.semaphore(f"tl_{e}_{i}")) for i in range(n_ep)]
        dsems = {}
        per_ep = DMA_EPOCH * DMAQ_K
        for e in self.ops:
            if self.ndma[e] == 0:
                continue
            n_ep = (self.ndma[e] + per_ep - 1) // per_ep
            dsems[e] = [[es.enter_context(nc.semaphore(f"dq_{e}_{i}_{k}")) for k in range(DMAQ_K)]
                        for i in range(n_ep)]

        def dma_sem(e, idx):
            return dsems[e][idx // per_ep][idx % DMAQ_K], 16 * ((idx % per_ep) // DMAQ_K + 1)

        def wait_for(engobj, d):
            if d.dma:
                s, v = dma_sem(d.eng, d.dma_idx)
            else:
                s = sems[d.eng][d.sig_idx // SEM_EPOCH]
                v = d.sig_idx % SEM_EPOCH + 1
            engobj.wait_ge(s, v)

        prog = self

        def run_engine(ename, engobj):
            for o in prog.ops[ename]:
                for d in o.deps:
                    wait_for(engobj, d)
                if o.dma:
                    if o.dma_idx >= DMAQ_K:
                        s, v = dma_sem(ename, o.dma_idx - DMAQ_K)
                        engobj.wait_ge(s, v)
                    inst = o.fn(engobj)
                    s, v = dma_sem(ename, o.dma_idx)
                    inst.then_inc(s, 16)
                elif o.fn is not None:
                    inst = o.fn(engobj)
                    if o.signal:
                        inst.then_inc(sems[ename][o.sig_idx // SEM_EPOCH], 1)
            n = prog.ndma[ename]
            for idx in range(max(0, n - DMAQ_K), n):
                s, v = dma_sem(ename, idx)
                engobj.wait_ge(s, v)

        with nc.Block() as block:
            @block.tensor
            def _(e):
                run_engine("pe", e)

            @block.scalar
            def _(e):
                run_engine("act", e)

            @block.vector
            def _(e):
                run_engine("dve", e)

            @block.gpsimd
            def _(e):
                run_engine("pool", e)

            @block.sync
            def _(e):
                run_engine("sp", e)
        es.close()


def build_program(n_layers=4, dbg=None):
    DBG = dbg or {}
    _REGCACHE.clear()
    nc = bass.Bass("TRN2", target_bir_lowering=False)
    P = Prog(nc)
    es = ExitStack()

    def din(name, shape, dt=F32):
        return nc.dram_tensor(name, list(shape), dt, kind="ExternalInput").ap()

    def dscr(name, shape, dt=F32):
        return nc.dram_tensor(name, list(shape), dt, kind="Internal").ap()

    def sb(name, shape, dt=F32):
        return es.enter_context(nc.sbuf_tensor("s_" + name, list(shape), dt))

    def ps(name, shape, dt=F32):
        return es.enter_context(nc.psum_tensor("p_" + name, list(shape), dt))

    x_in = din("x", [S, D])
    cT_in = din("cT", [128, 8])
    rel_bias = din("rel_bias", [32, 8])
    relT_in = din("relT", [8, 128, 256])
    mask0_in = din("mask0", [128, 256])
    ident_in = din("ident", [128, 128])
    tri_in = din("tri", [128, 128])
    bones_in = din("bones", [128, 128])
    cind_in = din("cind", [128, 2])
    stri_in = din("stri", [128, 128])
    tabinit_in = din("tabinit", [TABROWS, 4])
    rcst_in = din("rcst", [128, 172])
    pidx_in = din("pidx", [128, 2])
    NLW = n_layers
    ada_w = din("ada_w", [NLW, D, 6 * D])
    ada_b = din("ada_b", [4, 6 * D])
    norm1_w = din("norm1_w", [4, D])
    norm2_w = din("norm2_w", [4, D])
    even_w_in = din("even_w_in", [2, D, 2320])
    even_w_out = din("even_w_out", [2, D, D])
    a_q_norm = din("a_q_norm", [2, 64])
    a_k_norm = din("a_k_norm", [2, 64])
    a_sinks = din("a_sinks", [2, 8])
    b_gate_up = din("b_gate_up", [2, 16, 256])
    b_gate_bias = din("b_gate_bias", [2, 256])
    b_out_norm = din("b_out_norm", [2, 128])
    odd_w_in = din("odd_w_in", [2, D, 3072])
    odd_w_out = din("odd_w_out", [2, D, D])
    c_q_norm = din("c_q_norm", [2, 128])
    c_k_norm = din("c_k_norm", [2, 128])
    moe_wr = din("moe_wr", [4, D, 36])
    moe_br = din("moe_br", [4, 36])
    moe_w1 = din("moe_w1", [NLW, 32, D, 512])
    moe_w3 = din("moe_w3", [NLW, 32, D, 512])
    moe_w2 = din("moe_w2", [NLW, 32, 512, D])
    out = nc.dram_tensor("out", [S, D], F32, kind="ExternalOutput").ap()

    XR = dscr("XR", [S, D])
    H2 = dscr("H2", [S, D], BF16)
    TAB = dscr("TAB", [TABROWS, 4])
    MO = dscr("MO", [2 * MOH, D])
    ATT = dscr("ATT", [S, D], BF16)
    QT = dscr("QT", [8, 128, S], BF16)
    KT = dscr("KT", [8, 128, S], BF16)
    VA = dscr("VA", [S, 8, 129], BF16)

    R1 = sb("R1", [128, 24576], BF16)
    R2 = sb("R2", [128, 8448], F32)
    WOUT = sb("WOUT", [128, 8, 1024], BF16)
    rows = sb("rows", [128, 7, 1024])
    BS = sb("BS", [128, 8, 256])
    BM1 = sb("BM1", [128, 8, 128])
    relfar = sb("relfar", [128, 8])
    identf = sb("identf", [128, 128])
    identb = sb("identb", [128, 128], BF16)
    trif = sb("trif", [128, 128])
    bonesf = sb("bonesf", [128, 128])
    cindf = sb("cindf", [128, 2])
    strib = sb("strib", [128, 128], BF16)
    onesb = sb("onesb", [128, 128], BF16)
    bonesb = sb("bonesb", [128, 128], BF16)
    rcst = sb("rcst", [128, 172])
    pidx = sb("pidx", [128, 2])
    cTs = sb("cTs", [128, 8])
    xt0_ = sb("xt0", [128, D])
    xt = [xt0_, xt0_]
    m1 = sb("m1", [128, D])
    junk = sb("junk", [128, D])
    m2 = junk
    condbc = junk[:, :].rearrange("p (k n) -> p k n", k=8)
    tmpf = sb("tmpf", [128, D])
    hb = sb("hb", [128, D], BF16)
    hT = sb("hT", [128, 8, 128], BF16)
    proj = sb("proj", [128, 2320])
    att = sb("att", [128, D], BF16)
    h2b = hb
    sm = sb("sm", [128, 64])
    wrow = sb("wrow", [128, 640])
    WR = sb("WR", [128, 8, 36], BF16)
    brow = sb("brow", [128, 36])
    adab = m1[:, 0:512]
    EV = sb("EV", [128, 7888])

    class Carver:
        def __init__(self):
            self.off = 0

        def get(self, shape, dt=F32):
            np_ = shape[0]
            n = 1
            for d_ in shape[1:]:
                n *= d_
            sz = 2 if dt == BF16 else 4
            nb = (n * sz + 3) // 4 * 4
            c0 = self.off // 4
            self.off += nb
            assert self.off <= 7888 * 4, self.off
            v = EV[0:np_, c0:c0 + nb // 4]
            if dt != F32:
                v = v.bitcast(dt)[:, 0:n]
            if len(shape) == 3:
                v = v.rearrange("p (a b) -> p a b", a=shape[1])
            elif len(shape) == 4:
                v = v.rearrange("p (a b c) -> p a b c", a=shape[1], b=shape[2])
            return v

    cv = Carver()
    qkb = cv.get([128, 512], BF16)
    kdup = cv.get([128, 2, 128], BF16)
    qkT = [cv.get([128, 6, 128], BF16) for i in range(2)]
    vaS = [cv.get([128, 2, 65], BF16) for i in range(2)]
    sc = cv.get([128, 256])
    pexp = [cv.get([128, 256], BF16) for i in range(2)]
    abT = cv.get([32, 128], BF16)
    abp = cv.get([128, 32], BF16)
    gnb = cv.get([128, 256], BF16)
    zt = cv.get([128, 256])
    gneg = cv.get([128, 256])
    bsb = zt
    eb = cv.get([128, 256])
    enb = cv.get([128, 256])
    ekd = cv.get([128, 256])
    qd = cv.get([128, 256], BF16)
    kd = cv.get([128, 256], BF16)
    ku = cv.get([128, 256], BF16)
    vbb = cv.get([128, 512], BF16)
    sgn = cv.get([128, 512])
    dec = cv.get([64, 8])
    qdT = cv.get([64, 4, 128], BF16)
    qdm = cv.get([64, 4, 2, 128], BF16)
    kdT = cv.get([64, 4, 128], BF16)
    attT = cv.get([128, 128], BF16)
    St0 = cv.get([64, 4, 128])
    St1 = cv.get([64, 4, 128])
    St0b = cv.get([64, 4, 128], BF16)
    St1b = cv.get([64, 4, 128], BF16)
    gbias = cv.get([128, 256])
    onorm1 = cv.get([128, 128])
    gupf = cv.get([32, 256])
    gupb = cv.get([32, 256], BF16)
    esink = cv.get([128, 8])
    ev_even = cv.off
    cv = Carver()
    qnb = cv.get([128, 2048], BF16)
    vast = cv.get([128, 8, 129], BF16)
    kmsum = cv.get([128, 8, 32])
    kmean = cv.get([128, 8, 16], BF16)
    qTt = cv.get([128, 2, 256], BF16)
    scs = cv.get([128, 2, 16])
    sel = cv.get([128, 2, 2, 16])
    pT2 = [cv.get([128, 256], BF16) for i in range(4)]
    acc = cv.get([128, 2, 2, 129])
    attq = cv.get([128, 2, 256], BF16)
    sco_ = [cv.get([128, 128]) for i in range(2)]
    sco3 = cv.get([128, 384])
    pown = cv.get([128, 384], BF16)
    cv = Carver()
    xg = [cv.get([128, D], BF16) for i in range(3)]
    xgT = [cv.get([128, 8, 128], BF16) for i in range(2)]
    s1t4 = cv.get([128, 4, 128])
    actT = [cv.get([128, 4, 128], BF16) for i in range(2)]
    rtab = cv.get([128, 6, 32])
    blkE = cv.get([128, 96])
    widx1f = cv.get([128, 8, 96])
    widx1i = cv.get([128, 8, 96], I32)
    widx2f = cv.get([128, 4, 96])
    widx2i = cv.get([128, 4, 96], I32)
    M12all = R2[:, 0:2048].rearrange("p (t k e) -> p t k e", t=32, k=2)
    slall = R2[:, 2048:2112].rearrange("p (t k) -> p t k", t=32)
    recall = R2[:, 2112:2368].rearrange("p (t k c) -> p t k c", t=32, k=2)
    top8 = sb("top8", [128, 8])
    lg = sb("lg", [128, 36])
    msk = sb("msk", [128, 32])
    M12 = sb("M12", [128, 2, 32])
    Mb = sb("Mb", [128, 32], BF16)
    cbase = sb("cbase", [128, 32])
    Cc = sb("Cc", [128, 32])
    prod = sb("prod", [128, 2, 32])
    desti = [sb(f"desti{i}", [128, 2], I32) for i in range(2)]
    tabt = [cv.get([128, 4]) for i in range(3)]
    idxi = [cv.get([128, 2], I32) for i in range(3)]
    yo = [m1, junk]
    print("SBUF bytes remaining/partition:", nc.sbuf_bytes_remaining)

    pT = [ps(f"pT{i}", [128, 8, 128], BF16) for i in range(2)]
    pM = [ps(f"pM{i}", [128, 512]) for i in range(2)]
    pS = [ps(f"pS{i}", [128, 512]) for i in range(2)]
    pO = [ps(f"pO{i}", [128, 512]) for i in range(2)]

    R1w = R1[:, :].rearrange("p (k n) -> p k n", k=8)

    def ew(eb_, which):
        base = eb_ * 12288
        if which == 0:
            return R1[:, base:base + 4096].rearrange("p (k n) -> p k n", k=8)
        if which == 1:
            return R1[:, base + 4096:base + 8192].rearrange("p (k n) -> p k n", k=8)
        return R1[:, base + 8192:base + 12288].rearrange("p (k n) -> p k n", k=4)

    def stg(i):
        return R2[:, i * 4096:(i + 1) * 4096]

    WINT = [f"R1_{a}_{b}" for a in range(2) for b in range(3)]

    def bc(ap):
        return ap.partition_broadcast(128).squeeze(1)

    def fence(tags, eng="dve"):
        P.op(eng, lambda e: e.memset(sm[:, 62:63], 0.0), tags, tags + ["sm62"])

    R2b = R2[:, :].bitcast(BF16)
    KTs = R2b[:, 0:8192].rearrange("p (h t) -> p h t", h=2)
    VAs = R2b[:, 8192:8192 + 8256].rearrange("p (t h c) -> p t h c", t=32, h=2)
    VAs2 = R2b[:, 8192:8192 + 8256].rearrange("p (t c) -> p t c", t=32)

    def mm(out_, lhsT, rhs, start=True, stop=True, r=None, w=None):
        P.pe(lambda e: e.matmul(out_, lhsT=lhsT, rhs=rhs, start=start, stop=stop), r=r, w=w)

    def tp(out_, in_, ident, r=None, w=None):
        P.pe(lambda e: e.transpose(out=out_, in_=in_, identity=ident), r=r, w=w)

    P.dma("sp", identf[:], ident_in, w=["identf"])
    P.dma("sp", trif[:], tri_in, w=["trif"])
    P.dma("sp", bonesf[:], bones_in, w=["bonesf"])
    P.dma("sp", cindf[:], cind_in, w=["cindf"])
    P.dma("sp", rcst[:], rcst_in, w=["rcst"])
    P.dma("sp", pidx[:], pidx_in, w=["pidx"])
    P.dma("sp", cTs[:], cT_in, w=["cTs"])
    P.dma("sp", tmpf[:, 0:128], stri_in, w=["tmpf"])
    P.dve(lambda e: e.tensor_copy(out=strib[:], in_=tmpf[:, 0:128]), r=["tmpf"], w=["strib"])
    P.dve(lambda e: e.tensor_copy(out=identb[:], in_=identf[:]), r=["identf"], w=["identb"])
    P.dve(lambda e: e.memset(onesb[:], 1.0), w=["onesb"])
    P.dve(lambda e: e.tensor_copy(out=bonesb[:], in_=bonesf[:]), r=["bonesf"], w=["bonesb"])
    P.dma("sp", BS[:], relT_in.rearrange("h k q -> k h q"), w=["BS"])
    P.dma("sp", m1[:, 0:256], mask0_in, w=["m1"])
    P.dve(lambda e: e.tensor_copy(out=BM1[:], in_=BS[:, :, 128:256]), r=["BS"], w=["BM"])
    for h in range(8):
        P.dve(lambda e, h=h: e.tensor_tensor(out=BS[:, h, :], in0=BS[:, h, :], in1=m1[:, 0:256], op=ALU.add),
              r=["BS", "m1"], w=["BS"])
    P.dma("sp", relfar[:], bc(rel_bias[31:32, :]), w=["relfar"])
    P.act(lambda e: e.activation(out=cTs[:], in_=cTs[:], func=AF.Silu), r=["cTs"], w=["cTs"])

    def rstd_from_ssq(ssq_ap, n, inv_n, tag):
        P.dve(lambda e: e.tensor_scalar(out=ssq_ap, in0=ssq_ap, scalar1=inv_n, scalar2=EPS, op0=ALU.mult, op1=ALU.add),
              r=[tag], w=[tag])
        P.act(lambda e: e.activation(out=ssq_ap, in_=ssq_ap, func=AF.Sqrt), r=[tag], w=[tag])
        P.dve(lambda e: e.reciprocal(out=ssq_ap, in_=ssq_ap), r=[tag], w=[tag])

    def load_weight_bf16(dst_view, src_ap, nk, ncols, dst_tags, engs=("act", "dve", "pool")):
        srcv = src_ap.rearrange("(k p) n -> p k n", p=128)
        cpp = 4096 // nk
        c0 = 0
        i = 0
        while c0 < ncols:
            cw = min(cpp, ncols - c0)
            part = i % 2
            sv = stg(part)[:, 0:nk * cw].rearrange("p (k n) -> p k n", k=nk)
            P.dma("sp", sv, srcv[:, :, c0:c0 + cw], w=[f"R2_{part}"])
            en = engs[i % len(engs)]
            P.op(en, lambda e, sv=sv, c0=c0, cw=cw, en=en: (e.copy if en == "act" else e.tensor_copy)(
                out=dst_view[:, :, c0:c0 + cw], in_=sv),
                 [f"R2_{part}"], None, wacc=dst_tags)
            c0 += cw
            i += 1

    def layer_start(l):
        even = (l % 2 == 0)
        j = l // 2
        P.barrier(lambda e: e.memset(sm[:, 62:63], 0.0))
        if l > 0:
            P.pool(lambda e: e.tensor_copy(out=rows[:, 6, :], in_=rows[:, 5, :]), r=["rows5"], w=["rows6"])
        P.dve(lambda e: e.memset(tmpf[:, 0:128], 1.0), w=["tmpf"])
        for k in range(8):
            P.dve(lambda e, k=k: e.tensor_scalar(out=condbc[:, k, :], in0=tmpf[:, 0:128], scalar1=cTs[:, k:k + 1],
                                                 scalar2=None, op0=ALU.mult), r=["tmpf", "cTs"], w=["junk"])
        aw = ada_w[l].rearrange("(k p) n -> p k n", p=128)
        for cch in range(12):
            part = cch % 2
            sv = stg(part)[:, :].rearrange("p (k n) -> p k n", k=8)
            P.dma("sp", sv, aw[:, :, cch * 512:(cch + 1) * 512], w=[f"R2_{part}"])
            P.dma("sp", adab[:], bc(ada_b[l:l + 1, cch * 512:(cch + 1) * 512]), w=["m1"])
            pm = pM[cch % 2]
            for k in range(8):
                mm(pm[:, :], condbc[:, k, :], sv[:, k, :], start=(k == 0), stop=(k == 7),
                   r=["junk", f"R2_{part}"], w=[f"pM{cch % 2}"])
            rr = cch // 2
            hf = cch % 2
            P.dve(lambda e, pm=pm, rr=rr, hf=hf: e.tensor_tensor(out=rows[:, rr, hf * 512:(hf + 1) * 512], in0=pm[:, :],
                                                                  in1=adab[:], op=ALU.add),
                  r=[f"pM{cch % 2}", "m1"], w=[f"rows{rr}"])
        for rr, nw in ((1, norm1_w), (4, norm2_w)):
            P.dma("sp", tmpf[:], bc(nw[l:l + 1, :]), w=["tmpf"])
            P.dve(lambda e, rr=rr: e.scalar_tensor_tensor(out=rows[:, rr, :], in0=rows[:, rr, :], scalar=1.0, in1=tmpf[:],
                                                          op0=ALU.add, op1=ALU.mult),
                  r=[f"rows{rr}", "tmpf"], w=[f"rows{rr}"])
        P.dma("sp", tmpf[:, 0:288].rearrange("p (k n) -> p k n", k=8), moe_wr[l].rearrange("(k p) n -> p k n", p=128),
              w=["tmpf"])
        P.dve(lambda e: e.tensor_copy(out=WR[:], in_=tmpf[:, 0:288].rearrange("p (k n) -> p k n", k=8)),
              r=["tmpf"], w=["WR"])
        P.dma("sp", brow[:], bc(moe_br[l:l + 1, :]), w=["brow"])
        P.dve(lambda e: e.memset(cbase[:], 0.0), w=["cbase"])
        P.dma("sp", TAB, tabinit_in, w=["TAB"])
        win = even_w_in[j] if even else odd_w_in[j]
        ncols = 2320 if even else 3072
        wo = even_w_out[j] if even else odd_w_out[j]
        load_weight_bf16(R1w[:, :, 0:ncols], win, 8, ncols, WINT)
        load_weight_bf16(WOUT[:, :, :], wo, 8, 1024, ["WOUTt"])
        if even:
            P.dma("sp", tmpf[:, 0:64], bc(a_q_norm[j:j + 1, :]), w=["tmpf"])
            P.dma("sp", tmpf[:, 64:128], bc(a_k_norm[j:j + 1, :]), w=["tmpf2"])
            for i in range(10):
                src = tmpf[:, 0:64] if i < 8 else tmpf[:, 64:128]
                P.dve(lambda e, i=i, src=src: e.tensor_copy(out=wrow[:, i * 64:(i + 1) * 64], in_=src),
                      r=["tmpf", "tmpf2"], w=["wrow"])
            P.dma("sp", gbias[:], bc(b_gate_bias[j:j + 1, :]), w=["gbias"])
            P.dve(lambda e: e.memset(gupf[:, :], 0.0), w=["gupf"])
            P.dma("sp", gupf[0:16, :], b_gate_up[j], w=["gupf"])
            P.dve(lambda e: e.tensor_copy(out=gupb[:, :], in_=gupf[:, :]), r=["gupf"], w=["gupb"])
            P.dve(lambda e: e.memset(abp[:, :], 0.0), w=["abp"])
            P.dma("sp", onorm1[:, :], bc(b_out_norm[j:j + 1, :]), w=["onorm4"])
            P.dma("sp", esink[:], bc(a_sinks[j:j + 1, :]), w=["esink"])
            P.act(lambda e: e.activation(out=esink[:], in_=esink[:], func=AF.Exp), r=["esink"], w=["esink"])
            P.dve(lambda e: e.memset(St0[:], 0.0), w=["St0"])
            P.dve(lambda e: e.memset(qdm[:], 0.0), w=["qdm"])
            for i in range(2):
                P.dve(lambda e, i=i: e.memset(vaS[i][:], 1.0), w=[f"vaS{i}"])
        else:
            P.dma("sp", wrow[:, 0:128], bc(c_q_norm[j:j + 1, :]), w=["wrow"])
            P.dma("sp", wrow[:, 128:256], bc(c_k_norm[j:j + 1, :]), w=["wrow2"])
            P.dve(lambda e: e.memset(vast[:], 1.0), w=["vast"])

    def load_x(l, t, xb):
        xs = xt[xb]
        tg = "xt0"
        rs = slice(t * 128, (t + 1) * 128)
        if l == 0:
            P.dma("sp", xs[:], x_in[rs, :], w=[tg])
        else:
            P.dma("sp", xs[:], XR[rs, :], r=[f"XR{t}"], w=[tg])
            P.dma("sp", m1[:], MO[rs, :], r=["MO"], w=["m1"])
            P.dma("sp", m2[:], MO[MOH + t * 128:MOH + (t + 1) * 128, :], r=["MO"], w=["junk"])
            P.pool(lambda e: e.tensor_tensor(out=m1[:], in0=m1[:], in1=m2[:], op=ALU.add), r=["m1", "junk"], w=["m1"])
            P.pool(lambda e: e.tensor_tensor(out=m1[:], in0=m1[:], in1=rows[:, 6, :], op=ALU.mult),
                   r=["m1", "rows6"], w=["m1"])
            P.dve(lambda e: e.tensor_tensor(out=xs[:], in0=xs[:], in1=m1[:], op=ALU.add), r=[tg, "m1"], w=[tg])

    def norm_mod(xs, xtag, grow, srow, outb, otag):
        P.act(lambda e: e.activation(out=junk[:], in_=xs[:], func=AF.Square, accum_out=sm[:, 0:1]),
              r=[xtag], w=["junk", "sm0"])
        rstd_from_ssq(sm[:, 0:1], 1, 1.0 / D, "sm0")
        P.dve(lambda e: e.scalar_tensor_tensor(out=tmpf[:], in0=xs[:], scalar=sm[:, 0:1], in1=rows[:, grow, :],
                                               op0=ALU.mult, op1=ALU.mult),
              r=[xtag, "sm0", f"rows{grow}"], w=["tmpf"])
        P.pool(lambda e: e.tensor_tensor(out=outb[:], in0=tmpf[:], in1=rows[:, srow, :], op=ALU.add),
               r=["tmpf", f"rows{srow}"], w=[otag])

    tcount = [0]

    def transpose8(src, stag, dst, dtag):
        i = tcount[0] % 2
        tcount[0] += 1
        for k in range(8):
            tp(pT[i][:, k, :], src[:, k * 128:(k + 1) * 128], identb[:], r=[stag, "identb"], w=[f"pT{i}"])
        P.act(lambda e: e.copy(out=dst[:], in_=pT[i][:]), r=[f"pT{i}"], w=[dtag])

    mcount = [0]

    def project(srcT, stag, wview, wtag, ncols, dst, dtag):
        c0 = 0
        while c0 < ncols:
            cw = min(512, ncols - c0)
            i = mcount[0] % 2
            mcount[0] += 1
            for k in range(8):
                mm(pM[i][:, 0:cw], srcT[:, k, :], wview[:, k, c0:c0 + cw], start=(k == 0), stop=(k == 7),
                   r=[stag, wtag], w=[f"pM{i}"])
            if i == 0:
                P.act(lambda e, i=i, c0=c0, cw=cw: e.copy(out=dst[:, c0:c0 + cw], in_=pM[i][:, 0:cw]),
                      r=[f"pM{i}"], w=[dtag])
            else:
                P.dve(lambda e, i=i, c0=c0, cw=cw: e.tensor_copy(out=dst[:, c0:c0 + cw], in_=pM[i][:, 0:cw]),
                      r=[f"pM{i}"], w=[dtag])
            c0 += cw

    def tail(l, t, xb, attsrc, atag):
        xs = xt[xb]
        tg = "xt0"
        rs = slice(t * 128, (t + 1) * 128)
        transpose8(attsrc, atag, hT, "hT")
        for c in range(2):
            i = mcount[0] % 2
            mcount[0] += 1
            for k in range(8):
                mm(pM[i][:, :], hT[:, k, :], WOUT[:, k, c * 512:(c + 1) * 512], start=(k == 0), stop=(k == 7),
                   r=["hT", "WOUTt"], w=[f"pM{i}"])
            P.dve(lambda e, i=i, c=c: e.tensor_tensor(out=tmpf[:, c * 512:(c + 1) * 512], in0=pM[i][:, :],
                                                      in1=rows[:, 2, c * 512:(c + 1) * 512], op=ALU.mult),
                  r=[f"pM{i}", "rows2"], w=["tmpf"])
        P.pool(lambda e: e.tensor_tensor(out=xs[:], in0=xs[:], in1=tmpf[:], op=ALU.add), r=[tg, "tmpf"], w=[tg])
        P.dma("sp", XR[rs, :], xs[:], r=[tg], w=[f"XR{t}"])
        norm_mod(xs, tg, 4, 3, h2b, "hb")
        P.dma("sp", H2[rs, :], h2b[:], r=["hb"], wacc=["H2"])
        transpose8(h2b, "hb", hT, "hT")
        i = mcount[0] % 2
        mcount[0] += 1
        for k in range(8):
            mm(pM[i][:, 0:36], hT[:, k, :], WR[:, k, :], start=(k == 0), stop=(k == 7), r=["hT", "WR"], w=[f"pM{i}"])
        P.dve(lambda e: e.tensor_tensor(out=lg[:], in0=pM[i][:, 0:36], in1=brow[:], op=ALU.add),
              r=[f"pM{i}", "brow"], w=["lg"])
        RT = ["rt"]
        V = P.dve
        V(lambda e: e.tensor_reduce(out=sm[:, 8:9], in_=lg[:, 0:4], axis=AX.X, op=ALU.max), r=["lg"], w=RT)
        V(lambda e: e.tensor_scalar(out=sm[:, 12:16], in0=lg[:, 0:4], scalar1=sm[:, 8:9], scalar2=None, op0=ALU.subtract),
          r=["lg"] + RT, w=RT)
        P.act(lambda e: e.activation(out=sm[:, 16:20], in_=sm[:, 12:16], func=AF.Exp, accum_out=sm[:, 9:10]), r=RT, w=RT)
        V(lambda e: e.reciprocal(out=sm[:, 9:10], in_=sm[:, 9:10]), r=RT, w=RT)
        V(lambda e: e.tensor_scalar(out=sm[:, 20:24], in0=sm[:, 12:16], scalar1=0.0, scalar2=None, op0=ALU.is_ge),
          r=RT, w=RT)
        V(lambda e: e.tensor_scalar(out=sm[:, 20:24], in0=sm[:, 20:24], scalar1=BIG, scalar2=-BIG, op0=ALU.mult,
                                    op1=ALU.add), r=RT, w=RT)
        V(lambda e: e.tensor_tensor(out=msk[:, :].rearrange("p (g e) -> p g e", g=4),
                                    in0=lg[:, 4:36].rearrange("p (g e) -> p g e", g=4),
                                    in1=sm[:, 20:24].unsqueeze(2).to_broadcast([128, 4, 8]), op=ALU.add),
          r=["lg"] + RT, w=["msk"])
        V(lambda e: e.max(out=top8[:], in_=msk[:]), r=["msk"], w=["top8"])
        V(lambda e: e.tensor_scalar(out=M12[:, 0, :], in0=msk[:], scalar1=top8[:, 0:1], scalar2=None, op0=ALU.is_equal),
          r=["msk", "top8"], w=["M12"])
        V(lambda e: e.tensor_scalar(out=M12[:, 1, :], in0=msk[:], scalar1=top8[:, 1:2], scalar2=None, op0=ALU.is_equal),
          r=["msk", "top8"], w=["M12"])
        V(lambda e: e.tensor_tensor(out=sm[:, 10:11], in0=top8[:, 1:2], in1=top8[:, 0:1], op=ALU.subtract),
          r=["top8"], w=RT)
        P.act(lambda e: e.activation(out=sm[:, 10:11], in_=sm[:, 10:11], func=AF.Exp), r=RT, w=RT)
        V(lambda e: e.tensor_scalar(out=sm[:, 10:11], in0=sm[:, 10:11], scalar1=1.0, scalar2=None, op0=ALU.add), r=RT, w=RT)
        V(lambda e: e.reciprocal(out=sm[:, 10:11], in_=sm[:, 10:11]), r=RT, w=RT)
        rc = recall[:, t, :, :]
        rtag = "R2_0"
        V(lambda e: e.tensor_tensor(out=rc[:, 0, 2:3], in0=sm[:, 10:11], in1=sm[:, 9:10], op=ALU.mult), r=RT, w=[rtag])
        V(lambda e: e.tensor_tensor(out=rc[:, 1, 2:3], in0=sm[:, 9:10], in1=rc[:, 0, 2:3], op=ALU.subtract),
          r=RT + [rtag], w=[rtag])
        V(lambda e: e.tensor_scalar(out=rc[:, 0, 0:2], in0=pidx[:, 0:1].to_broadcast([128, 2]), scalar1=float(t * 128),
                                    scalar2=None, op0=ALU.add), r=["pidx"], w=[rtag])
        V(lambda e: e.tensor_scalar(out=rc[:, 1, 0:1], in0=pidx[:, 0:1], scalar1=float(t * 128), scalar2=None,
                                    op0=ALU.add), r=["pidx"], w=[rtag])
        V(lambda e: e.tensor_scalar(out=rc[:, 1, 1:2], in0=pidx[:, 0:1], scalar1=float(t * 128 + MOH), scalar2=None,
                                    op0=ALU.add), r=["pidx"], w=[rtag])
        V(lambda e: e.memset(rc[:, :, 3:4], 0.0), w=[rtag])
        V(lambda e: e.tensor_tensor(out=Mb[:], in0=M12[:, 0, :], in1=M12[:, 1, :], op=ALU.add), r=["M12"], w=["Mb"])
        V(lambda e: e.tensor_copy(out=M12all[:, t, :, :], in_=M12[:]), r=["M12"], w=[rtag])
        j_ = mcount[0] % 2
        mcount[0] += 1
        mm(pM[j_][:, 0:32], strib[:], Mb[:], r=["strib", "Mb"], w=[f"pM{j_}"])
        mm(pM[j_][:, 64:96], onesb[:], Mb[:], r=["onesb", "Mb"], w=[f"pM{j_}"])
        V(lambda e: e.tensor_tensor(out=Cc[:], in0=pM[j_][:, 0:32], in1=cbase[:], op=ALU.add),
          r=[f"pM{j_}", "cbase"], w=["Cc"])
        V(lambda e: e.tensor_tensor(out=cbase[:], in0=pM[j_][:, 64:96], in1=cbase[:], op=ALU.add),
          r=[f"pM{j_}", "cbase"], w=["cbase"])
        V(lambda e: e.tensor_tensor(out=prod[:], in0=M12[:], in1=Cc[:, :].unsqueeze(1).to_broadcast([128, 2, 32]),
                                    op=ALU.mult), r=["M12", "Cc"], w=["prod"])
        V(lambda e: e.tensor_reduce(out=slall[:, t, :], in_=prod[:], axis=AX.X, op=ALU.add), r=["prod"], w=[rtag])

    def route_finalize(l):
        P.barrier(lambda e: e.memset(sm[:, 62:63], 0.0))
        V = P.dve
        FT = ["rfin"]
        for half in range(2):
            es_ = slice(half * 16, (half + 1) * 16)
            t3 = tmpf[:, :].rearrange("p (a b) -> p a b", a=16)
            V(lambda e: e.tensor_tensor(out=t3, in0=cbase[:, es_].unsqueeze(2).to_broadcast([128, 16, 64]),
                                        in1=rcst[:, 0:64].unsqueeze(1).to_broadcast([128, 16, 64]), op=ALU.is_gt),
              r=["cbase", "rcst"], w=["tmpf"])
            V(lambda e: e.tensor_reduce(out=rtab[:, 0, es_], in_=t3, axis=AX.X, op=ALU.add), r=["tmpf"], w=FT)
        V(lambda e: e.tensor_scalar(out=rtab[:, 1, :], in0=rtab[:, 0, :], scalar1=128.0, scalar2=None, op0=ALU.mult),
          r=FT, w=FT)
        V(lambda e: e.memset(rtab[:, 5, :], 0.0), w=FT)
        V(lambda e: e.tensor_tensor_scan(out=rtab[:, 2, :], data0=rtab[:, 1, :], data1=rtab[:, 5, :], initial=0.0,
                                         op0=ALU.add, op1=ALU.add), r=FT, w=FT)
        V(lambda e: e.tensor_tensor(out=rtab[:, 3, :], in0=rtab[:, 2, :], in1=rtab[:, 1, :], op=ALU.subtract),
          r=FT, w=FT)
        V(lambda e: e.memset(blkE[:, :], 0.0), w=["blkE"])
        for ex in range(32):
            V(lambda e, ex=ex: e.scalar_tensor_tensor(out=blkE[:, :], in0=rcst[:, 64:160], scalar=rtab[:, 2, ex:ex + 1],
                                                      in1=blkE[:, :], op0=ALU.is_ge, op1=ALU.add),
              r=FT + ["rcst", "blkE"], w=["blkE"])
        V(lambda e: e.tensor_scalar(out=blkE[:, :], in0=blkE[:, :], scalar1=31.0, scalar2=float(32 * l), op0=ALU.min,
                                    op1=ALU.add), r=["blkE"], w=["blkE"])
        V(lambda e: e.tensor_scalar(out=widx1f[:, 0, :], in0=blkE[:, :], scalar1=128.0, scalar2=pidx[:, 0:1],
                                    op0=ALU.mult, op1=ALU.add), r=["blkE", "pidx"], w=["widx1f"])
        V(lambda e: e.memset(widx2f[:, 0, 0:1], 0.0), w=["widx2f"])
        V(lambda e: e.tensor_tensor(out=widx2f[:, 0, 1:96], in0=blkE[:, 1:96], in1=blkE[:, 0:95], op=ALU.is_equal),
          r=["blkE"], w=["widx2f"])
        V(lambda e: e.scalar_tensor_tensor(out=widx1f[:, 0, :], in0=widx2f[:, 0, :], scalar=268435456.0, in1=widx1f[:, 0, :],
                                           op0=ALU.mult, op1=ALU.add), r=["widx1f", "widx2f"], w=["widx1f"])
        V(lambda e: e.tensor_copy(out=widx1i[:, 0, :], in_=widx1f[:, 0, :]), r=["widx1f"], w=["widx1i"])
        for t in range(NT):
            rb_ = t % 2
            di = desti[rb_]
            dtag = f"desti{rb_}"
            V(lambda e, t=t: e.tensor_tensor(out=prod[:], in0=M12all[:, t, :, :],
                                             in1=rtab[:, 3, :].unsqueeze(1).to_broadcast([128, 2, 32]), op=ALU.mult),
              r=["R2_0"] + FT, w=["prod"])
            V(lambda e: e.tensor_reduce(out=sm[:, 26:28], in_=prod[:], axis=AX.X, op=ALU.add), r=["prod"], w=["rt"])
            V(lambda e, t=t: e.tensor_tensor(out=sm[:, 26:28], in0=sm[:, 26:28], in1=slall[:, t, :], op=ALU.add),
              r=["rt", "R2_0"], w=["rt"])
            V(lambda e, di=di: e.tensor_copy(out=di[:], in_=sm[:, 26:28]), r=["rt"], w=[dtag])
            for k in range(2):
                P.op("pool", lambda e, k=k, di=di, t=t: e.indirect_dma_start(
                    out=TAB, out_offset=bass.IndirectOffsetOnAxis(ap=di[:, k:k + 1], axis=0), in_=recall[:, t, k, :],
                    in_offset=None), ["R2_0", dtag], None, dma=True, wacc=["TAB"])

    def even_layer(l):
        for t in range(DBG.get("tiles", NT)):
            xb = t % 2
            cur = t % 2
            prv = 1 - cur
            load_x(l, t, xb)
            norm_mod(xt[xb], "xt0", 1, 0, hb, "hb")
            transpose8(hb, "hb", hT, "hT")
            project(hT, "hT", R1w, WINT, 2320, proj, "proj")
            if DBG.get("cut") == 1:
                P.dma("sp", out[t * 128:(t + 1) * 128, :], proj[:, 0:1024], r=["proj"], w=[f"out{t}"])
                continue
            P.dve(lambda e: e.tensor_tensor(out=tmpf[:, 0:640], in0=proj[:, 0:640], in1=proj[:, 0:640], op=ALU.mult),
                  r=["proj"], w=["tmpf"])
            P.dve(lambda e: e.tensor_reduce(out=sm[:, 32:42], in_=tmpf[:, 0:640].rearrange("p (g d) -> p g d", g=10),
                                            axis=AX.X, op=ALU.add), r=["tmpf"], w=["sm32"])
            rstd_from_ssq(sm[:, 32:42], 10, 1.0 / 64, "sm32")
            P.dve(lambda e: e.tensor_tensor(out=tmpf[:, 0:640].rearrange("p (g d) -> p g d", g=10),
                                            in0=proj[:, 0:640].rearrange("p (g d) -> p g d", g=10),
                                            in1=sm[:, 32:42].unsqueeze(2).to_broadcast([128, 10, 64]), op=ALU.mult),
                  r=["proj", "sm32"], w=["tmpf"])
            P.pool(lambda e: e.tensor_tensor(out=qkb[:], in0=tmpf[:, 0:512], in1=wrow[:, 0:512], op=ALU.mult),
                   r=["tmpf", "wrow"], w=["qkb"])
            for hf in range(2):
                P.pool(lambda e, hf=hf: e.tensor_tensor(out=kdup[:, :, hf * 64:(hf + 1) * 64],
                                                        in0=tmpf[:, 512:640].rearrange("p (g d) -> p g d", g=2),
                                                        in1=wrow[:, 512:640].rearrange("p (g d) -> p g d", g=2),
                                                        op=ALU.mult), r=["tmpf", "wrow"], w=["kdup"])
            P.act(lambda e: e.copy(out=vaS[cur][:, :, 0:64], in_=proj[:, 640:768].rearrange("p (g d) -> p g d", g=2)),
                  r=["proj"], w=[f"vaS{cur}"])
            i = tcount[0] % 2
            tcount[0] += 1
            for pr in range(4):
                tp(pT[i][:, pr, :], qkb[:, pr * 128:(pr + 1) * 128], identb[:], r=["qkb", "identb"], w=[f"pT{i}"])
            for g in range(2):
                tp(pT[i][:, 4 + g, :], kdup[:, g, :], identb[:], r=["kdup", "identb"], w=[f"pT{i}"])
            P.act(lambda e, i=i: e.copy(out=qkT[cur][:, :, :], in_=pT[i][:, 0:6, :]), r=[f"pT{i}"], w=[f"qkT{cur}"])
            if DBG.get("cut") == 2:
                P.dve(lambda e: e.tensor_copy(out=tmpf[:, 0:768], in_=qkT[cur][:, :, :].rearrange("p a b -> p (a b)")), r=[f"qkT{cur}"], w=["tmpf"])
                P.dma("sp", out[t * 128:(t + 1) * 128, 0:768], tmpf[:, 0:768], r=["tmpf"], w=[f"out{t}"])
                continue
            for hg in range(2):
                po = pO[hg]
                for hh in range(4):
                    h = hg * 4 + hh
                    g = h // 4
                    pr = h // 2
                    b0 = (h % 2) * 64
                    psx = pS[h % 2]
                    mm(psx[:, 0:128], qkT[cur][b0:b0 + 64, 4 + g, :], qkT[cur][b0:b0 + 64, pr, :],
                       r=[f"qkT{cur}"], w=[f"pS{h % 2}"])
                    if t > 0:
                        mm(psx[:, 128:256], qkT[prv][b0:b0 + 64, 4 + g, :], qkT[cur][b0:b0 + 64, pr, :],
                           r=[f"qkT{cur}", f"qkT{prv}"], w=[f"pS{h % 2}"])
                    P.dve(lambda e, psx=psx, h=h: e.scalar_tensor_tensor(out=sc[:], in0=psx[:, 0:256], scalar=0.125,
                                                                         in1=BS[:, h, :], op0=ALU.mult, op1=ALU.add),
                          r=[f"pS{h % 2}", "BS"], w=["sc"])
                    pe_ = pexp[h % 2]
                    P.act(lambda e, pe_=pe_: e.activation(out=pe_[:], in_=sc[:], func=AF.Exp), r=["sc"], w=[f"pexp{h % 2}"])
                    mm(po[:, hh * 65:(hh + 1) * 65], pe_[:, 0:128], vaS[cur][:, g, :], start=True, stop=(t == 0),
                       r=[f"pexp{h % 2}", f"vaS{cur}"], w=[f"pO{hg}"])
                    if t > 0:
                        mm(po[:, hh * 65:(hh + 1) * 65], pe_[:, 128:256], vaS[prv][:, g, :], start=False, stop=True,
                           r=[f"pexp{h % 2}", f"vaS{prv}"], w=[f"pO{hg}"])
                pov = po[:, 0:260].rearrange("p (h c) -> p h c", h=4)
                P.dve(lambda e, pov=pov, hg=hg: e.tensor_tensor(out=sm[:, 44:48].unsqueeze(2), in0=pov[:, :, 64:65],
                                                                in1=esink[:, hg * 4:(hg + 1) * 4].unsqueeze(2), op=ALU.add),
                      r=[f"pO{hg}", "esink"], w=["sm44"])
                P.dve(lambda e: e.reciprocal(out=sm[:, 44:48], in_=sm[:, 44:48]), r=["sm44"], w=["sm44"])
                P.dve(lambda e, pov=pov, hg=hg: e.tensor_tensor(
                    out=att[:, hg * 256:(hg + 1) * 256].rearrange("p (h d) -> p h d", h=4), in0=pov[:, :, 0:64],
                    in1=sm[:, 44:48].unsqueeze(2).to_broadcast([128, 4, 64]), op=ALU.mult),
                    r=[f"pO{hg}", "sm44"], w=["att"])
            if DBG.get("cut") == 31:
                P.dve(lambda e: e.tensor_copy(out=tmpf[:, 0:256], in_=sc[:, :]), r=["sc"], w=["tmpf"])
                P.dve(lambda e: e.tensor_copy(out=tmpf[:, 256:516], in_=pO[1][:, 0:260]), r=["pO1"], w=["tmpf"])
                P.dve(lambda e: e.tensor_copy(out=tmpf[:, 520:528], in_=esink[:, :]), r=["esink"], w=["tmpf"])
                P.dve(lambda e: e.tensor_copy(out=tmpf[:, 528:532], in_=sm[:, 44:48]), r=["sm44"], w=["tmpf"])
                P.dve(lambda e: e.tensor_copy(out=tmpf[:, 532:788], in_=pexp[1][:, :]), r=["pexp1"], w=["tmpf"])
                P.dve(lambda e: e.tensor_copy(out=tmpf[:, 788:918], in_=vaS[cur][:, :, :].rearrange("p a b -> p (a b)")), r=[f"vaS{cur}"], w=["tmpf"])
                P.dma("sp", out[t * 128:(t + 1) * 128, :], tmpf[:, :], r=["tmpf"], w=[f"out{t}"])
                continue
            if DBG.get("cut") == 3:
                P.dve(lambda e: e.tensor_copy(out=tmpf[:, 0:512], in_=att[:, 0:512]), r=["att"], w=["tmpf"])
                P.dma("sp", out[t * 128:(t + 1) * 128, 0:512], tmpf[:, 0:512], r=["tmpf"], w=[f"out{t}"])
                continue
            P.dve(lambda e: e.tensor_copy(out=abp[:, 0:16], in_=proj[:, 2304:2320]), r=["proj"], w=["abp"])
            i = tcount[0] % 2
            tcount[0] += 1
            tp(pT[i][0:32, 0, :], abp[:, :], identb[:], r=["abp", "identb"], w=[f"pT{i}"])
            P.act(lambda e: e.copy(out=abT[:, :], in_=pT[i][0:32, 0, :]), r=[f"pT{i}"], w=["abT"])
            mm(pS[1][:, 0:256], abT[:, :], gupb[:, :], r=["abT", "gupb"], w=["pS1"])
            P.dve(lambda e: e.tensor_tensor(out=zt[:], in0=pS[1][:, 0:256], in1=gbias[:], op=ALU.add),
                  r=["pS1", "gbias"], w=["zt"])
            if DBG.get("cut") == 41:
                P.dve(lambda e: e.tensor_copy(out=tmpf[:, 0:256], in_=zt[:, :]), r=["zt"], w=["tmpf"])
                P.dma("sp", out[t * 128:(t + 1) * 128, 0:256], tmpf[:, 0:256], r=["tmpf"], w=[f"out{t}"])
                continue
            P.act(lambda e: e.activation(out=zt[:], in_=zt[:], func=AF.Exp, scale=-1.0), r=["zt"], w=["zt"])
            P.dve(lambda e: e.tensor_scalar(out=zt[:], in0=zt[:], scalar1=1.0, scalar2=None, op0=ALU.add), r=["zt"], w=["zt"])
            P.act(lambda e: e.activation(out=zt[:], in_=zt[:], func=AF.Ln), r=["zt"], w=["zt"])
            P.dve(lambda e: e.tensor_scalar(out=gneg[:], in0=zt[:], scalar1=-1.0 / 16.0, scalar2=None, op0=ALU.mult),
                  r=["zt"], w=["gneg"])
            if DBG.get("cut") == 42:
                P.dve(lambda e: e.tensor_copy(out=tmpf[:, 0:256], in_=gneg[:, :]), r=["gneg"], w=["tmpf"])
                P.dma("sp", out[t * 128:(t + 1) * 128, 0:256], tmpf[:, 0:256], r=["tmpf"], w=[f"out{t}"])
                continue
            mm(pS[0][:, 0:256], trif[:], gneg[:], r=["trif", "gneg"], w=["pS0"])
            mm(pS[0][:, 256:512], bonesf[:], gneg[:], r=["bonesf", "gneg"], w=["pS0"])
            P.act(lambda e: e.copy(out=bsb[:], in_=pS[0][:, 0:256]), r=["pS0"], w=["zt"])
            P.act(lambda e: e.activation(out=eb[:], in_=pS[0][:, 0:256], func=AF.Exp), r=["pS0"], w=["eb"])
            P.act(lambda e: e.activation(out=enb[:], in_=pS[0][:, 0:256], func=AF.Exp, scale=-1.0), r=["pS0"], w=["enb"])
            P.dve(lambda e: e.tensor_tensor(out=ekd[:], in0=pS[0][:, 256:512], in1=bsb[:], op=ALU.subtract),
                  r=["pS0", "zt"], w=["ekd"])
            P.act(lambda e: e.activation(out=ekd[:], in_=ekd[:], func=AF.Exp), r=["ekd"], w=["ekd"])
            if DBG.get("cut") == 43:
                P.dve(lambda e: e.tensor_copy(out=tmpf[:, 0:256], in_=ekd[:, :]), r=["ekd"], w=["tmpf"])
                P.dma("sp", out[t * 128:(t + 1) * 128, 0:256], tmpf[:, 0:256], r=["tmpf"], w=[f"out{t}"])
                continue
            P.dve(lambda e: e.scalar_tensor_tensor(out=qd[:], in0=proj[:, 768:1024], scalar=0.125, in1=eb[:],
                                                   op0=ALU.mult, op1=ALU.mult), r=["proj", "eb"], w=["qd"])
            P.pool(lambda e: e.tensor_tensor(out=kd[:], in0=proj[:, 1024:1280], in1=enb[:], op=ALU.mult),
                   r=["proj", "enb"], w=["kd"])
            P.pool(lambda e: e.tensor_tensor(out=ku[:], in0=proj[:, 1024:1280], in1=ekd[:], op=ALU.mult),
                   r=["proj", "ekd"], w=["ku"])
            P.pool(lambda e: e.tensor_copy(out=vbb[:], in_=proj[:, 1280:1792]), r=["proj"], w=["vbb"])
            P.act(lambda e: e.activation(out=sgn[:], in_=proj[:, 1792:2304], func=AF.Silu), r=["proj"], w=["sgn"])
            P.dve(lambda e: e.tensor_tensor(out=sgn[:, :].rearrange("p (h d) -> p h d", h=4),
                                             in0=sgn[:, :].rearrange("p (h d) -> p h d", h=4),
                                             in1=onorm1[:, :].unsqueeze(1).to_broadcast([128, 4, 128]), op=ALU.mult),
                   r=["sgn", "onorm4"], w=["sgn"])
            P.dve(lambda e: e.tensor_copy(out=gnb[:, :], in_=gneg[:, :]), r=["gneg"], w=["gnb"])
            for hh in range(4):
                mm(pS[1][0:64, hh * 128:(hh + 1) * 128], gnb[:, hh * 64:(hh + 1) * 64], bonesb[:, :], r=["gnb", "bonesb"], w=["pS1"])
            P.act(lambda e: e.activation(out=dec[:, :].rearrange("p (h c) -> p h c", h=4),
                                         in_=pS[1][0:64, :].rearrange("p (h c r) -> p h c r", h=4, c=2)[:, :, :, 0],
                                         func=AF.Exp), r=["pS1"], w=["dec"])
            if DBG.get("cut") == 4:
                P.dve(lambda e: e.tensor_copy(out=tmpf[:, 0:256], in_=qd[:, :]), r=["qd"], w=["tmpf"])
                P.dve(lambda e: e.tensor_copy(out=tmpf[:, 256:512], in_=ku[:, :]), r=["ku"], w=["tmpf"])
                P.dve(lambda e: e.tensor_copy(out=tmpf[0:64, 512:520], in_=dec[:, :]), r=["dec"], w=["tmpf"])
                P.dma("sp", out[t * 128:(t + 1) * 128, 0:520], tmpf[:, 0:520], r=["tmpf"], w=[f"out{t}"])
                continue
            i = tcount[0] % 2
            tcount[0] += 1
            for hh in range(4):
                tp(pT[i][0:64, hh, :], qd[:, hh * 64:(hh + 1) * 64], identb[:], r=["qd", "identb"], w=[f"pT{i}"])
                tp(pT[i][0:64, 4 + hh, :], kd[:, hh * 64:(hh + 1) * 64], identb[:], r=["kd", "identb"], w=[f"pT{i}"])
            P.act(lambda e, i=i: e.copy(out=qdT[:], in_=pT[i][0:64, 0:4, :]), r=[f"pT{i}"], w=["qdT"])
            P.act(lambda e, i=i: e.copy(out=kdT[:], in_=pT[i][0:64, 4:8, :]), r=[f"pT{i}"], w=["kdT"])
            P.dve(lambda e, i=i: e.tensor_copy(out=qdm[:, :, 0, 0:64], in_=pT[i][0:64, 0:4, 0:64]), r=[f"pT{i}"], w=["qdm"])
            P.dve(lambda e, i=i: e.tensor_copy(out=qdm[:, :, 1, 64:128], in_=pT[i][0:64, 0:4, 64:128]),
                  r=[f"pT{i}"], w=["qdm"])
            P.act(lambda e: e.copy(out=St0b[:], in_=St0[:]), r=["St0"], w=["St0b"])
            for hh in range(4):
                vs = slice(hh * 128, (hh + 1) * 128)
                ks = slice(hh * 64, (hh + 1) * 64)
                pa = pS[hh % 2]
                mm(pa[:, 0:128], kdT[:, hh, :], qdT[:, hh, :], r=["kdT", "qdT"], w=[f"pS{hh % 2}"])
                P.dve(lambda e, pa=pa: e.tensor_tensor(out=attT[:], in0=pa[:, 0:128], in1=trif[:], op=ALU.mult),
                      r=[f"pS{hh % 2}", "trif"], w=["attT"])
                mm(pO[0][0:64, vs], ku[0:64, ks], vbb[0:64, vs], r=["ku", "vbb"], w=["pO0"])
                mm(pO[1][0:64, vs], ku[64:128, ks], vbb[64:128, vs], r=["ku", "vbb"], w=["pO1"])
                P.dve(lambda e, hh=hh: e.scalar_tensor_tensor(out=St1[:, hh, :], in0=St0[:, hh, :],
                                                              scalar=dec[:, 2 * hh:2 * hh + 1], in1=pO[0][0:64, vs],
                                                              op0=ALU.mult, op1=ALU.add),
                      r=["St0", "dec", "pO0"], w=["St1"])
                P.act(lambda e, hh=hh: e.copy(out=St1b[:, hh, :], in_=St1[:, hh, :]), r=["St1"], w=["St1b"])
                mm(pa[:, 256:384], attT[:, :], vbb[:, vs], start=True, stop=False, r=["attT", "vbb"], w=[f"pS{hh % 2}"])
                mm(pa[:, 256:384], qdm[:, hh, 0, :], St0b[:, hh, :], start=False, stop=False, r=["qdm", "St0b"],
                   w=[f"pS{hh % 2}"])
                mm(pa[:, 256:384], qdm[:, hh, 1, :], St1b[:, hh, :], start=False, stop=True, r=["qdm", "St1b"],
                   w=[f"pS{hh % 2}"])
                P.dve(lambda e, hh=hh: e.scalar_tensor_tensor(out=St0[:, hh, :], in0=St1[:, hh, :],
                                                              scalar=dec[:, 2 * hh + 1:2 * hh + 2],
                                                              in1=pO[1][0:64, vs], op0=ALU.mult, op1=ALU.add),
                      r=["St1", "dec", "pO1"], w=["St0"])
                P.act(lambda e, pa=pa: e.activation(out=junk[:, 0:128], in_=pa[:, 256:384], func=AF.Square,
                                                    accum_out=sm[:, 50:51]), r=[f"pS{hh % 2}"], w=["junk", "sm50"])
                rstd_from_ssq(sm[:, 50:51], 1, 1.0 / 128, "sm50")
                P.dve(lambda e, pa=pa, hh=hh: e.scalar_tensor_tensor(out=att[:, 512 + hh * 128:512 + (hh + 1) * 128],
                                                                     in0=pa[:, 256:384], scalar=sm[:, 50:51],
                                                                     in1=sgn[:, hh * 128:(hh + 1) * 128], op0=ALU.mult,
                                                                     op1=ALU.mult),
                      r=[f"pS{hh % 2}", "sm50", "sgn"], w=["att"])
            if DBG.get("notail"):
                P.dve(lambda e: e.tensor_copy(out=tmpf[:], in_=att[:]), r=["att"], w=["tmpf"])
                P.dma("sp", out[t * 128:(t + 1) * 128, :], tmpf[:], r=["tmpf"], w=[f"out{t}"])
                continue
            tail(l, t, xb, att, "att")
            if DBG.get("dumph2"):
                P.dve(lambda e: e.tensor_copy(out=tmpf[:], in_=hb[:]), r=["hb"], w=["tmpf"])
                P.dma("sp", out[t * 128:(t + 1) * 128, :], tmpf[:], r=["tmpf"], w=[f"out{t}"])

    def odd_layer(l):
        P.dve(lambda e: e.memset(kmsum[:], 0.0), w=["kmsum"])
        for t in range(NT):
            xb = t % 2
            rs = slice(t * 128, (t + 1) * 128)
            load_x(l, t, xb)
            if l > 0:
                P.dma("sp", XR[rs, :], xt[xb][:], r=["xt0"], w=[f"XR{t}"])
            norm_mod(xt[xb], "xt0", 1, 0, hb, "hb")
            transpose8(hb, "hb", hT, "hT")
            project(hT, "hT", R1w, WINT, 2048, proj, "proj")
            for half in range(2):
                src = proj[:, half * 1024:(half + 1) * 1024]
                P.dve(lambda e, src=src: e.tensor_tensor(out=tmpf[:], in0=src, in1=src, op=ALU.mult), r=["proj"], w=["tmpf"])
                P.dve(lambda e: e.tensor_reduce(out=sm[:, 32:40], in_=tmpf[:, :].rearrange("p (g d) -> p g d", g=8),
                                                axis=AX.X, op=ALU.add), r=["tmpf"], w=["sm32"])
                rstd_from_ssq(sm[:, 32:40], 8, 1.0 / 128, "sm32")
                P.dve(lambda e, src=src: e.tensor_tensor(out=tmpf[:, :].rearrange("p (g d) -> p g d", g=8),
                                                         in0=src.rearrange("p (g d) -> p g d", g=8),
                                                         in1=sm[:, 32:40].unsqueeze(2).to_broadcast([128, 8, 128]),
                                                         op=ALU.mult), r=["proj", "sm32"], w=["tmpf"])
                P.pool(lambda e, half=half: e.tensor_tensor(
                    out=qnb[:, half * 1024:(half + 1) * 1024].rearrange("p (g d) -> p g d", g=8),
                    in0=tmpf[:, :].rearrange("p (g d) -> p g d", g=8),
                    in1=wrow[:, half * 128:(half + 1) * 128].unsqueeze(1).to_broadcast([128, 8, 128]), op=ALU.mult),
                    r=["tmpf", "wrow", "wrow2"], w=["qnb"])
            project(hT, "hT", R1w[:, :, 2048:3072], WINT, 1024, proj, "proj")
            P.act(lambda e: e.copy(out=vast[:, :, 0:128], in_=proj[:, 0:1024].rearrange("p (g d) -> p g d", g=8)),
                  r=["proj"], w=["vast"])
            P.dma("sp", VA[rs, :, :].rearrange("t h c -> t (h c)"), vast[:, :, :].rearrange("p h c -> p (h c)"), r=["vast"], wacc=["VA"])
            for half in range(2):
                dstD = QT if half == 0 else KT
                transpose8(qnb[:, half * 1024:(half + 1) * 1024], "qnb", hT, "hT")
                P.dma("sp", dstD[:, :, rs].rearrange("h d t -> d h t"), hT[:], r=["hT"], wacc=["QT" if half == 0 else "KT"])
                if half == 1:
                    P.dve(lambda e, t=t: e.tensor_reduce(out=kmsum[:, :, t], in_=hT[:], axis=AX.X, op=ALU.add),
                          r=["hT"], w=["kmsum"])
        P.dve(lambda e: e.tensor_tensor(out=tmpf[:, 0:128].rearrange("p (h b) -> p h b", h=8), in0=kmsum[:, :, :].rearrange("p h (b two) -> p h b two", two=2)[:, :, :, 0],
                                        in1=kmsum[:, :, :].rearrange("p h (b two) -> p h b two", two=2)[:, :, :, 1], op=ALU.add), r=["kmsum"], w=["tmpf"])
        P.dve(lambda e: e.tensor_scalar(out=kmean[:], in0=tmpf[:, 0:128].rearrange("p (h b) -> p h b", h=8),
                                        scalar1=1.0 / 256, scalar2=None, op0=ALU.mult), r=["tmpf"], w=["kmean"])
        pcount = [0]
        ocount = [0]
        scount = [0]
        pTf = [pT[i][:, :, :].rearrange("p a b -> p (a b)").bitcast(F32) for i in range(2)]
        sbanks = ((pS[0], "pS0"), (pS[1], "pS1"), (pTf[0], "pT0"), (pTf[1], "pT1"))
        for hgp in range(4):
            h0 = hgp * 2
            P.dma("sp", KTs, KT[h0:h0 + 2, :, :].rearrange("h d t -> d h t"), r=["KT"], w=["R2_0"])
            P.dma("sp", VAs2, VA[:, h0:h0 + 2, :].rearrange("(t p) h c -> p t (h c)", p=128), r=["VA"], w=["R2_1"])
            for qb_ in range(16):
                qs = slice(qb_ * 256, (qb_ + 1) * 256)
                P.dma("sp", qTt[:], QT[h0:h0 + 2, :, qs].rearrange("h d t -> d h t"), r=["QT"], w=["qTt"])
                for hh in range(2):
                    h = h0 + hh
                    for qt in range(2):
                        mm(pM[0][:, qt * 16:(qt + 1) * 16], qTt[:, hh, qt * 128:(qt + 1) * 128], kmean[:, h, :],
                           r=["qTt", "kmean"], w=["pM0"])
                    P.dve(lambda e: e.memset(scs[:], NEG), w=["scs"])
                    if qb_ > 0:
                        P.dve(lambda e, qb_=qb_: e.tensor_copy(
                            out=scs[:, :, 0:qb_], in_=pM[0][:, 0:32].rearrange("p (q b) -> p q b", q=2)[:, :, 0:qb_]),
                            r=["pM0"], w=["scs"])
                    for qt in range(2):
                        P.dve(lambda e, qt=qt: e.max(out=top8[:], in_=scs[:, qt, :]), r=["scs"], w=["top8"])
                        P.dve(lambda e, qt=qt, hh=hh: e.tensor_scalar(out=sel[:, hh, qt, :], in0=scs[:, qt, :],
                                                                      scalar1=top8[:, 2:3], scalar2=None, op0=ALU.is_ge),
                              r=["scs", "top8"], w=["sel"])
                    combos = ((0, 2 * qb_, 0), (1, 2 * qb_, 1), (1, 2 * qb_ + 1, 0))
                    psx, ptag = sbanks[scount[0] % 4]
                    scount[0] += 1
                    for ci, (qt, kt, dl) in enumerate(combos):
                        mm(psx[:, ci * 128:(ci + 1) * 128], KTs[:, hh, kt * 128:(kt + 1) * 128],
                           qTt[:, hh, qt * 128:(qt + 1) * 128], r=["R2_0", "qTt"], w=[ptag])
                    for ci, (qt, kt, dl) in enumerate(combos):
                        P.dve(lambda e, psx=psx, h=h, dl=dl, ci=ci: e.scalar_tensor_tensor(
                            out=sco3[:, ci * 128:(ci + 1) * 128], in0=psx[:, ci * 128:(ci + 1) * 128], scalar=128 ** -0.5,
                            in1=(BS[:, h, 0:128] if dl == 0 else BM1[:, h, :]), op0=ALU.mult, op1=ALU.add),
                            r=[ptag, "BM", "BS"], w=["sco3"])
                    P.act(lambda e: e.activation(out=pown[:, :], in_=sco3[:, :], func=AF.Exp), r=["sco3"], w=["pown"])
                    po = pO[0]
                    mm(po[:, 0:129], pown[:, 0:128], VAs[:, 2 * qb_, hh, :], start=True, stop=True, r=["pown", "R2_1"], w=["pO0"])
                    mm(po[:, 129:258], pown[:, 128:256], VAs[:, 2 * qb_, hh, :], start=True, stop=False,
                       r=["pown", "R2_1"], w=["pO0"])
                    mm(po[:, 129:258], pown[:, 256:384], VAs[:, 2 * qb_ + 1, hh, :], start=False, stop=True,
                       r=["pown", "R2_1"], w=["pO0"])
                    P.dve(lambda e, hh=hh: e.tensor_copy(out=acc[:, hh, :, :],
                                                         in_=pO[0][:, 0:258].rearrange("p (q c) -> p q c", q=2)),
                          r=["pO0"], w=["acc"])
                    def stage1(jb, hh=hh, h=h, qb_=qb_):
                        pbs = []
                        for kk in range(2):
                            kt = 2 * jb + kk
                            psx, ptag = sbanks[scount[0] % 4]
                            scount[0] += 1
                            pb = pT2[pcount[0] % 4]
                            pbt = f"pT2_{pcount[0] % 4}"
                            sci = pcount[0] % 2
                            pcount[0] += 1
                            mm(psx[:, 0:256], KTs[:, hh, kt * 128:(kt + 1) * 128], qTt[:, hh, :], r=["R2_0", "qTt"], w=[ptag])
                            if kt == 2 * qb_ - 1:
                                P.dve(lambda e, psx=psx, h=h, sci=sci: e.scalar_tensor_tensor(
                                    out=sco_[sci][:, 0:128], in0=psx[:, 0:128], scalar=128 ** -0.5, in1=BM1[:, h, :],
                                    op0=ALU.mult, op1=ALU.add), r=[ptag, "BM"], w=[f"sco{sci}"])
                                P.act(lambda e, pb=pb, sci=sci: e.activation(out=pb[:, 0:128], in_=sco_[sci][:, 0:128], func=AF.Exp),
                                      r=[f"sco{sci}"], w=[pbt])
                                P.act(lambda e, pb=pb, psx=psx, h=h: e.activation(out=pb[:, 128:256], in_=psx[:, 128:256],
                                                                                   func=AF.Exp, bias=relfar[:, h:h + 1],
                                                                                   scale=128 ** -0.5),
                                      r=[ptag, "relfar"], w=[pbt])
                            else:
                                P.act(lambda e, pb=pb, psx=psx, h=h: e.activation(out=pb[:, 0:256], in_=psx[:, 0:256],
                                                                                   func=AF.Exp, bias=relfar[:, h:h + 1],
                                                                                   scale=128 ** -0.5),
                                      r=[ptag, "relfar"], w=[pbt])
                            pbs.append((pb, pbt, kt))
                        return pbs

                    def stage2(jb, pbs, hh=hh):
                        po, potag = ((pO[1], "pO1"), (pM[1], "pM1"))[ocount[0] % 2]
                        ocount[0] += 1
                        for qt in range(2):
                            for kk, (pb, pbt, kt) in enumerate(pbs):
                                mm(po[:, qt * 129:(qt + 1) * 129], pb[:, qt * 128:(qt + 1) * 128], VAs[:, kt, hh, :],
                                   start=(kk == 0), stop=(kk == 1), r=[pbt, "R2_1"], w=[potag])
                        for qt in range(2):
                            P.dve(lambda e, qt=qt, hh=hh, jb=jb, po=po: e.scalar_tensor_tensor(
                                out=acc[:, hh, qt, :], in0=po[:, qt * 129:(qt + 1) * 129], scalar=sel[:, hh, qt, jb:jb + 1],
                                in1=acc[:, hh, qt, :], op0=ALU.mult, op1=ALU.add), r=[potag, "sel", "acc"], w=["acc"])

                    if qb_ > 0:
                        cur_pbs = stage1(0)
                        for jb in range(qb_):
                            nxt = stage1(jb + 1) if jb + 1 < qb_ else None
                            stage2(jb, cur_pbs)
                            cur_pbs = nxt
                    for qt in range(2):
                        P.dve(lambda e, qt=qt, hh=hh: e.reciprocal(out=sm[:, 52:53], in_=acc[:, hh, qt, 128:129]),
                              r=["acc"], w=["sm52"])
                        P.dve(lambda e, qt=qt, hh=hh: e.tensor_scalar(out=attq[:, qt, hh * 128:(hh + 1) * 128],
                                                                      in0=acc[:, hh, qt, 0:128], scalar1=sm[:, 52:53],
                                                                      scalar2=None, op0=ALU.mult),
                              r=["acc", "sm52"], w=["attq"])
                for qt in range(2):
                    t = 2 * qb_ + qt
                    P.dma("sp", ATT[t * 128:(t + 1) * 128, h0 * 128:(h0 + 2) * 128], attq[:, qt, :], r=["attq"], wacc=["ATT"])
        for t in range(NT):
            xb = t % 2
            rs = slice(t * 128, (t + 1) * 128)
            if l == 0:
                P.dma("sp", xt[xb][:], x_in[rs, :], w=["xt0"])
            else:
                P.dma("sp", xt[xb][:], XR[rs, :], r=[f"XR{t}"], w=["xt0"])
            P.dma("sp", att[:], ATT[rs, :], r=["ATT"], w=["att"])
            tail(l, t, xb, att, "att")

    def expert_phase(l):
        w1r = moe_w1.rearrange("l e (p j) n -> (l e p) (j n)", j=8)
        w3r = moe_w3.rearrange("l e (p j) n -> (l e p) (j n)", j=8)
        w2r = moe_w2.rearrange("l e (p j) n -> (l e p) (j n)", j=4)
        s0f = stg(0)[:, :]
        s1f = stg(1)[:, :]
        s2f = WOUT[:, :, :].rearrange("p a b -> p (a b)").bitcast(F32)
        s0 = stg(0)[:, :].rearrange("p (k n) -> p k n", k=8)
        s1 = stg(1)[:, :].rearrange("p (k n) -> p k n", k=8)
        s2 = WOUT[:, :, :].rearrange("p a b -> p (a b)").bitcast(F32).rearrange("p (k n) -> p k n", k=4)
        nblk = DBG.get("nblk", NBLK)

        def stage_tok(b):
            tb_ = b % 3
            tb = tabt[tb_]
            ix = idxi[tb_]
            P.dma("sp", tb[:], TAB[b * 128:(b + 1) * 128, :], r=["TAB"], w=[f"tabt{tb_}"])
            P.dve(lambda e, tb=tb, ix=ix: e.tensor_copy(out=ix[:], in_=tb[:, 0:2]), r=[f"tabt{tb_}"], w=[f"idxi{tb_}"])
            P.op("pool", lambda e, ix=ix, tb_=tb_: e.indirect_dma_start(
                out=xg[tb_][:, :], out_offset=None, in_=H2,
                in_offset=bass.IndirectOffsetOnAxis(ap=ix[:, 0:1], axis=0)),
                ["H2", f"idxi{tb_}"], [f"xg{tb_}"], dma=True)

        def stage_w(b):
            if not DBG.get("nowdma"):
                for sv, wr_, tg in ((s0f, w1r, "R2_0"), (s1f, w3r, "R2_1"), (s2f, w2r, "WOUTt")):
                    P.op("pool", lambda e, b=b, sv=sv, wr_=wr_: e.indirect_dma_start(
                        out=sv, out_offset=None, in_=wr_,
                        in_offset=bass.IndirectOffsetOnAxis(ap=widx1i[:, 0, b:b + 1], axis=0),
                        bounds_check=_LazyReg(NLW * 32 * 128 - 1), oob_is_err=False),
                        ["widx1i"], [tg], dma=True)

        def stage_cast(b):
            eb_ = b % 2
            w1v, w3v, w2v = ew(eb_, 0), ew(eb_, 1), ew(eb_, 2)
            wt = [f"R1_{eb_}_{i}" for i in range(3)]
            P.act(lambda e, w1v=w1v: e.copy(out=w1v, in_=s0), r=["R2_0"], w=[wt[0]])
            P.dve(lambda e, w3v=w3v: e.tensor_copy(out=w3v, in_=s1), r=["R2_1"], w=[wt[1]])
            P.act(lambda e, w2v=w2v: e.copy(out=w2v, in_=s2), r=["WOUTt"], w=[wt[2]])

        def stage_b1(b):
            eb_ = b % 2
            tb_ = b % 3
            i = tcount[0] % 2
            tcount[0] += 1
            for k in range(8):
                tp(pT[i][:, k, :], xg[tb_][:, :].rearrange("t (p j) -> t j p", j=8)[:, k, :], identb[:],
                   r=[f"xg{tb_}", "identb"], w=[f"pT{i}"])
            P.dve(lambda e, i=i, eb_=eb_: e.tensor_copy(out=xgT[eb_][:, :, :], in_=pT[i][:]), r=[f"pT{i}"], w=[f"xgT{eb_}"])

        def stage_b(b):
            eb_ = b % 2
            tb_ = b % 3
            w1v, w3v, w2v = ew(eb_, 0), ew(eb_, 1), ew(eb_, 2)
            wt = [f"R1_{eb_}_{i}" for i in range(3)]
            tb = tabt[tb_]
            ix = idxi[tb_]
            hbanks = ((pM[0], "pM0"), (pM[1], "pM1"), (pO[0], "pO0"), (pO[1], "pO1"))
            for hc in range(4):
                hb_, htag = hbanks[hc]
                for k in range(8):
                    mm(hb_[:, 0:128], w1v[:, k, :].rearrange("p (m f) -> p f m", f=4)[:, hc, :], xgT[eb_][:, k, :],
                       start=(k == 0), stop=(k == 7), r=[f"xgT{eb_}", wt[0]], w=[htag])
                for k in range(8):
                    mm(hb_[:, 128:256], w3v[:, k, :].rearrange("p (m f) -> p f m", f=4)[:, hc, :], xgT[eb_][:, k, :],
                       start=(k == 0), stop=(k == 7), r=[f"xgT{eb_}", wt[1]], w=[htag])
            for hc in range(4):
                hb_, htag = hbanks[hc]
                P.act(lambda e, hb_=hb_, hc=hc: e.activation(out=s1t4[:, hc, :], in_=hb_[:, 0:128], func=AF.Silu),
                      r=[htag], w=[f"s1t{hc}"])
                P.dve(lambda e, hc=hc, eb_=eb_, hb_=hb_: e.tensor_tensor(out=actT[eb_][:, hc, :], in0=hb_[:, 128:256],
                                                                         in1=s1t4[:, hc, :], op=ALU.mult),
                      r=[htag, f"s1t{hc}"], w=[f"actT{eb_}"])
            ytag = ("m1", "junk")[eb_]
            for half in range(2):
                for hc in range(4):
                    mm(pS[half][:, :], actT[eb_][:, hc, :], w2v[:, hc, half * 512:(half + 1) * 512],
                       start=(hc == 0), stop=(hc == 3), r=[f"actT{eb_}", wt[2]], w=[f"pS{half}"])
                if half == 0:
                    P.act(lambda e, eb_=eb_, tb=tb: e.activation(out=yo[eb_][:, 0:512], in_=pS[0][:, :], func=AF.Copy,
                                                                 scale=tb[:, 2:3]),
                          r=["pS0", f"tabt{tb_}"], w=[ytag])
                else:
                    P.dve(lambda e, eb_=eb_, tb=tb: e.tensor_scalar(out=yo[eb_][:, 512:1024], in0=pS[1][:, :],
                                                                     scalar1=tb[:, 2:3], scalar2=None, op0=ALU.mult),
                          r=["pS1", f"tabt{tb_}"], w=[ytag])
            P.op("pool", lambda e, eb_=eb_, ix=ix: e.indirect_dma_start(
                out=MO, out_offset=bass.IndirectOffsetOnAxis(ap=ix[:, 1:2], axis=0), in_=yo[eb_][:], in_offset=None),
                [ytag, f"idxi{tb_}"], None, dma=True, wacc=["MO"])

        stage_tok(0)
        stage_w(0)
        stage_cast(0)
        if nblk > 1:
            stage_tok(1)
            stage_w(1)
        for b in range(nblk):
            stage_b1(b)
            if b + 1 < nblk:
                stage_cast(b + 1)
            if b + 2 < nblk:
                stage_tok(b + 2)
                stage_w(b + 2)
            stage_b(b)

    for l in range(n_layers):
        layer_start(l)
        if DBG.get("stage") == "A":
            for r_ in range(6):
                P.dma("sp", out[r_ * 128:(r_ + 1) * 128, :], rows[:, r_, :], r=[f"rows{r_}"], w=[f"out{r_}"])
            break
        if l % 2 == 0:
            even_layer(l)
        else:
            odd_layer(l)
        if not DBG.get("noexp"):
            route_finalize(l)
            expert_phase(l)
    if DBG.get("nofinal"):
        P.emit()
        es.close()
        return nc
    P.pool(lambda e: e.tensor_copy(out=rows[:, 6, :], in_=rows[:, 5, :]), r=["rows5"], w=["rows6"])
    P.barrier(lambda e: e.memset(sm[:, 62:63], 0.0))
    fsets = ((xt[0][:, :], m1[:, :], junk[:, :], ("xt0", "m1", "junk")),
             (tmpf[:, :], proj[:, 0:1024], proj[:, 1024:2048], ("tmpf", "fmA", "fmB")),
             (EV[:, 0:1024], EV[:, 1024:2048], EV[:, 2048:3072], ("fmC", "fmD", "fmE")))
    for t in range(NT):
        xs, ma, mb, (tx, ta, tb2) = fsets[t % 3]
        rs = slice(t * 128, (t + 1) * 128)
        P.dma("sp", xs, XR[rs, :], r=[f"XR{t}"], w=[tx])
        P.dma("sp", ma, MO[rs, :], r=["MO"], w=[ta])
        P.dma("sp", mb, MO[MOH + t * 128:MOH + (t + 1) * 128, :], r=["MO"], w=[tb2])
        P.pool(lambda e, ma=ma, mb=mb: e.tensor_tensor(out=ma, in0=ma, in1=mb, op=ALU.add), r=[ta, tb2], w=[ta])
        P.dve(lambda e, ma=ma: e.tensor_tensor(out=ma, in0=ma, in1=rows[:, 6, :], op=ALU.mult), r=[ta, "rows6"], w=[ta])
        P.dve(lambda e, xs=xs, ma=ma: e.tensor_tensor(out=xs, in0=xs, in1=ma, op=ALU.add), r=[tx, ta], w=[tx])
        P.dma("sp", out[rs, :], xs, r=[tx], w=[f"out{t}"])
    P.emit()
    es.close()
    return nc


def _rel_bucket_np(d):
    d = np.maximum(d, 0)
    logd = np.log(np.maximum(d, 1).astype(np.float32) / 16) / math.log(128 / 16)
    far = np.minimum(16 + (logd * 16).astype(np.int32), 31)
    return np.where(d < 16, d, far)


def _constants():
    k = np.arange(128)[:, None]
    q = np.arange(128)[None, :]
    c = {}
    c["ident"] = np.eye(128, dtype=np.float32)
    same = (k // 64) == (q // 64)
    c["tri"] = (same & (k <= q)).astype(np.float32)
    c["bones"] = same.astype(np.float32)
    c["cind"] = (np.arange(128)[:, None] // 64 == np.arange(2)[None, :]).astype(np.float32)
    c["stri"] = (k < q).astype(np.float32)
    mask0 = np.zeros((128, 256), np.float32)
    mask0[:, 0:128] = np.where(q >= k, 0.0, NEG)
    mask0[:, 128:256] = np.where(q < k, 0.0, NEG)
    c["mask0"] = mask0
    r = np.arange(TABROWS)
    tab = np.zeros((TABROWS, 4), np.float32)
    tab[:, 1] = S + (r % 128)
    c["tabinit"] = tab
    rc_ = np.zeros((128, 172), np.float32)
    rc_[:, 0:64] = (np.arange(64) * 128)[None, :]
    rc_[:, 64:160] = (np.arange(96) * 128)[None, :]
    rc_[:, 160:168] = np.arange(8)[None, :] * 128 + np.arange(128)[:, None]
    rc_[:, 168:172] = np.arange(4)[None, :] * 128 + np.arange(128)[:, None]
    c["rcst"] = rc_
    pid = np.zeros((128, 2), np.float32)
    pid[:, 0] = np.arange(128)
    pid[:, 1] = 32 * CAP + np.arange(128)
    c["pidx"] = pid
    d0 = q - k
    d1 = 128 + q - k
    c["_b0"] = _rel_bucket_np(d0)
    c["_b1"] = _rel_bucket_np(d1)
    return c


_CACHE = {}


def kernel(x, c, rel_bias, ada_w, ada_b, norm1_w, norm2_w, even_w_in, even_w_out, a_q_norm, a_k_norm, a_sinks,
           b_gate_up, b_gate_bias, b_out_norm, odd_w_in, odd_w_out, c_q_norm, c_k_norm, moe_w_group, moe_b_group,
           moe_w_expert, moe_b_expert, moe_w1, moe_w3, moe_w2, _n_layers=4, _cores=None, _dbg=None):
    f = lambda a: np.ascontiguousarray(np.asarray(a, dtype=np.float32))
    cst = _constants()
    rel_bias = f(rel_bias)
    relT = np.empty((8, 128, 256), np.float32)
    relT[:, :, 0:128] = np.transpose(rel_bias[cst["_b0"]], (2, 0, 1))
    relT[:, :, 128:256] = np.transpose(rel_bias[cst["_b1"]], (2, 0, 1))
    shared = {
        "rel_bias": rel_bias, "relT": relT, "ada_w": f(ada_w[:_n_layers]), "ada_b": f(ada_b), "norm1_w": f(norm1_w),
        "norm2_w": f(norm2_w), "even_w_in": f(even_w_in), "even_w_out": f(even_w_out), "a_q_norm": f(a_q_norm),
        "a_k_norm": f(a_k_norm), "a_sinks": f(a_sinks), "b_gate_up": f(b_gate_up), "b_gate_bias": f(b_gate_bias),
        "b_out_norm": f(b_out_norm), "odd_w_in": f(odd_w_in), "odd_w_out": f(odd_w_out), "c_q_norm": f(c_q_norm),
        "c_k_norm": f(c_k_norm),
        "moe_wr": np.ascontiguousarray(np.concatenate([f(moe_w_group), f(moe_w_expert)], axis=-1)),
        "moe_br": np.ascontiguousarray(np.concatenate([f(moe_b_group), f(moe_b_expert)], axis=-1)),
        "moe_w1": f(moe_w1[:_n_layers]), "moe_w3": f(moe_w3[:_n_layers]), "moe_w2": f(moe_w2[:_n_layers]),
    }
    for k_ in ("ident", "tri", "bones", "cind", "stri", "mask0", "tabinit", "rcst", "pidx"):
        shared[k_] = cst[k_]
    x = f(x)
    c = f(c)
    cores = list(range(8)) if _cores is None else _cores
    in_maps = []
    for b in cores:
        m = dict(shared)
        m["x"] = x[b]
        m["cT"] = np.ascontiguousarray(c[b].reshape(8, 128).T)
        in_maps.append(m)
    key = (_n_layers, str(_dbg))
    if key not in _CACHE:
        _CACHE[key] = build_program(_n_layers, _dbg)
    nc = _CACHE[key]
    res = run_bass_kernel_spmd(nc, in_maps, core_ids=list(range(len(cores))))
    outs = [r["out"] for r in res.results]
    return np.stack(outs, axis=0).astype(np.float32)
```
